# Optimizing a Trainium2 kernel written in Bass

```python
import math
import jax
import jax.numpy as jnp
from jax import lax
import numpy as np

D_MODEL = 1024
BATCH = 4
SEQ = 8192
DEPTH = 4

CTX_LEN = 256
GRID_W = 64
EPS = 1e-6
CONV_W = D_MODEL // 4
CONV_K = 31
MLSTM_DH = 64
MLSTM_W = D_MODEL // 4
MLSTM_HEADS = MLSTM_W // MLSTM_DH
MLSTM_CHUNK = 64
DIFF_DH = 64
DIFF_DV = 2 * DIFF_DH
DIFF_W = D_MODEL // 2
DIFF_HEADS = DIFF_W // DIFF_DV
MIX_W = CONV_W + MLSTM_W + DIFF_W
Q_BLOCK = 128
ROPE_BASE = 10000.0
AXIS_ROT = DIFF_DH // 2
IN_SIZES = (2 * CONV_W, MLSTM_W, MLSTM_W, MLSTM_W, MLSTM_W, 4 * MLSTM_HEADS, 2 * DIFF_HEADS * DIFF_DH, 2 * DIFF_HEADS * DIFF_DH, DIFF_W)
IN_W = sum(IN_SIZES)
PEER_HEADS = 8
PEER_DK = 128
N_KEYS = 128
N_EXPERTS = N_KEYS * N_KEYS
PEER_TOPK = 16
TOKEN_BLOCK = 128

kernel_name = "hybrid_conv_mlstm_diffattn_peer_block"


def rmsnorm(x, g):
    xf = x.astype(jnp.float32)
    y = xf * lax.rsqrt(jnp.mean(xf * xf, axis=-1, keepdims=True) + EPS)
    return (y * g.astype(jnp.float32)).astype(x.dtype)


def layernorm(x, g, b):
    xf = x.astype(jnp.float32)
    mu = jnp.mean(xf, axis=-1, keepdims=True)
    var = jnp.mean(jnp.square(xf - mu), axis=-1, keepdims=True)
    y = (xf - mu) * lax.rsqrt(var + EPS) * g.astype(jnp.float32) + b.astype(jnp.float32)
    return y.astype(x.dtype)


def heads(t, n):
    b, l, w = t.shape
    return t.reshape(b, l, n, w // n).transpose(0, 2, 1, 3)


def merge_heads(t):
    b, n, l, d = t.shape
    return t.transpose(0, 2, 1, 3).reshape(b, l, n * d)


def pair_heads(t):
    b, l, _ = t.shape
    t = t.reshape(b, l, DIFF_HEADS, 2, DIFF_DH)
    return t[:, :, :, 0].transpose(0, 2, 1, 3), t[:, :, :, 1].transpose(0, 2, 1, 3)


def flip_seq(t, rev):
    return jnp.flip(t, axis=2) if rev else t


def axial_rope(n_tokens):
    rows = n_tokens // GRID_W
    row = jnp.repeat(jnp.arange(rows), GRID_W).astype(jnp.float32)
    col = jnp.tile(jnp.arange(GRID_W), rows).astype(jnp.float32)
    inv = ROPE_BASE ** (-jnp.arange(0, AXIS_ROT, 2, dtype=jnp.float32) / AXIS_ROT)
    ang = jnp.concatenate([row[:, None] * inv, col[:, None] * inv], axis=-1)
    return jnp.cos(ang), jnp.sin(ang)


def apply_rope(t, cos, sin):
    tp = t.reshape(t.shape[:-1] + (t.shape[-1] // 2, 2))
    t1, t2 = tp[..., 0], tp[..., 1]
    c = cos.astype(t.dtype)
    s = sin.astype(t.dtype)
    return jnp.stack([t1 * c - t2 * s, t1 * s + t2 * c], axis=-1).reshape(t.shape)


def conv_module(a, conv_w, conv_b, ln_g, ln_b):
    u = a[..., :CONV_W] * jax.nn.sigmoid(a[..., CONV_W:])
    y = lax.conv_general_dilated(u, conv_w.astype(u.dtype), window_strides=(1,),
                                 padding=[(CONV_K // 2, CONV_K // 2)],
                                 dimension_numbers=("NWC", "WIO", "NWC"),
                                 feature_group_count=CONV_W) + conv_b
    return jax.nn.silu(layernorm(y, ln_g, ln_b))


def mlstm_chunk_scan(q, k, v, ig, lf, state):
    bsz, nh, seq, dh = q.shape
    nc = seq // MLSTM_CHUNK

    def chunks(t):
        t = t.reshape(t.shape[:2] + (nc, MLSTM_CHUNK) + t.shape[3:])
        return jnp.moveaxis(t, 2, 0)

    xs = tuple(chunks(t) for t in (q, k, v, ig, lf))
    tril = jnp.tril(jnp.ones((MLSTM_CHUNK, MLSTM_CHUNK), dtype=bool))

    def step(carry, inp):
        C, n, m = carry
        qc, kc, vc, ic, fc = inp
        qf, kf, vf = qc.astype(jnp.float32), kc.astype(jnp.float32), vc.astype(jnp.float32)
        b = jnp.cumsum(fc, axis=-1)
        dlog = jnp.where(tril, b[..., :, None] - b[..., None, :] + ic[..., None, :], -jnp.inf)
        inter = b + m[..., None]
        mt = jnp.maximum(inter, jnp.max(dlog, axis=-1))
        dw = jnp.exp(dlog - mt[..., None])
        iw = jnp.exp(inter - mt)
        s = jnp.einsum("bhtd,bhsd->bhts", qf, kf) * dw
        num = jnp.einsum("bhts,bhse->bhte", s, vf) + iw[..., None] * jnp.einsum("bhed,bhtd->bhte", C, qf)
        den = jnp.sum(s, axis=-1) + iw * jnp.einsum("bhd,bhtd->bht", n, qf)
        h = num / jnp.maximum(jnp.abs(den), jnp.exp(-mt))[..., None]
        bl = b[..., -1]
        wlog = bl[..., None] - b + ic
        mn = jnp.maximum(bl + m, jnp.max(wlog, axis=-1))
        wk = jnp.exp(wlog - mn[..., None])
        decay = jnp.exp(bl + m - mn)
        C = decay[..., None, None] * C + jnp.einsum("bhs,bhse,bhsd->bhed", wk, vf, kf)
        n = decay[..., None] * n + jnp.einsum("bhs,bhsd->bhd", wk, kf)
        return (C, n, mn), h

    state, hs = lax.scan(step, state, xs)
    h = jnp.moveaxis(hs, 0, 2).reshape(bsz, nh, seq, dh)
    return h, state


def mlstm_mixer(lat, ctx, gate_b, norm_g, need_ctx):
    gb = gate_b.reshape(-1).astype(jnp.float32)

    def prep(q, k, v, g):
        b, l, _ = q.shape
        g = (g.astype(jnp.float32) + gb).reshape(b, l, 4, MLSTM_HEADS).transpose(2, 0, 3, 1)
        return heads(q, MLSTM_HEADS), heads(k, MLSTM_HEADS) * (MLSTM_DH ** -0.5), heads(v, MLSTM_HEADS), g

    ql, kl, vl, gl = prep(lat[0], lat[1], lat[2], lat[4])
    qc, kc, vc, gc = prep(ctx[0], ctx[1], ctx[2], ctx[4])
    bsz = ql.shape[0]
    outs_l, outs_c = [], []
    for d, rev in enumerate((False, True)):
        state = (jnp.zeros((bsz, MLSTM_HEADS, MLSTM_DH, MLSTM_DH), jnp.float32),
                 jnp.zeros((bsz, MLSTM_HEADS, MLSTM_DH), jnp.float32),
                 jnp.zeros((bsz, MLSTM_HEADS), jnp.float32))
        hc, state = mlstm_chunk_scan(*[flip_seq(t, rev) for t in (qc, kc, vc, gc[2 * d], jax.nn.log_sigmoid(gc[2 * d + 1]))], state)
        hl, _ = mlstm_chunk_scan(*[flip_seq(t, rev) for t in (ql, kl, vl, gl[2 * d], jax.nn.log_sigmoid(gl[2 * d + 1]))], state)
        outs_c.append(flip_seq(hc, rev))
        outs_l.append(flip_seq(hl, rev))

    def finish(h, o):
        h = merge_heads(rmsnorm(h, norm_g))
        return (h * jax.nn.sigmoid(o.astype(jnp.float32))).astype(o.dtype)

    y_lat = finish(outs_l[0] + outs_l[1], lat[3])
    y_ctx = finish(outs_c[0] + outs_c[1], ctx[3]) if need_ctx else None
    return y_lat, y_ctx


def diff_attention(q1, q2, k1, k2, v, lam):
    bsz, nh, lq, dh = q1.shape
    nb = lq // Q_BLOCK
    scale = dh ** -0.5

    def blocks(t):
        return jnp.moveaxis(t.reshape(bsz, nh, nb, Q_BLOCK, dh), 2, 0)

    def one(args):
        a1, a2 = args
        p1 = jax.nn.softmax(jnp.einsum("bhqd,bhkd->bhqk", a1, k1).astype(jnp.float32) * scale, axis=-1)
        p2 = jax.nn.softmax(jnp.einsum("bhqd,bhkd->bhqk", a2, k2).astype(jnp.float32) * scale, axis=-1)
        p = p1 - lam * p2
        return jnp.einsum("bhqk,bhkd->bhqd", p.astype(v.dtype), v)

    o = lax.map(one, (blocks(q1), blocks(q2)))
    return jnp.moveaxis(o, 0, 2).reshape(bsz, nh, lq, v.shape[-1])


def diff_mixer(lat, ctx, lam_params, lam_init, norm_g, cos, sin, need_ctx):
    dql, dkl, dvl = lat
    dqc, dkc, dvc = ctx
    lp = lam_params.astype(jnp.float32)
    lam = jnp.exp(jnp.sum(lp[0] * lp[1])) - jnp.exp(jnp.sum(lp[2] * lp[3])) + lam_init
    ql1, ql2 = (apply_rope(t, cos, sin) for t in pair_heads(dql))
    kl1, kl2 = (apply_rope(t, cos, sin) for t in pair_heads(dkl))
    qc1, qc2 = pair_heads(dqc)
    kc1, kc2 = pair_heads(dkc)
    vl = heads(dvl, DIFF_HEADS)
    vc = heads(dvc, DIFF_HEADS)
    k1 = jnp.concatenate([kc1, kl1], axis=2)
    k2 = jnp.concatenate([kc2, kl2], axis=2)
    v = jnp.concatenate([vc, vl], axis=2)

    def finish(o):
        return merge_heads(rmsnorm(o, norm_g) * (1.0 - lam_init))

    y_lat = finish(diff_attention(ql1, ql2, k1, k2, v, lam))
    y_ctx = finish(diff_attention(qc1, qc2, kc1, kc2, vc, lam)) if need_ctx else None
    return y_lat, y_ctx


def peer_ffn(h, wq, sub_keys, eu, ev):
    bsz, seq, dm = h.shape
    nb = (bsz * seq) // TOKEN_BLOCK
    hb = h.reshape(nb, TOKEN_BLOCK, dm)

    def one(t):
        q = (t @ wq).reshape(TOKEN_BLOCK, PEER_HEADS, 2, PEER_DK // 2)
        s = jnp.einsum("thpd,hpnd->thpn", q, sub_keys).astype(jnp.float32)
        st, it = lax.top_k(s, PEER_TOPK)
        cand = (st[:, :, 0, :, None] + st[:, :, 1, None, :]).reshape(TOKEN_BLOCK, PEER_HEADS, PEER_TOPK * PEER_TOPK)
        cidx = (it[:, :, 0, :, None] * N_KEYS + it[:, :, 1, None, :]).reshape(TOKEN_BLOCK, PEER_HEADS, PEER_TOPK * PEER_TOPK)
        best, pos = lax.top_k(cand, PEER_TOPK)
        e = jnp.take_along_axis(cidx, pos, axis=-1)
        g = jax.nn.softmax(best, axis=-1)
        u = jnp.take(eu, e, axis=0)
        act = jax.nn.gelu(jnp.einsum("thkd,td->thk", u, t).astype(jnp.float32), approximate=False)
        return jnp.einsum("thk,thkd->td", (g * act).astype(ev.dtype), jnp.take(ev, e, axis=0))

    return lax.map(one, hb).reshape(bsz, seq, dm)


def trunk_layer(x, xc, mod, modc, lam_init, cos, sin, need_ctx,
                norm1_g, norm2_g, w_in, conv_w, conv_b, conv_ln_g, conv_ln_b,
                mlstm_gate_b, mlstm_norm_g, diff_lambda, diff_norm_g, w_out,
                peer_wq, peer_keys, peer_u, peer_v):
    sh1, sc1, g1, sh2, sc2, g2 = [m[:, None, :] for m in jnp.split(mod, 6, axis=-1)]
    sh1c, sc1c, g1c, sh2c, sc2c, g2c = jnp.split(modc, 6, axis=-1)
    splits = np.cumsum(IN_SIZES)[:-1].tolist()
    h = rmsnorm(x, norm1_g) * (1.0 + sc1) + sh1
    hc = rmsnorm(xc, norm1_g) * (1.0 + sc1c) + sh1c
    a, mq, mk, mv, mo, mg, dq, dk, dv = jnp.split(h @ w_in, splits, axis=-1)
    ac, mqc, mkc, mvc, moc, mgc, dqc, dkc, dvc = jnp.split(hc @ w_in, splits, axis=-1)
    conv_l = conv_module(a, conv_w, conv_b, conv_ln_g, conv_ln_b)
    mlstm_l, mlstm_c = mlstm_mixer((mq, mk, mv, mo, mg), (mqc, mkc, mvc, moc, mgc), mlstm_gate_b, mlstm_norm_g, need_ctx)
    diff_l, diff_c = diff_mixer((dq, dk, dv), (dqc, dkc, dvc), diff_lambda, lam_init, diff_norm_g, cos, sin, need_ctx)
    mix = jnp.concatenate([conv_l.astype(x.dtype), mlstm_l.astype(x.dtype), diff_l.astype(x.dtype)], axis=-1)
    x = x + g1 * (mix @ w_out)
    h2 = rmsnorm(x, norm2_g) * (1.0 + sc2) + sh2
    x = x + g2 * peer_ffn(h2, peer_wq, peer_keys, peer_u, peer_v)
    if need_ctx:
        conv_c = conv_module(ac, conv_w, conv_b, conv_ln_g, conv_ln_b)
        mix_c = jnp.concatenate([conv_c.astype(xc.dtype), mlstm_c.astype(xc.dtype), diff_c.astype(xc.dtype)], axis=-1)
        xc = xc + g1c * (mix_c @ w_out)
        h2c = rmsnorm(xc, norm2_g) * (1.0 + sc2c) + sh2c
        xc = xc + g2c * peer_ffn(h2c, peer_wq, peer_keys, peer_u, peer_v)
    return x, xc


def setup_inputs(seed: int = 0) -> dict:
    key = jax.random.key(seed)
    ks = jax.random.split(key, 24)
    f32 = jnp.float32

    def nrm(k, shape, scale):
        return jax.random.normal(k, shape, f32) * scale

    gate_offset = jnp.array([0.0, 3.0, 0.0, 3.0], f32)[None, :, None]
    return {
        "x": nrm(ks[0], (BATCH, SEQ, D_MODEL), 1.0),
        "c": nrm(ks[1], (BATCH, D_MODEL), 1.0),
        "ctx": nrm(ks[2], (BATCH, CTX_LEN, D_MODEL), 1.0),
        "c_ctx": nrm(ks[3], (D_MODEL,), 1.0),
        "ada_w": nrm(ks[4], (DEPTH, D_MODEL, 6 * D_MODEL), 0.5 * D_MODEL ** -0.5),
        "ada_b": nrm(ks[5], (DEPTH, 6 * D_MODEL), 0.02),
        "norm1_g": 1.0 + nrm(ks[6], (DEPTH, D_MODEL), 0.02),
        "norm2_g": 1.0 + nrm(ks[7], (DEPTH, D_MODEL), 0.02),
        "w_in": nrm(ks[8], (DEPTH, D_MODEL, IN_W), D_MODEL ** -0.5),
        "conv_w": nrm(ks[9], (DEPTH, CONV_K, 1, CONV_W), CONV_K ** -0.5),
        "conv_b": nrm(ks[10], (DEPTH, CONV_W), 0.02),
        "conv_ln_g": 1.0 + nrm(ks[11], (DEPTH, CONV_W), 0.02),
        "conv_ln_b": nrm(ks[12], (DEPTH, CONV_W), 0.02),
        "mlstm_gate_b": gate_offset + nrm(ks[13], (DEPTH, 4, MLSTM_HEADS), 0.5),
        "mlstm_norm_g": 1.0 + nrm(ks[14], (DEPTH, MLSTM_DH), 0.02),
        "diff_lambda": nrm(ks[15], (DEPTH, 4, DIFF_DH), 0.1),
        "diff_norm_g": 1.0 + nrm(ks[16], (DEPTH, DIFF_DV), 0.02),
        "w_out": nrm(ks[17], (DEPTH, MIX_W, D_MODEL), MIX_W ** -0.5),
        "peer_wq": nrm(ks[18], (DEPTH, D_MODEL, PEER_HEADS * PEER_DK), D_MODEL ** -0.5),
        "peer_keys": nrm(ks[19], (DEPTH, PEER_HEADS, 2, N_KEYS, PEER_DK // 2), (PEER_DK // 2) ** -0.5),
        "peer_u": nrm(ks[20], (DEPTH, N_EXPERTS, D_MODEL), D_MODEL ** -0.5),
        "peer_v": nrm(ks[21], (DEPTH, N_EXPERTS, D_MODEL), PEER_HEADS ** -0.5),
        "final_g": 1.0 + nrm(ks[22], (D_MODEL,), 0.02),
    }


def reference(x, c, ctx, c_ctx, ada_w, ada_b, norm1_g, norm2_g, w_in, conv_w, conv_b,
              conv_ln_g, conv_ln_b, mlstm_gate_b, mlstm_norm_g, diff_lambda, diff_norm_g,
              w_out, peer_wq, peer_keys, peer_u, peer_v, final_g):
    cos, sin = axial_rope(x.shape[1])
    sc = jax.nn.silu(c)
    scc = jax.nn.silu(c_ctx)
    xc = ctx
    for l in range(DEPTH):
        mod = sc @ ada_w[l] + ada_b[l]
        modc = scc @ ada_w[l] + ada_b[l]
        lam_init = 0.8 - 0.6 * math.exp(-0.3 * l)
        x, xc = trunk_layer(x, xc, mod, modc, lam_init, cos, sin, l < DEPTH - 1,
                            norm1_g[l], norm2_g[l], w_in[l], conv_w[l], conv_b[l],
                            conv_ln_g[l], conv_ln_b[l], mlstm_gate_b[l], mlstm_norm_g[l],
                            diff_lambda[l], diff_norm_g[l], w_out[l], peer_wq[l],
                            peer_keys[l], peer_u[l], peer_v[l])
    return rmsnorm(x, final_g)
```

```python
import math
from contextlib import ExitStack
import numpy as np
import ml_dtypes
import concourse.bass as bass
import concourse.mybir as mybir
from concourse.bass_utils import run_bass_kernel_spmd

F32 = mybir.dt.float32
BF16 = mybir.dt.bfloat16
I32 = mybir.dt.int32
U32 = mybir.dt.uint32
AF = mybir.ActivationFunctionType
ALU = mybir.AluOpType
AX = mybir.AxisListType

D = 1024
IN_W = 3088
EPS = 1e-6
R_DMA = 8
NCORES = 8


class Prog:
    ENGS = ("pe", "act", "dve", "pool", "sp")

    def __init__(self, nc):
        self.nc = nc
        self.gs = ExitStack()
        self.sems = {}
        for e in ("pe", "act", "dve"):
            self.sems[e] = self.gs.enter_context(nc.semaphore(f"s_{e}"))
        for e in ("sp", "pool"):
            self.sems[e] = [self.gs.enter_context(nc.semaphore(f"s_{e}{i}")) for i in range(R_DMA)]
        self.cnt = {e: 0 for e in self.ENGS}
        self.waited = {e: {} for e in self.ENGS}
        self.nalloc = 0
        self.phase_open = False
        self.begin_phase()

    def begin_phase(self):
        assert not self.phase_open
        self.phase_open = True
        self.es = ExitStack()
        self.ops = {e: [] for e in self.ENGS}
        self.last_w = {}
        self.readers = {}
        self.bar = dict(self.cnt)

    def sb(self, shape, dtype=F32, name=None):
        self.nalloc += 1
        name = f"{name or 'sb'}_{self.nalloc}"
        return self.es.enter_context(self.nc.sbuf_tensor(name, list(shape), dtype))

    def ps(self, shape, dtype=F32, name=None):
        self.nalloc += 1
        name = f"{name or 'ps'}_{self.nalloc}"
        return self.es.enter_context(self.nc.psum_tensor(name, list(shape), dtype))

    def add(self, eng, fn, reads=(), writes=()):
        deps = set()
        for b in reads:
            if b in self.last_w:
                deps.add(self.last_w[b])
        for b in writes:
            if b in self.last_w:
                deps.add(self.last_w[b])
            for r in self.readers.get(b, ()):
                deps.add(r)
        self.cnt[eng] += 1
        idx = self.cnt[eng]
        me = (eng, idx)
        for b in reads:
            self.readers.setdefault(b, []).append(me)
        for b in writes:
            self.last_w[b] = me
            self.readers[b] = []
        deps.discard(me)
        self.ops[eng].append((fn, deps, idx))
        return me

    def dma(self, out, in_, reads=(), writes=(), eng="sp", **kw):
        return self.add(eng, lambda e: e.dma_start(out=out, in_=in_, **kw), reads, writes)

    def _dep_wait(self, engname, engobj, dep):
        waited = self.waited[engname]
        f, k = dep
        if f in ("sp", "pool"):
            n = k - 1
            sem = self.sems[f][n % R_DMA]
            val = 16 * (n // R_DMA + 1)
            key = (f, n % R_DMA)
        else:
            sem = self.sems[f]
            val = k
            key = f
        if waited.get(key, 0) < val:
            engobj.wait_ge(sem, val)
            waited[key] = val

    def _wait_all(self, engname, engobj, counts):
        for f in self.ENGS:
            tot = counts[f]
            if tot == 0:
                continue
            if f in ("sp", "pool"):
                for k in range(max(1, tot - R_DMA + 1), tot + 1):
                    self._dep_wait(engname, engobj, (f, k))
            else:
                self._dep_wait(engname, engobj, (f, tot))

    def end_phase(self, final=False):
        assert self.phase_open
        self.phase_open = False
        nc = self.nc

        def run(engname, engobj):
            first = True
            for fn, deps, idx in self.ops[engname]:
                if first:
                    self._wait_all(engname, engobj, self.bar)
                    first = False
                for d in sorted(deps):
                    if d[0] == engname and engname == "pe":
                        continue
                    self._dep_wait(engname, engobj, d)
                if engname in ("sp", "pool"):
                    n = idx - 1
                    if n >= R_DMA:
                        self._dep_wait(engname, engobj, (engname, idx - R_DMA))
                    inst = fn(engobj)
                    inst.then_inc(self.sems[engname][n % R_DMA], 16)
                else:
                    inst = fn(engobj)
                    inst.then_inc(self.sems[engname], 1)
            if final and engname in ("sp", "pool"):
                self._wait_all(engname, engobj, {e: (self.cnt[e] if e == engname else 0) for e in self.ENGS})

        with nc.Block() as block:
            @block.tensor
            def _(e):
                run("pe", e)

            @block.scalar
            def _(e):
                run("act", e)

            @block.vector
            def _(e):
                run("dve", e)

            @block.gpsimd
            def _(e):
                run("pool", e)

            @block.sync
            def _(e):
                run("sp", e)
        self.es.close()

    def emit(self):
        self.end_phase(final=True)
        self.gs.close()


def _din(nc, name, shape, dt=F32):
    return nc.dram_tensor(name, list(shape), dt, kind="ExternalInput").ap()


def _dout(nc, name, shape, dt=F32):
    return nc.dram_tensor(name, list(shape), dt, kind="ExternalOutput").ap()


def build_P0():
    nc = bass.Bass("TRN2", target_bir_lowering=False)
    w = _din(nc, "w", [1024, 3072])
    b = _din(nc, "b", [128, 24])
    cT = _din(nc, "cT", [128, 8, 5])
    o = _dout(nc, "modT", [128, 24, 5])
    p = Prog(nc)
    ct = p.sb([128, 8, 5]); st = p.sb([128, 8, 5]); bt = p.sb([128, 24]); ot = p.sb([128, 24, 5])
    p.dma(ct[:], cT, writes=["ct"])
    p.dma(bt[:], b, writes=["bt"])
    p.add("act", lambda e: e.activation(out=st[:], in_=ct[:], func=AF.Silu), ["ct"], ["st"])
    wv = w.rearrange("(k p) n -> p k n", p=128)
    NB = 6
    wts = [p.sb([128, 8, 512], name=f"wt{i}") for i in range(2)]
    pss = [p.ps([128, 4, 8], name=f"pp{i}") for i in range(2)]
    for blk in range(NB):
        wt = wts[blk % 2]; ps = pss[blk % 2]
        p.dma(wt[:], wv[:, :, blk * 512:(blk + 1) * 512], writes=[f"wt{blk%2}"])
        for jj in range(4):
            for k in range(8):
                p.add("pe", lambda e, wt=wt, ps=ps, jj=jj, k=k: e.matmul(
                    ps[:, jj, 0:5], lhsT=wt[:, k, jj * 128:(jj + 1) * 128], rhs=st[:, k, :],
                    start=(k == 0), stop=(k == 7)), [f"wt{blk%2}", "st"], [f"pp{blk%2}"])
        for jj in range(4):
            j = blk * 4 + jj
            p.add("dve", lambda e, ps=ps, jj=jj, j=j: e.tensor_scalar(
                out=ot[:, j, :], in0=ps[:, jj, 0:5], scalar1=bt[:, j:j + 1], scalar2=None,
                op0=ALU.add), [f"pp{blk%2}", "bt"], ["ot"])
    p.dma(o, ot[:], reads=["ot"])
    p.emit()
    return nc


def load_cast_weight(p, wb, w, ncols, key, nk=8, bw=512):
    wv = w.rearrange("(k p) n -> p k n", p=128)
    stg = [p.sb([128, nk, bw], name=f"stg_{key}{i}") for i in range(2)]
    for bi, c0 in enumerate(range(0, ncols, bw)):
        cw = min(bw, ncols - c0)
        st = stg[bi % 2]
        p.dma(st[:, :, 0:cw], wv[:, :, c0:c0 + cw], writes=[f"stg_{key}{bi%2}"])
        h = nk // 2
        p.add("act", lambda e, st=st, c0=c0, cw=cw: e.activation(out=wb[:, 0:h, c0:c0 + cw], in_=st[:, 0:h, 0:cw], func=AF.Copy),
              [f"stg_{key}{bi%2}"], [f"{key}_a{bi}"])
        p.add("dve", lambda e, st=st, c0=c0, cw=cw: e.tensor_copy(out=wb[:, h:nk, c0:c0 + cw], in_=st[:, h:nk, 0:cw]),
              [f"stg_{key}{bi%2}"], [f"{key}_b{bi}"])
    p.add("act", lambda e: e.activation(out=wb[0:1, 0, 0:1], in_=wb[0:1, 0, 0:1], func=AF.Copy),
          [f"{key}_a{bi}" for bi in range((ncols + bw - 1) // bw)] + [f"{key}_b{bi}" for bi in range((ncols + bw - 1) // bw)], [key])


def build_A(NT, ctx_tiles=(0,), stage=9):
    nc = bass.Bass("TRN2", target_bir_lowering=False)
    xs = _din(nc, "xs", [NT * 128, D])
    w_in = _din(nc, "w_in", [D, IN_W])
    pv = _din(nc, "pv", [128, 8, 5])
    cs = _din(nc, "cs", [NT * 128, 64])
    ident = _din(nc, "ident", [128, 128])
    proj = _dout(nc, "proj", [NT * 128, IN_W])
    p = Prog(nc)
    wb = p.sb([128, 8, IN_W], BF16, name="wb")
    load_cast_weight(p, wb, w_in, IN_W, "wb")
    idt = p.sb([128, 128]); p.dma(idt[:], ident, writes=["idt"])
    pvt = p.sb([128, 8, 5]); p.dma(pvt[:], pv, writes=["pvt"])
    gm = p.sb([128, 8, 2])
    for i, col in enumerate((1, 3)):
        p.add("dve", lambda e, i=i, col=col: e.scalar_tensor_tensor(
            out=gm[:, :, i], in0=pvt[:, :, col], scalar=1.0, in1=pvt[:, :, 0],
            op0=ALU.add, op1=ALU.mult), ["pvt"], ["gm"])
    NB = 2
    xt = [p.sb([128, D], name=f"xt{i}") for i in range(NB)]
    xn = [p.sb([128, D], name=f"xn{i}") for i in range(NB)]
    junk = p.sb([128, D], name="junk")
    ss = [p.sb([128, 1], name=f"ss{i}") for i in range(NB)]
    rstd = [p.sb([128, 1], name=f"rstd{i}") for i in range(NB)]
    hT = [p.sb([128, 8, 128], BF16, name=f"hT{i}") for i in range(NB)]
    ot = [p.sb([128, IN_W], name=f"ot{i}") for i in range(NB)]
    ro = [p.sb([128, 1024], name=f"ro{i}") for i in range(NB)]
    tmp = [p.sb([128, 4, 512], name=f"tmp{i}") for i in range(NB)]
    cst = [p.sb([128, 64], name=f"cst{i}") for i in range(NB)]
    tp = [p.ps([128, 512], name=f"tp{i}") for i in range(4)]
    acc = [p.ps([128, 512], name=f"acc{i}") for i in range(4)]
    nacc = 0
    for t in range(NT):
        i = t % NB
        isc = t in ctx_tiles
        r0 = t * 128
        p.dma(xt[i][:], xs[r0:r0 + 128, :], writes=[f"xt{i}"])
        p.dma(cst[i][:], cs[r0:r0 + 128, :], writes=[f"cst{i}"])
        p.add("act", lambda e, i=i: e.activation(out=junk[:], in_=xt[i][:], func=AF.Square,
                                                  accum_out=ss[i][:, 0:1]), [f"xt{i}"], ["junk", f"ss{i}"])
        p.add("dve", lambda e, i=i: e.tensor_scalar(out=rstd[i][:], in0=ss[i][:], scalar1=1.0 / D, scalar2=EPS,
                                                     op0=ALU.mult, op1=ALU.add), [f"ss{i}"], [f"rstd{i}"])
        p.add("act", lambda e, i=i: e.activation(out=ss[i][:], in_=rstd[i][:], func=AF.Sqrt), [f"rstd{i}"], [f"ss{i}"])
        p.add("dve", lambda e, i=i: e.reciprocal(out=rstd[i][:], in_=ss[i][:]), [f"ss{i}"], [f"rstd{i}"])
        p.add("dve", lambda e, i=i: e.tensor_scalar(out=xn[i][:], in0=xt[i][:], scalar1=rstd[i][:, 0:1], scalar2=None,
                                                     op0=ALU.mult), [f"xt{i}", f"rstd{i}"], [f"xn{i}"])
        if stage == 1:
            p.dma(proj[r0:r0 + 128, 0:1024], xn[i][:], reads=[f"xn{i}"])
            continue
        for half in range(2):
            tpi = (2 * t + half) % 4
            for k in range(half * 4, half * 4 + 4):
                p.add("pe", lambda e, i=i, k=k, tpi=tpi: e.transpose(
                    tp[tpi][:, (k % 4) * 128:(k % 4 + 1) * 128], xn[i][:, k * 128:(k + 1) * 128], idt[:]),
                    [f"xn{i}", "idt"], [f"tp{tpi}"])
            sc_col = 1 if isc else 0
            sh_col = 4 if isc else 2
            for k in range(half * 4, half * 4 + 4):
                if half == 0:
                    p.add("act", lambda e, i=i, k=k, tpi=tpi, sc_col=sc_col, sh_col=sh_col: e.activation(
                        out=hT[i][:, k, :], in_=tp[tpi][:, (k % 4) * 128:(k % 4 + 1) * 128], func=AF.Identity,
                        scale=gm[:, k, sc_col:sc_col + 1], bias=pvt[:, k, sh_col:sh_col + 1]),
                        [f"tp{tpi}", "gm", "pvt"], [f"hT{i}_{k}"])
                else:
                    p.add("dve", lambda e, i=i, k=k, tpi=tpi, sc_col=sc_col, sh_col=sh_col: e.tensor_scalar(
                        out=hT[i][:, k, :], in0=tp[tpi][:, (k % 4) * 128:(k % 4 + 1) * 128],
                        scalar1=gm[:, k, sc_col:sc_col + 1], scalar2=pvt[:, k, sh_col:sh_col + 1],
                        op0=ALU.mult, op1=ALU.add), [f"tp{tpi}", "gm", "pvt"], [f"hT{i}_{k}"])
        if stage == 2:
            p.add("dve", lambda e, i=i: e.tensor_copy(out=ot[i][:, 0:1024], in_=hT[i][:].rearrange("p k n -> p (k n)")),
                  [f"hT{i}_{k}" for k in range(8)], [f"ot{i}_0"])
            p.dma(proj[r0:r0 + 128, 0:1024], ot[i][:, 0:1024], reads=[f"ot{i}_0"])
            continue
        for blk in range(7):
            c0 = blk * 512
            cw = min(512, IN_W - c0)
            a = nacc % 4; nacc += 1
            for k in range(8):
                p.add("pe", lambda e, i=i, k=k, a=a, c0=c0, cw=cw: e.matmul(
                    acc[a][:, 0:cw], lhsT=hT[i][:, k, :], rhs=wb[:, k, c0:c0 + cw],
                    start=(k == 0), stop=(k == 7)), [f"hT{i}_{k}", "wb"], [f"acc{a}"])
            if blk % 2 == 0:
                p.add("act", lambda e, i=i, a=a, c0=c0, cw=cw: e.activation(
                    out=ot[i][:, c0:c0 + cw], in_=acc[a][:, 0:cw], func=AF.Copy), [f"acc{a}"], [f"ot{i}_{blk}"])
            else:
                p.add("dve", lambda e, i=i, a=a, c0=c0, cw=cw: e.tensor_copy(
                    out=ot[i][:, c0:c0 + cw], in_=acc[a][:, 0:cw]), [f"acc{a}"], [f"ot{i}_{blk}"])
        if stage == 3:
            p.dma(proj[r0:r0 + 128, :], ot[i][:], reads=[f"ot{i}_{b}" for b in range(7)])
            continue
        src = ot[i][:, 1552:2576].rearrange("p (g r two) -> p g r two", g=16, two=2)
        dst = ro[i][:].rearrange("p (g r two) -> p g r two", g=16, two=2)
        t1 = src[:, :, :, 0]; t2 = src[:, :, :, 1]
        cosb = cst[i][:, None, 0:32].to_broadcast([128, 16, 32])
        sinb = cst[i][:, None, 32:64].to_broadcast([128, 16, 32])
        tm = [tmp[i][:, j, :].rearrange("p (g r) -> p g r", g=16) for j in range(4)]
        rk = [f"ot{i}_3", f"ot{i}_4", f"ot{i}_5", f"cst{i}"]
        p.add("dve", lambda e, tm=tm, t1=t1, cosb=cosb: e.tensor_tensor(out=tm[0], in0=t1, in1=cosb, op=ALU.mult), rk, [f"tmp{i}_0"])
        p.add("dve", lambda e, tm=tm, t2=t2, sinb=sinb: e.tensor_tensor(out=tm[1], in0=t2, in1=sinb, op=ALU.mult), rk, [f"tmp{i}_1"])
        p.add("dve", lambda e, tm=tm, t1=t1, sinb=sinb: e.tensor_tensor(out=tm[2], in0=t1, in1=sinb, op=ALU.mult), rk, [f"tmp{i}_2"])
        p.add("dve", lambda e, tm=tm, t2=t2, cosb=cosb: e.tensor_tensor(out=tm[3], in0=t2, in1=cosb, op=ALU.mult), rk, [f"tmp{i}_3"])
        p.add("dve", lambda e, tm=tm, dst=dst: e.tensor_tensor(out=dst[:, :, :, 0], in0=tm[0], in1=tm[1], op=ALU.subtract),
              [f"tmp{i}_0", f"tmp{i}_1"], [f"ro{i}a"])
        p.add("dve", lambda e, tm=tm, dst=dst: e.tensor_tensor(out=dst[:, :, :, 1], in0=tm[2], in1=tm[3], op=ALU.add),
              [f"tmp{i}_2", f"tmp{i}_3"], [f"ro{i}b"])
        p.dma(proj[r0:r0 + 128, 0:1552], ot[i][:, 0:1552], reads=[f"ot{i}_{b}" for b in range(4)])
        p.dma(proj[r0:r0 + 128, 1552:2576], ro[i][:], reads=[f"ro{i}a", f"ro{i}b"])
        p.dma(proj[r0:r0 + 128, 2576:IN_W], ot[i][:, 2576:IN_W], reads=[f"ot{i}_5", f"ot{i}_6"])
    p.emit()
    return nc


def build_M1(NCH, NS=4, GC=4):
    nc = bass.Bass("TRN2", target_bir_lowering=False)
    L = NCH * 128
    mqT = _din(nc, "mqT", [NS, 64, L])
    mkT = _din(nc, "mkT", [NS, 64, L])
    mvk = _din(nc, "mvk", [NS, L, 128])
    mg = _din(nc, "mg", [NS, L, 2])
    mgb = _din(nc, "mgb", [NS, 128, 2])
    tri_d = _din(nc, "tri", [128, 128])
    ones_d = _din(nc, "ones", [128, 128])
    mh = _dout(nc, "mh", [NS, L, 64])
    p = Prog(nc)
    tri = p.sb([128, 128], name="tri_sb"); p.dma(tri[:], tri_d, writes=["tri"])
    ones = p.sb([128, 128], name="ones_sb"); p.dma(ones[:], ones_d, writes=["ones"])
    banks = [p.ps([128, 512], name=f"bank{i}") for i in range(7)]
    NBUF = 2
    B = {}
    for s in range(NS):
        gb = p.sb([128, 2], name=f"gb{s}"); p.dma(gb[:], mgb[s], writes=[f"gb{s}"])
        B[s, "gb"] = gb
        B[s, "Cf"] = p.sb([64, 65], name=f"Cf{s}")
        B[s, "Cb"] = p.sb([64, 65], BF16, name=f"Cb{s}")
        B[s, "tmpC"] = p.sb([64, 65], name=f"tmpC{s}")
        p.add("dve", lambda e, s=s: e.memset(B[s, "Cf"][:], 0.0), [], [f"Cf{s}"])
        p.add("dve", lambda e, s=s: e.memset(B[s, "Cb"][:], 0.0), [], [f"Cb{s}"])
        for i in range(NBUF):
            k = (s, i)
            B[k, "qTf"] = p.sb([64, GC * 128], name=f"qTf{s}_{i}")
            B[k, "kTf"] = p.sb([64, GC * 128], name=f"kTf{s}_{i}")
            B[k, "vkf"] = p.sb([128, GC, 128], name=f"vkf{s}_{i}")
            B[k, "gf"] = p.sb([128, GC, 2], name=f"gf{s}_{i}")
            B[k, "qTb"] = p.sb([64, GC * 128], BF16, name=f"qTb{s}_{i}")
            B[k, "kTb"] = p.sb([64, GC * 128], BF16, name=f"kTb{s}_{i}")
            B[k, "vaug"] = p.sb([128, GC, 65], BF16, name=f"vaug{s}_{i}")
            B[k, "kb"] = p.sb([128, GC, 64], BF16, name=f"kb{s}_{i}")
            B[k, "h"] = p.sb([128, GC, 64], name=f"h{s}_{i}")
            B[k, "gi"] = p.sb([128, GC], name=f"gi{s}_{i}")
            B[k, "sp"] = p.sb([128, GC], name=f"sp{s}_{i}")
            B[k, "a"] = p.sb([128, GC], name=f"a{s}_{i}")
            B[k, "b"] = p.sb([128, GC], name=f"b{s}_{i}")
            B[k, "eG"] = p.sb([128, GC], name=f"eG{s}_{i}")
            B[k, "WT"] = p.sb([128, 128], BF16, name=f"WT{s}_{i}")
            B[k, "d"] = p.sb([128, 4], name=f"d{s}_{i}")
            p.add("dve", lambda e, k=k: e.memset(B[k, "vaug"][:, :, 64:65], 1.0), [], [f"vaug1_{s}_{i}"])
    ngroups = (NCH + GC - 1) // GC
    for g in range(ngroups):
        c0 = g * GC
        gc = min(GC, NCH - c0)
        i = g % NBUF
        for s in range(NS):
            k = (s, i)
            sfx = f"{s}_{i}"
            T = {n: B[k, n] for n in ("qTf", "kTf", "vkf", "gf", "qTb", "kTb", "vaug", "kb", "h", "gi", "sp", "a", "b", "eG", "WT", "d")}
            Cf, Cb, tmpC, gb = B[s, "Cf"], B[s, "Cb"], B[s, "tmpC"], B[s, "gb"]
            bS, bX, bU = banks[(s % 2) * 3], banks[(s % 2) * 3 + 1], banks[(s % 2) * 3 + 2]
            kS, kX, kU = f"bank{(s%2)*3}", f"bank{(s%2)*3+1}", f"bank{(s%2)*3+2}"
            bP = banks[6]
            W = gc * 128
            p.dma(T["qTf"][:, 0:W], mqT[s, :, c0 * 128:c0 * 128 + W], writes=[f"qTf{sfx}"])
            p.dma(T["kTf"][:, 0:W], mkT[s, :, c0 * 128:c0 * 128 + W], writes=[f"kTf{sfx}"])
            p.dma(T["vkf"][:, 0:gc, :], mvk[s].rearrange("(c p) f -> p c f", p=128)[:, c0:c0 + gc, :], writes=[f"vkf{sfx}"])
            p.dma(T["gf"][:, 0:gc, :], mg[s].rearrange("(c p) f -> p c f", p=128)[:, c0:c0 + gc, :], writes=[f"gf{sfx}"])
            p.add("dve", lambda e, T=T, gb=gb, gc=gc: e.tensor_scalar(out=T["gi"][:, 0:gc], in0=T["gf"][:, 0:gc, 0], scalar1=gb[:, 0:1],
                                                                     scalar2=None, op0=ALU.add), [f"gf{sfx}", f"gb{s}"], [f"gi{sfx}"])
            p.add("dve", lambda e, T=T, gb=gb, gc=gc: e.tensor_scalar(out=T["sp"][:, 0:gc], in0=T["gf"][:, 0:gc, 1], scalar1=gb[:, 1:2],
                                                                     scalar2=None, op0=ALU.add), [f"gf{sfx}", f"gb{s}"], [f"sp{sfx}"])
            p.add("act", lambda e, T=T, gc=gc: e.activation(out=T["sp"][:, 0:gc], in_=T["sp"][:, 0:gc], func=AF.Exp, scale=-1.0),
                  [f"sp{sfx}"], [f"sp{sfx}"])
            p.add("dve", lambda e, T=T, gc=gc: e.tensor_scalar(out=T["sp"][:, 0:gc], in0=T["sp"][:, 0:gc], scalar1=1.0, scalar2=None,
                                                              op0=ALU.add), [f"sp{sfx}"], [f"sp{sfx}"])
            p.add("act", lambda e, T=T, gc=gc: e.activation(out=T["sp"][:, 0:gc], in_=T["sp"][:, 0:gc], func=AF.Ln),
                  [f"sp{sfx}"], [f"sp{sfx}"])
            p.add("pe", lambda e, T=T, gc=gc, bP=bP: e.matmul(bP[:, 0:gc], lhsT=tri[:], rhs=T["sp"][:, 0:gc], start=True, stop=True),
                  ["tri", f"sp{sfx}"], ["bank6"])
            p.add("pe", lambda e, T=T, gc=gc, bP=bP: e.matmul(bP[:, 64:64 + gc], lhsT=ones[:], rhs=T["sp"][:, 0:gc], start=True, stop=True),
                  ["ones", f"sp{sfx}"], ["bank6"])
            p.add("act", lambda e, T=T, gc=gc, bP=bP: e.activation(out=T["a"][:, 0:gc], in_=bP[:, 0:gc], func=AF.Exp, scale=-1.0),
                  ["bank6"], [f"a{sfx}"])
            p.add("act", lambda e, T=T, gc=gc, bP=bP: e.activation(out=T["eG"][:, 0:gc], in_=bP[:, 64:64 + gc], func=AF.Exp, scale=-1.0),
                  ["bank6"], [f"eG{sfx}"])
            p.add("act", lambda e, T=T, gc=gc, bP=bP: e.activation(out=T["b"][:, 0:gc], in_=bP[:, 0:gc], func=AF.Identity),
                  ["bank6"], [f"b{sfx}"])
            p.add("dve", lambda e, T=T, gc=gc: e.tensor_tensor(out=T["b"][:, 0:gc], in0=T["b"][:, 0:gc], in1=T["gi"][:, 0:gc], op=ALU.add),
                  [f"b{sfx}", f"gi{sfx}"], [f"b{sfx}"])
            p.add("act", lambda e, T=T, gc=gc: e.activation(out=T["b"][:, 0:gc], in_=T["b"][:, 0:gc], func=AF.Exp),
                  [f"b{sfx}"], [f"b{sfx}"])
            p.add("act", lambda e, T=T, W=W: e.activation(out=T["qTb"][:, 0:W], in_=T["qTf"][:, 0:W], func=AF.Copy),
                  [f"qTf{sfx}"], [f"qTb{sfx}"])
            p.add("dve", lambda e, T=T, W=W: e.tensor_scalar(out=T["kTb"][:, 0:W], in0=T["kTf"][:, 0:W], scalar1=0.125, scalar2=None,
                                                            op0=ALU.mult), [f"kTf{sfx}"], [f"kTb{sfx}"])
            p.add("act", lambda e, T=T, gc=gc: e.activation(out=T["vaug"][:, 0:gc, 0:64], in_=T["vkf"][:, 0:gc, 0:64], func=AF.Copy),
                  [f"vkf{sfx}"], [f"vaug{sfx}"])
            p.add("dve", lambda e, T=T, gc=gc: e.scalar_tensor_tensor(
                out=T["kb"][:, 0:gc, :], in0=T["vkf"][:, 0:gc, 64:128], scalar=0.125,
                in1=T["b"][:, 0:gc, None].to_broadcast([128, gc, 64]), op0=ALU.mult, op1=ALU.mult),
                [f"vkf{sfx}", f"b{sfx}"], [f"kb{sfx}"])
            for cc in range(gc):
                cs = slice(cc * 128, (cc + 1) * 128)
                p.add("pe", lambda e, T=T, cs=cs, bS=bS: e.matmul(bS[:, 0:128], lhsT=T["kTb"][:, cs], rhs=T["qTb"][:, cs],
                                                                  start=True, stop=True), [f"kTb{sfx}", f"qTb{sfx}"], [kS])
                p.add("dve", lambda e, T=T, cc=cc, bS=bS: e.scalar_tensor_tensor(
                    out=T["WT"][:], in0=bS[:, 0:128], scalar=T["b"][:, cc:cc + 1], in1=tri[:], op0=ALU.mult, op1=ALU.mult),
                    [kS, f"b{sfx}", "tri"], [f"WT{sfx}"])
                p.add("pe", lambda e, T=T, cc=cc, bX=bX: e.matmul(bX[:, 0:65], lhsT=T["WT"][:], rhs=T["vaug"][:, cc, :],
                                                                  start=True, stop=False), [f"WT{sfx}", f"vaug{sfx}", f"vaug1_{sfx}"], [kX])
                p.add("pe", lambda e, T=T, cs=cs, bX=bX, Cb=Cb: e.matmul(bX[:, 0:65], lhsT=T["qTb"][:, cs], rhs=Cb[:],
                                                                         start=False, stop=True), [f"qTb{sfx}", f"Cb{s}"], [kX])
                p.add("pe", lambda e, T=T, cc=cc, bU=bU: e.matmul(bU[0:64, 0:65], lhsT=T["kb"][:, cc, :], rhs=T["vaug"][:, cc, :],
                                                                  start=True, stop=True), [f"kb{sfx}", f"vaug{sfx}", f"vaug1_{sfx}"], [kU])
                d = T["d"]
                p.add("act", lambda e, T=T, cc=cc, bX=bX, d=d: e.activation(
                    out=d[:, 0:1], in_=bX[:, 64:65], func=AF.Abs, scale=T["a"][:, cc:cc + 1]),
                    [kX, f"a{sfx}"], [f"d0{sfx}"])
                p.add("dve", lambda e, d=d: e.tensor_scalar(out=d[:, 3:4], in0=d[:, 0:1], scalar1=1.0, scalar2=None, op0=ALU.max),
                      [f"d0{sfx}"], [f"d3{sfx}"])
                p.add("dve", lambda e, d=d: e.reciprocal(out=d[:, 1:2], in_=d[:, 3:4]), [f"d3{sfx}"], [f"d1{sfx}"])
                p.add("dve", lambda e, T=T, cc=cc, d=d: e.tensor_tensor(out=d[:, 2:3], in0=d[:, 1:2], in1=T["a"][:, cc:cc + 1], op=ALU.mult),
                      [f"d1{sfx}", f"a{sfx}"], [f"d2{sfx}"])
                p.add("dve", lambda e, T=T, cc=cc, bX=bX, d=d: e.tensor_scalar(
                    out=T["h"][:, cc, :], in0=bX[:, 0:64], scalar1=d[:, 2:3], scalar2=None, op0=ALU.mult),
                    [kX, f"d2{sfx}"], [f"h{sfx}"])
                p.add("dve", lambda e, bU=bU, Cf=Cf, tmpC=tmpC: e.tensor_tensor(out=tmpC[:], in0=bU[0:64, 0:65], in1=Cf[:], op=ALU.add),
                      [kU, f"Cf{s}"], [f"tmpC{s}"])
                p.add("dve", lambda e, T=T, cc=cc, Cf=Cf, tmpC=tmpC: e.tensor_scalar(
                    out=Cf[:], in0=tmpC[:], scalar1=T["eG"][0:64, cc:cc + 1], scalar2=None, op0=ALU.mult),
                    [f"tmpC{s}", f"eG{sfx}"], [f"Cf{s}"])
                p.add("act", lambda e, T=T, cc=cc, Cb=Cb, tmpC=tmpC: e.activation(
                    out=Cb[:], in_=tmpC[:], func=AF.Copy, scale=T["eG"][0:64, cc:cc + 1]),
                    [f"tmpC{s}", f"eG{sfx}"], [f"Cb{s}"])
            p.dma(mh[s].rearrange("(c p) f -> p c f", p=128)[:, c0:c0 + gc, :], T["h"][:, 0:gc, :], reads=[f"h{sfx}"])
    p.emit()
    return nc


def build_M2(NKT, NCT, NSL=2):
    nc = bass.Bass("TRN2", target_bir_lowering=False)
    L = NKT * 128
    aqT = _din(nc, "aqT", [NSL, 2, 64, L])
    akT = _din(nc, "akT", [NSL, 2, 64, L])
    av = _din(nc, "av", [NSL, L, 128])
    dl_d = _din(nc, "dl", [128, 256])
    dng_d = _din(nc, "dng", [128, 128])
    lami_d = _din(nc, "lami", [128, 2])
    ones_d = _din(nc, "ones", [128, 128])
    ao = _dout(nc, "ao", [NSL, L, 128])
    p = Prog(nc)
    ones = p.sb([128, 128], name="ones_sb"); p.dma(ones[:], ones_d, writes=["ones"])
    dl = p.sb([128, 256], name="dl_sb"); p.dma(dl[:], dl_d, writes=["dl"])
    gsc = p.sb([128, 128], name="gsc"); p.dma(gsc[:], dng_d, writes=["gsc"])
    lami = p.sb([128, 2], name="lami_sb"); p.dma(lami[:], lami_d, writes=["lami"])
    junk = p.sb([128, 128], name="junk")
    lm = p.sb([128, 4], name="lm")
    p.add("dve", lambda e: e.scalar_tensor_tensor(out=junk[:, 0:64], in0=dl[:, 0:64], scalar=1.0, in1=dl[:, 64:128],
                                                  op0=ALU.mult, op1=ALU.mult, accum_out=lm[:, 0:1]), ["dl"], ["junk", "lm0"])
    p.add("dve", lambda e: e.scalar_tensor_tensor(out=junk[:, 64:128], in0=dl[:, 128:192], scalar=1.0, in1=dl[:, 192:256],
                                                  op0=ALU.mult, op1=ALU.mult, accum_out=lm[:, 1:2]), ["dl"], ["junk", "lm1"])
    p.add("act", lambda e: e.activation(out=lm[:, 0:2], in_=lm[:, 0:2], func=AF.Exp), ["lm0", "lm1"], ["lm01"])
    p.add("dve", lambda e: e.tensor_tensor(out=lm[:, 2:3], in0=lm[:, 1:2], in1=lm[:, 0:1], op=ALU.subtract), ["lm01"], ["lm2"])
    p.add("dve", lambda e: e.tensor_scalar(out=lm[:, 3:4], in0=lm[:, 2:3], scalar1=lami[:, 0:1], scalar2=None, op0=ALU.subtract),
          ["lm2", "lami"], ["nlam"])
    p.add("dve", lambda e: e.tensor_scalar(out=gsc[:], in0=gsc[:], scalar1=lami[:, 1:2], scalar2=None, op0=ALU.mult),
          ["gsc", "lami"], ["gsc"])
    banks = [p.ps([128, 512], name=f"bank{i}") for i in range(8)]
    qTb = p.sb([64, 2, L], BF16, name="qTb")
    kTb = p.sb([64, 2, L], BF16, name="kTb")
    vaug = p.sb([128, NKT, 129], BF16, name="vaug")
    p.add("dve", lambda e: e.memset(vaug[:, :, 128:129], 1.0), [], ["vaug1"])
    PW = 2048
    stg = [p.sb([64, PW], name=f"stg{i}") for i in range(2)]
    sq = p.sb([64, PW], name="sq")
    VG = 16
    vst = [p.sb([128, VG, 128], name=f"vst{i}") for i in range(2)]
    mx = p.sb([128, 8], name="mx")
    nb = p.sb([128, 4], name="nb")
    pT = [p.sb([128, 512], BF16, name=f"pT{i}") for i in range(3)]
    ev = [p.sb([128, 8], name=f"ev{i}") for i in range(2)]
    o1 = [p.sb([128, 128], name=f"o1_{i}") for i in range(2)]
    oo = [p.sb([128, 128], name=f"oo_{i}") for i in range(2)]
    yy = [p.sb([128, 128], name=f"yy_{i}") for i in range(2)]
    nstg = 0
    nvst = 0
    nit = 0
    nblk = 0
    nev = 0
    for s in range(NSL):
        p.add("dve", lambda e: e.memset(mx[:], 0.0), [], ["mx", "mx4"])
        for which, (src, dst) in enumerate(((aqT, qTb), (akT, kTb))):
            for comp in range(2):
                for c0 in range(0, L, PW):
                    cw = min(PW, L - c0)
                    si = nstg % 2; nstg += 1
                    st = stg[si]
                    p.dma(st[:, 0:cw], src[s, comp, :, c0:c0 + cw], writes=[f"stg{si}"])
                    p.add("dve", lambda e, st=st, dst=dst, comp=comp, c0=c0, cw=cw: e.tensor_copy(out=dst[:, comp, c0:c0 + cw], in_=st[:, 0:cw]),
                          [f"stg{si}"], ["qTb" if which == 0 else "kTb"])
                    p.add("act", lambda e, st=st, cw=cw: e.activation(out=sq[:, 0:cw], in_=st[:, 0:cw], func=AF.Square), [f"stg{si}"], ["sq"])
                    for b0 in range(0, cw, 512):
                        bw = min(512, cw - b0)
                        p.add("pe", lambda e, b0=b0, bw=bw: e.matmul(banks[6][:, 0:bw], lhsT=ones[0:64, :], rhs=sq[:, b0:b0 + bw], start=True, stop=True),
                              ["ones", "sq"], ["bank6"])
                        p.add("dve", lambda e, bw=bw: e.reduce_max(out=mx[:, 4:5], in_=banks[6][:, 0:bw], axis=AX.X), ["bank6"], ["mx4"])
                        col = which * 2 + comp
                        p.add("dve", lambda e, col=col: e.tensor_tensor(out=mx[:, col:col + 1], in0=mx[:, col:col + 1], in1=mx[:, 4:5], op=ALU.max),
                              ["mx4", "mx"], ["mx"])
        for g0 in range(0, NKT, VG):
            gn = min(VG, NKT - g0)
            vi = nvst % 2; nvst += 1
            p.dma(vst[vi][:, 0:gn, :], av[s].rearrange("(c p) f -> p c f", p=128)[:, g0:g0 + gn, :], writes=[f"vst{vi}"])
            p.add("act", lambda e, vi=vi, g0=g0, gn=gn: e.activation(out=vaug[:, g0:g0 + gn, 0:128], in_=vst[vi][:, 0:gn, :], func=AF.Copy),
                  [f"vst{vi}"], ["vaug"])
        p.add("dve", lambda e: e.tensor_tensor(out=nb[:, 2:4], in0=mx[:, 0:2], in1=mx[:, 2:4], op=ALU.mult), ["mx"], ["nb2"])
        p.add("act", lambda e: e.activation(out=nb[:, 2:4], in_=nb[:, 2:4], func=AF.Sqrt, scale=1.0 / 64.0), ["nb2"], ["nb2"])
        p.add("dve", lambda e: e.tensor_scalar(out=nb[:, 0:2], in0=nb[:, 2:4], scalar1=60.0, scalar2=-1.0, op0=ALU.min, op1=ALU.mult),
              ["nb2"], ["nb"])
        blocks = [(0, NCT, 0, NCT)]
        for q0 in range(NCT, NKT, 4):
            blocks.append((q0, min(4, NKT - q0), 0, NKT))
        for (q0, nq, k0, nk) in blocks:
            oset = nblk % 2; nblk += 1
            obanks = [banks[oset * 3 + i] for i in range(3)]
            okeys = [f"bank{oset*3+i}" for i in range(3)]
            for i in range(3):
                p.add("dve", lambda e, i=i, obanks=obanks: e.memset(obanks[i][:], 0.0), [], [okeys[i]])

            def oacc(comp, j):
                a = comp * 4 + j
                return obanks[a // 3][:, (a % 3) * 129:(a % 3) * 129 + 129], okeys[a // 3]
            QW = nq * 128
            for kt in range(k0, k0 + nk):
                for comp in range(2):
                    sb_i = 6 + nit % 2
                    pi = nit % 3
                    nit += 1
                    p.add("pe", lambda e, comp=comp, kt=kt, sb_i=sb_i, q0=q0, QW=QW: e.matmul(
                        banks[sb_i][:, 0:QW], lhsT=kTb[:, comp, kt * 128:(kt + 1) * 128], rhs=qTb[:, comp, q0 * 128:q0 * 128 + QW],
                        start=True, stop=True), ["kTb", "qTb"], [f"bank{sb_i}"])
                    p.add("act", lambda e, comp=comp, sb_i=sb_i, pi=pi, QW=QW: e.activation(
                        out=pT[pi][:, 0:QW], in_=banks[sb_i][:, 0:QW], func=AF.Exp, scale=0.125, bias=nb[:, comp:comp + 1]),
                        [f"bank{sb_i}", "nb"], [f"pT{pi}"])
                    for j in range(nq):
                        oap, okey = oacc(comp, j)
                        p.add("pe", lambda e, oap=oap, pi=pi, j=j, kt=kt: e.matmul(
                            oap, lhsT=pT[pi][:, j * 128:(j + 1) * 128], rhs=vaug[:, kt, :], start=False, stop=False,
                            skip_group_check=True), [f"pT{pi}", "vaug", "vaug1"], [okey])
            for j in range(nq):
                ei = nev % 2; nev += 1
                E = ev[ei]
                (o1ap, k1), (o2ap, k2) = oacc(0, j), oacc(1, j)
                sfx = f"_{ei}"
                p.add("dve", lambda e, E=E, o1ap=o1ap: e.reciprocal(out=E[:, 0:1], in_=o1ap[:, 128:129]), [k1], ["ev0" + sfx])
                p.add("dve", lambda e, E=E, o2ap=o2ap: e.reciprocal(out=E[:, 1:2], in_=o2ap[:, 128:129]), [k2], ["ev1" + sfx])
                p.add("dve", lambda e, E=E: e.tensor_tensor(out=E[:, 2:3], in0=E[:, 1:2], in1=lm[:, 3:4], op=ALU.mult),
                      ["ev1" + sfx, "nlam"], ["ev2" + sfx])
                p.add("dve", lambda e, E=E, o1ap=o1ap, ei=ei: e.tensor_scalar(out=o1[ei][:], in0=o1ap[:, 0:128], scalar1=E[:, 0:1], scalar2=None,
                                                                           op0=ALU.mult), [k1, "ev0" + sfx], ["o1" + sfx])
                p.add("dve", lambda e, E=E, o2ap=o2ap, ei=ei: e.scalar_tensor_tensor(
                    out=oo[ei][:], in0=o2ap[:, 0:128], scalar=E[:, 2:3], in1=o1[ei][:], op0=ALU.mult, op1=ALU.add),
                    [k2, "ev2" + sfx, "o1" + sfx], ["oo" + sfx])
                p.add("dve", lambda e, E=E, ei=ei: e.scalar_tensor_tensor(
                    out=junk[:], in0=oo[ei][:], scalar=1.0, in1=oo[ei][:], op0=ALU.mult, op1=ALU.mult, accum_out=E[:, 3:4]),
                    ["oo" + sfx], ["junk", "ev3" + sfx])
                p.add("dve", lambda e, E=E: e.tensor_scalar(out=E[:, 4:5], in0=E[:, 3:4], scalar1=1.0 / 128.0, scalar2=EPS,
                                                            op0=ALU.mult, op1=ALU.add), ["ev3" + sfx], ["ev4" + sfx])
                p.add("act", lambda e, E=E: e.activation(out=E[:, 5:6], in_=E[:, 4:5], func=AF.Sqrt), ["ev4" + sfx], ["ev5" + sfx])
                p.add("dve", lambda e, E=E: e.reciprocal(out=E[:, 6:7], in_=E[:, 5:6]), ["ev5" + sfx], ["ev6" + sfx])
                p.add("dve", lambda e, E=E, ei=ei: e.scalar_tensor_tensor(
                    out=yy[ei][:], in0=oo[ei][:], scalar=E[:, 6:7], in1=gsc[:], op0=ALU.mult, op1=ALU.mult),
                    ["oo" + sfx, "ev6" + sfx, "gsc"], ["yy" + sfx])
                r0 = (q0 + j) * 128
                p.dma(ao[s, r0:r0 + 128, :], yy[ei][:], reads=["yy" + sfx])
    p.emit()
    return nc


def build_C1(segs):
    nc = bass.Bass("TRN2", target_bir_lowering=False)
    W = sum(n + 30 for n in segs)
    NT = sum(segs)
    aT = _din(nc, "aT", [512, W])
    cw_d = _din(nc, "cw", [128, 2, 31])
    cp_d = _din(nc, "cp", [128, 2, 3])
    ones_d = _din(nc, "ones", [128, 128])
    co = _dout(nc, "convT", [2, 128, NT])
    p = Prog(nc)
    ones = p.sb([128, 128], name="ones_sb"); p.dma(ones[:], ones_d, writes=["ones"])
    cw = p.sb([128, 2, 31], name="cw_sb"); p.dma(cw[:], cw_d, writes=["cw"])
    cp = p.sb([128, 2, 3], name="cp_sb"); p.dma(cp[:], cp_d, writes=["cp"])
    N = 512
    a1 = [p.sb([128, N + 30], name=f"a1_{i}") for i in range(2)]
    a2 = [p.sb([128, N + 30], name=f"a2_{i}") for i in range(2)]
    u = [p.sb([128, N + 30], name=f"u_{i}") for i in range(2)]
    acc = [[p.sb([128, N], name=f"acc_{i}_{r}") for r in range(2)] for i in range(2)]
    y = [p.sb([128, N], name=f"y_{i}") for i in range(2)]
    ysq = [p.sb([128, N], name=f"ysq_{i}") for i in range(2)]
    mean = p.sb([128, N], name="mean"); msq = p.sb([128, N], name="msq"); rstd = p.sb([128, N], name="rstd")
    zz = [p.sb([128, N], name=f"zz_{i}") for i in range(2)]
    bA = p.ps([128, 512], name="bankA"); bB = p.ps([128, 512], name="bankB")
    off = 0
    tok0 = 0
    for ns in segs:
        for t0 in range(0, ns, N):
            n = min(N, ns - t0)
            for j in range(2):
                c0 = off + t0
                p.dma(a1[j][:, 0:n + 30], aT[j * 128:(j + 1) * 128, c0:c0 + n + 30], writes=[f"a1_{j}"])
                p.dma(a2[j][:, 0:n + 30], aT[256 + j * 128:256 + (j + 1) * 128, c0:c0 + n + 30], writes=[f"a2_{j}"])
                p.add("act", lambda e, j=j, n=n: e.activation(out=a2[j][:, 0:n + 30], in_=a2[j][:, 0:n + 30], func=AF.Sigmoid),
                      [f"a2_{j}"], [f"a2_{j}"])
                p.add("dve", lambda e, j=j, n=n: e.tensor_tensor(out=u[j][:, 0:n + 30], in0=a1[j][:, 0:n + 30], in1=a2[j][:, 0:n + 30], op=ALU.mult),
                      [f"a1_{j}", f"a2_{j}"], [f"u_{j}"])
                p.add("dve", lambda e, j=j, n=n: e.tensor_scalar(out=acc[j][0][:, 0:n], in0=u[j][:, 0:n], scalar1=cw[:, j, 0:1], scalar2=None,
                                                                op0=ALU.mult), [f"u_{j}", "cw"], [f"acc_{j}_0"])
                for k in range(1, 31):
                    src, dst = acc[j][(k - 1) % 2], acc[j][k % 2]
                    p.add("dve", lambda e, j=j, n=n, k=k, src=src, dst=dst: e.scalar_tensor_tensor(
                        out=dst[:, 0:n], in0=u[j][:, k:k + n], scalar=cw[:, j, k:k + 1], in1=src[:, 0:n], op0=ALU.mult, op1=ALU.add),
                        [f"u_{j}", "cw", f"acc_{j}_{(k-1)%2}"], [f"acc_{j}_{k%2}"])
                p.add("dve", lambda e, j=j, n=n: e.tensor_scalar(out=y[j][:, 0:n], in0=acc[j][0][:, 0:n], scalar1=cp[:, j, 0:1], scalar2=None,
                                                                op0=ALU.add), [f"acc_{j}_0", "cp"], [f"y_{j}"])
                p.add("act", lambda e, j=j, n=n: e.activation(out=ysq[j][:, 0:n], in_=y[j][:, 0:n], func=AF.Square), [f"y_{j}"], [f"ysq_{j}"])
            for j in range(2):
                p.add("pe", lambda e, j=j, n=n: e.matmul(bA[:, 0:n], lhsT=ones[:], rhs=y[j][:, 0:n], start=(j == 0), stop=(j == 1)),
                      ["ones", f"y_{j}"], ["bankA"])
            for j in range(2):
                p.add("pe", lambda e, j=j, n=n: e.matmul(bB[:, 0:n], lhsT=ones[:], rhs=ysq[j][:, 0:n], start=(j == 0), stop=(j == 1)),
                      ["ones", f"ysq_{j}"], ["bankB"])
            p.add("act", lambda e, n=n: e.activation(out=mean[:, 0:n], in_=bA[:, 0:n], func=AF.Copy, scale=1.0 / 256), ["bankA"], ["mean"])
            p.add("act", lambda e, n=n: e.activation(out=msq[:, 0:n], in_=bA[:, 0:n], func=AF.Square, scale=1.0 / 256), ["bankA"], ["msq"])
            p.add("dve", lambda e, n=n: e.scalar_tensor_tensor(out=rstd[:, 0:n], in0=bB[:, 0:n], scalar=1.0 / 256, in1=msq[:, 0:n],
                                                               op0=ALU.mult, op1=ALU.subtract), ["bankB", "msq"], ["rstd"])
            p.add("dve", lambda e, n=n: e.tensor_scalar(out=rstd[:, 0:n], in0=rstd[:, 0:n], scalar1=EPS, scalar2=None, op0=ALU.add),
                  ["rstd"], ["rstd"])
            p.add("act", lambda e, n=n: e.activation(out=rstd[:, 0:n], in_=rstd[:, 0:n], func=AF.Sqrt), ["rstd"], ["rstd"])
            p.add("dve", lambda e, n=n: e.reciprocal(out=rstd[:, 0:n], in_=rstd[:, 0:n]), ["rstd"], ["rstd"])
            for j in range(2):
                p.add("dve", lambda e, j=j, n=n: e.tensor_tensor(out=zz[j][:, 0:n], in0=y[j][:, 0:n], in1=mean[:, 0:n], op=ALU.subtract),
                      [f"y_{j}", "mean"], [f"zz_{j}"])
                p.add("dve", lambda e, j=j, n=n: e.tensor_tensor(out=zz[j][:, 0:n], in0=zz[j][:, 0:n], in1=rstd[:, 0:n], op=ALU.mult),
                      [f"zz_{j}", "rstd"], [f"zz_{j}"])
                p.add("act", lambda e, j=j, n=n: e.activation(out=zz[j][:, 0:n], in_=zz[j][:, 0:n], func=AF.Silu,
                                                              scale=cp[:, j, 1:2], bias=cp[:, j, 2:3]), [f"zz_{j}", "cp"], [f"zz_{j}"])
                p.dma(co[j, :, tok0 + t0:tok0 + t0 + n], zz[j][:, 0:n], reads=[f"zz_{j}"])
        off += ns + 30
        tok0 += ns
    p.emit()
    return nc


def build_C2(NT, ctx_tiles=(0,)):
    nc = bass.Bass("TRN2", target_bir_lowering=False)
    L = NT * 128
    xs = _din(nc, "xs", [L, D])
    convT = _din(nc, "convT", [2, 128, L])
    hf_d = _din(nc, "hf", [L, 256])
    hb_d = _din(nc, "hb", [L, 256])
    mo_d = _din(nc, "mo", [L, 256])
    ao_d = _din(nc, "ao", [L, 512])
    mng_d = _din(nc, "mng", [128, 64])
    w_out = _din(nc, "w_out", [D, D])
    bc_d = _din(nc, "bc", [7, 128, D])
    ident = _din(nc, "ident", [128, 128])
    x1_o = _dout(nc, "x1", [L, D])
    h2_o = _dout(nc, "h2", [L, D])
    p = Prog(nc)
    wob = p.sb([128, 8, D], BF16, name="wob")
    load_cast_weight(p, wob, w_out, D, "wob")
    idt = p.sb([128, 128], name="idt"); p.dma(idt[:], ident, writes=["idt"])
    mng = p.sb([128, 64], name="mng_sb"); p.dma(mng[:], mng_d, writes=["mng"])
    bc = [p.sb([128, D], name=f"bc{i}") for i in range(7)]
    for i in range(7):
        p.dma(bc[i][:], bc_d[i], writes=[f"bc{i}"])
    for i in (3, 4):
        p.add("dve", lambda e, i=i: e.scalar_tensor_tensor(out=bc[i][:], in0=bc[i][:], scalar=1.0, in1=bc[0][:], op0=ALU.add, op1=ALU.mult),
              [f"bc{i}", "bc0"], [f"bc{i}"])
    NB = 2
    def mk(shape, nm, dt=F32):
        return [p.sb(shape, dt, name=f"{nm}{i}") for i in range(NB)]
    xt = mk([128, D], "xt"); hf = mk([128, 256], "hf"); hb = mk([128, 256], "hb"); mo = mk([128, 256], "mo")
    aot = mk([128, 512], "aot"); cvt = mk([128, 2, 128], "cvt"); ym = mk([128, 256], "ym"); sqm = mk([128, 256], "sqm")
    st4 = mk([128, 8], "st4"); mixT = mk([128, 8, 128], "mixT", BF16); tmp = mk([128, D], "tmp"); x1 = mk([128, D], "x1")
    h2 = mk([128, D], "h2"); ss = mk([128, 4], "ss")
    junk = p.sb([128, D], name="junk")
    bT = [p.ps([128, 512], name=f"bT{i}") for i in range(4)]
    bAcc = [p.ps([128, 512], name=f"bAcc{i}") for i in range(4)]
    for t in range(NT):
        i = t % NB
        isc = t in ctx_tiles
        r0 = t * 128
        g1b = bc[2] if isc else bc[1]
        gm2 = bc[4] if isc else bc[3]
        sh2 = bc[6] if isc else bc[5]
        kg1, kgm2, ksh2 = (f"bc{2 if isc else 1}", f"bc{4 if isc else 3}", f"bc{6 if isc else 5}")
        p.dma(xt[i][:], xs[r0:r0 + 128, :], writes=[f"xt{i}"])
        p.dma(hf[i][:], hf_d[r0:r0 + 128, :], writes=[f"hf{i}"])
        p.dma(hb[i][:], hb_d[r0:r0 + 128, :], writes=[f"hb{i}"])
        p.dma(mo[i][:], mo_d[r0:r0 + 128, :], writes=[f"mo{i}"])
        p.dma(aot[i][:], ao_d[r0:r0 + 128, :], writes=[f"aot{i}"])
        p.dma(cvt[i][:], convT[:, :, r0:r0 + 128].rearrange("j p n -> p j n"), writes=[f"cvt{i}"])
        p.add("dve", lambda e, i=i: e.tensor_tensor(out=ym[i][:], in0=hf[i][:], in1=hb[i][:], op=ALU.add), [f"hf{i}", f"hb{i}"], [f"ym{i}"])
        p.add("act", lambda e, i=i: e.activation(out=sqm[i][:], in_=ym[i][:], func=AF.Square), [f"ym{i}"], [f"sqm{i}"])
        p.add("dve", lambda e, i=i: e.tensor_reduce(out=st4[i][:, 0:4], in_=sqm[i][:].rearrange("p (h d) -> p h d", h=4), axis=AX.X, op=ALU.add),
              [f"sqm{i}"], [f"st4a{i}"])
        p.add("dve", lambda e, i=i: e.tensor_scalar(out=st4[i][:, 4:8], in0=st4[i][:, 0:4], scalar1=1.0 / 64, scalar2=EPS, op0=ALU.mult, op1=ALU.add),
              [f"st4a{i}"], [f"st4b{i}"])
        p.add("act", lambda e, i=i: e.activation(out=st4[i][:, 0:4], in_=st4[i][:, 4:8], func=AF.Sqrt), [f"st4b{i}"], [f"st4a{i}"])
        p.add("dve", lambda e, i=i: e.reciprocal(out=st4[i][:, 4:8], in_=st4[i][:, 0:4]), [f"st4a{i}"], [f"st4b{i}"])
        p.add("act", lambda e, i=i: e.activation(out=mo[i][:], in_=mo[i][:], func=AF.Sigmoid), [f"mo{i}"], [f"mo{i}"])
        ym3 = ym[i][:].rearrange("p (h d) -> p h d", h=4)
        p.add("dve", lambda e, i=i, ym3=ym3: e.tensor_tensor(out=ym3, in0=ym3, in1=st4[i][:, 4:8, None].to_broadcast([128, 4, 64]), op=ALU.mult),
              [f"ym{i}", f"st4b{i}"], [f"ym{i}"])
        p.add("dve", lambda e, i=i, ym3=ym3: e.tensor_tensor(out=ym3, in0=ym3, in1=mng[:, None, :].to_broadcast([128, 4, 64]), op=ALU.mult),
              [f"ym{i}", "mng"], [f"ym{i}"])
        p.add("dve", lambda e, i=i: e.tensor_tensor(out=ym[i][:], in0=ym[i][:], in1=mo[i][:], op=ALU.mult), [f"ym{i}", f"mo{i}"], [f"ym{i}"])
        p.add("act", lambda e, i=i: e.activation(out=mixT[i][:, 0:2, :], in_=cvt[i][:], func=AF.Copy), [f"cvt{i}"], [f"mixT{i}_c"])
        ta, tb = (2 * t) % 4, (2 * t + 1) % 4
        srcs = [(ym[i], 0, f"ym{i}"), (ym[i], 128, f"ym{i}"), (aot[i], 0, f"aot{i}"), (aot[i], 128, f"aot{i}"),
                (aot[i], 256, f"aot{i}"), (aot[i], 384, f"aot{i}")]
        for n_, (src, c0, key) in enumerate(srcs):
            bank = ta if n_ < 4 else tb
            col = (n_ % 4) * 128
            p.add("pe", lambda e, src=src, c0=c0, bank=bank, col=col: e.transpose(bT[bank][:, col:col + 128], src[:, c0:c0 + 128], idt[:]),
                  [key, "idt"], [f"bT{bank}"])
        p.add("act", lambda e, i=i, ta=ta: e.activation(out=mixT[i][:, 2:6, :], in_=bT[ta][:].rearrange("p (k n) -> p k n", k=4), func=AF.Copy),
              [f"bT{ta}"], [f"mixT{i}_a"])
        p.add("dve", lambda e, i=i, tb=tb: e.tensor_copy(out=mixT[i][:, 6:8, :], in_=bT[tb][:, 0:256].rearrange("p (k n) -> p k n", k=2)),
              [f"bT{tb}"], [f"mixT{i}_b"])
        for blk in range(2):
            a = (2 * t + blk) % 4
            for k in range(8):
                p.add("pe", lambda e, i=i, k=k, a=a, blk=blk: e.matmul(bAcc[a][:], lhsT=mixT[i][:, k, :], rhs=wob[:, k, blk * 512:(blk + 1) * 512],
                                                                      start=(k == 0), stop=(k == 7)),
                      [f"mixT{i}_c", f"mixT{i}_a", f"mixT{i}_b", "wob"], [f"bAcc{a}"])
            cs = slice(blk * 512, (blk + 1) * 512)
            p.add("dve", lambda e, i=i, a=a, cs=cs, g1b=g1b: e.tensor_tensor(out=tmp[i][:, cs], in0=bAcc[a][:], in1=g1b[:, cs], op=ALU.mult),
                  [f"bAcc{a}", kg1], [f"tmp{i}_{blk}"])
            p.add("dve", lambda e, i=i, cs=cs: e.tensor_tensor(out=x1[i][:, cs], in0=tmp[i][:, cs], in1=xt[i][:, cs], op=ALU.add),
                  [f"tmp{i}_{blk}", f"xt{i}"], [f"x1{i}_{blk}"])
        p.dma(x1_o[r0:r0 + 128, :], x1[i][:], reads=[f"x1{i}_0", f"x1{i}_1"])
        p.add("act", lambda e, i=i: e.activation(out=junk[:], in_=x1[i][:], func=AF.Square, accum_out=ss[i][:, 0:1]),
              [f"x1{i}_0", f"x1{i}_1"], ["junk", f"ss0{i}"])
        p.add("dve", lambda e, i=i: e.tensor_scalar(out=ss[i][:, 1:2], in0=ss[i][:, 0:1], scalar1=1.0 / D, scalar2=EPS, op0=ALU.mult, op1=ALU.add),
              [f"ss0{i}"], [f"ss1{i}"])
        p.add("act", lambda e, i=i: e.activation(out=ss[i][:, 2:3], in_=ss[i][:, 1:2], func=AF.Sqrt), [f"ss1{i}"], [f"ss2{i}"])
        p.add("dve", lambda e, i=i: e.reciprocal(out=ss[i][:, 3:4], in_=ss[i][:, 2:3]), [f"ss2{i}"], [f"ss3{i}"])
        p.add("dve", lambda e, i=i, gm2=gm2: e.scalar_tensor_tensor(out=h2[i][:], in0=x1[i][:], scalar=ss[i][:, 3:4], in1=gm2[:],
                                                                     op0=ALU.mult, op1=ALU.mult),
              [f"x1{i}_0", f"x1{i}_1", f"ss3{i}", kgm2], [f"h2{i}"])
        p.add("dve", lambda e, i=i, sh2=sh2: e.tensor_tensor(out=h2[i][:], in0=h2[i][:], in1=sh2[:], op=ALU.add), [f"h2{i}", ksh2], [f"h2{i}"])
        p.dma(h2_o[r0:r0 + 128, :], h2[i][:], reads=[f"h2{i}"])
    p.emit()
    return nc


def build_C3(NT, ctx_tiles=(0,), NGB=4):
    nc = bass.Bass("TRN2", target_bir_lowering=False)
    L = NT * 128
    NE = 16384
    h2_d = _din(nc, "h2", [L, D])
    x1_d = _din(nc, "x1", [L, D])
    wq = _din(nc, "wq", [D, D])
    kbd_d = _din(nc, "kbd", [128, 8, 256])
    pu = _din(nc, "peer_u", [NE, D])
    pv = _din(nc, "peer_v", [NE, D])
    g2_d = _din(nc, "g2b", [2, 128, D])
    ident = _din(nc, "ident", [128, 128])
    x2_o = _dout(nc, "x2", [L, D])
    p = Prog(nc)
    wqb = p.sb([128, 8, D], F32, name="wqb")
    p.dma(wqb[:], wq.rearrange("(k p) n -> p k n", p=128), writes=["wqb"])
    idt = p.sb([128, 128], name="idt"); p.dma(idt[:], ident, writes=["idt"])
    kbd = p.sb([128, 8, 256], name="kbd_sb"); p.dma(kbd[:], kbd_d, writes=["kbd"])
    g2b = [p.sb([128, D], name=f"g2b{i}") for i in range(2)]
    for i in range(2):
        p.dma(g2b[i][:], g2_d[i], writes=[f"g2b{i}"])
    h2t = p.sb([128, D], name="h2t"); x1t = p.sb([128, D], name="x1t")
    h2T = p.sb([128, 8, 128], F32, name="h2T")
    qTf = p.sb([128, 8, 128], name="qTf")
    sc = p.sb([128, 16, 128], name="sc"); wk = p.sb([128, 16, 128], name="wk")
    st = p.sb([128, 16, 16], name="st"); it = p.sb([128, 16, 16], U32, name="it"); itf = p.sb([128, 16, 16], name="itf")
    cand = p.sb([128, 8, 256], name="cand"); cidx = p.sb([128, 8, 256], name="cidx"); wk2 = p.sb([128, 8, 256], name="wk2")
    best = p.sb([128, 8, 16], name="best"); gw = p.sb([128, 8, 16], name="gw")
    eidf = p.sb([128, 128], name="eidf"); eidi = p.sb([128, 128], I32, name="eidi")
    sm = p.sb([128, 16], name="sm")
    act = p.sb([128, 128], name="act_sb"); coef = p.sb([128, 128], name="coef")
    junk = p.sb([128, D], name="junk")
    acc = p.sb([128, D], name="acc"); x2t = p.sb([128, D], name="x2t")
    rows = [p.sb([128, D], name=f"rows{i}") for i in range(NGB)]
    banks = [p.ps([128, 512], name=f"bank{i}") for i in range(8)]
    ngat = 0
    for t in range(NT):
        isc = t in ctx_tiles
        r0 = t * 128
        gb = g2b[1] if isc else g2b[0]
        kgb = "g2b1" if isc else "g2b0"
        p.dma(h2t[:], h2_d[r0:r0 + 128, :], writes=["h2t"])
        p.dma(x1t[:], x1_d[r0:r0 + 128, :], writes=["x1t"])
        for half in range(2):
            for k in range(half * 4, half * 4 + 4):
                p.add("pe", lambda e, k=k, half=half: e.transpose(banks[half][:, (k % 4) * 128:(k % 4 + 1) * 128], h2t[:, k * 128:(k + 1) * 128], idt[:]),
                      ["h2t", "idt"], [f"bank{half}"])
        p.add("act", lambda e: e.activation(out=h2T[:, 0:4, :], in_=banks[0][:].rearrange("p (k n) -> p k n", k=4), func=AF.Copy), ["bank0"], ["h2T_a"])
        p.add("dve", lambda e: e.tensor_copy(out=h2T[:, 4:8, :], in_=banks[1][:].rearrange("p (k n) -> p k n", k=4)), ["bank1"], ["h2T_b"])
        for c in range(8):
            bk = 2 + c // 4
            for k in range(8):
                p.add("pe", lambda e, c=c, k=k, bk=bk: e.matmul(banks[bk][:, (c % 4) * 128:(c % 4 + 1) * 128], lhsT=wqb[:, k, c * 128:(c + 1) * 128],
                                                              rhs=h2T[:, k, :], start=(k == 0), stop=(k == 7)),
                      ["wqb", "h2T_a", "h2T_b"], [f"bank{bk}"])
        p.add("act", lambda e: e.activation(out=qTf[:, 0:4, :], in_=banks[2][:].rearrange("p (k n) -> p k n", k=4), func=AF.Copy), ["bank2"], ["qTf_a"])
        p.add("dve", lambda e: e.tensor_copy(out=qTf[:, 4:8, :], in_=banks[3][:].rearrange("p (k n) -> p k n", k=4)), ["bank3"], ["qTf_b"])
        for h in range(8):
            bk = 4 + h // 2
            p.add("pe", lambda e, h=h, bk=bk: e.matmul(banks[bk][:, (h % 2) * 256:(h % 2 + 1) * 256], lhsT=qTf[:, h, :], rhs=kbd[:, h, :],
                                                      start=True, stop=True), ["qTf_a", "qTf_b", "kbd"], [f"bank{bk}"])
        for bk in range(4, 8):
            g0 = (bk - 4) * 4
            if bk % 2 == 0:
                p.add("act", lambda e, bk=bk, g0=g0: e.activation(out=sc[:, g0:g0 + 4, :], in_=banks[bk][:].rearrange("p (g n) -> p g n", g=4), func=AF.Copy),
                      [f"bank{bk}"], [f"sc{bk}"])
            else:
                p.add("dve", lambda e, bk=bk, g0=g0: e.tensor_copy(out=sc[:, g0:g0 + 4, :], in_=banks[bk][:].rearrange("p (g n) -> p g n", g=4)),
                      [f"bank{bk}"], [f"sc{bk}"])
        for g in range(16):
            ks = f"sc{4 + g // 4}"
            p.add("dve", lambda e, g=g: e.max(out=st[:, g, 0:8], in_=sc[:, g, :]), [ks], [f"st{g}a"])
            p.add("dve", lambda e, g=g: e.max_index(out=it[:, g, 0:8], in_max=st[:, g, 0:8], in_values=sc[:, g, :]), [ks, f"st{g}a"], [f"it{g}a"])
            p.add("dve", lambda e, g=g: e.match_replace(out=wk[:, g, :], in_to_replace=st[:, g, 0:8], in_values=sc[:, g, :], imm_value=-1e30),
                  [ks, f"st{g}a"], [f"wk{g}"])
            p.add("dve", lambda e, g=g: e.max(out=st[:, g, 8:16], in_=wk[:, g, :]), [f"wk{g}"], [f"st{g}b"])
            p.add("dve", lambda e, g=g: e.max_index(out=it[:, g, 8:16], in_max=st[:, g, 8:16], in_values=wk[:, g, :]), [f"wk{g}", f"st{g}b"], [f"it{g}b"])
        allst = [f"st{g}{x}" for g in range(16) for x in "ab"]
        allit = [f"it{g}{x}" for g in range(16) for x in "ab"]
        p.add("dve", lambda e: e.tensor_copy(out=itf[:], in_=it[:]), allit, ["itf"])
        st4 = st[:].rearrange("p (h two) k -> p h two k", two=2)
        itf4 = itf[:].rearrange("p (h two) k -> p h two k", two=2)
        cand4 = cand[:].rearrange("p h (i j) -> p h i j", i=16)
        cidx4 = cidx[:].rearrange("p h (i j) -> p h i j", i=16)
        for h in range(8):
            p.add("dve", lambda e, h=h: e.tensor_tensor(out=cand4[:, h], in0=st4[:, h, 0, :, None].to_broadcast([128, 16, 16]),
                                                        in1=st4[:, h, 1, None, :].to_broadcast([128, 16, 16]), op=ALU.add), allst, [f"cand{h}"])
            p.add("dve", lambda e, h=h: e.scalar_tensor_tensor(out=cidx4[:, h], in0=itf4[:, h, 0, :, None].to_broadcast([128, 16, 16]), scalar=128.0,
                                                               in1=itf4[:, h, 1, None, :].to_broadcast([128, 16, 16]), op0=ALU.mult, op1=ALU.add),
                  ["itf"], [f"cidx{h}"])
            p.add("dve", lambda e, h=h: e.max(out=best[:, h, 0:8], in_=cand[:, h, :]), [f"cand{h}"], [f"best{h}a"])
            p.add("dve", lambda e, h=h: e.match_replace(out=wk2[:, h, :], in_to_replace=best[:, h, 0:8], in_values=cand[:, h, :], imm_value=-1e30),
                  [f"cand{h}", f"best{h}a"], [f"wk2{h}"])
            p.add("dve", lambda e, h=h: e.max(out=best[:, h, 8:16], in_=wk2[:, h, :]), [f"wk2{h}"], [f"best{h}b"])
            for k in range(16):
                hk = h * 16 + k
                p.add("dve", lambda e, h=h, k=k, hk=hk: e.scalar_tensor_tensor(
                    out=junk[:, 0:256], in0=cand[:, h, :], scalar=best[:, h, k:k + 1], in1=cidx[:, h, :], op0=ALU.is_equal, op1=ALU.mult,
                    accum_out=eidf[:, hk:hk + 1]), [f"cand{h}", f"cidx{h}", f"best{h}a", f"best{h}b"], ["junk", f"eidf{h}"])
        alle = [f"eidf{h}" for h in range(8)]
        allb = [f"best{h}{x}" for h in range(8) for x in "ab"]
        p.add("dve", lambda e: e.tensor_scalar(out=eidf[:], in0=eidf[:], scalar1=float(NE - 1), scalar2=0.0, op0=ALU.min, op1=ALU.max), alle, ["eidf"])
        p.add("dve", lambda e: e.tensor_copy(out=eidi[:], in_=eidf[:]), ["eidf"], ["eidi"])
        p.add("dve", lambda e: e.tensor_tensor(out=gw[:], in0=best[:], in1=best[:, :, 0:1].to_broadcast([128, 8, 16]), op=ALU.subtract), allb, ["gw"])
        p.add("act", lambda e: e.activation(out=gw[:], in_=gw[:], func=AF.Exp), ["gw"], ["gw"])
        p.add("dve", lambda e: e.tensor_reduce(out=sm[:, 0:8], in_=gw[:], axis=AX.X, op=ALU.add), ["gw"], ["sm0"])
        p.add("dve", lambda e: e.reciprocal(out=sm[:, 8:16], in_=sm[:, 0:8]), ["sm0"], ["sm1"])
        p.add("dve", lambda e: e.tensor_tensor(out=gw[:], in0=gw[:], in1=sm[:, 8:16, None].to_broadcast([128, 8, 16]), op=ALU.mult), ["gw", "sm1"], ["gw"])
        for hk in range(128):
            b = ngat % NGB; ngat += 1
            p.add("pool", lambda e, b=b, hk=hk: e.indirect_dma_start(
                out=rows[b][:], out_offset=None, in_=pu, in_offset=bass.IndirectOffsetOnAxis(ap=eidi[:, hk:hk + 1], axis=0)),
                ["eidi"], [f"rows{b}"])
            p.add("dve", lambda e, b=b, hk=hk: e.scalar_tensor_tensor(out=junk[:], in0=rows[b][:], scalar=1.0, in1=h2t[:], op0=ALU.mult, op1=ALU.mult,
                                                                     accum_out=act[:, hk:hk + 1]), [f"rows{b}", "h2t"], ["junk", "act"])
        p.add("act", lambda e: e.activation(out=coef[:], in_=act[:], func=AF.Gelu), ["act"], ["coef"])
        p.add("dve", lambda e: e.tensor_tensor(out=coef[:], in0=coef[:], in1=gw[:].rearrange("p h k -> p (h k)"), op=ALU.mult), ["coef", "gw"], ["coef"])
        p.add("dve", lambda e: e.memset(acc[:], 0.0), [], ["acc"])
        for hk in range(128):
            b = ngat % NGB; ngat += 1
            p.add("pool", lambda e, b=b, hk=hk: e.indirect_dma_start(
                out=rows[b][:], out_offset=None, in_=pv, in_offset=bass.IndirectOffsetOnAxis(ap=eidi[:, hk:hk + 1], axis=0)),
                ["eidi"], [f"rows{b}"])
            p.add("dve", lambda e, b=b, hk=hk: e.scalar_tensor_tensor(out=acc[:], in0=rows[b][:], scalar=coef[:, hk:hk + 1], in1=acc[:],
                                                                     op0=ALU.mult, op1=ALU.add), [f"rows{b}", "coef", "acc"], ["acc"])
        p.add("dve", lambda e, gb=gb: e.tensor_tensor(out=x2t[:], in0=acc[:], in1=gb[:], op=ALU.mult), ["acc", kgb], ["x2t"])
        p.add("dve", lambda e: e.tensor_tensor(out=x2t[:], in0=x2t[:], in1=x1t[:], op=ALU.add), ["x2t", "x1t"], ["x2t"])
        p.dma(x2_o[r0:r0 + 128, :], x2t[:], reads=["x2t"])
    p.emit()
    return nc


def build_F(NT):
    nc = bass.Bass("TRN2", target_bir_lowering=False)
    L = NT * 128
    xs = _din(nc, "xs", [L, D])
    fg_d = _din(nc, "fg", [128, D])
    yo = _dout(nc, "y", [L, D])
    p = Prog(nc)
    fg = p.sb([128, D], name="fg_sb"); p.dma(fg[:], fg_d, writes=["fg"])
    junk = p.sb([128, D], name="junk")
    NB = 2
    xt = [p.sb([128, D], name=f"xt{i}") for i in range(NB)]
    yt = [p.sb([128, D], name=f"yt{i}") for i in range(NB)]
    ss = [p.sb([128, 4], name=f"ss{i}") for i in range(NB)]
    for t in range(NT):
        i = t % NB
        r0 = t * 128
        p.dma(xt[i][:], xs[r0:r0 + 128, :], writes=[f"xt{i}"])
        p.add("act", lambda e, i=i: e.activation(out=junk[:], in_=xt[i][:], func=AF.Square, accum_out=ss[i][:, 0:1]), [f"xt{i}"], ["junk", f"ss0{i}"])
        p.add("dve", lambda e, i=i: e.tensor_scalar(out=ss[i][:, 1:2], in0=ss[i][:, 0:1], scalar1=1.0 / D, scalar2=EPS, op0=ALU.mult, op1=ALU.add),
              [f"ss0{i}"], [f"ss1{i}"])
        p.add("act", lambda e, i=i: e.activation(out=ss[i][:, 2:3], in_=ss[i][:, 1:2], func=AF.Sqrt), [f"ss1{i}"], [f"ss2{i}"])
        p.add("dve", lambda e, i=i: e.reciprocal(out=ss[i][:, 3:4], in_=ss[i][:, 2:3]), [f"ss2{i}"], [f"ss3{i}"])
        p.add("dve", lambda e, i=i: e.scalar_tensor_tensor(out=yt[i][:], in0=xt[i][:], scalar=ss[i][:, 3:4], in1=fg[:], op0=ALU.mult, op1=ALU.mult),
              [f"xt{i}", f"ss3{i}", "fg"], [f"yt{i}"])
        p.dma(yo[r0:r0 + 128, :], yt[i][:], reads=[f"yt{i}"])
    p.emit()
    return nc


_PROGS = {}


def _prog(name, fn):
    if name not in _PROGS:
        _PROGS[name] = fn()
    return _PROGS[name]


def _run(nc, maps):
    res = run_bass_kernel_spmd(nc, maps, core_ids=list(range(NCORES)))
    return res.results


def _rep(v, n=128):
    return np.ascontiguousarray(np.broadcast_to(np.asarray(v, np.float32)[None], (n, v.shape[0])))


def _rope_tables(n_tokens):
    rows = n_tokens // 64
    row = np.repeat(np.arange(rows), 64).astype(np.float32)
    col = np.tile(np.arange(64), rows).astype(np.float32)
    inv = (np.float32(10000.0) ** (-np.arange(0, 32, 2, dtype=np.float32) / np.float32(32))).astype(np.float32)
    ang = np.concatenate([row[:, None] * inv, col[:, None] * inv], axis=-1).astype(np.float32)
    return np.cos(ang).astype(np.float32), np.sin(ang).astype(np.float32)


def kernel_unfused(x, c, ctx, c_ctx, ada_w, ada_b, norm1_g, norm2_g, w_in, conv_w, conv_b, conv_ln_g, conv_ln_b,
           mlstm_gate_b, mlstm_norm_g, diff_lambda, diff_norm_g, w_out, peer_wq, peer_keys, peer_u, peer_v, final_g):
    f32 = np.float32
    x = np.asarray(x, f32); ctx = np.asarray(ctx, f32)
    B, S, _ = x.shape
    CT = ctx.shape[1]
    DEPTH = ada_w.shape[0]
    HS, HC = S // 2, CT // 2
    NT = (HS + HC) // 128
    NKT = (S + CT) // 128
    NCT = CT // 128
    ident = np.eye(128, dtype=f32)
    ones = np.ones((128, 128), f32)
    tri = np.triu(np.ones((128, 128), f32))
    cos, sin = _rope_tables(S)
    cores = [(cc // 2, cc % 2) for cc in range(NCORES)]

    cT = np.concatenate([np.asarray(c, f32), np.asarray(c_ctx, f32)[None]], 0).T
    cTl = np.ascontiguousarray(cT.reshape(8, 128, 5).transpose(1, 0, 2))
    maps = []
    for cc in range(NCORES):
        ll, half = cc // 2, cc % 2
        maps.append({"w": np.ascontiguousarray(ada_w[ll][:, half * 3072:(half + 1) * 3072]),
                     "b": np.ascontiguousarray(np.asarray(ada_b[ll], f32)[half * 3072:(half + 1) * 3072].reshape(24, 128).T),
                     "cT": cTl})
    res = _run(_prog("P0", build_P0), maps)
    mod = np.zeros((DEPTH, 6144, 5), f32)
    for cc in range(NCORES):
        ll, half = cc // 2, cc % 2
        mod[ll, half * 3072:(half + 1) * 3072] = res[cc]["modT"].transpose(1, 0, 2).reshape(3072, 5)

    xs = [np.concatenate([ctx[b, z * HC:(z + 1) * HC], x[b, z * HS:(z + 1) * HS]], 0) for (b, z) in cores]
    cs_core = []
    for (b, z) in cores:
        cs = np.zeros((HC + HS, 64), f32)
        cs[:HC, :32] = 1.0
        cs[HC:, :32] = cos[z * HS:(z + 1) * HS]
        cs[HC:, 32:] = sin[z * HS:(z + 1) * HS]
        cs_core.append(cs)

    def fv(v):
        return np.asarray(v, f32).reshape(8, 128).T

    for l in range(DEPTH):
        m6 = mod[l]
        sh1, sc1, g1, sh2, sc2, g2 = [m6[i * 1024:(i + 1) * 1024] for i in range(6)]
        lam_init = 0.8 - 0.6 * math.exp(-0.3 * l)
        maps = []
        for cc, (b, z) in enumerate(cores):
            pv = np.ascontiguousarray(np.stack([fv(norm1_g[l]), fv(sc1[:, b]), fv(sh1[:, b]), fv(sc1[:, 4]), fv(sh1[:, 4])], -1))
            maps.append({"xs": xs[cc], "w_in": np.asarray(w_in[l], f32), "pv": pv, "cs": cs_core[cc], "ident": ident})
        res = _run(_prog("A", lambda: build_A(NT, ctx_tiles=tuple(range(HC // 128)))), maps)
        proj = []
        for b in range(B):
            p0, p1 = res[2 * b]["proj"], res[2 * b + 1]["proj"]
            proj.append(np.concatenate([p0[:HC], p1[:HC], p0[HC:], p1[HC:]], 0))
        del res

        def flipseq(a):
            return np.concatenate([a[:CT][::-1], a[CT:][::-1]], 0)

        maps = []
        for cc, (b, z) in enumerate(cores):
            P = proj[b]
            mqT = np.zeros((4, 64, CT + S), f32); mkT = np.zeros((4, 64, CT + S), f32)
            mvk = np.zeros((4, CT + S, 128), f32); mg = np.zeros((4, CT + S, 2), f32); mgb = np.zeros((4, 128, 2), f32)
            for j in range(2):
                h = 2 * z + j
                q = P[:, 512 + h * 64:512 + (h + 1) * 64]; k = P[:, 768 + h * 64:768 + (h + 1) * 64]
                v = P[:, 1024 + h * 64:1024 + (h + 1) * 64]
                for d in range(2):
                    s = j * 2 + d
                    gi = P[:, 1536 + (2 * d) * 4 + h]; gf = P[:, 1536 + (2 * d + 1) * 4 + h]
                    qq, kk, vv, ii, ff = (q, k, v, gi, gf) if d == 0 else tuple(flipseq(a) for a in (q, k, v, gi, gf))
                    mqT[s] = qq.T; mkT[s] = kk.T
                    mvk[s, :, :64] = vv; mvk[s, :, 64:] = kk
                    mg[s, :, 0] = ii; mg[s, :, 1] = ff
                    mgb[s, :, 0] = mlstm_gate_b[l][2 * d, h]; mgb[s, :, 1] = mlstm_gate_b[l][2 * d + 1, h]
            maps.append({"mqT": mqT, "mkT": mkT, "mvk": mvk, "mg": mg, "mgb": mgb, "tri": tri, "ones": ones})
        res = _run(_prog("M1", lambda: build_M1(NKT)), maps)
        hf = [np.zeros((CT + S, 256), f32) for _ in range(B)]
        hb = [np.zeros((CT + S, 256), f32) for _ in range(B)]
        for cc, (b, z) in enumerate(cores):
            mh = res[cc]["mh"]
            for j in range(2):
                h = 2 * z + j
                hf[b][:, h * 64:(h + 1) * 64] = mh[j * 2]
                hb[b][:, h * 64:(h + 1) * 64] = flipseq(mh[j * 2 + 1])
        del res, maps

        maps = []
        dl = _rep(np.asarray(diff_lambda[l], f32).reshape(256))
        dng = _rep(np.asarray(diff_norm_g[l], f32))
        lami = _rep(np.array([lam_init, 1.0 - lam_init], f32))
        for cc, (b, z) in enumerate(cores):
            P = proj[b]
            aqT = np.zeros((2, 2, 64, CT + S), f32); akT = np.zeros((2, 2, 64, CT + S), f32); av = np.zeros((2, CT + S, 128), f32)
            for j in range(2):
                h = 2 * z + j
                for comp in range(2):
                    o = h * 128 + comp * 64
                    aqT[j, comp] = P[:, 1552 + o:1552 + o + 64].T
                    akT[j, comp] = P[:, 2064 + o:2064 + o + 64].T
                av[j] = P[:, 2576 + h * 128:2576 + (h + 1) * 128]
            maps.append({"aqT": aqT, "akT": akT, "av": av, "dl": dl, "dng": dng, "lami": lami, "ones": ones})
        res = _run(_prog("M2", lambda: build_M2(NKT, NCT)), maps)
        ao = [np.zeros((CT + S, 512), f32) for _ in range(B)]
        for cc, (b, z) in enumerate(cores):
            for j in range(2):
                h = 2 * z + j
                ao[b][:, h * 128:(h + 1) * 128] = res[cc]["ao"][j]
        del res, maps

        cw = np.ascontiguousarray(np.asarray(conv_w[l], f32)[:, 0, :].T.reshape(2, 128, 31).transpose(1, 0, 2))
        cp = np.ascontiguousarray(np.stack([conv_b[l], conv_ln_g[l], conv_ln_b[l]], -1).astype(f32).reshape(2, 128, 3).transpose(1, 0, 2))
        maps = []
        for cc, (b, z) in enumerate(cores):
            P = proj[b]
            aT = np.zeros((512, HC + 30 + HS + 30), f32)
            actx = np.zeros((CT + 30, 512), f32); actx[15:15 + CT] = P[:CT, 0:512]
            alat = np.zeros((S + 30, 512), f32); alat[15:15 + S] = P[CT:, 0:512]
            aT[:, 0:HC + 30] = actx[z * HC:z * HC + HC + 30].T
            aT[:, HC + 30:] = alat[z * HS:z * HS + HS + 30].T
            maps.append({"aT": aT, "cw": cw, "cp": cp, "ones": ones})
        res = _run(_prog("C1", lambda: build_C1([HC, HS])), maps)
        convT = [res[cc]["convT"] for cc in range(NCORES)]
        del res, maps

        maps = []
        mng = _rep(np.asarray(mlstm_norm_g[l], f32))
        for cc, (b, z) in enumerate(cores):
            sel = lambda a: np.ascontiguousarray(np.concatenate([a[z * HC:(z + 1) * HC], a[CT + z * HS:CT + (z + 1) * HS]], 0))
            bc = np.stack([_rep(norm2_g[l]), _rep(g1[:, b]), _rep(g1[:, 4]), _rep(sc2[:, b]), _rep(sc2[:, 4]), _rep(sh2[:, b]), _rep(sh2[:, 4])])
            maps.append({"xs": xs[cc], "convT": convT[cc], "hf": sel(hf[b]), "hb": sel(hb[b]), "mo": sel(proj[b][:, 1280:1536]),
                         "ao": sel(ao[b]), "mng": mng, "w_out": np.asarray(w_out[l], f32), "bc": bc, "ident": ident})
        res = _run(_prog("C2", lambda: build_C2(NT, ctx_tiles=tuple(range(HC // 128)))), maps)
        x1 = [res[cc]["x1"] for cc in range(NCORES)]
        h2 = [res[cc]["h2"] for cc in range(NCORES)]
        del res, maps, proj, hf, hb, ao, convT

        keys = np.asarray(peer_keys[l], f32)
        kbd = np.zeros((128, 8, 256), f32)
        for pp in range(2):
            kbd[pp * 64:(pp + 1) * 64, :, pp * 128:(pp + 1) * 128] = keys[:, pp].transpose(2, 0, 1)
        pu = np.asarray(peer_u[l], f32); pvv = np.asarray(peer_v[l], f32); wq = np.asarray(peer_wq[l], f32)
        maps = []
        for cc, (b, z) in enumerate(cores):
            maps.append({"h2": h2[cc], "x1": x1[cc], "wq": wq, "kbd": kbd, "peer_u": pu, "peer_v": pvv,
                         "g2b": np.stack([_rep(g2[:, b]), _rep(g2[:, 4])]), "ident": ident})
        res = _run(_prog("C3", lambda: build_C3(NT, ctx_tiles=tuple(range(HC // 128)))), maps)
        xs = [res[cc]["x2"] for cc in range(NCORES)]
        del res, maps, x1, h2

    fg = _rep(np.asarray(final_g, f32))
    maps = [{"xs": np.ascontiguousarray(xs[cc][HC:]), "fg": fg} for cc in range(NCORES)]
    res = _run(_prog("F", lambda: build_F(HS // 128)), maps)
    out = np.zeros((B, S, D), f32)
    for cc, (b, z) in enumerate(cores):
        out[b, z * HS:(z + 1) * HS] = res[cc]["y"]
    return out


class Cfg:
    def __init__(self, S, CT, DEPTH):
        self.S, self.CT, self.DEPTH = S, CT, DEPTH
        self.NTOK = S + CT
        self.NKT = self.NTOK // 128
        self.NCT = CT // 128
        self.NE = 16384


def _ph_P0(p, g, T):
    D_ = g.DEPTH
    ct = p.sb([128, 8, 2], name="ct"); st = p.sb([128, 8, 2], name="st")
    p.dma(ct[:], T["cT"], writes=["ct"])
    p.add("act", lambda e: e.activation(out=st[:], in_=ct[:], func=AF.Silu), ["ct"], ["st"])
    sbc = p.sb([128, 8, 2, 128], name="sbc")
    p.add("dve", lambda e: e.tensor_copy(out=sbc[:], in_=st[:, :, :, None].to_broadcast([128, 8, 2, 128])), ["st"], ["sbc"])
    wts = [p.sb([128, 8, 512], name=f"wt{i}") for i in range(2)]
    bbt = [p.sb([128, 512], name=f"bbt{i}") for i in range(2)]
    ob = [p.sb([128, 2, 512], name=f"ob{i}") for i in range(2)]
    btT = p.sb([128, 48], name="btT")
    oT = p.sb([128, 48, 2], name="oT")
    pTb = [p.ps([128, 512], name=f"ppT{i}") for i in range(2)]
    pT = [t[:, 0:32].rearrange("p (a b) -> p a b", a=4) for t in pTb]
    pB = [p.ps([128, 512], name=f"ppB{i}") for i in range(4)]
    nblk = 0
    for l in range(D_):
        p.dma(btT[:], T["ada_bT"][l], writes=["btT"])
        wv = T["ada_w"][l].rearrange("(k p) n -> p k n", p=128)
        for blk in range(12):
            i = nblk % 2; nblk += 1
            wt = wts[i]
            p.dma(wt[:], wv[:, :, blk * 512:(blk + 1) * 512], writes=[f"wt{i}"])
            p.dma(bbt[i][:], T["ada_bB"][l, :, blk * 512:(blk + 1) * 512], writes=[f"bbt{i}"])
            for jj in range(4):
                for k in range(8):
                    p.add("pe", lambda e, wt=wt, i=i, jj=jj, k=k: e.matmul(pT[i][:, jj, 0:2], lhsT=wt[:, k, jj * 128:(jj + 1) * 128], rhs=st[:, k, :],
                                                                          start=(k == 0), stop=(k == 7)), [f"wt{i}", "st"], [f"ppT{i}"])
            for jj in range(4):
                j = blk * 4 + jj
                p.add("dve", lambda e, i=i, jj=jj, j=j: e.tensor_scalar(out=oT[:, j, :], in0=pT[i][:, jj, 0:2], scalar1=btT[:, j:j + 1], scalar2=None,
                                                                       op0=ALU.add), [f"ppT{i}", "btT"], ["oT"])
            for n in range(2):
                pb = (2 * blk + n) % 4
                for k in range(8):
                    p.add("pe", lambda e, wt=wt, n=n, k=k, pb=pb: e.matmul(pB[pb][:], lhsT=sbc[:, k, n, :], rhs=wt[:, k, :], start=(k == 0), stop=(k == 7)),
                          [f"wt{i}", "sbc"], [f"ppB{pb}"])
                p.add("dve", lambda e, i=i, n=n, pb=pb: e.tensor_tensor(out=ob[i][:, n, :], in0=pB[pb][:], in1=bbt[i][:], op=ALU.add),
                      [f"ppB{pb}", f"bbt{i}"], [f"ob{i}_{n}"])
                p.dma(T["modB"][l, n, :, blk * 512:(blk + 1) * 512], ob[i][:, n, :], reads=[f"ob{i}_{n}"])
        p.dma(T["modT"][l], oT[:], reads=["oT"])


def _ph_A(p, g, T, l):
    NT = g.NKT
    xs, proj, projT = T["xs"], T["proj"], T["projT"]
    wb = p.sb([128, 8, IN_W], BF16, name="wb")
    load_cast_weight(p, wb, T["w_in"][l], IN_W, "wb", bw=256)
    idt = p.sb([128, 128], name="idt"); p.dma(idt[:], T["ident"], writes=["idt"])
    mT = p.sb([128, 48, 2], name="mT"); p.dma(mT[:], T["modT"][l], writes=["mT"])
    n1 = p.sb([128, 8], name="n1"); p.dma(n1[:], T["n1g"][l], writes=["n1"])
    gm = p.sb([128, 8, 2], name="gm")
    for n in range(2):
        p.add("dve", lambda e, n=n: e.scalar_tensor_tensor(out=gm[:, :, n], in0=mT[:, 8:16, n], scalar=1.0, in1=n1[:], op0=ALU.add, op1=ALU.mult),
              ["mT", "n1"], ["gm"])
    NB = 2
    mk = lambda shape, nm, dt=F32: [p.sb(shape, dt, name=f"{nm}{i}") for i in range(NB)]
    xt = mk([128, D], "xt"); xn = mk([128, D], "xn"); junk = p.sb([128, D], name="junk")
    ss = mk([128, 1], "ss"); rstd = mk([128, 1], "rstd"); hT = mk([128, 8, 128], "hT", BF16)
    ot = mk([128, IN_W], "ot"); ro = mk([128, 1024], "ro"); tmp = mk([128, 4, 512], "tmp"); cst = mk([128, 64], "cst")
    tT = mk([128, 16, 128], "tT")
    tp = [p.ps([128, 512], name=f"tp{i}") for i in range(4)]
    acc = [p.ps([128, 512], name=f"acc{i}") for i in range(4)]
    nacc = 0
    ntp = 0
    for t in range(NT):
        i = t % NB
        isc = t < g.NCT
        n_ = 1 if isc else 0
        r0 = t * 128
        p.dma(xt[i][:], xs[r0:r0 + 128, :], writes=[f"xt{i}"])
        p.dma(cst[i][:], T["cs"][r0:r0 + 128, :], writes=[f"cst{i}"])
        p.add("act", lambda e, i=i: e.activation(out=junk[:], in_=xt[i][:], func=AF.Square, accum_out=ss[i][:, 0:1]), [f"xt{i}"], ["junk", f"ss{i}"])
        p.add("dve", lambda e, i=i: e.tensor_scalar(out=rstd[i][:], in0=ss[i][:], scalar1=1.0 / D, scalar2=EPS, op0=ALU.mult, op1=ALU.add),
              [f"ss{i}"], [f"rstd{i}"])
        p.add("act", lambda e, i=i: e.activation(out=ss[i][:], in_=rstd[i][:], func=AF.Sqrt), [f"rstd{i}"], [f"ss{i}"])
        p.add("dve", lambda e, i=i: e.reciprocal(out=rstd[i][:], in_=ss[i][:]), [f"ss{i}"], [f"rstd{i}"])
        p.add("dve", lambda e, i=i: e.tensor_scalar(out=xn[i][:], in0=xt[i][:], scalar1=rstd[i][:, 0:1], scalar2=None, op0=ALU.mult),
              [f"xt{i}", f"rstd{i}"], [f"xn{i}"])
        for half in range(2):
            tpi = ntp % 4; ntp += 1
            for k in range(half * 4, half * 4 + 4):
                p.add("pe", lambda e, i=i, k=k, tpi=tpi: e.transpose(tp[tpi][:, (k % 4) * 128:(k % 4 + 1) * 128], xn[i][:, k * 128:(k + 1) * 128], idt[:]),
                      [f"xn{i}", "idt"], [f"tp{tpi}"])
            for k in range(half * 4, half * 4 + 4):
                if half == 0:
                    p.add("act", lambda e, i=i, k=k, tpi=tpi, n_=n_: e.activation(
                        out=hT[i][:, k, :], in_=tp[tpi][:, (k % 4) * 128:(k % 4 + 1) * 128], func=AF.Identity,
                        scale=gm[:, k, n_:n_ + 1], bias=mT[:, k, n_:n_ + 1]), [f"tp{tpi}", "gm", "mT"], [f"hT{i}_{k}"])
                else:
                    p.add("dve", lambda e, i=i, k=k, tpi=tpi, n_=n_: e.tensor_scalar(
                        out=hT[i][:, k, :], in0=tp[tpi][:, (k % 4) * 128:(k % 4 + 1) * 128],
                        scalar1=gm[:, k, n_:n_ + 1], scalar2=mT[:, k, n_:n_ + 1], op0=ALU.mult, op1=ALU.add), [f"tp{tpi}", "gm", "mT"], [f"hT{i}_{k}"])
        for blk in range(7):
            c0 = blk * 512
            cw = min(512, IN_W - c0)
            a = nacc % 4; nacc += 1
            for k in range(8):
                p.add("pe", lambda e, i=i, k=k, a=a, c0=c0, cw=cw: e.matmul(acc[a][:, 0:cw], lhsT=hT[i][:, k, :], rhs=wb[:, k, c0:c0 + cw],
                                                                            start=(k == 0), stop=(k == 7)), [f"hT{i}_{k}", "wb"], [f"acc{a}"])
            if blk % 2 == 0:
                p.add("act", lambda e, i=i, a=a, c0=c0, cw=cw: e.activation(out=ot[i][:, c0:c0 + cw], in_=acc[a][:, 0:cw], func=AF.Copy),
                      [f"acc{a}"], [f"ot{i}_{blk}"])
            else:
                p.add("dve", lambda e, i=i, a=a, c0=c0, cw=cw: e.tensor_copy(out=ot[i][:, c0:c0 + cw], in_=acc[a][:, 0:cw]), [f"acc{a}"], [f"ot{i}_{blk}"])
        src = ot[i][:, 1552:2576].rearrange("p (g r two) -> p g r two", g=16, two=2)
        dst = ro[i][:].rearrange("p (g r two) -> p g r two", g=16, two=2)
        t1 = src[:, :, :, 0]; t2 = src[:, :, :, 1]
        cosb = cst[i][:, None, 0:32].to_broadcast([128, 16, 32])
        sinb = cst[i][:, None, 32:64].to_broadcast([128, 16, 32])
        tm = [tmp[i][:, j, :].rearrange("p (g r) -> p g r", g=16) for j in range(4)]
        rk = [f"ot{i}_3", f"ot{i}_4", f"ot{i}_5", f"cst{i}"]
        p.add("dve", lambda e, tm=tm, t1=t1, cosb=cosb: e.tensor_tensor(out=tm[0], in0=t1, in1=cosb, op=ALU.mult), rk, [f"tmp{i}_0"])
        p.add("dve", lambda e, tm=tm, t2=t2, sinb=sinb: e.tensor_tensor(out=tm[1], in0=t2, in1=sinb, op=ALU.mult), rk, [f"tmp{i}_1"])
        p.add("dve", lambda e, tm=tm, t1=t1, sinb=sinb: e.tensor_tensor(out=tm[2], in0=t1, in1=sinb, op=ALU.mult), rk, [f"tmp{i}_2"])
        p.add("dve", lambda e, tm=tm, t2=t2, cosb=cosb: e.tensor_tensor(out=tm[3], in0=t2, in1=cosb, op=ALU.mult), rk, [f"tmp{i}_3"])
        p.add("dve", lambda e, tm=tm, dst=dst: e.tensor_tensor(out=dst[:, :, :, 0], in0=tm[0], in1=tm[1], op=ALU.subtract),
              [f"tmp{i}_0", f"tmp{i}_1"], [f"ro{i}a"])
        p.add("dve", lambda e, tm=tm, dst=dst: e.tensor_tensor(out=dst[:, :, :, 1], in0=tm[2], in1=tm[3], op=ALU.add),
              [f"tmp{i}_2", f"tmp{i}_3"], [f"ro{i}b"])
        p.dma(proj[r0:r0 + 128, 0:1552], ot[i][:, 0:1552], reads=[f"ot{i}_{b}" for b in range(4)])
        p.dma(proj[r0:r0 + 128, 1552:2576], ro[i][:], reads=[f"ro{i}a", f"ro{i}b"])
        p.dma(proj[r0:r0 + 128, 2576:IN_W], ot[i][:, 2576:IN_W], reads=[f"ot{i}_5", f"ot{i}_6"])
        srcs = [(ot[i], c * 128, [f"ot{i}_0", f"ot{i}_1"]) for c in range(8)] + [(ro[i], c * 128, [f"ro{i}a", f"ro{i}b"]) for c in range(8)]
        for q4 in range(4):
            tpi = ntp % 4; ntp += 1
            for c in range(4):
                sap, c0, keys = srcs[q4 * 4 + c]
                p.add("pe", lambda e, sap=sap, c0=c0, tpi=tpi, c=c: e.transpose(tp[tpi][:, c * 128:(c + 1) * 128], sap[:, c0:c0 + 128], idt[:]),
                      keys + ["idt"], [f"tp{tpi}"])
            if q4 % 2 == 0:
                p.add("act", lambda e, i=i, q4=q4, tpi=tpi: e.activation(out=tT[i][:, q4 * 4:q4 * 4 + 4, :], in_=tp[tpi][:].rearrange("p (k n) -> p k n", k=4),
                                                                         func=AF.Copy), [f"tp{tpi}"], [f"tT{i}_{q4}"])
            else:
                p.add("dve", lambda e, i=i, q4=q4, tpi=tpi: e.tensor_copy(out=tT[i][:, q4 * 4:q4 * 4 + 4, :], in_=tp[tpi][:].rearrange("p (k n) -> p k n", k=4)),
                      [f"tp{tpi}"], [f"tT{i}_{q4}"])
        p.dma(projT[:, r0:r0 + 128].rearrange("(k p) n -> p k n", p=128), tT[i][:], reads=[f"tT{i}_{q}" for q in range(4)])


def _ph_M1(p, g, T, l, GC=4):
    NKT, NCT = g.NKT, g.NCT
    proj, projT, mixo = T["proj"], T["projT"], T["mixo"]
    tri = p.sb([128, 128], name="tri_sb"); p.dma(tri[:], T["tri"], writes=["tri"])
    triL = p.sb([128, 128], name="triL_sb"); p.dma(triL[:], T["triL"], writes=["triL"])
    ones = p.sb([128, 128], name="ones_sb"); p.dma(ones[:], T["ones"], writes=["ones"])
    banks = [p.ps([128, 512], name=f"bank{i}") for i in range(7)]
    NS = 4
    NBUF = 2
    B = {}
    for s in range(NS):
        B[s, "gb"] = p.sb([128, 2], name=f"gb{s}")
        B[s, "Cf"] = p.sb([64, 65], name=f"Cf{s}")
        B[s, "Cb"] = p.sb([64, 65], BF16, name=f"Cb{s}")
        B[s, "tmpC"] = p.sb([64, 65], name=f"tmpC{s}")
        for i in range(NBUF):
            k = (s, i)
            B[k, "qTf"] = p.sb([64, GC * 128], name=f"qTf{s}_{i}")
            B[k, "kTf"] = p.sb([64, GC * 128], name=f"kTf{s}_{i}")
            B[k, "vf"] = p.sb([128, GC, 64], name=f"vf{s}_{i}")
            B[k, "kf"] = p.sb([128, GC, 64], name=f"kf{s}_{i}")
            B[k, "gf"] = p.sb([128, GC, 16], name=f"gf{s}_{i}")
            B[k, "qTb"] = p.sb([64, GC * 128], BF16, name=f"qTb{s}_{i}")
            B[k, "kTb"] = p.sb([64, GC * 128], BF16, name=f"kTb{s}_{i}")
            B[k, "vaug"] = p.sb([128, GC, 65], BF16, name=f"vaug{s}_{i}")
            B[k, "kb"] = p.sb([128, GC, 64], BF16, name=f"kb{s}_{i}")
            B[k, "h"] = p.sb([128, GC, 64], name=f"h{s}_{i}")
            for nm in ("gi", "sp", "a", "b", "eG"):
                B[k, nm] = p.sb([128, GC], name=f"{nm}{s}_{i}")
            B[k, "WT"] = p.sb([128, 128], BF16, name=f"WT{s}_{i}")
            B[k, "d"] = p.sb([128, 4], name=f"d{s}_{i}")
            p.add("dve", lambda e, k=k: e.memset(B[k, "vaug"][:, :, 64:65], 1.0), [], [f"vaug1_{s}_{i}"])
    def groups(rev):
        out = []
        for lo, hi in ((0, NCT), (NCT, NKT)):
            rng = list(range(lo, hi))
            for g0 in range(0, len(rng), GC):
                out.append(rng[g0:g0 + GC])
        if rev:
            out = []
            for lo, hi in ((0, NCT), (NCT, NKT)):
                rng = list(range(lo, hi))[::-1]
                for g0 in range(0, len(rng), GC):
                    out.append(rng[g0:g0 + GC])
        return out
    gcount = {s: 0 for s in range(NS)}
    for hp in range(2):
        for s in range(NS):
            h, dr = 2 * hp + s // 2, s % 2
            p.dma(B[s, "gb"][:], T["mgb"][l, h * 2 + dr], writes=[f"gb{s}"])
            p.add("dve", lambda e, s=s: e.memset(B[s, "Cf"][:], 0.0), [], [f"Cf{s}"])
            p.add("dve", lambda e, s=s: e.memset(B[s, "Cb"][:], 0.0), [], [f"Cb{s}"])
        glists = [groups(s % 2 == 1) for s in range(NS)]
        for gi_ in range(len(glists[0])):
            for s in range(NS):
                h, dr = 2 * hp + s // 2, s % 2
                chunks = glists[s][gi_]
                lo, hi = min(chunks), max(chunks) + 1
                gc = hi - lo
                i = gcount[s] % NBUF; gcount[s] += 1
                k = (s, i)
                sfx = f"{s}_{i}"
                Tt = {n: B[k, n] for n in ("qTf", "kTf", "vf", "kf", "gf", "qTb", "kTb", "vaug", "kb", "h", "gi", "sp", "a", "b", "eG", "WT", "d")}
                Cf, Cb, tmpC, gb = B[s, "Cf"], B[s, "Cb"], B[s, "tmpC"], B[s, "gb"]
                bS, bX, bU = banks[(s % 2) * 3], banks[(s % 2) * 3 + 1], banks[(s % 2) * 3 + 2]
                kS, kX, kU = f"bank{(s%2)*3}", f"bank{(s%2)*3+1}", f"bank{(s%2)*3+2}"
                bP = banks[6]
                trm = triL if dr else tri
                ktr = "triL" if dr else "tri"
                W = gc * 128
                t0 = lo * 128
                p.dma(Tt["qTf"][:, 0:W], projT[512 + h * 64:512 + (h + 1) * 64, t0:t0 + W], writes=[f"qTf{sfx}"])
                p.dma(Tt["kTf"][:, 0:W], projT[768 + h * 64:768 + (h + 1) * 64, t0:t0 + W], writes=[f"kTf{sfx}"])
                pr = proj[t0:t0 + W, :].rearrange("(c p) f -> p c f", p=128)
                p.dma(Tt["vf"][:, 0:gc, :], pr[:, :, 1024 + h * 64:1024 + (h + 1) * 64], writes=[f"vf{sfx}"])
                p.dma(Tt["kf"][:, 0:gc, :], pr[:, :, 768 + h * 64:768 + (h + 1) * 64], writes=[f"kf{sfx}"])
                ci = (2 * dr) * 4 + h
                cf = (2 * dr + 1) * 4 + h
                p.dma(Tt["gf"][:, 0:gc, :], pr[:, :, 1536:1552], writes=[f"gf{sfx}a"])
                gk = [f"gf{sfx}a"]
                p.add("dve", lambda e, Tt=Tt, gb=gb, gc=gc, ci=ci: e.tensor_scalar(out=Tt["gi"][:, 0:gc], in0=Tt["gf"][:, 0:gc, ci], scalar1=gb[:, 0:1],
                                                                       scalar2=None, op0=ALU.add), gk + [f"gb{s}"], [f"gi{sfx}"])
                p.add("dve", lambda e, Tt=Tt, gb=gb, gc=gc, cf=cf: e.tensor_scalar(out=Tt["sp"][:, 0:gc], in0=Tt["gf"][:, 0:gc, cf], scalar1=gb[:, 1:2],
                                                                       scalar2=None, op0=ALU.add), gk + [f"gb{s}"], [f"sp{sfx}"])
                p.add("act", lambda e, Tt=Tt, gc=gc: e.activation(out=Tt["sp"][:, 0:gc], in_=Tt["sp"][:, 0:gc], func=AF.Exp, scale=-1.0),
                      [f"sp{sfx}"], [f"sp{sfx}"])
                p.add("dve", lambda e, Tt=Tt, gc=gc: e.tensor_scalar(out=Tt["sp"][:, 0:gc], in0=Tt["sp"][:, 0:gc], scalar1=1.0, scalar2=None,
                                                                op0=ALU.add), [f"sp{sfx}"], [f"sp{sfx}"])
                p.add("act", lambda e, Tt=Tt, gc=gc: e.activation(out=Tt["sp"][:, 0:gc], in_=Tt["sp"][:, 0:gc], func=AF.Ln), [f"sp{sfx}"], [f"sp{sfx}"])
                p.add("pe", lambda e, Tt=Tt, gc=gc, bP=bP, trm=trm: e.matmul(bP[:, 0:gc], lhsT=trm[:], rhs=Tt["sp"][:, 0:gc], start=True, stop=True),
                      [ktr, f"sp{sfx}"], ["bank6"])
                p.add("pe", lambda e, Tt=Tt, gc=gc, bP=bP: e.matmul(bP[:, 64:64 + gc], lhsT=ones[:], rhs=Tt["sp"][:, 0:gc], start=True, stop=True),
                      ["ones", f"sp{sfx}"], ["bank6"])
                p.add("act", lambda e, Tt=Tt, gc=gc, bP=bP: e.activation(out=Tt["a"][:, 0:gc], in_=bP[:, 0:gc], func=AF.Exp, scale=-1.0), ["bank6"], [f"a{sfx}"])
                p.add("act", lambda e, Tt=Tt, gc=gc, bP=bP: e.activation(out=Tt["eG"][:, 0:gc], in_=bP[:, 64:64 + gc], func=AF.Exp, scale=-1.0), ["bank6"], [f"eG{sfx}"])
                p.add("act", lambda e, Tt=Tt, gc=gc, bP=bP: e.activation(out=Tt["b"][:, 0:gc], in_=bP[:, 0:gc], func=AF.Identity), ["bank6"], [f"b{sfx}"])
                p.add("dve", lambda e, Tt=Tt, gc=gc: e.tensor_tensor(out=Tt["b"][:, 0:gc], in0=Tt["b"][:, 0:gc], in1=Tt["gi"][:, 0:gc], op=ALU.add),
                      [f"b{sfx}", f"gi{sfx}"], [f"b{sfx}"])
                p.add("act", lambda e, Tt=Tt, gc=gc: e.activation(out=Tt["b"][:, 0:gc], in_=Tt["b"][:, 0:gc], func=AF.Exp), [f"b{sfx}"], [f"b{sfx}"])
                p.add("act", lambda e, Tt=Tt, W=W: e.activation(out=Tt["qTb"][:, 0:W], in_=Tt["qTf"][:, 0:W], func=AF.Copy), [f"qTf{sfx}"], [f"qTb{sfx}"])
                p.add("dve", lambda e, Tt=Tt, W=W: e.tensor_scalar(out=Tt["kTb"][:, 0:W], in0=Tt["kTf"][:, 0:W], scalar1=0.125, scalar2=None, op0=ALU.mult),
                      [f"kTf{sfx}"], [f"kTb{sfx}"])
                p.add("act", lambda e, Tt=Tt, gc=gc: e.activation(out=Tt["vaug"][:, 0:gc, 0:64], in_=Tt["vf"][:, 0:gc, :], func=AF.Copy),
                      [f"vf{sfx}"], [f"vaug{sfx}"])
                p.add("dve", lambda e, Tt=Tt, gc=gc: e.scalar_tensor_tensor(
                    out=Tt["kb"][:, 0:gc, :], in0=Tt["kf"][:, 0:gc, :], scalar=0.125,
                    in1=Tt["b"][:, 0:gc, None].to_broadcast([128, gc, 64]), op0=ALU.mult, op1=ALU.mult), [f"kf{sfx}", f"b{sfx}"], [f"kb{sfx}"])
                for c in chunks:
                    cc = c - lo
                    cs = slice(cc * 128, (cc + 1) * 128)
                    p.add("pe", lambda e, Tt=Tt, cs=cs, bS=bS: e.matmul(bS[:, 0:128], lhsT=Tt["kTb"][:, cs], rhs=Tt["qTb"][:, cs], start=True, stop=True),
                          [f"kTb{sfx}", f"qTb{sfx}"], [kS])
                    p.add("dve", lambda e, Tt=Tt, cc=cc, bS=bS, trm=trm: e.scalar_tensor_tensor(
                        out=Tt["WT"][:], in0=bS[:, 0:128], scalar=Tt["b"][:, cc:cc + 1], in1=trm[:], op0=ALU.mult, op1=ALU.mult),
                        [kS, f"b{sfx}", ktr], [f"WT{sfx}"])
                    p.add("pe", lambda e, Tt=Tt, cc=cc, bX=bX: e.matmul(bX[:, 0:65], lhsT=Tt["WT"][:], rhs=Tt["vaug"][:, cc, :], start=True, stop=False),
                          [f"WT{sfx}", f"vaug{sfx}", f"vaug1_{sfx}"], [kX])
                    p.add("pe", lambda e, Tt=Tt, cs=cs, bX=bX, Cb=Cb: e.matmul(bX[:, 0:65], lhsT=Tt["qTb"][:, cs], rhs=Cb[:], start=False, stop=True),
                          [f"qTb{sfx}", f"Cb{s}"], [kX])
                    p.add("pe", lambda e, Tt=Tt, cc=cc, bU=bU: e.matmul(bU[0:64, 0:65], lhsT=Tt["kb"][:, cc, :], rhs=Tt["vaug"][:, cc, :], start=True, stop=True),
                          [f"kb{sfx}", f"vaug{sfx}", f"vaug1_{sfx}"], [kU])
                    d = Tt["d"]
                    p.add("act", lambda e, Tt=Tt, cc=cc, bX=bX, d=d: e.activation(out=d[:, 0:1], in_=bX[:, 64:65], func=AF.Abs, scale=Tt["a"][:, cc:cc + 1]),
                          [kX, f"a{sfx}"], [f"d0{sfx}"])
                    p.add("dve", lambda e, d=d: e.tensor_scalar(out=d[:, 3:4], in0=d[:, 0:1], scalar1=1.0, scalar2=None, op0=ALU.max), [f"d0{sfx}"], [f"d3{sfx}"])
                    p.add("dve", lambda e, d=d: e.reciprocal(out=d[:, 1:2], in_=d[:, 3:4]), [f"d3{sfx}"], [f"d1{sfx}"])
                    p.add("dve", lambda e, Tt=Tt, cc=cc, d=d: e.tensor_tensor(out=d[:, 2:3], in0=d[:, 1:2], in1=Tt["a"][:, cc:cc + 1], op=ALU.mult),
                          [f"d1{sfx}", f"a{sfx}"], [f"d2{sfx}"])
                    p.add("dve", lambda e, Tt=Tt, cc=cc, bX=bX, d=d: e.tensor_scalar(out=Tt["h"][:, cc, :], in0=bX[:, 0:64], scalar1=d[:, 2:3], scalar2=None,
                                                                                op0=ALU.mult), [kX, f"d2{sfx}"], [f"h{sfx}"])
                    p.add("dve", lambda e, bU=bU, Cf=Cf, tmpC=tmpC: e.tensor_tensor(out=tmpC[:], in0=bU[0:64, 0:65], in1=Cf[:], op=ALU.add),
                          [kU, f"Cf{s}"], [f"tmpC{s}"])
                    p.add("dve", lambda e, Tt=Tt, cc=cc, Cf=Cf, tmpC=tmpC: e.tensor_scalar(out=Cf[:], in0=tmpC[:], scalar1=Tt["eG"][0:64, cc:cc + 1], scalar2=None,
                                                                                      op0=ALU.mult), [f"tmpC{s}", f"eG{sfx}"], [f"Cf{s}"])
                    p.add("act", lambda e, Tt=Tt, cc=cc, Cb=Cb, tmpC=tmpC: e.activation(out=Cb[:], in_=tmpC[:], func=AF.Copy, scale=Tt["eG"][0:64, cc:cc + 1]),
                          [f"tmpC{s}", f"eG{sfx}"], [f"Cb{s}"])
                oc = dr * 256 + h * 64
                p.dma(mixo[t0:t0 + W, oc:oc + 64].rearrange("(c p) f -> p c f", p=128), Tt["h"][:, 0:gc, :], reads=[f"h{sfx}"])


def _ph_M2(p, g, T, l):
    NKT, NCT = g.NKT, g.NCT
    L = NKT * 128
    proj, projT, mixo = T["proj"], T["projT"], T["mixo"]
    ones = p.sb([128, 128], name="ones_sb"); p.dma(ones[:], T["ones"], writes=["ones"])
    dl = p.sb([128, 256], name="dl_sb"); p.dma(dl[:], T["dl"][l], writes=["dl"])
    gsc = p.sb([128, 128], name="gsc"); p.dma(gsc[:], T["dng"][l], writes=["gsc"])
    lami = p.sb([128, 2], name="lami_sb"); p.dma(lami[:], T["lami"][l], writes=["lami"])
    junk = p.sb([128, 128], name="junk")
    lm = p.sb([128, 4], name="lm")
    p.add("dve", lambda e: e.scalar_tensor_tensor(out=junk[:, 0:64], in0=dl[:, 0:64], scalar=1.0, in1=dl[:, 64:128],
                                                  op0=ALU.mult, op1=ALU.mult, accum_out=lm[:, 0:1]), ["dl"], ["junk", "lm0"])
    p.add("dve", lambda e: e.scalar_tensor_tensor(out=junk[:, 64:128], in0=dl[:, 128:192], scalar=1.0, in1=dl[:, 192:256],
                                                  op0=ALU.mult, op1=ALU.mult, accum_out=lm[:, 1:2]), ["dl"], ["junk", "lm1"])
    p.add("act", lambda e: e.activation(out=lm[:, 0:2], in_=lm[:, 0:2], func=AF.Exp), ["lm0", "lm1"], ["lm01"])
    p.add("dve", lambda e: e.tensor_tensor(out=lm[:, 2:3], in0=lm[:, 1:2], in1=lm[:, 0:1], op=ALU.subtract), ["lm01"], ["lm2"])
    p.add("dve", lambda e: e.tensor_scalar(out=lm[:, 3:4], in0=lm[:, 2:3], scalar1=lami[:, 0:1], scalar2=None, op0=ALU.subtract), ["lm2", "lami"], ["nlam"])
    p.add("dve", lambda e: e.tensor_scalar(out=gsc[:], in0=gsc[:], scalar1=lami[:, 1:2], scalar2=None, op0=ALU.mult), ["gsc", "lami"], ["gsc"])
    banks = [p.ps([128, 512], name=f"bank{i}") for i in range(8)]
    qTb = p.sb([64, 2, L], BF16, name="qTb")
    kTb = p.sb([64, 2, L], BF16, name="kTb")
    vaug = p.sb([128, NKT, 129], BF16, name="vaug")
    p.add("dve", lambda e: e.memset(vaug[:, :, 128:129], 1.0), [], ["vaug1"])
    PW = 2048
    stg = [p.sb([64, PW], name=f"stg{i}") for i in range(2)]
    sq = p.sb([64, PW], name="sq")
    VG = 16
    vst = [p.sb([128, VG, 128], name=f"vst{i}") for i in range(2)]
    mx = p.sb([128, 8], name="mx")
    nb = p.sb([128, 4], name="nb")
    pT = [p.sb([128, 512], BF16, name=f"pT{i}") for i in range(3)]
    ev = [p.sb([128, 8], name=f"ev{i}") for i in range(2)]
    o1 = [p.sb([128, 128], name=f"o1_{i}") for i in range(2)]
    oo = [p.sb([128, 128], name=f"oo_{i}") for i in range(2)]
    yy = [p.sb([128, 128], name=f"yy_{i}") for i in range(2)]
    nstg = nvst = nit = nblk = nev = 0
    for s in range(4):
        h = s
        p.add("dve", lambda e: e.memset(mx[:], 0.0), [], ["mx", "mx4"])
        for which, (rbase, dst) in enumerate(((1024, qTb), (1536, kTb))):
            for comp in range(2):
                rr = rbase + h * 128 + comp * 64
                for c0 in range(0, L, PW):
                    cw = min(PW, L - c0)
                    si = nstg % 2; nstg += 1
                    st = stg[si]
                    p.dma(st[:, 0:cw], projT[rr:rr + 64, c0:c0 + cw], writes=[f"stg{si}"])
                    p.add("dve", lambda e, st=st, dst=dst, comp=comp, c0=c0, cw=cw: e.tensor_copy(out=dst[:, comp, c0:c0 + cw], in_=st[:, 0:cw]),
                          [f"stg{si}"], ["qTb" if which == 0 else "kTb"])
                    p.add("act", lambda e, st=st, cw=cw: e.activation(out=sq[:, 0:cw], in_=st[:, 0:cw], func=AF.Square), [f"stg{si}"], ["sq"])
                    for b0 in range(0, cw, 512):
                        bw = min(512, cw - b0)
                        p.add("pe", lambda e, b0=b0, bw=bw: e.matmul(banks[6][:, 0:bw], lhsT=ones[0:64, :], rhs=sq[:, b0:b0 + bw], start=True, stop=True),
                              ["ones", "sq"], ["bank6"])
                        p.add("dve", lambda e, bw=bw: e.reduce_max(out=mx[:, 4:5], in_=banks[6][:, 0:bw], axis=AX.X), ["bank6"], ["mx4"])
                        col = which * 2 + comp
                        p.add("dve", lambda e, col=col: e.tensor_tensor(out=mx[:, col:col + 1], in0=mx[:, col:col + 1], in1=mx[:, 4:5], op=ALU.max),
                              ["mx4", "mx"], ["mx"])
        for g0 in range(0, NKT, VG):
            gn = min(VG, NKT - g0)
            vi = nvst % 2; nvst += 1
            p.dma(vst[vi][:, 0:gn, :], proj[g0 * 128:(g0 + gn) * 128, 2576 + h * 128:2576 + (h + 1) * 128].rearrange("(c p) f -> p c f", p=128),
                  writes=[f"vst{vi}"])
            p.add("act", lambda e, vi=vi, g0=g0, gn=gn: e.activation(out=vaug[:, g0:g0 + gn, 0:128], in_=vst[vi][:, 0:gn, :], func=AF.Copy),
                  [f"vst{vi}"], ["vaug"])
        p.add("dve", lambda e: e.tensor_tensor(out=nb[:, 2:4], in0=mx[:, 0:2], in1=mx[:, 2:4], op=ALU.mult), ["mx"], ["nb2"])
        p.add("act", lambda e: e.activation(out=nb[:, 2:4], in_=nb[:, 2:4], func=AF.Sqrt, scale=1.0 / 64.0), ["nb2"], ["nb2"])
        p.add("dve", lambda e: e.tensor_scalar(out=nb[:, 0:2], in0=nb[:, 2:4], scalar1=60.0, scalar2=-1.0, op0=ALU.min, op1=ALU.mult), ["nb2"], ["nb"])
        blocks = [(0, NCT, 0, NCT)]
        for q0 in range(NCT, NKT, 4):
            blocks.append((q0, min(4, NKT - q0), 0, NKT))
        for (q0, nq, k0, nk) in blocks:
            oset = nblk % 2; nblk += 1
            obanks = [banks[oset * 3 + i] for i in range(3)]
            okeys = [f"bank{oset*3+i}" for i in range(3)]
            for i in range(3):
                p.add("dve", lambda e, i=i, obanks=obanks: e.memset(obanks[i][:], 0.0), [], [okeys[i]])

            def oacc(comp, j, obanks=obanks, okeys=okeys):
                a = comp * 4 + j
                return obanks[a // 3][:, (a % 3) * 129:(a % 3) * 129 + 129], okeys[a // 3]
            QW = nq * 128
            for kt in range(k0, k0 + nk):
                for comp in range(2):
                    sb_i = 6 + nit % 2
                    pi = nit % 3
                    nit += 1
                    p.add("pe", lambda e, comp=comp, kt=kt, sb_i=sb_i, q0=q0, QW=QW: e.matmul(
                        banks[sb_i][:, 0:QW], lhsT=kTb[:, comp, kt * 128:(kt + 1) * 128], rhs=qTb[:, comp, q0 * 128:q0 * 128 + QW],
                        start=True, stop=True), ["kTb", "qTb"], [f"bank{sb_i}"])
                    p.add("act", lambda e, comp=comp, sb_i=sb_i, pi=pi, QW=QW: e.activation(
                        out=pT[pi][:, 0:QW], in_=banks[sb_i][:, 0:QW], func=AF.Exp, scale=0.125, bias=nb[:, comp:comp + 1]),
                        [f"bank{sb_i}", "nb"], [f"pT{pi}"])
                    for j in range(nq):
                        oap, okey = oacc(comp, j)
                        p.add("pe", lambda e, oap=oap, pi=pi, j=j, kt=kt: e.matmul(
                            oap, lhsT=pT[pi][:, j * 128:(j + 1) * 128], rhs=vaug[:, kt, :], start=False, stop=False,
                            skip_group_check=True), [f"pT{pi}", "vaug", "vaug1"], [okey])
            for j in range(nq):
                ei = nev % 2; nev += 1
                E = ev[ei]
                (o1ap, k1), (o2ap, k2) = oacc(0, j), oacc(1, j)
                sfx = f"_{ei}"
                p.add("dve", lambda e, E=E, o1ap=o1ap: e.reciprocal(out=E[:, 0:1], in_=o1ap[:, 128:129]), [k1], ["ev0" + sfx])
                p.add("dve", lambda e, E=E, o2ap=o2ap: e.reciprocal(out=E[:, 1:2], in_=o2ap[:, 128:129]), [k2], ["ev1" + sfx])
                p.add("dve", lambda e, E=E: e.tensor_tensor(out=E[:, 2:3], in0=E[:, 1:2], in1=lm[:, 3:4], op=ALU.mult), ["ev1" + sfx, "nlam"], ["ev2" + sfx])
                p.add("dve", lambda e, E=E, o1ap=o1ap, ei=ei: e.tensor_scalar(out=o1[ei][:], in0=o1ap[:, 0:128], scalar1=E[:, 0:1], scalar2=None, op0=ALU.mult),
                      [k1, "ev0" + sfx], ["o1" + sfx])
                p.add("dve", lambda e, E=E, o2ap=o2ap, ei=ei: e.scalar_tensor_tensor(out=oo[ei][:], in0=o2ap[:, 0:128], scalar=E[:, 2:3], in1=o1[ei][:],
                                                                                     op0=ALU.mult, op1=ALU.add), [k2, "ev2" + sfx, "o1" + sfx], ["oo" + sfx])
                p.add("dve", lambda e, E=E, ei=ei: e.scalar_tensor_tensor(out=junk[:], in0=oo[ei][:], scalar=1.0, in1=oo[ei][:], op0=ALU.mult, op1=ALU.mult,
                                                                          accum_out=E[:, 3:4]), ["oo" + sfx], ["junk", "ev3" + sfx])
                p.add("dve", lambda e, E=E: e.tensor_scalar(out=E[:, 4:5], in0=E[:, 3:4], scalar1=1.0 / 128.0, scalar2=EPS, op0=ALU.mult, op1=ALU.add),
                      ["ev3" + sfx], ["ev4" + sfx])
                p.add("act", lambda e, E=E: e.activation(out=E[:, 5:6], in_=E[:, 4:5], func=AF.Sqrt), ["ev4" + sfx], ["ev5" + sfx])
                p.add("dve", lambda e, E=E: e.reciprocal(out=E[:, 6:7], in_=E[:, 5:6]), ["ev5" + sfx], ["ev6" + sfx])
                p.add("dve", lambda e, E=E, ei=ei: e.scalar_tensor_tensor(out=yy[ei][:], in0=oo[ei][:], scalar=E[:, 6:7], in1=gsc[:], op0=ALU.mult, op1=ALU.mult),
                      ["oo" + sfx, "ev6" + sfx, "gsc"], ["yy" + sfx])
                r0 = (q0 + j) * 128
                p.dma(mixo[r0:r0 + 128, 512 + h * 128:512 + (h + 1) * 128], yy[ei][:], reads=["yy" + sfx])


def _ph_C1(p, g, T, l):
    projT, co = T["projT"], T["convT"]
    ones = p.sb([128, 128], name="ones_sb"); p.dma(ones[:], T["ones"], writes=["ones"])
    cw = p.sb([128, 2, 31], name="cw_sb"); p.dma(cw[:], T["cw"][l], writes=["cw"])
    cp = p.sb([128, 2, 3], name="cp_sb"); p.dma(cp[:], T["cp"][l], writes=["cp"])
    N = 512
    a1 = [p.sb([128, N + 30], name=f"a1_{i}") for i in range(2)]
    a2 = [p.sb([128, N + 30], name=f"a2_{i}") for i in range(2)]
    u = [p.sb([128, N + 30], name=f"u_{i}") for i in range(2)]
    acc = [[p.sb([128, N], name=f"acc_{i}_{r}") for r in range(2)] for i in range(2)]
    y = [p.sb([128, N], name=f"y_{i}") for i in range(2)]
    ysq = [p.sb([128, N], name=f"ysq_{i}") for i in range(2)]
    mean = p.sb([128, N], name="mean"); msq = p.sb([128, N], name="msq"); rstd = p.sb([128, N], name="rstd")
    zz = [p.sb([128, N], name=f"zz_{i}") for i in range(2)]
    bA = p.ps([128, 512], name="bankA"); bB = p.ps([128, 512], name="bankB")
    for (s0, ns) in ((0, g.CT), (g.CT, g.S)):
        for t0 in range(0, ns, N):
            n = min(N, ns - t0)
            lo = max(t0 - 15, 0); hi = min(t0 + n + 15, ns)
            d0 = lo - (t0 - 15)
            clip = (lo != t0 - 15) or (hi != t0 + n + 15)
            for j in range(2):
                for (buf, nm, rb) in ((a1[j], f"a1_{j}", j * 128), (a2[j], f"a2_{j}", 256 + j * 128)):
                    if clip:
                        p.add("dve", lambda e, buf=buf, n=n: e.memset(buf[:, 0:n + 30], 0.0), [], [nm])
                    p.dma(buf[:, d0:d0 + hi - lo], projT[rb:rb + 128, s0 + lo:s0 + hi], writes=[nm])
                p.add("act", lambda e, j=j, n=n: e.activation(out=a2[j][:, 0:n + 30], in_=a2[j][:, 0:n + 30], func=AF.Sigmoid), [f"a2_{j}"], [f"a2_{j}"])
                p.add("dve", lambda e, j=j, n=n: e.tensor_tensor(out=u[j][:, 0:n + 30], in0=a1[j][:, 0:n + 30], in1=a2[j][:, 0:n + 30], op=ALU.mult),
                      [f"a1_{j}", f"a2_{j}"], [f"u_{j}"])
                p.add("dve", lambda e, j=j, n=n: e.tensor_scalar(out=acc[j][0][:, 0:n], in0=u[j][:, 0:n], scalar1=cw[:, j, 0:1], scalar2=None, op0=ALU.mult),
                      [f"u_{j}", "cw"], [f"acc_{j}_0"])
                for k in range(1, 31):
                    src, dst = acc[j][(k - 1) % 2], acc[j][k % 2]
                    p.add("dve", lambda e, j=j, n=n, k=k, src=src, dst=dst: e.scalar_tensor_tensor(
                        out=dst[:, 0:n], in0=u[j][:, k:k + n], scalar=cw[:, j, k:k + 1], in1=src[:, 0:n], op0=ALU.mult, op1=ALU.add),
                        [f"u_{j}", "cw", f"acc_{j}_{(k-1)%2}"], [f"acc_{j}_{k%2}"])
                p.add("dve", lambda e, j=j, n=n: e.tensor_scalar(out=y[j][:, 0:n], in0=acc[j][0][:, 0:n], scalar1=cp[:, j, 0:1], scalar2=None, op0=ALU.add),
                      [f"acc_{j}_0", "cp"], [f"y_{j}"])
                p.add("act", lambda e, j=j, n=n: e.activation(out=ysq[j][:, 0:n], in_=y[j][:, 0:n], func=AF.Square), [f"y_{j}"], [f"ysq_{j}"])
            for j in range(2):
                p.add("pe", lambda e, j=j, n=n: e.matmul(bA[:, 0:n], lhsT=ones[:], rhs=y[j][:, 0:n], start=(j == 0), stop=(j == 1)), ["ones", f"y_{j}"], ["bankA"])
            for j in range(2):
                p.add("pe", lambda e, j=j, n=n: e.matmul(bB[:, 0:n], lhsT=ones[:], rhs=ysq[j][:, 0:n], start=(j == 0), stop=(j == 1)), ["ones", f"ysq_{j}"], ["bankB"])
            p.add("act", lambda e, n=n: e.activation(out=mean[:, 0:n], in_=bA[:, 0:n], func=AF.Copy, scale=1.0 / 256), ["bankA"], ["mean"])
            p.add("act", lambda e, n=n: e.activation(out=msq[:, 0:n], in_=bA[:, 0:n], func=AF.Square, scale=1.0 / 256), ["bankA"], ["msq"])
            p.add("dve", lambda e, n=n: e.scalar_tensor_tensor(out=rstd[:, 0:n], in0=bB[:, 0:n], scalar=1.0 / 256, in1=msq[:, 0:n], op0=ALU.mult, op1=ALU.subtract),
                  ["bankB", "msq"], ["rstd"])
            p.add("dve", lambda e, n=n: e.tensor_scalar(out=rstd[:, 0:n], in0=rstd[:, 0:n], scalar1=EPS, scalar2=None, op0=ALU.add), ["rstd"], ["rstd"])
            p.add("act", lambda e, n=n: e.activation(out=rstd[:, 0:n], in_=rstd[:, 0:n], func=AF.Sqrt), ["rstd"], ["rstd"])
            p.add("dve", lambda e, n=n: e.reciprocal(out=rstd[:, 0:n], in_=rstd[:, 0:n]), ["rstd"], ["rstd"])
            for j in range(2):
                p.add("dve", lambda e, j=j, n=n: e.tensor_tensor(out=zz[j][:, 0:n], in0=y[j][:, 0:n], in1=mean[:, 0:n], op=ALU.subtract), [f"y_{j}", "mean"], [f"zz_{j}"])
                p.add("dve", lambda e, j=j, n=n: e.tensor_tensor(out=zz[j][:, 0:n], in0=zz[j][:, 0:n], in1=rstd[:, 0:n], op=ALU.mult), [f"zz_{j}", "rstd"], [f"zz_{j}"])
                p.add("act", lambda e, j=j, n=n: e.activation(out=zz[j][:, 0:n], in_=zz[j][:, 0:n], func=AF.Silu, scale=cp[:, j, 1:2], bias=cp[:, j, 2:3]),
                      [f"zz_{j}", "cp"], [f"zz_{j}"])
                p.dma(co[j, :, s0 + t0:s0 + t0 + n], zz[j][:, 0:n], reads=[f"zz_{j}"])


def _ph_C2(p, g, T, l):
    NT = g.NKT
    xs, convT, mixo, proj = T["xs"], T["convT"], T["mixo"], T["proj"]
    wob = p.sb([128, 8, D], BF16, name="wob")
    load_cast_weight(p, wob, T["w_out"][l], D, "wob")
    idt = p.sb([128, 128], name="idt"); p.dma(idt[:], T["ident"], writes=["idt"])
    mng = p.sb([128, 64], name="mng_sb"); p.dma(mng[:], T["mng"][l], writes=["mng"])
    bc = [p.sb([128, D], name=f"bc{i}") for i in range(7)]
    mB = T["modB"]
    srcs = [T["n2gB"][l], mB[l, 0, :, 2048:3072], mB[l, 1, :, 2048:3072], mB[l, 0, :, 4096:5120], mB[l, 1, :, 4096:5120],
            mB[l, 0, :, 3072:4096], mB[l, 1, :, 3072:4096]]
    for i in range(7):
        p.dma(bc[i][:], srcs[i], writes=[f"bc{i}"])
    for i in (3, 4):
        p.add("dve", lambda e, i=i: e.scalar_tensor_tensor(out=bc[i][:], in0=bc[i][:], scalar=1.0, in1=bc[0][:], op0=ALU.add, op1=ALU.mult),
              [f"bc{i}", "bc0"], [f"bc{i}"])
    NB = 2
    mk = lambda shape, nm, dt=F32: [p.sb(shape, dt, name=f"{nm}{i}") for i in range(NB)]
    xt = mk([128, D], "xt"); hf = mk([128, 256], "hf"); hb = mk([128, 256], "hb"); mo = mk([128, 256], "mo")
    aot = mk([128, 512], "aot"); cvt = mk([128, 2, 128], "cvt"); ym = mk([128, 256], "ym"); sqm = mk([128, 256], "sqm")
    st4 = mk([128, 8], "st4"); mixT = mk([128, 8, 128], "mixT", BF16); tmp = mk([128, D], "tmp"); x1 = mk([128, D], "x1")
    h2 = mk([128, D], "h2"); ss = mk([128, 4], "ss")
    junk = p.sb([128, D], name="junk")
    bT = [p.ps([128, 512], name=f"bT{i}") for i in range(4)]
    bAcc = [p.ps([128, 512], name=f"bAcc{i}") for i in range(4)]
    for t in range(NT):
        i = t % NB
        isc = t < g.NCT
        r0 = t * 128
        g1b = bc[2] if isc else bc[1]
        gm2 = bc[4] if isc else bc[3]
        sh2 = bc[6] if isc else bc[5]
        kg1, kgm2, ksh2 = (f"bc{2 if isc else 1}", f"bc{4 if isc else 3}", f"bc{6 if isc else 5}")
        p.dma(xt[i][:], xs[r0:r0 + 128, :], writes=[f"xt{i}"])
        p.dma(hf[i][:], mixo[r0:r0 + 128, 0:256], writes=[f"hf{i}"])
        p.dma(hb[i][:], mixo[r0:r0 + 128, 256:512], writes=[f"hb{i}"])
        p.dma(mo[i][:], proj[r0:r0 + 128, 1280:1536], writes=[f"mo{i}"])
        p.dma(aot[i][:], mixo[r0:r0 + 128, 512:1024], writes=[f"aot{i}"])
        p.dma(cvt[i][:], convT[:, :, r0:r0 + 128].rearrange("j p n -> p j n"), writes=[f"cvt{i}"])
        p.add("dve", lambda e, i=i: e.tensor_tensor(out=ym[i][:], in0=hf[i][:], in1=hb[i][:], op=ALU.add), [f"hf{i}", f"hb{i}"], [f"ym{i}"])
        p.add("act", lambda e, i=i: e.activation(out=sqm[i][:], in_=ym[i][:], func=AF.Square), [f"ym{i}"], [f"sqm{i}"])
        p.add("dve", lambda e, i=i: e.tensor_reduce(out=st4[i][:, 0:4], in_=sqm[i][:].rearrange("p (h d) -> p h d", h=4), axis=AX.X, op=ALU.add),
              [f"sqm{i}"], [f"st4a{i}"])
        p.add("dve", lambda e, i=i: e.tensor_scalar(out=st4[i][:, 4:8], in0=st4[i][:, 0:4], scalar1=1.0 / 64, scalar2=EPS, op0=ALU.mult, op1=ALU.add),
              [f"st4a{i}"], [f"st4b{i}"])
        p.add("act", lambda e, i=i: e.activation(out=st4[i][:, 0:4], in_=st4[i][:, 4:8], func=AF.Sqrt), [f"st4b{i}"], [f"st4a{i}"])
        p.add("dve", lambda e, i=i: e.reciprocal(out=st4[i][:, 4:8], in_=st4[i][:, 0:4]), [f"st4a{i}"], [f"st4b{i}"])
        p.add("act", lambda e, i=i: e.activation(out=mo[i][:], in_=mo[i][:], func=AF.Sigmoid), [f"mo{i}"], [f"mo{i}"])
        ym3 = ym[i][:].rearrange("p (h d) -> p h d", h=4)
        p.add("dve", lambda e, i=i, ym3=ym3: e.tensor_tensor(out=ym3, in0=ym3, in1=st4[i][:, 4:8, None].to_broadcast([128, 4, 64]), op=ALU.mult),
              [f"ym{i}", f"st4b{i}"], [f"ym{i}"])
        p.add("dve", lambda e, i=i, ym3=ym3: e.tensor_tensor(out=ym3, in0=ym3, in1=mng[:, None, :].to_broadcast([128, 4, 64]), op=ALU.mult),
              [f"ym{i}", "mng"], [f"ym{i}"])
        p.add("dve", lambda e, i=i: e.tensor_tensor(out=ym[i][:], in0=ym[i][:], in1=mo[i][:], op=ALU.mult), [f"ym{i}", f"mo{i}"], [f"ym{i}"])
        p.add("act", lambda e, i=i: e.activation(out=mixT[i][:, 0:2, :], in_=cvt[i][:], func=AF.Copy), [f"cvt{i}"], [f"mixT{i}_c"])
        ta, tb = (2 * t) % 4, (2 * t + 1) % 4
        tsrc = [(ym[i], 0, f"ym{i}"), (ym[i], 128, f"ym{i}"), (aot[i], 0, f"aot{i}"), (aot[i], 128, f"aot{i}"),
                (aot[i], 256, f"aot{i}"), (aot[i], 384, f"aot{i}")]
        for n_, (src, c0, key) in enumerate(tsrc):
            bank = ta if n_ < 4 else tb
            col = (n_ % 4) * 128
            p.add("pe", lambda e, src=src, c0=c0, bank=bank, col=col: e.transpose(bT[bank][:, col:col + 128], src[:, c0:c0 + 128], idt[:]),
                  [key, "idt"], [f"bT{bank}"])
        p.add("act", lambda e, i=i, ta=ta: e.activation(out=mixT[i][:, 2:6, :], in_=bT[ta][:].rearrange("p (k n) -> p k n", k=4), func=AF.Copy),
              [f"bT{ta}"], [f"mixT{i}_a"])
        p.add("dve", lambda e, i=i, tb=tb: e.tensor_copy(out=mixT[i][:, 6:8, :], in_=bT[tb][:, 0:256].rearrange("p (k n) -> p k n", k=2)),
              [f"bT{tb}"], [f"mixT{i}_b"])
        for blk in range(2):
            a = (2 * t + blk) % 4
            for k in range(8):
                p.add("pe", lambda e, i=i, k=k, a=a, blk=blk: e.matmul(bAcc[a][:], lhsT=mixT[i][:, k, :], rhs=wob[:, k, blk * 512:(blk + 1) * 512],
                                                                      start=(k == 0), stop=(k == 7)),
                      [f"mixT{i}_c", f"mixT{i}_a", f"mixT{i}_b", "wob"], [f"bAcc{a}"])
            cs = slice(blk * 512, (blk + 1) * 512)
            p.add("dve", lambda e, i=i, a=a, cs=cs, g1b=g1b: e.tensor_tensor(out=tmp[i][:, cs], in0=bAcc[a][:], in1=g1b[:, cs], op=ALU.mult),
                  [f"bAcc{a}", kg1], [f"tmp{i}_{blk}"])
            p.add("dve", lambda e, i=i, cs=cs: e.tensor_tensor(out=x1[i][:, cs], in0=tmp[i][:, cs], in1=xt[i][:, cs], op=ALU.add),
                  [f"tmp{i}_{blk}", f"xt{i}"], [f"x1{i}_{blk}"])
        p.dma(T["x1"][r0:r0 + 128, :], x1[i][:], reads=[f"x1{i}_0", f"x1{i}_1"])
        p.add("act", lambda e, i=i: e.activation(out=junk[:], in_=x1[i][:], func=AF.Square, accum_out=ss[i][:, 0:1]),
              [f"x1{i}_0", f"x1{i}_1"], ["junk", f"ss0{i}"])
        p.add("dve", lambda e, i=i: e.tensor_scalar(out=ss[i][:, 1:2], in0=ss[i][:, 0:1], scalar1=1.0 / D, scalar2=EPS, op0=ALU.mult, op1=ALU.add),
              [f"ss0{i}"], [f"ss1{i}"])
        p.add("act", lambda e, i=i: e.activation(out=ss[i][:, 2:3], in_=ss[i][:, 1:2], func=AF.Sqrt), [f"ss1{i}"], [f"ss2{i}"])
        p.add("dve", lambda e, i=i: e.reciprocal(out=ss[i][:, 3:4], in_=ss[i][:, 2:3]), [f"ss2{i}"], [f"ss3{i}"])
        p.add("dve", lambda e, i=i, gm2=gm2: e.scalar_tensor_tensor(out=h2[i][:], in0=x1[i][:], scalar=ss[i][:, 3:4], in1=gm2[:], op0=ALU.mult, op1=ALU.mult),
              [f"x1{i}_0", f"x1{i}_1", f"ss3{i}", kgm2], [f"h2{i}"])
        p.add("dve", lambda e, i=i, sh2=sh2: e.tensor_tensor(out=h2[i][:], in0=h2[i][:], in1=sh2[:], op=ALU.add), [f"h2{i}", ksh2], [f"h2{i}"])
        p.dma(T["h2"][r0:r0 + 128, :], h2[i][:], reads=[f"h2{i}"])


def _ph_C3(p, g, T, l, NGB=4):
    NT = g.NKT
    NE = g.NE
    pu, pv = T["peer_u"][l], T["peer_v"][l]
    wqb = p.sb([128, 8, D], F32, name="wqb")
    p.dma(wqb[:], T["wq"][l].rearrange("(k p) n -> p k n", p=128), writes=["wqb"])
    idt = p.sb([128, 128], name="idt"); p.dma(idt[:], T["ident"], writes=["idt"])
    kbd = p.sb([128, 8, 256], name="kbd_sb"); p.dma(kbd[:], T["kbd"][l], writes=["kbd"])
    g2b = [p.sb([128, D], name=f"g2b{i}") for i in range(2)]
    for i in range(2):
        p.dma(g2b[i][:], T["modB"][l, i, :, 5120:6144], writes=[f"g2b{i}"])
    h2t = p.sb([128, D], name="h2t"); x1t = p.sb([128, D], name="x1t")
    h2T = p.sb([128, 8, 128], F32, name="h2T")
    qTf = p.sb([128, 8, 128], name="qTf")
    sc = p.sb([128, 16, 128], name="sc"); wk = p.sb([128, 16, 128], name="wk")
    st = p.sb([128, 16, 16], name="st"); it = p.sb([128, 16, 16], U32, name="it"); itf = p.sb([128, 16, 16], name="itf")
    cand = p.sb([128, 8, 256], name="cand"); cidx = p.sb([128, 8, 256], name="cidx"); wk2 = p.sb([128, 8, 256], name="wk2")
    best = p.sb([128, 8, 16], name="best"); gw = p.sb([128, 8, 16], name="gw")
    eidf = p.sb([128, 128], name="eidf"); eidi = p.sb([128, 128], I32, name="eidi")
    sm = p.sb([128, 16], name="sm")
    act = p.sb([128, 128], name="act_sb"); coef = p.sb([128, 128], name="coef")
    junk = p.sb([128, D], name="junk")
    acc = p.sb([128, D], name="acc"); x2t = p.sb([128, D], name="x2t")
    rows = [p.sb([128, D], name=f"rows{i}") for i in range(NGB)]
    banks = [p.ps([128, 512], name=f"bank{i}") for i in range(8)]
    ngat = 0
    for t in range(NT):
        isc = t < g.NCT
        r0 = t * 128
        gb = g2b[1] if isc else g2b[0]
        kgb = "g2b1" if isc else "g2b0"
        p.dma(h2t[:], T["h2"][r0:r0 + 128, :], writes=["h2t"])
        p.dma(x1t[:], T["x1"][r0:r0 + 128, :], writes=["x1t"])
        for half in range(2):
            for k in range(half * 4, half * 4 + 4):
                p.add("pe", lambda e, k=k, half=half: e.transpose(banks[half][:, (k % 4) * 128:(k % 4 + 1) * 128], h2t[:, k * 128:(k + 1) * 128], idt[:]),
                      ["h2t", "idt"], [f"bank{half}"])
        p.add("act", lambda e: e.activation(out=h2T[:, 0:4, :], in_=banks[0][:].rearrange("p (k n) -> p k n", k=4), func=AF.Copy), ["bank0"], ["h2T_a"])
        p.add("dve", lambda e: e.tensor_copy(out=h2T[:, 4:8, :], in_=banks[1][:].rearrange("p (k n) -> p k n", k=4)), ["bank1"], ["h2T_b"])
        for c in range(8):
            bk = 2 + c // 4
            for k in range(8):
                p.add("pe", lambda e, c=c, k=k, bk=bk: e.matmul(banks[bk][:, (c % 4) * 128:(c % 4 + 1) * 128], lhsT=wqb[:, k, c * 128:(c + 1) * 128],
                                                              rhs=h2T[:, k, :], start=(k == 0), stop=(k == 7)), ["wqb", "h2T_a", "h2T_b"], [f"bank{bk}"])
        p.add("act", lambda e: e.activation(out=qTf[:, 0:4, :], in_=banks[2][:].rearrange("p (k n) -> p k n", k=4), func=AF.Copy), ["bank2"], ["qTf_a"])
        p.add("dve", lambda e: e.tensor_copy(out=qTf[:, 4:8, :], in_=banks[3][:].rearrange("p (k n) -> p k n", k=4)), ["bank3"], ["qTf_b"])
        for h in range(8):
            bk = 4 + h // 2
            p.add("pe", lambda e, h=h, bk=bk: e.matmul(banks[bk][:, (h % 2) * 256:(h % 2 + 1) * 256], lhsT=qTf[:, h, :], rhs=kbd[:, h, :],
                                                      start=True, stop=True), ["qTf_a", "qTf_b", "kbd"], [f"bank{bk}"])
        for bk in range(4, 8):
            g0 = (bk - 4) * 4
            if bk % 2 == 0:
                p.add("act", lambda e, bk=bk, g0=g0: e.activation(out=sc[:, g0:g0 + 4, :], in_=banks[bk][:].rearrange("p (g n) -> p g n", g=4), func=AF.Copy),
                      [f"bank{bk}"], [f"sc{bk}"])
            else:
                p.add("dve", lambda e, bk=bk, g0=g0: e.tensor_copy(out=sc[:, g0:g0 + 4, :], in_=banks[bk][:].rearrange("p (g n) -> p g n", g=4)),
                      [f"bank{bk}"], [f"sc{bk}"])
        for gq in range(16):
            ks = f"sc{4 + gq // 4}"
            p.add("dve", lambda e, gq=gq: e.max(out=st[:, gq, 0:8], in_=sc[:, gq, :]), [ks], [f"st{gq}a"])
            p.add("dve", lambda e, gq=gq: e.max_index(out=it[:, gq, 0:8], in_max=st[:, gq, 0:8], in_values=sc[:, gq, :]), [ks, f"st{gq}a"], [f"it{gq}a"])
            p.add("dve", lambda e, gq=gq: e.match_replace(out=wk[:, gq, :], in_to_replace=st[:, gq, 0:8], in_values=sc[:, gq, :], imm_value=-1e30),
                  [ks, f"st{gq}a"], [f"wk{gq}"])
            p.add("dve", lambda e, gq=gq: e.max(out=st[:, gq, 8:16], in_=wk[:, gq, :]), [f"wk{gq}"], [f"st{gq}b"])
            p.add("dve", lambda e, gq=gq: e.max_index(out=it[:, gq, 8:16], in_max=st[:, gq, 8:16], in_values=wk[:, gq, :]), [f"wk{gq}", f"st{gq}b"], [f"it{gq}b"])
        allst = [f"st{gq}{x}" for gq in range(16) for x in "ab"]
        allit = [f"it{gq}{x}" for gq in range(16) for x in "ab"]
        p.add("dve", lambda e: e.tensor_copy(out=itf[:], in_=it[:]), allit, ["itf"])
        st4 = st[:].rearrange("p (h two) k -> p h two k", two=2)
        itf4 = itf[:].rearrange("p (h two) k -> p h two k", two=2)
        cand4 = cand[:].rearrange("p h (i j) -> p h i j", i=16)
        cidx4 = cidx[:].rearrange("p h (i j) -> p h i j", i=16)
        for h in range(8):
            p.add("dve", lambda e, h=h: e.tensor_tensor(out=cand4[:, h], in0=st4[:, h, 0, :, None].to_broadcast([128, 16, 16]),
                                                        in1=st4[:, h, 1, None, :].to_broadcast([128, 16, 16]), op=ALU.add), allst, [f"cand{h}"])
            p.add("dve", lambda e, h=h: e.scalar_tensor_tensor(out=cidx4[:, h], in0=itf4[:, h, 0, :, None].to_broadcast([128, 16, 16]), scalar=128.0,
                                                               in1=itf4[:, h, 1, None, :].to_broadcast([128, 16, 16]), op0=ALU.mult, op1=ALU.add),
                  ["itf"], [f"cidx{h}"])
            p.add("dve", lambda e, h=h: e.max(out=best[:, h, 0:8], in_=cand[:, h, :]), [f"cand{h}"], [f"best{h}a"])
            p.add("dve", lambda e, h=h: e.match_replace(out=wk2[:, h, :], in_to_replace=best[:, h, 0:8], in_values=cand[:, h, :], imm_value=-1e30),
                  [f"cand{h}", f"best{h}a"], [f"wk2{h}"])
            p.add("dve", lambda e, h=h: e.max(out=best[:, h, 8:16], in_=wk2[:, h, :]), [f"wk2{h}"], [f"best{h}b"])
            for k in range(16):
                hk = h * 16 + k
                p.add("dve", lambda e, h=h, k=k, hk=hk: e.scalar_tensor_tensor(
                    out=junk[:, 0:256], in0=cand[:, h, :], scalar=best[:, h, k:k + 1], in1=cidx[:, h, :], op0=ALU.is_equal, op1=ALU.mult,
                    accum_out=eidf[:, hk:hk + 1]), [f"cand{h}", f"cidx{h}", f"best{h}a", f"best{h}b"], ["junk", f"eidf{h}"])
        alle = [f"eidf{h}" for h in range(8)]
        allb = [f"best{h}{x}" for h in range(8) for x in "ab"]
        p.add("dve", lambda e: e.tensor_scalar(out=eidf[:], in0=eidf[:], scalar1=float(NE - 1), scalar2=0.0, op0=ALU.min, op1=ALU.max), alle, ["eidf"])
        p.add("dve", lambda e: e.tensor_copy(out=eidi[:], in_=eidf[:]), ["eidf"], ["eidi"])
        p.add("dve", lambda e: e.tensor_tensor(out=gw[:], in0=best[:], in1=best[:, :, 0:1].to_broadcast([128, 8, 16]), op=ALU.subtract), allb, ["gw"])
        p.add("act", lambda e: e.activation(out=gw[:], in_=gw[:], func=AF.Exp), ["gw"], ["gw"])
        p.add("dve", lambda e: e.tensor_reduce(out=sm[:, 0:8], in_=gw[:], axis=AX.X, op=ALU.add), ["gw"], ["sm0"])
        p.add("dve", lambda e: e.reciprocal(out=sm[:, 8:16], in_=sm[:, 0:8]), ["sm0"], ["sm1"])
        p.add("dve", lambda e: e.tensor_tensor(out=gw[:], in0=gw[:], in1=sm[:, 8:16, None].to_broadcast([128, 8, 16]), op=ALU.mult), ["gw", "sm1"], ["gw"])
        for hk in range(128):
            b = ngat % NGB; ngat += 1
            p.add("pool", lambda e, b=b, hk=hk: e.indirect_dma_start(
                out=rows[b][:], out_offset=None, in_=pu, in_offset=bass.IndirectOffsetOnAxis(ap=eidi[:, hk:hk + 1], axis=0)), ["eidi"], [f"rows{b}"])
            p.add("dve", lambda e, b=b, hk=hk: e.scalar_tensor_tensor(out=junk[:], in0=rows[b][:], scalar=1.0, in1=h2t[:], op0=ALU.mult, op1=ALU.mult,
                                                                     accum_out=act[:, hk:hk + 1]), [f"rows{b}", "h2t"], ["junk", "act"])
        p.add("act", lambda e: e.activation(out=coef[:], in_=act[:], func=AF.Gelu), ["act"], ["coef"])
        p.add("dve", lambda e: e.tensor_tensor(out=coef[:], in0=coef[:], in1=gw[:].rearrange("p h k -> p (h k)"), op=ALU.mult), ["coef", "gw"], ["coef"])
        p.add("dve", lambda e: e.memset(acc[:], 0.0), [], ["acc"])
        for hk in range(128):
            b = ngat % NGB; ngat += 1
            p.add("pool", lambda e, b=b, hk=hk: e.indirect_dma_start(
                out=rows[b][:], out_offset=None, in_=pv, in_offset=bass.IndirectOffsetOnAxis(ap=eidi[:, hk:hk + 1], axis=0)), ["eidi"], [f"rows{b}"])
            p.add("dve", lambda e, b=b, hk=hk: e.scalar_tensor_tensor(out=acc[:], in0=rows[b][:], scalar=coef[:, hk:hk + 1], in1=acc[:],
                                                                     op0=ALU.mult, op1=ALU.add), [f"rows{b}", "coef", "acc"], ["acc"])
        p.add("dve", lambda e, gb=gb: e.tensor_tensor(out=x2t[:], in0=acc[:], in1=gb[:], op=ALU.mult), ["acc", kgb], ["x2t"])
        p.add("dve", lambda e: e.tensor_tensor(out=x2t[:], in0=x2t[:], in1=x1t[:], op=ALU.add), ["x2t", "x1t"], ["x2t"])
        p.dma(T["xs"][r0:r0 + 128, :], x2t[:], reads=["x2t"])


def _ph_F(p, g, T):
    fg = p.sb([128, D], name="fg_sb"); p.dma(fg[:], T["fg"], writes=["fg"])
    junk = p.sb([128, D], name="junk")
    NB = 2
    xt = [p.sb([128, D], name=f"xt{i}") for i in range(NB)]
    yt = [p.sb([128, D], name=f"yt{i}") for i in range(NB)]
    ss = [p.sb([128, 4], name=f"ss{i}") for i in range(NB)]
    for t in range(g.S // 128):
        i = t % NB
        r0 = t * 128
        p.dma(xt[i][:], T["xs"][g.CT + r0:g.CT + r0 + 128, :], writes=[f"xt{i}"])
        p.add("act", lambda e, i=i: e.activation(out=junk[:], in_=xt[i][:], func=AF.Square, accum_out=ss[i][:, 0:1]), [f"xt{i}"], ["junk", f"ss0{i}"])
        p.add("dve", lambda e, i=i: e.tensor_scalar(out=ss[i][:, 1:2], in0=ss[i][:, 0:1], scalar1=1.0 / D, scalar2=EPS, op0=ALU.mult, op1=ALU.add),
              [f"ss0{i}"], [f"ss1{i}"])
        p.add("act", lambda e, i=i: e.activation(out=ss[i][:, 2:3], in_=ss[i][:, 1:2], func=AF.Sqrt), [f"ss1{i}"], [f"ss2{i}"])
        p.add("dve", lambda e, i=i: e.reciprocal(out=ss[i][:, 3:4], in_=ss[i][:, 2:3]), [f"ss2{i}"], [f"ss3{i}"])
        p.add("dve", lambda e, i=i: e.scalar_tensor_tensor(out=yt[i][:], in0=xt[i][:], scalar=ss[i][:, 3:4], in1=fg[:], op0=ALU.mult, op1=ALU.mult),
              [f"xt{i}", f"ss3{i}", "fg"], [f"yt{i}"])
        p.dma(T["y"][r0:r0 + 128, :], yt[i][:], reads=[f"yt{i}"])


def build_fused(S, CT, DEPTH):
    g = Cfg(S, CT, DEPTH)
    nc = bass.Bass("TRN2", target_bir_lowering=False)
    NTOK = g.NTOK
    T = {}
    ins = {"xs0": [NTOK, D], "cT": [128, 8, 2], "ada_w": [DEPTH, D, 6144], "ada_bB": [DEPTH, 128, 6144], "ada_bT": [DEPTH, 128, 48],
           "n1g": [DEPTH, 128, 8], "n2gB": [DEPTH, 128, D], "w_in": [DEPTH, D, IN_W], "cs": [NTOK, 64],
           "cw": [DEPTH, 128, 2, 31], "cp": [DEPTH, 128, 2, 3], "mgb": [DEPTH, 8, 128, 2], "mng": [DEPTH, 128, 64],
           "dl": [DEPTH, 128, 256], "dng": [DEPTH, 128, 128], "lami": [DEPTH, 128, 2],
           "w_out": [DEPTH, D, D], "wq": [DEPTH, D, D], "kbd": [DEPTH, 128, 8, 256],
           "fg": [128, D],
           "ident": [128, 128], "ones": [128, 128], "tri": [128, 128], "triL": [128, 128]}
    for k, shp in ins.items():
        T[k] = _din(nc, k, shp)
    T["peer_u"] = [_din(nc, f"peer_u{l}", [g.NE, D]) for l in range(DEPTH)]
    T["peer_v"] = [_din(nc, f"peer_v{l}", [g.NE, D]) for l in range(DEPTH)]
    T["y"] = _dout(nc, "y", [S, D])
    scr = {"xs": [NTOK, D], "proj": [NTOK, IN_W], "projT": [2048, NTOK], "mixo": [NTOK, D], "convT": [2, 128, NTOK],
           "x1": [NTOK, D], "h2": [NTOK, D], "modT": [DEPTH, 128, 48, 2], "modB": [DEPTH, 2, 128, 6144]}
    for k, shp in scr.items():
        T[k] = nc.dram_tensor("scr_" + k, shp, F32, kind="Internal").ap()
    p = Prog(nc)
    CH = 2048
    for r0 in range(0, NTOK, CH):
        r1 = min(NTOK, r0 + CH)
        p.dma(T["xs"][r0:r1, :], T["xs0"][r0:r1, :])
    _ph_P0(p, g, T)
    p.end_phase()
    for l in range(DEPTH):
        for ph in (_ph_A, _ph_M1, _ph_M2, _ph_C1, _ph_C2, _ph_C3):
            p.begin_phase()
            ph(p, g, T, l)
            p.end_phase()
    p.begin_phase()
    _ph_F(p, g, T)
    p.emit()
    return nc


def fused_inputs(b, x, c, ctx, c_ctx, ada_w, ada_b, norm1_g, norm2_g, w_in, conv_w, conv_b, conv_ln_g, conv_ln_b,
                 mlstm_gate_b, mlstm_norm_g, diff_lambda, diff_norm_g, w_out, peer_wq, peer_keys, peer_u, peer_v, final_g, shared=None):
    f32 = np.float32
    DEPTH = ada_w.shape[0]
    S, CT = x.shape[1], ctx.shape[1]
    if shared is None:
        shared = {}
    if not shared:
        cos, sin = _rope_tables(S)
        cs = np.zeros((CT + S, 64), f32); cs[:CT, :32] = 1.0; cs[CT:, :32] = cos; cs[CT:, 32:] = sin
        shared["cs"] = cs
        shared["ada_w"] = np.asarray(ada_w, f32)
        ab = np.asarray(ada_b, f32)
        shared["ada_bB"] = np.ascontiguousarray(np.broadcast_to(ab[:, None, :], (DEPTH, 128, 6144)))
        shared["ada_bT"] = np.ascontiguousarray(ab.reshape(DEPTH, 48, 128).transpose(0, 2, 1))
        shared["n1g"] = np.ascontiguousarray(np.asarray(norm1_g, f32).reshape(DEPTH, 8, 128).transpose(0, 2, 1))
        shared["n2gB"] = np.ascontiguousarray(np.broadcast_to(np.asarray(norm2_g, f32)[:, None, :], (DEPTH, 128, D)))
        shared["w_in"] = np.asarray(w_in, f32)
        shared["cw"] = np.ascontiguousarray(np.asarray(conv_w, f32)[:, :, 0, :].transpose(0, 2, 1).reshape(DEPTH, 2, 128, 31).transpose(0, 2, 1, 3))
        cp = np.stack([np.asarray(conv_b, f32), np.asarray(conv_ln_g, f32), np.asarray(conv_ln_b, f32)], -1)
        shared["cp"] = np.ascontiguousarray(cp.reshape(DEPTH, 2, 128, 3).transpose(0, 2, 1, 3))
        gbv = np.asarray(mlstm_gate_b, f32)
        mgb = np.zeros((DEPTH, 8, 128, 2), f32)
        for h in range(4):
            for d in range(2):
                mgb[:, h * 2 + d, :, 0] = gbv[:, 2 * d, h][:, None]
                mgb[:, h * 2 + d, :, 1] = gbv[:, 2 * d + 1, h][:, None]
        shared["mgb"] = mgb
        shared["mng"] = np.ascontiguousarray(np.broadcast_to(np.asarray(mlstm_norm_g, f32)[:, None, :], (DEPTH, 128, 64)))
        shared["dl"] = np.ascontiguousarray(np.broadcast_to(np.asarray(diff_lambda, f32).reshape(DEPTH, 1, 256), (DEPTH, 128, 256)))
        shared["dng"] = np.ascontiguousarray(np.broadcast_to(np.asarray(diff_norm_g, f32)[:, None, :], (DEPTH, 128, 128)))
        lami = np.zeros((DEPTH, 128, 2), f32)
        for l in range(DEPTH):
            li = 0.8 - 0.6 * math.exp(-0.3 * l)
            lami[l, :, 0] = li; lami[l, :, 1] = 1.0 - li
        shared["lami"] = lami
        shared["w_out"] = np.asarray(w_out, f32)
        shared["wq"] = np.asarray(peer_wq, f32)
        keys = np.asarray(peer_keys, f32)
        kbd = np.zeros((DEPTH, 128, 8, 256), f32)
        for pp in range(2):
            kbd[:, pp * 64:(pp + 1) * 64, :, pp * 128:(pp + 1) * 128] = keys[:, :, pp].transpose(0, 3, 1, 2)
        shared["kbd"] = kbd
        for l in range(DEPTH):
            shared[f"peer_u{l}"] = np.ascontiguousarray(np.asarray(peer_u[l], f32))
            shared[f"peer_v{l}"] = np.ascontiguousarray(np.asarray(peer_v[l], f32))
        shared["fg"] = _rep(np.asarray(final_g, f32))
        shared["ident"] = np.eye(128, dtype=f32)
        shared["ones"] = np.ones((128, 128), f32)
        shared["tri"] = np.triu(np.ones((128, 128), f32))
        shared["triL"] = np.tril(np.ones((128, 128), f32))
    m = dict(shared)
    m["xs0"] = np.ascontiguousarray(np.concatenate([np.asarray(ctx[b], f32), np.asarray(x[b], f32)], 0))
    cT = np.stack([np.asarray(c[b], f32), np.asarray(c_ctx, f32)], -1)
    m["cT"] = np.ascontiguousarray(cT.reshape(8, 128, 2).transpose(1, 0, 2))
    return m


_FUSED = {}


def kernel(**inp):
    x = np.asarray(inp["x"], np.float32)
    B, S, _ = x.shape
    CT = inp["ctx"].shape[1]
    DEPTH = inp["ada_w"].shape[0]
    key = (S, CT, DEPTH)
    if key not in _FUSED:
        _FUSED[key] = build_fused(S, CT, DEPTH)
    nc = _FUSED[key]
    shared = {}
    per_b = [fused_inputs(b, shared=shared, **inp) for b in range(B)]
    maps = [per_b[(cc // 2) % B] for cc in range(NCORES)]
    res = run_bass_kernel_spmd(nc, maps, core_ids=list(range(NCORES))).results
    out = np.zeros((B, S, D), np.float32)
    for b in range(B):
        out[b] = res[2 * b]["y"]
    return out
```

```python
import math
from contextlib import ExitStack
import numpy as np
import ml_dtypes
import concourse.bass as bass
import concourse.mybir as mybir
from concourse.bass_utils import run_bass_kernel_spmd

F32 = mybir.dt.float32
BF16 = mybir.dt.bfloat16
I32 = mybir.dt.int32
U32 = mybir.dt.uint32
AF = mybir.ActivationFunctionType
ALU = mybir.AluOpType
AX = mybir.AxisListType

D = 1024
IN_W = 3088
EPS = 1e-6
R_DMA = 8
NCORES = 8


class Prog:
    ENGS = ("pe", "act", "dve", "pool", "sp")

    def __init__(self, nc):
        self.nc = nc
        self.gs = ExitStack()
        self.sems = {}
        for e in ("pe", "act", "dve"):
            self.sems[e] = self.gs.enter_context(nc.semaphore(f"s_{e}"))
        for e in ("sp", "pool"):
            self.sems[e] = [self.gs.enter_context(nc.semaphore(f"s_{e}{i}")) for i in range(R_DMA)]
        self.cnt = {e: 0 for e in self.ENGS}
        self.waited = {e: {} for e in self.ENGS}
        self.nalloc = 0
        self.phase_open = False
        self.begin_phase()

    def begin_phase(self):
        assert not self.phase_open
        self.phase_open = True
        self.es = ExitStack()
        self.ops = {e: [] for e in self.ENGS}
        self.last_w = {}
        self.readers = {}
        self.bar = dict(self.cnt)

    def sb(self, shape, dtype=F32, name=None):
        self.nalloc += 1
        name = f"{name or 'sb'}_{self.nalloc}"
        return self.es.enter_context(self.nc.sbuf_tensor(name, list(shape), dtype))

    def ps(self, shape, dtype=F32, name=None):
        self.nalloc += 1
        name = f"{name or 'ps'}_{self.nalloc}"
        return self.es.enter_context(self.nc.psum_tensor(name, list(shape), dtype))

    def add(self, eng, fn, reads=(), writes=()):
        deps = set()
        for b in reads:
            if b in self.last_w:
                deps.add(self.last_w[b])
        for b in writes:
            if b in self.last_w:
                deps.add(self.last_w[b])
            for r in self.readers.get(b, ()):
                deps.add(r)
        self.cnt[eng] += 1
        idx = self.cnt[eng]
        me = (eng, idx)
        for b in reads:
            self.readers.setdefault(b, []).append(me)
        for b in writes:
            self.last_w[b] = me
            self.readers[b] = []
        deps.discard(me)
        self.ops[eng].append((fn, deps, idx))
        return me

    def dma(self, out, in_, reads=(), writes=(), eng="sp", **kw):
        return self.add(eng, lambda e: e.dma_start(out=out, in_=in_, **kw), reads, writes)

    def _dep_wait(self, engname, engobj, dep):
        waited = self.waited[engname]
        f, k = dep
        if f in ("sp", "pool"):
            n = k - 1
            sem = self.sems[f][n % R_DMA]
            val = 16 * (n // R_DMA + 1)
            key = (f, n % R_DMA)
        else:
            sem = self.sems[f]
            val = k
            key = f
        if waited.get(key, 0) < val:
            engobj.wait_ge(sem, val)
            waited[key] = val

    def _wait_all(self, engname, engobj, counts):
        for f in self.ENGS:
            tot = counts[f]
            if tot == 0:
                continue
            if f in ("sp", "pool"):
                for k in range(max(1, tot - R_DMA + 1), tot + 1):
                    self._dep_wait(engname, engobj, (f, k))
            else:
                self._dep_wait(engname, engobj, (f, tot))

    def end_phase(self, final=False):
        assert self.phase_open
        self.phase_open = False
        nc = self.nc

        def run(engname, engobj):
            first = True
            for fn, deps, idx in self.ops[engname]:
                if first:
                    self._wait_all(engname, engobj, self.bar)
                    first = False
                for d in sorted(deps):
                    if d[0] == engname and engname == "pe":
                        continue
                    self._dep_wait(engname, engobj, d)
                if engname in ("sp", "pool"):
                    n = idx - 1
                    if n >= R_DMA:
                        self._dep_wait(engname, engobj, (engname, idx - R_DMA))
                    inst = fn(engobj)
                    inst.then_inc(self.sems[engname][n % R_DMA], 16)
                else:
                    inst = fn(engobj)
                    inst.then_inc(self.sems[engname], 1)
            if final and engname in ("sp", "pool"):
                self._wait_all(engname, engobj, {e: (self.cnt[e] if e == engname else 0) for e in self.ENGS})

        with nc.Block() as block:
            @block.tensor
            def _(e):
                run("pe", e)

            @block.scalar
            def _(e):
                run("act", e)

            @block.vector
            def _(e):
                run("dve", e)

            @block.gpsimd
            def _(e):
                run("pool", e)

            @block.sync
            def _(e):
                run("sp", e)
        self.es.close()

    def emit(self):
        self.end_phase(final=True)
        self.gs.close()


def _din(nc, name, shape, dt=F32):
    return nc.dram_tensor(name, list(shape), dt, kind="ExternalInput").ap()


def _dout(nc, name, shape, dt=F32):
    return nc.dram_tensor(name, list(shape), dt, kind="ExternalOutput").ap()


def build_P0():
    nc = bass.Bass("TRN2", target_bir_lowering=False)
    w = _din(nc, "w", [1024, 3072])
    b = _din(nc, "b", [128, 24])
    cT = _din(nc, "cT", [128, 8, 5])
    o = _dout(nc, "modT", [128, 24, 5])
    p = Prog(nc)
    ct = p.sb([128, 8, 5]); st = p.sb([128, 8, 5]); bt = p.sb([128, 24]); ot = p.sb([128, 24, 5])
    p.dma(ct[:], cT, writes=["ct"])
    p.dma(bt[:], b, writes=["bt"])
    p.add("act", lambda e: e.activation(out=st[:], in_=ct[:], func=AF.Silu), ["ct"], ["st"])
    wv = w.rearrange("(k p) n -> p k n", p=128)
    NB = 6
    wts = [p.sb([128, 8, 512], name=f"wt{i}") for i in range(2)]
    pss = [p.ps([128, 4, 8], name=f"pp{i}") for i in range(2)]
    for blk in range(NB):
        wt = wts[blk % 2]; ps = pss[blk % 2]
        p.dma(wt[:], wv[:, :, blk * 512:(blk + 1) * 512], writes=[f"wt{blk%2}"])
        for jj in range(4):
            for k in range(8):
                p.add("pe", lambda e, wt=wt, ps=ps, jj=jj, k=k: e.matmul(
                    ps[:, jj, 0:5], lhsT=wt[:, k, jj * 128:(jj + 1) * 128], rhs=st[:, k, :],
                    start=(k == 0), stop=(k == 7)), [f"wt{blk%2}", "st"], [f"pp{blk%2}"])
        for jj in range(4):
            j = blk * 4 + jj
            p.add("dve", lambda e, ps=ps, jj=jj, j=j: e.tensor_scalar(
                out=ot[:, j, :], in0=ps[:, jj, 0:5], scalar1=bt[:, j:j + 1], scalar2=None,
                op0=ALU.add), [f"pp{blk%2}", "bt"], ["ot"])
    p.dma(o, ot[:], reads=["ot"])
    p.emit()
    return nc


def load_cast_weight(p, wb, w, ncols, key, nk=8, bw=512):
    wv = w.rearrange("(k p) n -> p k n", p=128)
    stg = [p.sb([128, nk, bw], name=f"stg_{key}{i}") for i in range(2)]
    for bi, c0 in enumerate(range(0, ncols, bw)):
        cw = min(bw, ncols - c0)
        st = stg[bi % 2]
        p.dma(st[:, :, 0:cw], wv[:, :, c0:c0 + cw], writes=[f"stg_{key}{bi%2}"])
        h = nk // 2
        p.add("act", lambda e, st=st, c0=c0, cw=cw: e.activation(out=wb[:, 0:h, c0:c0 + cw], in_=st[:, 0:h, 0:cw], func=AF.Copy),
              [f"stg_{key}{bi%2}"], [f"{key}_a{bi}"])
        p.add("dve", lambda e, st=st, c0=c0, cw=cw: e.tensor_copy(out=wb[:, h:nk, c0:c0 + cw], in_=st[:, h:nk, 0:cw]),
              [f"stg_{key}{bi%2}"], [f"{key}_b{bi}"])
    p.add("act", lambda e: e.activation(out=wb[0:1, 0, 0:1], in_=wb[0:1, 0, 0:1], func=AF.Copy),
          [f"{key}_a{bi}" for bi in range((ncols + bw - 1) // bw)] + [f"{key}_b{bi}" for bi in range((ncols + bw - 1) // bw)], [key])


def build_A(NT, ctx_tiles=(0,), stage=9):
    nc = bass.Bass("TRN2", target_bir_lowering=False)
    xs = _din(nc, "xs", [NT * 128, D])
    w_in = _din(nc, "w_in", [D, IN_W])
    pv = _din(nc, "pv", [128, 8, 5])
    cs = _din(nc, "cs", [NT * 128, 64])
    ident = _din(nc, "ident", [128, 128])
    proj = _dout(nc, "proj", [NT * 128, IN_W])
    p = Prog(nc)
    wb = p.sb([128, 8, IN_W], BF16, name="wb")
    load_cast_weight(p, wb, w_in, IN_W, "wb")
    idt = p.sb([128, 128]); p.dma(idt[:], ident, writes=["idt"])
    pvt = p.sb([128, 8, 5]); p.dma(pvt[:], pv, writes=["pvt"])
    gm = p.sb([128, 8, 2])
    for i, col in enumerate((1, 3)):
        p.add("dve", lambda e, i=i, col=col: e.scalar_tensor_tensor(
            out=gm[:, :, i], in0=pvt[:, :, col], scalar=1.0, in1=pvt[:, :, 0],
            op0=ALU.add, op1=ALU.mult), ["pvt"], ["gm"])
    NB = 2
    xt = [p.sb([128, D], name=f"xt{i}") for i in range(NB)]
    xn = [p.sb([128, D], name=f"xn{i}") for i in range(NB)]
    junk = p.sb([128, D], name="junk")
    ss = [p.sb([128, 1], name=f"ss{i}") for i in range(NB)]
    rstd = [p.sb([128, 1], name=f"rstd{i}") for i in range(NB)]
    hT = [p.sb([128, 8, 128], BF16, name=f"hT{i}") for i in range(NB)]
    ot = [p.sb([128, IN_W], name=f"ot{i}") for i in range(NB)]
    ro = [p.sb([128, 1024], name=f"ro{i}") for i in range(NB)]
    tmp = [p.sb([128, 4, 512], name=f"tmp{i}") for i in range(NB)]
    cst = [p.sb([128, 64], name=f"cst{i}") for i in range(NB)]
    tp = [p.ps([128, 512], name=f"tp{i}") for i in range(4)]
    acc = [p.ps([128, 512], name=f"acc{i}") for i in range(4)]
    nacc = 0
    for t in range(NT):
        i = t % NB
        isc = t in ctx_tiles
        r0 = t * 128
        p.dma(xt[i][:], xs[r0:r0 + 128, :], writes=[f"xt{i}"])
        p.dma(cst[i][:], cs[r0:r0 + 128, :], writes=[f"cst{i}"])
        p.add("act", lambda e, i=i: e.activation(out=junk[:], in_=xt[i][:], func=AF.Square,
                                                  accum_out=ss[i][:, 0:1]), [f"xt{i}"], ["junk", f"ss{i}"])
        p.add("dve", lambda e, i=i: e.tensor_scalar(out=rstd[i][:], in0=ss[i][:], scalar1=1.0 / D, scalar2=EPS,
                                                     op0=ALU.mult, op1=ALU.add), [f"ss{i}"], [f"rstd{i}"])
        p.add("act", lambda e, i=i: e.activation(out=ss[i][:], in_=rstd[i][:], func=AF.Sqrt), [f"rstd{i}"], [f"ss{i}"])
        p.add("dve", lambda e, i=i: e.reciprocal(out=rstd[i][:], in_=ss[i][:]), [f"ss{i}"], [f"rstd{i}"])
        p.add("dve", lambda e, i=i: e.tensor_scalar(out=xn[i][:], in0=xt[i][:], scalar1=rstd[i][:, 0:1], scalar2=None,
                                                     op0=ALU.mult), [f"xt{i}", f"rstd{i}"], [f"xn{i}"])
        if stage == 1:
            p.dma(proj[r0:r0 + 128, 0:1024], xn[i][:], reads=[f"xn{i}"])
            continue
        for half in range(2):
            tpi = (2 * t + half) % 4
            for k in range(half * 4, half * 4 + 4):
                p.add("pe", lambda e, i=i, k=k, tpi=tpi: e.transpose(
                    tp[tpi][:, (k % 4) * 128:(k % 4 + 1) * 128], xn[i][:, k * 128:(k + 1) * 128], idt[:]),
                    [f"xn{i}", "idt"], [f"tp{tpi}"])
            sc_col = 1 if isc else 0
            sh_col = 4 if isc else 2
            for k in range(half * 4, half * 4 + 4):
                if half == 0:
                    p.add("act", lambda e, i=i, k=k, tpi=tpi, sc_col=sc_col, sh_col=sh_col: e.activation(
                        out=hT[i][:, k, :], in_=tp[tpi][:, (k % 4) * 128:(k % 4 + 1) * 128], func=AF.Identity,
                        scale=gm[:, k, sc_col:sc_col + 1], bias=pvt[:, k, sh_col:sh_col + 1]),
                        [f"tp{tpi}", "gm", "pvt"], [f"hT{i}_{k}"])
                else:
                    p.add("dve", lambda e, i=i, k=k, tpi=tpi, sc_col=sc_col, sh_col=sh_col: e.tensor_scalar(
                        out=hT[i][:, k, :], in0=tp[tpi][:, (k % 4) * 128:(k % 4 + 1) * 128],
                        scalar1=gm[:, k, sc_col:sc_col + 1], scalar2=pvt[:, k, sh_col:sh_col + 1],
                        op0=ALU.mult, op1=ALU.add), [f"tp{tpi}", "gm", "pvt"], [f"hT{i}_{k}"])
        if stage == 2:
            p.add("dve", lambda e, i=i: e.tensor_copy(out=ot[i][:, 0:1024], in_=hT[i][:].rearrange("p k n -> p (k n)")),
                  [f"hT{i}_{k}" for k in range(8)], [f"ot{i}_0"])
            p.dma(proj[r0:r0 + 128, 0:1024], ot[i][:, 0:1024], reads=[f"ot{i}_0"])
            continue
        for blk in range(7):
            c0 = blk * 512
            cw = min(512, IN_W - c0)
            a = nacc % 4; nacc += 1
            for k in range(8):
                p.add("pe", lambda e, i=i, k=k, a=a, c0=c0, cw=cw: e.matmul(
                    acc[a][:, 0:cw], lhsT=hT[i][:, k, :], rhs=wb[:, k, c0:c0 + cw],
                    start=(k == 0), stop=(k == 7)), [f"hT{i}_{k}", "wb"], [f"acc{a}"])
            if blk % 2 == 0:
                p.add("act", lambda e, i=i, a=a, c0=c0, cw=cw: e.activation(
                    out=ot[i][:, c0:c0 + cw], in_=acc[a][:, 0:cw], func=AF.Copy), [f"acc{a}"], [f"ot{i}_{blk}"])
            else:
                p.add("dve", lambda e, i=i, a=a, c0=c0, cw=cw: e.tensor_copy(
                    out=ot[i][:, c0:c0 + cw], in_=acc[a][:, 0:cw]), [f"acc{a}"], [f"ot{i}_{blk}"])
        if stage == 3:
            p.dma(proj[r0:r0 + 128, :], ot[i][:], reads=[f"ot{i}_{b}" for b in range(7)])
            continue
        src = ot[i][:, 1552:2576].rearrange("p (g r two) -> p g r two", g=16, two=2)
        dst = ro[i][:].rearrange("p (g r two) -> p g r two", g=16, two=2)
        t1 = src[:, :, :, 0]; t2 = src[:, :, :, 1]
        cosb = cst[i][:, None, 0:32].to_broadcast([128, 16, 32])
        sinb = cst[i][:, None, 32:64].to_broadcast([128, 16, 32])
        tm = [tmp[i][:, j, :].rearrange("p (g r) -> p g r", g=16) for j in range(4)]
        rk = [f"ot{i}_3", f"ot{i}_4", f"ot{i}_5", f"cst{i}"]
        p.add("dve", lambda e, tm=tm, t1=t1, cosb=cosb: e.tensor_tensor(out=tm[0], in0=t1, in1=cosb, op=ALU.mult), rk, [f"tmp{i}_0"])
        p.add("dve", lambda e, tm=tm, t2=t2, sinb=sinb: e.tensor_tensor(out=tm[1], in0=t2, in1=sinb, op=ALU.mult), rk, [f"tmp{i}_1"])
        p.add("dve", lambda e, tm=tm, t1=t1, sinb=sinb: e.tensor_tensor(out=tm[2], in0=t1, in1=sinb, op=ALU.mult), rk, [f"tmp{i}_2"])
        p.add("dve", lambda e, tm=tm, t2=t2, cosb=cosb: e.tensor_tensor(out=tm[3], in0=t2, in1=cosb, op=ALU.mult), rk, [f"tmp{i}_3"])
        p.add("dve", lambda e, tm=tm, dst=dst: e.tensor_tensor(out=dst[:, :, :, 0], in0=tm[0], in1=tm[1], op=ALU.subtract),
              [f"tmp{i}_0", f"tmp{i}_1"], [f"ro{i}a"])
        p.add("dve", lambda e, tm=tm, dst=dst: e.tensor_tensor(out=dst[:, :, :, 1], in0=tm[2], in1=tm[3], op=ALU.add),
              [f"tmp{i}_2", f"tmp{i}_3"], [f"ro{i}b"])
        p.dma(proj[r0:r0 + 128, 0:1552], ot[i][:, 0:1552], reads=[f"ot{i}_{b}" for b in range(4)])
        p.dma(proj[r0:r0 + 128, 1552:2576], ro[i][:], reads=[f"ro{i}a", f"ro{i}b"])
        p.dma(proj[r0:r0 + 128, 2576:IN_W], ot[i][:, 2576:IN_W], reads=[f"ot{i}_5", f"ot{i}_6"])
    p.emit()
    return nc


def build_M1(NCH, NS=4, GC=4):
    nc = bass.Bass("TRN2", target_bir_lowering=False)
    L = NCH * 128
    mqT = _din(nc, "mqT", [NS, 64, L])
    mkT = _din(nc, "mkT", [NS, 64, L])
    mvk = _din(nc, "mvk", [NS, L, 128])
    mg = _din(nc, "mg", [NS, L, 2])
    mgb = _din(nc, "mgb", [NS, 128, 2])
    tri_d = _din(nc, "tri", [128, 128])
    ones_d = _din(nc, "ones", [128, 128])
    mh = _dout(nc, "mh", [NS, L, 64])
    p = Prog(nc)
    tri = p.sb([128, 128], name="tri_sb"); p.dma(tri[:], tri_d, writes=["tri"])
    ones = p.sb([128, 128], name="ones_sb"); p.dma(ones[:], ones_d, writes=["ones"])
    banks = [p.ps([128, 512], name=f"bank{i}") for i in range(7)]
    NBUF = 2
    B = {}
    for s in range(NS):
        gb = p.sb([128, 2], name=f"gb{s}"); p.dma(gb[:], mgb[s], writes=[f"gb{s}"])
        B[s, "gb"] = gb
        B[s, "Cf"] = p.sb([64, 65], name=f"Cf{s}")
        B[s, "Cb"] = p.sb([64, 65], BF16, name=f"Cb{s}")
        B[s, "tmpC"] = p.sb([64, 65], name=f"tmpC{s}")
        p.add("dve", lambda e, s=s: e.memset(B[s, "Cf"][:], 0.0), [], [f"Cf{s}"])
        p.add("dve", lambda e, s=s: e.memset(B[s, "Cb"][:], 0.0), [], [f"Cb{s}"])
        for i in range(NBUF):
            k = (s, i)
            B[k, "qTf"] = p.sb([64, GC * 128], name=f"qTf{s}_{i}")
            B[k, "kTf"] = p.sb([64, GC * 128], name=f"kTf{s}_{i}")
            B[k, "vkf"] = p.sb([128, GC, 128], name=f"vkf{s}_{i}")
            B[k, "gf"] = p.sb([128, GC, 2], name=f"gf{s}_{i}")
            B[k, "qTb"] = p.sb([64, GC * 128], BF16, name=f"qTb{s}_{i}")
            B[k, "kTb"] = p.sb([64, GC * 128], BF16, name=f"kTb{s}_{i}")
            B[k, "vaug"] = p.sb([128, GC, 65], BF16, name=f"vaug{s}_{i}")
            B[k, "kb"] = p.sb([128, GC, 64], BF16, name=f"kb{s}_{i}")
            B[k, "h"] = p.sb([128, GC, 64], name=f"h{s}_{i}")
            B[k, "gi"] = p.sb([128, GC], name=f"gi{s}_{i}")
            B[k, "sp"] = p.sb([128, GC], name=f"sp{s}_{i}")
            B[k, "a"] = p.sb([128, GC], name=f"a{s}_{i}")
            B[k, "b"] = p.sb([128, GC], name=f"b{s}_{i}")
            B[k, "eG"] = p.sb([128, GC], name=f"eG{s}_{i}")
            B[k, "WT"] = p.sb([128, 128], BF16, name=f"WT{s}_{i}")
            B[k, "d"] = p.sb([128, 4], name=f"d{s}_{i}")
            p.add("dve", lambda e, k=k: e.memset(B[k, "vaug"][:, :, 64:65], 1.0), [], [f"vaug1_{s}_{i}"])
    ngroups = (NCH + GC - 1) // GC
    for g in range(ngroups):
        c0 = g * GC
        gc = min(GC, NCH - c0)
        i = g % NBUF
        for s in range(NS):
            k = (s, i)
            sfx = f"{s}_{i}"
            T = {n: B[k, n] for n in ("qTf", "kTf", "vkf", "gf", "qTb", "kTb", "vaug", "kb", "h", "gi", "sp", "a", "b", "eG", "WT", "d")}
            Cf, Cb, tmpC, gb = B[s, "Cf"], B[s, "Cb"], B[s, "tmpC"], B[s, "gb"]
            bS, bX, bU = banks[(s % 2) * 3], banks[(s % 2) * 3 + 1], banks[(s % 2) * 3 + 2]
            kS, kX, kU = f"bank{(s%2)*3}", f"bank{(s%2)*3+1}", f"bank{(s%2)*3+2}"
            bP = banks[6]
            W = gc * 128
            p.dma(T["qTf"][:, 0:W], mqT[s, :, c0 * 128:c0 * 128 + W], writes=[f"qTf{sfx}"])
            p.dma(T["kTf"][:, 0:W], mkT[s, :, c0 * 128:c0 * 128 + W], writes=[f"kTf{sfx}"])
            p.dma(T["vkf"][:, 0:gc, :], mvk[s].rearrange("(c p) f -> p c f", p=128)[:, c0:c0 + gc, :], writes=[f"vkf{sfx}"])
            p.dma(T["gf"][:, 0:gc, :], mg[s].rearrange("(c p) f -> p c f", p=128)[:, c0:c0 + gc, :], writes=[f"gf{sfx}"])
            p.add("dve", lambda e, T=T, gb=gb, gc=gc: e.tensor_scalar(out=T["gi"][:, 0:gc], in0=T["gf"][:, 0:gc, 0], scalar1=gb[:, 0:1],
                                                                     scalar2=None, op0=ALU.add), [f"gf{sfx}", f"gb{s}"], [f"gi{sfx}"])
            p.add("dve", lambda e, T=T, gb=gb, gc=gc: e.tensor_scalar(out=T["sp"][:, 0:gc], in0=T["gf"][:, 0:gc, 1], scalar1=gb[:, 1:2],
                                                                     scalar2=None, op0=ALU.add), [f"gf{sfx}", f"gb{s}"], [f"sp{sfx}"])
            p.add("act", lambda e, T=T, gc=gc: e.activation(out=T["sp"][:, 0:gc], in_=T["sp"][:, 0:gc], func=AF.Exp, scale=-1.0),
                  [f"sp{sfx}"], [f"sp{sfx}"])
            p.add("dve", lambda e, T=T, gc=gc: e.tensor_scalar(out=T["sp"][:, 0:gc], in0=T["sp"][:, 0:gc], scalar1=1.0, scalar2=None,
                                                              op0=ALU.add), [f"sp{sfx}"], [f"sp{sfx}"])
            p.add("act", lambda e, T=T, gc=gc: e.activation(out=T["sp"][:, 0:gc], in_=T["sp"][:, 0:gc], func=AF.Ln),
                  [f"sp{sfx}"], [f"sp{sfx}"])
            p.add("pe", lambda e, T=T, gc=gc, bP=bP: e.matmul(bP[:, 0:gc], lhsT=tri[:], rhs=T["sp"][:, 0:gc], start=True, stop=True),
                  ["tri", f"sp{sfx}"], ["bank6"])
            p.add("pe", lambda e, T=T, gc=gc, bP=bP: e.matmul(bP[:, 64:64 + gc], lhsT=ones[:], rhs=T["sp"][:, 0:gc], start=True, stop=True),
                  ["ones", f"sp{sfx}"], ["bank6"])
            p.add("act", lambda e, T=T, gc=gc, bP=bP: e.activation(out=T["a"][:, 0:gc], in_=bP[:, 0:gc], func=AF.Exp, scale=-1.0),
                  ["bank6"], [f"a{sfx}"])
            p.add("act", lambda e, T=T, gc=gc, bP=bP: e.activation(out=T["eG"][:, 0:gc], in_=bP[:, 64:64 + gc], func=AF.Exp, scale=-1.0),
                  ["bank6"], [f"eG{sfx}"])
            p.add("act", lambda e, T=T, gc=gc, bP=bP: e.activation(out=T["b"][:, 0:gc], in_=bP[:, 0:gc], func=AF.Identity),
                  ["bank6"], [f"b{sfx}"])
            p.add("dve", lambda e, T=T, gc=gc: e.tensor_tensor(out=T["b"][:, 0:gc], in0=T["b"][:, 0:gc], in1=T["gi"][:, 0:gc], op=ALU.add),
                  [f"b{sfx}", f"gi{sfx}"], [f"b{sfx}"])
            p.add("act", lambda e, T=T, gc=gc: e.activation(out=T["b"][:, 0:gc], in_=T["b"][:, 0:gc], func=AF.Exp),
                  [f"b{sfx}"], [f"b{sfx}"])
            p.add("act", lambda e, T=T, W=W: e.activation(out=T["qTb"][:, 0:W], in_=T["qTf"][:, 0:W], func=AF.Copy),
                  [f"qTf{sfx}"], [f"qTb{sfx}"])
            p.add("dve", lambda e, T=T, W=W: e.tensor_scalar(out=T["kTb"][:, 0:W], in0=T["kTf"][:, 0:W], scalar1=0.125, scalar2=None,
                                                            op0=ALU.mult), [f"kTf{sfx}"], [f"kTb{sfx}"])
            p.add("act", lambda e, T=T, gc=gc: e.activation(out=T["vaug"][:, 0:gc, 0:64], in_=T["vkf"][:, 0:gc, 0:64], func=AF.Copy),
                  [f"vkf{sfx}"], [f"vaug{sfx}"])
            p.add("dve", lambda e, T=T, gc=gc: e.scalar_tensor_tensor(
                out=T["kb"][:, 0:gc, :], in0=T["vkf"][:, 0:gc, 64:128], scalar=0.125,
                in1=T["b"][:, 0:gc, None].to_broadcast([128, gc, 64]), op0=ALU.mult, op1=ALU.mult),
                [f"vkf{sfx}", f"b{sfx}"], [f"kb{sfx}"])
            for cc in range(gc):
                cs = slice(cc * 128, (cc + 1) * 128)
                p.add("pe", lambda e, T=T, cs=cs, bS=bS: e.matmul(bS[:, 0:128], lhsT=T["kTb"][:, cs], rhs=T["qTb"][:, cs],
                                                                  start=True, stop=True), [f"kTb{sfx}", f"qTb{sfx}"], [kS])
                p.add("dve", lambda e, T=T, cc=cc, bS=bS: e.scalar_tensor_tensor(
                    out=T["WT"][:], in0=bS[:, 0:128], scalar=T["b"][:, cc:cc + 1], in1=tri[:], op0=ALU.mult, op1=ALU.mult),
                    [kS, f"b{sfx}", "tri"], [f"WT{sfx}"])
                p.add("pe", lambda e, T=T, cc=cc, bX=bX: e.matmul(bX[:, 0:65], lhsT=T["WT"][:], rhs=T["vaug"][:, cc, :],
                                                                  start=True, stop=False), [f"WT{sfx}", f"vaug{sfx}", f"vaug1_{sfx}"], [kX])
                p.add("pe", lambda e, T=T, cs=cs, bX=bX, Cb=Cb: e.matmul(bX[:, 0:65], lhsT=T["qTb"][:, cs], rhs=Cb[:],
                                                                         start=False, stop=True), [f"qTb{sfx}", f"Cb{s}"], [kX])
                p.add("pe", lambda e, T=T, cc=cc, bU=bU: e.matmul(bU[0:64, 0:65], lhsT=T["kb"][:, cc, :], rhs=T["vaug"][:, cc, :],
                                                                  start=True, stop=True), [f"kb{sfx}", f"vaug{sfx}", f"vaug1_{sfx}"], [kU])
                d = T["d"]
                p.add("act", lambda e, T=T, cc=cc, bX=bX, d=d: e.activation(
                    out=d[:, 0:1], in_=bX[:, 64:65], func=AF.Abs, scale=T["a"][:, cc:cc + 1]),
                    [kX, f"a{sfx}"], [f"d0{sfx}"])
                p.add("dve", lambda e, d=d: e.tensor_scalar(out=d[:, 3:4], in0=d[:, 0:1], scalar1=1.0, scalar2=None, op0=ALU.max),
                      [f"d0{sfx}"], [f"d3{sfx}"])
                p.add("dve", lambda e, d=d: e.reciprocal(out=d[:, 1:2], in_=d[:, 3:4]), [f"d3{sfx}"], [f"d1{sfx}"])
                p.add("dve", lambda e, T=T, cc=cc, d=d: e.tensor_tensor(out=d[:, 2:3], in0=d[:, 1:2], in1=T["a"][:, cc:cc + 1], op=ALU.mult),
                      [f"d1{sfx}", f"a{sfx}"], [f"d2{sfx}"])
                p.add("dve", lambda e, T=T, cc=cc, bX=bX, d=d: e.tensor_scalar(
                    out=T["h"][:, cc, :], in0=bX[:, 0:64], scalar1=d[:, 2:3], scalar2=None, op0=ALU.mult),
                    [kX, f"d2{sfx}"], [f"h{sfx}"])
                p.add("dve", lambda e, bU=bU, Cf=Cf, tmpC=tmpC: e.tensor_tensor(out=tmpC[:], in0=bU[0:64, 0:65], in1=Cf[:], op=ALU.add),
                      [kU, f"Cf{s}"], [f"tmpC{s}"])
                p.add("dve", lambda e, T=T, cc=cc, Cf=Cf, tmpC=tmpC: e.tensor_scalar(
                    out=Cf[:], in0=tmpC[:], scalar1=T["eG"][0:64, cc:cc + 1], scalar2=None, op0=ALU.mult),
                    [f"tmpC{s}", f"eG{sfx}"], [f"Cf{s}"])
                p.add("act", lambda e, T=T, cc=cc, Cb=Cb, tmpC=tmpC: e.activation(
                    out=Cb[:], in_=tmpC[:], func=AF.Copy, scale=T["eG"][0:64, cc:cc + 1]),
                    [f"tmpC{s}", f"eG{sfx}"], [f"Cb{s}"])
            p.dma(mh[s].rearrange("(c p) f -> p c f", p=128)[:, c0:c0 + gc, :], T["h"][:, 0:gc, :], reads=[f"h{sfx}"])
    p.emit()
    return nc


def build_M2(NKT, NCT, NSL=2):
    nc = bass.Bass("TRN2", target_bir_lowering=False)
    L = NKT * 128
    aqT = _din(nc, "aqT", [NSL, 2, 64, L])
    akT = _din(nc, "akT", [NSL, 2, 64, L])
    av = _din(nc, "av", [NSL, L, 128])
    dl_d = _din(nc, "dl", [128, 256])
    dng_d = _din(nc, "dng", [128, 128])
    lami_d = _din(nc, "lami", [128, 2])
    ones_d = _din(nc, "ones", [128, 128])
    ao = _dout(nc, "ao", [NSL, L, 128])
    p = Prog(nc)
    ones = p.sb([128, 128], name="ones_sb"); p.dma(ones[:], ones_d, writes=["ones"])
    dl = p.sb([128, 256], name="dl_sb"); p.dma(dl[:], dl_d, writes=["dl"])
    gsc = p.sb([128, 128], name="gsc"); p.dma(gsc[:], dng_d, writes=["gsc"])
    lami = p.sb([128, 2], name="lami_sb"); p.dma(lami[:], lami_d, writes=["lami"])
    junk = p.sb([128, 128], name="junk")
    lm = p.sb([128, 4], name="lm")
    p.add("dve", lambda e: e.scalar_tensor_tensor(out=junk[:, 0:64], in0=dl[:, 0:64], scalar=1.0, in1=dl[:, 64:128],
                                                  op0=ALU.mult, op1=ALU.mult, accum_out=lm[:, 0:1]), ["dl"], ["junk", "lm0"])
    p.add("dve", lambda e: e.scalar_tensor_tensor(out=junk[:, 64:128], in0=dl[:, 128:192], scalar=1.0, in1=dl[:, 192:256],
                                                  op0=ALU.mult, op1=ALU.mult, accum_out=lm[:, 1:2]), ["dl"], ["junk", "lm1"])
    p.add("act", lambda e: e.activation(out=lm[:, 0:2], in_=lm[:, 0:2], func=AF.Exp), ["lm0", "lm1"], ["lm01"])
    p.add("dve", lambda e: e.tensor_tensor(out=lm[:, 2:3], in0=lm[:, 1:2], in1=lm[:, 0:1], op=ALU.subtract), ["lm01"], ["lm2"])
    p.add("dve", lambda e: e.tensor_scalar(out=lm[:, 3:4], in0=lm[:, 2:3], scalar1=lami[:, 0:1], scalar2=None, op0=ALU.subtract),
          ["lm2", "lami"], ["nlam"])
    p.add("dve", lambda e: e.tensor_scalar(out=gsc[:], in0=gsc[:], scalar1=lami[:, 1:2], scalar2=None, op0=ALU.mult),
          ["gsc", "lami"], ["gsc"])
    banks = [p.ps([128, 512], name=f"bank{i}") for i in range(8)]
    qTb = p.sb([64, 2, L], BF16, name="qTb")
    kTb = p.sb([64, 2, L], BF16, name="kTb")
    vaug = p.sb([128, NKT, 129], BF16, name="vaug")
    p.add("dve", lambda e: e.memset(vaug[:, :, 128:129], 1.0), [], ["vaug1"])
    PW = 2048
    stg = [p.sb([64, PW], name=f"stg{i}") for i in range(2)]
    sq = p.sb([64, PW], name="sq")
    VG = 16
    vst = [p.sb([128, VG, 128], name=f"vst{i}") for i in range(2)]
    mx = p.sb([128, 8], name="mx")
    nb = p.sb([128, 4], name="nb")
    pT = [p.sb([128, 512], BF16, name=f"pT{i}") for i in range(3)]
    ev = [p.sb([128, 8], name=f"ev{i}") for i in range(2)]
    o1 = [p.sb([128, 128], name=f"o1_{i}") for i in range(2)]
    oo = [p.sb([128, 128], name=f"oo_{i}") for i in range(2)]
    yy = [p.sb([128, 128], name=f"yy_{i}") for i in range(2)]
    nstg = 0
    nvst = 0
    nit = 0
    nblk = 0
    nev = 0
    for s in range(NSL):
        p.add("dve", lambda e: e.memset(mx[:], 0.0), [], ["mx", "mx4"])
        for which, (src, dst) in enumerate(((aqT, qTb), (akT, kTb))):
            for comp in range(2):
                for c0 in range(0, L, PW):
                    cw = min(PW, L - c0)
                    si = nstg % 2; nstg += 1
                    st = stg[si]
                    p.dma(st[:, 0:cw], src[s, comp, :, c0:c0 + cw], writes=[f"stg{si}"])
                    p.add("dve", lambda e, st=st, dst=dst, comp=comp, c0=c0, cw=cw: e.tensor_copy(out=dst[:, comp, c0:c0 + cw], in_=st[:, 0:cw]),
                          [f"stg{si}"], ["qTb" if which == 0 else "kTb"])
                    p.add("act", lambda e, st=st, cw=cw: e.activation(out=sq[:, 0:cw], in_=st[:, 0:cw], func=AF.Square), [f"stg{si}"], ["sq"])
                    for b0 in range(0, cw, 512):
                        bw = min(512, cw - b0)
                        p.add("pe", lambda e, b0=b0, bw=bw: e.matmul(banks[6][:, 0:bw], lhsT=ones[0:64, :], rhs=sq[:, b0:b0 + bw], start=True, stop=True),
                              ["ones", "sq"], ["bank6"])
                        p.add("dve", lambda e, bw=bw: e.reduce_max(out=mx[:, 4:5], in_=banks[6][:, 0:bw], axis=AX.X), ["bank6"], ["mx4"])
                        col = which * 2 + comp
                        p.add("dve", lambda e, col=col: e.tensor_tensor(out=mx[:, col:col + 1], in0=mx[:, col:col + 1], in1=mx[:, 4:5], op=ALU.max),
                              ["mx4", "mx"], ["mx"])
        for g0 in range(0, NKT, VG):
            gn = min(VG, NKT - g0)
            vi = nvst % 2; nvst += 1
            p.dma(vst[vi][:, 0:gn, :], av[s].rearrange("(c p) f -> p c f", p=128)[:, g0:g0 + gn, :], writes=[f"vst{vi}"])
            p.add("act", lambda e, vi=vi, g0=g0, gn=gn: e.activation(out=vaug[:, g0:g0 + gn, 0:128], in_=vst[vi][:, 0:gn, :], func=AF.Copy),
                  [f"vst{vi}"], ["vaug"])
        p.add("dve", lambda e: e.tensor_tensor(out=nb[:, 2:4], in0=mx[:, 0:2], in1=mx[:, 2:4], op=ALU.mult), ["mx"], ["nb2"])
        p.add("act", lambda e: e.activation(out=nb[:, 2:4], in_=nb[:, 2:4], func=AF.Sqrt, scale=1.0 / 64.0), ["nb2"], ["nb2"])
        p.add("dve", lambda e: e.tensor_scalar(out=nb[:, 0:2], in0=nb[:, 2:4], scalar1=60.0, scalar2=-1.0, op0=ALU.min, op1=ALU.mult),
              ["nb2"], ["nb"])
        blocks = [(0, NCT, 0, NCT)]
        for q0 in range(NCT, NKT, 4):
            blocks.append((q0, min(4, NKT - q0), 0, NKT))
        for (q0, nq, k0, nk) in blocks:
            oset = nblk % 2; nblk += 1
            obanks = [banks[oset * 3 + i] for i in range(3)]
            okeys = [f"bank{oset*3+i}" for i in range(3)]
            for i in range(3):
                p.add("dve", lambda e, i=i, obanks=obanks: e.memset(obanks[i][:], 0.0), [], [okeys[i]])

            def oacc(comp, j):
                a = comp * 4 + j
                return obanks[a // 3][:, (a % 3) * 129:(a % 3) * 129 + 129], okeys[a // 3]
            QW = nq * 128
            for kt in range(k0, k0 + nk):
                for comp in range(2):
                    sb_i = 6 + nit % 2
                    pi = nit % 3
                    nit += 1
                    p.add("pe", lambda e, comp=comp, kt=kt, sb_i=sb_i, q0=q0, QW=QW: e.matmul(
                        banks[sb_i][:, 0:QW], lhsT=kTb[:, comp, kt * 128:(kt + 1) * 128], rhs=qTb[:, comp, q0 * 128:q0 * 128 + QW],
                        start=True, stop=True), ["kTb", "qTb"], [f"bank{sb_i}"])
                    p.add("act", lambda e, comp=comp, sb_i=sb_i, pi=pi, QW=QW: e.activation(
                        out=pT[pi][:, 0:QW], in_=banks[sb_i][:, 0:QW], func=AF.Exp, scale=0.125, bias=nb[:, comp:comp + 1]),
                        [f"bank{sb_i}", "nb"], [f"pT{pi}"])
                    for j in range(nq):
                        oap, okey = oacc(comp, j)
                        p.add("pe", lambda e, oap=oap, pi=pi, j=j, kt=kt: e.matmul(
                            oap, lhsT=pT[pi][:, j * 128:(j + 1) * 128], rhs=vaug[:, kt, :], start=False, stop=False,
                            skip_group_check=True), [f"pT{pi}", "vaug", "vaug1"], [okey])
            for j in range(nq):
                ei = nev % 2; nev += 1
                E = ev[ei]
                (o1ap, k1), (o2ap, k2) = oacc(0, j), oacc(1, j)
                sfx = f"_{ei}"
                p.add("dve", lambda e, E=E, o1ap=o1ap: e.reciprocal(out=E[:, 0:1], in_=o1ap[:, 128:129]), [k1], ["ev0" + sfx])
                p.add("dve", lambda e, E=E, o2ap=o2ap: e.reciprocal(out=E[:, 1:2], in_=o2ap[:, 128:129]), [k2], ["ev1" + sfx])
                p.add("dve", lambda e, E=E: e.tensor_tensor(out=E[:, 2:3], in0=E[:, 1:2], in1=lm[:, 3:4], op=ALU.mult),
                      ["ev1" + sfx, "nlam"], ["ev2" + sfx])
                p.add("dve", lambda e, E=E, o1ap=o1ap, ei=ei: e.tensor_scalar(out=o1[ei][:], in0=o1ap[:, 0:128], scalar1=E[:, 0:1], scalar2=None,
                                                                           op0=ALU.mult), [k1, "ev0" + sfx], ["o1" + sfx])
                p.add("dve", lambda e, E=E, o2ap=o2ap, ei=ei: e.scalar_tensor_tensor(
                    out=oo[ei][:], in0=o2ap[:, 0:128], scalar=E[:, 2:3], in1=o1[ei][:], op0=ALU.mult, op1=ALU.add),
                    [k2, "ev2" + sfx, "o1" + sfx], ["oo" + sfx])
                p.add("dve", lambda e, E=E, ei=ei: e.scalar_tensor_tensor(
                    out=junk[:], in0=oo[ei][:], scalar=1.0, in1=oo[ei][:], op0=ALU.mult, op1=ALU.mult, accum_out=E[:, 3:4]),
                    ["oo" + sfx], ["junk", "ev3" + sfx])
                p.add("dve", lambda e, E=E: e.tensor_scalar(out=E[:, 4:5], in0=E[:, 3:4], scalar1=1.0 / 128.0, scalar2=EPS,
                                                            op0=ALU.mult, op1=ALU.add), ["ev3" + sfx], ["ev4" + sfx])
                p.add("act", lambda e, E=E: e.activation(out=E[:, 5:6], in_=E[:, 4:5], func=AF.Sqrt), ["ev4" + sfx], ["ev5" + sfx])
                p.add("dve", lambda e, E=E: e.reciprocal(out=E[:, 6:7], in_=E[:, 5:6]), ["ev5" + sfx], ["ev6" + sfx])
                p.add("dve", lambda e, E=E, ei=ei: e.scalar_tensor_tensor(
                    out=yy[ei][:], in0=oo[ei][:], scalar=E[:, 6:7], in1=gsc[:], op0=ALU.mult, op1=ALU.mult),
                    ["oo" + sfx, "ev6" + sfx, "gsc"], ["yy" + sfx])
                r0 = (q0 + j) * 128
                p.dma(ao[s, r0:r0 + 128, :], yy[ei][:], reads=["yy" + sfx])
    p.emit()
    return nc


def build_C1(segs):
    nc = bass.Bass("TRN2", target_bir_lowering=False)
    W = sum(n + 30 for n in segs)
    NT = sum(segs)
    aT = _din(nc, "aT", [512, W])
    cw_d = _din(nc, "cw", [128, 2, 31])
    cp_d = _din(nc, "cp", [128, 2, 3])
    ones_d = _din(nc, "ones", [128, 128])
    co = _dout(nc, "convT", [2, 128, NT])
    p = Prog(nc)
    ones = p.sb([128, 128], name="ones_sb"); p.dma(ones[:], ones_d, writes=["ones"])
    cw = p.sb([128, 2, 31], name="cw_sb"); p.dma(cw[:], cw_d, writes=["cw"])
    cp = p.sb([128, 2, 3], name="cp_sb"); p.dma(cp[:], cp_d, writes=["cp"])
    N = 512
    a1 = [p.sb([128, N + 30], name=f"a1_{i}") for i in range(2)]
    a2 = [p.sb([128, N + 30], name=f"a2_{i}") for i in range(2)]
    u = [p.sb([128, N + 30], name=f"u_{i}") for i in range(2)]
    acc = [[p.sb([128, N], name=f"acc_{i}_{r}") for r in range(2)] for i in range(2)]
    y = [p.sb([128, N], name=f"y_{i}") for i in range(2)]
    ysq = [p.sb([128, N], name=f"ysq_{i}") for i in range(2)]
    mean = p.sb([128, N], name="mean"); msq = p.sb([128, N], name="msq"); rstd = p.sb([128, N], name="rstd")
    zz = [p.sb([128, N], name=f"zz_{i}") for i in range(2)]
    bA = p.ps([128, 512], name="bankA"); bB = p.ps([128, 512], name="bankB")
    off = 0
    tok0 = 0
    for ns in segs:
        for t0 in range(0, ns, N):
            n = min(N, ns - t0)
            for j in range(2):
                c0 = off + t0
                p.dma(a1[j][:, 0:n + 30], aT[j * 128:(j + 1) * 128, c0:c0 + n + 30], writes=[f"a1_{j}"])
                p.dma(a2[j][:, 0:n + 30], aT[256 + j * 128:256 + (j + 1) * 128, c0:c0 + n + 30], writes=[f"a2_{j}"])
                p.add("act", lambda e, j=j, n=n: e.activation(out=a2[j][:, 0:n + 30], in_=a2[j][:, 0:n + 30], func=AF.Sigmoid),
                      [f"a2_{j}"], [f"a2_{j}"])
                p.add("dve", lambda e, j=j, n=n: e.tensor_tensor(out=u[j][:, 0:n + 30], in0=a1[j][:, 0:n + 30], in1=a2[j][:, 0:n + 30], op=ALU.mult),
                      [f"a1_{j}", f"a2_{j}"], [f"u_{j}"])
                p.add("dve", lambda e, j=j, n=n: e.tensor_scalar(out=acc[j][0][:, 0:n], in0=u[j][:, 0:n], scalar1=cw[:, j, 0:1], scalar2=None,
                                                                op0=ALU.mult), [f"u_{j}", "cw"], [f"acc_{j}_0"])
                for k in range(1, 31):
                    src, dst = acc[j][(k - 1) % 2], acc[j][k % 2]
                    p.add("dve", lambda e, j=j, n=n, k=k, src=src, dst=dst: e.scalar_tensor_tensor(
                        out=dst[:, 0:n], in0=u[j][:, k:k + n], scalar=cw[:, j, k:k + 1], in1=src[:, 0:n], op0=ALU.mult, op1=ALU.add),
                        [f"u_{j}", "cw", f"acc_{j}_{(k-1)%2}"], [f"acc_{j}_{k%2}"])
                p.add("dve", lambda e, j=j, n=n: e.tensor_scalar(out=y[j][:, 0:n], in0=acc[j][0][:, 0:n], scalar1=cp[:, j, 0:1], scalar2=None,
                                                                op0=ALU.add), [f"acc_{j}_0", "cp"], [f"y_{j}"])
                p.add("act", lambda e, j=j, n=n: e.activation(out=ysq[j][:, 0:n], in_=y[j][:, 0:n], func=AF.Square), [f"y_{j}"], [f"ysq_{j}"])
            for j in range(2):
                p.add("pe", lambda e, j=j, n=n: e.matmul(bA[:, 0:n], lhsT=ones[:], rhs=y[j][:, 0:n], start=(j == 0), stop=(j == 1)),
                      ["ones", f"y_{j}"], ["bankA"])
            for j in range(2):
                p.add("pe", lambda e, j=j, n=n: e.matmul(bB[:, 0:n], lhsT=ones[:], rhs=ysq[j][:, 0:n], start=(j == 0), stop=(j == 1)),
                      ["ones", f"ysq_{j}"], ["bankB"])
            p.add("act", lambda e, n=n: e.activation(out=mean[:, 0:n], in_=bA[:, 0:n], func=AF.Copy, scale=1.0 / 256), ["bankA"], ["mean"])
            p.add("act", lambda e, n=n: e.activation(out=msq[:, 0:n], in_=bA[:, 0:n], func=AF.Square, scale=1.0 / 256), ["bankA"], ["msq"])
            p.add("dve", lambda e, n=n: e.scalar_tensor_tensor(out=rstd[:, 0:n], in0=bB[:, 0:n], scalar=1.0 / 256, in1=msq[:, 0:n],
                                                               op0=ALU.mult, op1=ALU.subtract), ["bankB", "msq"], ["rstd"])
            p.add("dve", lambda e, n=n: e.tensor_scalar(out=rstd[:, 0:n], in0=rstd[:, 0:n], scalar1=EPS, scalar2=None, op0=ALU.add),
                  ["rstd"], ["rstd"])
            p.add("act", lambda e, n=n: e.activation(out=rstd[:, 0:n], in_=rstd[:, 0:n], func=AF.Sqrt), ["rstd"], ["rstd"])
            p.add("dve", lambda e, n=n: e.reciprocal(out=rstd[:, 0:n], in_=rstd[:, 0:n]), ["rstd"], ["rstd"])
            for j in range(2):
                p.add("dve", lambda e, j=j, n=n: e.tensor_tensor(out=zz[j][:, 0:n], in0=y[j][:, 0:n], in1=mean[:, 0:n], op=ALU.subtract),
                      [f"y_{j}", "mean"], [f"zz_{j}"])
                p.add("dve", lambda e, j=j, n=n: e.tensor_tensor(out=zz[j][:, 0:n], in0=zz[j][:, 0:n], in1=rstd[:, 0:n], op=ALU.mult),
                      [f"zz_{j}", "rstd"], [f"zz_{j}"])
                p.add("act", lambda e, j=j, n=n: e.activation(out=zz[j][:, 0:n], in_=zz[j][:, 0:n], func=AF.Silu,
                                                              scale=cp[:, j, 1:2], bias=cp[:, j, 2:3]), [f"zz_{j}", "cp"], [f"zz_{j}"])
                p.dma(co[j, :, tok0 + t0:tok0 + t0 + n], zz[j][:, 0:n], reads=[f"zz_{j}"])
        off += ns + 30
        tok0 += ns
    p.emit()
    return nc


def build_C2(NT, ctx_tiles=(0,)):
    nc = bass.Bass("TRN2", target_bir_lowering=False)
    L = NT * 128
    xs = _din(nc, "xs", [L, D])
    convT = _din(nc, "convT", [2, 128, L])
    hf_d = _din(nc, "hf", [L, 256])
    hb_d = _din(nc, "hb", [L, 256])
    mo_d = _din(nc, "mo", [L, 256])
    ao_d = _din(nc, "ao", [L, 512])
    mng_d = _din(nc, "mng", [128, 64])
    w_out = _din(nc, "w_out", [D, D])
    bc_d = _din(nc, "bc", [7, 128, D])
    ident = _din(nc, "ident", [128, 128])
    x1_o = _dout(nc, "x1", [L, D])
    h2_o = _dout(nc, "h2", [L, D])
    p = Prog(nc)
    wob = p.sb([128, 8, D], BF16, name="wob")
    load_cast_weight(p, wob, w_out, D, "wob")
    idt = p.sb([128, 128], name="idt"); p.dma(idt[:], ident, writes=["idt"])
    mng = p.sb([128, 64], name="mng_sb"); p.dma(mng[:], mng_d, writes=["mng"])
    bc = [p.sb([128, D], name=f"bc{i}") for i in range(7)]
    for i in range(7):
        p.dma(bc[i][:], bc_d[i], writes=[f"bc{i}"])
    for i in (3, 4):
        p.add("dve", lambda e, i=i: e.scalar_tensor_tensor(out=bc[i][:], in0=bc[i][:], scalar=1.0, in1=bc[0][:], op0=ALU.add, op1=ALU.mult),
              [f"bc{i}", "bc0"], [f"bc{i}"])
    NB = 2
    def mk(shape, nm, dt=F32):
        return [p.sb(shape, dt, name=f"{nm}{i}") for i in range(NB)]
    xt = mk([128, D], "xt"); hf = mk([128, 256], "hf"); hb = mk([128, 256], "hb"); mo = mk([128, 256], "mo")
    aot = mk([128, 512], "aot"); cvt = mk([128, 2, 128], "cvt"); ym = mk([128, 256], "ym"); sqm = mk([128, 256], "sqm")
    st4 = mk([128, 8], "st4"); mixT = mk([128, 8, 128], "mixT", BF16); tmp = mk([128, D], "tmp"); x1 = mk([128, D], "x1")
    h2 = mk([128, D], "h2"); ss = mk([128, 4], "ss")
    junk = p.sb([128, D], name="junk")
    bT = [p.ps([128, 512], name=f"bT{i}") for i in range(4)]
    bAcc = [p.ps([128, 512], name=f"bAcc{i}") for i in range(4)]
    for t in range(NT):
        i = t % NB
        isc = t in ctx_tiles
        r0 = t * 128
        g1b = bc[2] if isc else bc[1]
        gm2 = bc[4] if isc else bc[3]
        sh2 = bc[6] if isc else bc[5]
        kg1, kgm2, ksh2 = (f"bc{2 if isc else 1}", f"bc{4 if isc else 3}", f"bc{6 if isc else 5}")
        p.dma(xt[i][:], xs[r0:r0 + 128, :], writes=[f"xt{i}"])
        p.dma(hf[i][:], hf_d[r0:r0 + 128, :], writes=[f"hf{i}"])
        p.dma(hb[i][:], hb_d[r0:r0 + 128, :], writes=[f"hb{i}"])
        p.dma(mo[i][:], mo_d[r0:r0 + 128, :], writes=[f"mo{i}"])
        p.dma(aot[i][:], ao_d[r0:r0 + 128, :], writes=[f"aot{i}"])
        p.dma(cvt[i][:], convT[:, :, r0:r0 + 128].rearrange("j p n -> p j n"), writes=[f"cvt{i}"])
        p.add("dve", lambda e, i=i: e.tensor_tensor(out=ym[i][:], in0=hf[i][:], in1=hb[i][:], op=ALU.add), [f"hf{i}", f"hb{i}"], [f"ym{i}"])
        p.add("act", lambda e, i=i: e.activation(out=sqm[i][:], in_=ym[i][:], func=AF.Square), [f"ym{i}"], [f"sqm{i}"])
        p.add("dve", lambda e, i=i: e.tensor_reduce(out=st4[i][:, 0:4], in_=sqm[i][:].rearrange("p (h d) -> p h d", h=4), axis=AX.X, op=ALU.add),
              [f"sqm{i}"], [f"st4a{i}"])
        p.add("dve", lambda e, i=i: e.tensor_scalar(out=st4[i][:, 4:8], in0=st4[i][:, 0:4], scalar1=1.0 / 64, scalar2=EPS, op0=ALU.mult, op1=ALU.add),
              [f"st4a{i}"], [f"st4b{i}"])
        p.add("act", lambda e, i=i: e.activation(out=st4[i][:, 0:4], in_=st4[i][:, 4:8], func=AF.Sqrt), [f"st4b{i}"], [f"st4a{i}"])
        p.add("dve", lambda e, i=i: e.reciprocal(out=st4[i][:, 4:8], in_=st4[i][:, 0:4]), [f"st4a{i}"], [f"st4b{i}"])
        p.add("act", lambda e, i=i: e.activation(out=mo[i][:], in_=mo[i][:], func=AF.Sigmoid), [f"mo{i}"], [f"mo{i}"])
        ym3 = ym[i][:].rearrange("p (h d) -> p h d", h=4)
        p.add("dve", lambda e, i=i, ym3=ym3: e.tensor_tensor(out=ym3, in0=ym3, in1=st4[i][:, 4:8, None].to_broadcast([128, 4, 64]), op=ALU.mult),
              [f"ym{i}", f"st4b{i}"], [f"ym{i}"])
        p.add("dve", lambda e, i=i, ym3=ym3: e.tensor_tensor(out=ym3, in0=ym3, in1=mng[:, None, :].to_broadcast([128, 4, 64]), op=ALU.mult),
              [f"ym{i}", "mng"], [f"ym{i}"])
        p.add("dve", lambda e, i=i: e.tensor_tensor(out=ym[i][:], in0=ym[i][:], in1=mo[i][:], op=ALU.mult), [f"ym{i}", f"mo{i}"], [f"ym{i}"])
        p.add("act", lambda e, i=i: e.activation(out=mixT[i][:, 0:2, :], in_=cvt[i][:], func=AF.Copy), [f"cvt{i}"], [f"mixT{i}_c"])
        ta, tb = (2 * t) % 4, (2 * t + 1) % 4
        srcs = [(ym[i], 0, f"ym{i}"), (ym[i], 128, f"ym{i}"), (aot[i], 0, f"aot{i}"), (aot[i], 128, f"aot{i}"),
                (aot[i], 256, f"aot{i}"), (aot[i], 384, f"aot{i}")]
        for n_, (src, c0, key) in enumerate(srcs):
            bank = ta if n_ < 4 else tb
            col = (n_ % 4) * 128
            p.add("pe", lambda e, src=src, c0=c0, bank=bank, col=col: e.transpose(bT[bank][:, col:col + 128], src[:, c0:c0 + 128], idt[:]),
                  [key, "idt"], [f"bT{bank}"])
        p.add("act", lambda e, i=i, ta=ta: e.activation(out=mixT[i][:, 2:6, :], in_=bT[ta][:].rearrange("p (k n) -> p k n", k=4), func=AF.Copy),
              [f"bT{ta}"], [f"mixT{i}_a"])
        p.add("dve", lambda e, i=i, tb=tb: e.tensor_copy(out=mixT[i][:, 6:8, :], in_=bT[tb][:, 0:256].rearrange("p (k n) -> p k n", k=2)),
              [f"bT{tb}"], [f"mixT{i}_b"])
        for blk in range(2):
            a = (2 * t + blk) % 4
            for k in range(8):
                p.add("pe", lambda e, i=i, k=k, a=a, blk=blk: e.matmul(bAcc[a][:], lhsT=mixT[i][:, k, :], rhs=wob[:, k, blk * 512:(blk + 1) * 512],
                                                                      start=(k == 0), stop=(k == 7)),
                      [f"mixT{i}_c", f"mixT{i}_a", f"mixT{i}_b", "wob"], [f"bAcc{a}"])
            cs = slice(blk * 512, (blk + 1) * 512)
            p.add("dve", lambda e, i=i, a=a, cs=cs, g1b=g1b: e.tensor_tensor(out=tmp[i][:, cs], in0=bAcc[a][:], in1=g1b[:, cs], op=ALU.mult),
                  [f"bAcc{a}", kg1], [f"tmp{i}_{blk}"])
            p.add("dve", lambda e, i=i, cs=cs: e.tensor_tensor(out=x1[i][:, cs], in0=tmp[i][:, cs], in1=xt[i][:, cs], op=ALU.add),
                  [f"tmp{i}_{blk}", f"xt{i}"], [f"x1{i}_{blk}"])
        p.dma(x1_o[r0:r0 + 128, :], x1[i][:], reads=[f"x1{i}_0", f"x1{i}_1"])
        p.add("act", lambda e, i=i: e.activation(out=junk[:], in_=x1[i][:], func=AF.Square, accum_out=ss[i][:, 0:1]),
              [f"x1{i}_0", f"x1{i}_1"], ["junk", f"ss0{i}"])
        p.add("dve", lambda e, i=i: e.tensor_scalar(out=ss[i][:, 1:2], in0=ss[i][:, 0:1], scalar1=1.0 / D, scalar2=EPS, op0=ALU.mult, op1=ALU.add),
              [f"ss0{i}"], [f"ss1{i}"])
        p.add("act", lambda e, i=i: e.activation(out=ss[i][:, 2:3], in_=ss[i][:, 1:2], func=AF.Sqrt), [f"ss1{i}"], [f"ss2{i}"])
        p.add("dve", lambda e, i=i: e.reciprocal(out=ss[i][:, 3:4], in_=ss[i][:, 2:3]), [f"ss2{i}"], [f"ss3{i}"])
        p.add("dve", lambda e, i=i, gm2=gm2: e.scalar_tensor_tensor(out=h2[i][:], in0=x1[i][:], scalar=ss[i][:, 3:4], in1=gm2[:],
                                                                     op0=ALU.mult, op1=ALU.mult),
              [f"x1{i}_0", f"x1{i}_1", f"ss3{i}", kgm2], [f"h2{i}"])
        p.add("dve", lambda e, i=i, sh2=sh2: e.tensor_tensor(out=h2[i][:], in0=h2[i][:], in1=sh2[:], op=ALU.add), [f"h2{i}", ksh2], [f"h2{i}"])
        p.dma(h2_o[r0:r0 + 128, :], h2[i][:], reads=[f"h2{i}"])
    p.emit()
    return nc


def build_C3(NT, ctx_tiles=(0,), NGB=4):
    nc = bass.Bass("TRN2", target_bir_lowering=False)
    L = NT * 128
    NE = 16384
    h2_d = _din(nc, "h2", [L, D])
    x1_d = _din(nc, "x1", [L, D])
    wq = _din(nc, "wq", [D, D])
    kbd_d = _din(nc, "kbd", [128, 8, 256])
    pu = _din(nc, "peer_u", [NE, D])
    pv = _din(nc, "peer_v", [NE, D])
    g2_d = _din(nc, "g2b", [2, 128, D])
    ident = _din(nc, "ident", [128, 128])
    x2_o = _dout(nc, "x2", [L, D])
    p = Prog(nc)
    wqb = p.sb([128, 8, D], F32, name="wqb")
    p.dma(wqb[:], wq.rearrange("(k p) n -> p k n", p=128), writes=["wqb"])
    idt = p.sb([128, 128], name="idt"); p.dma(idt[:], ident, writes=["idt"])
    kbd = p.sb([128, 8, 256], name="kbd_sb"); p.dma(kbd[:], kbd_d, writes=["kbd"])
    g2b = [p.sb([128, D], name=f"g2b{i}") for i in range(2)]
    for i in range(2):
        p.dma(g2b[i][:], g2_d[i], writes=[f"g2b{i}"])
    h2t = p.sb([128, D], name="h2t"); x1t = p.sb([128, D], name="x1t")
    h2T = p.sb([128, 8, 128], F32, name="h2T")
    qTf = p.sb([128, 8, 128], name="qTf")
    sc = p.sb([128, 16, 128], name="sc"); wk = p.sb([128, 16, 128], name="wk")
    st = p.sb([128, 16, 16], name="st"); it = p.sb([128, 16, 16], U32, name="it"); itf = p.sb([128, 16, 16], name="itf")
    cand = p.sb([128, 8, 256], name="cand"); cidx = p.sb([128, 8, 256], name="cidx"); wk2 = p.sb([128, 8, 256], name="wk2")
    best = p.sb([128, 8, 16], name="best"); gw = p.sb([128, 8, 16], name="gw")
    eidf = p.sb([128, 128], name="eidf"); eidi = p.sb([128, 128], I32, name="eidi")
    sm = p.sb([128, 16], name="sm")
    act = p.sb([128, 128], name="act_sb"); coef = p.sb([128, 128], name="coef")
    junk = p.sb([128, D], name="junk")
    acc = p.sb([128, D], name="acc"); x2t = p.sb([128, D], name="x2t")
    rows = [p.sb([128, D], name=f"rows{i}") for i in range(NGB)]
    banks = [p.ps([128, 512], name=f"bank{i}") for i in range(8)]
    ngat = 0
    for t in range(NT):
        isc = t in ctx_tiles
        r0 = t * 128
        gb = g2b[1] if isc else g2b[0]
        kgb = "g2b1" if isc else "g2b0"
        p.dma(h2t[:], h2_d[r0:r0 + 128, :], writes=["h2t"])
        p.dma(x1t[:], x1_d[r0:r0 + 128, :], writes=["x1t"])
        for half in range(2):
            for k in range(half * 4, half * 4 + 4):
                p.add("pe", lambda e, k=k, half=half: e.transpose(banks[half][:, (k % 4) * 128:(k % 4 + 1) * 128], h2t[:, k * 128:(k + 1) * 128], idt[:]),
                      ["h2t", "idt"], [f"bank{half}"])
        p.add("act", lambda e: e.activation(out=h2T[:, 0:4, :], in_=banks[0][:].rearrange("p (k n) -> p k n", k=4), func=AF.Copy), ["bank0"], ["h2T_a"])
        p.add("dve", lambda e: e.tensor_copy(out=h2T[:, 4:8, :], in_=banks[1][:].rearrange("p (k n) -> p k n", k=4)), ["bank1"], ["h2T_b"])
        for c in range(8):
            bk = 2 + c // 4
            for k in range(8):
                p.add("pe", lambda e, c=c, k=k, bk=bk: e.matmul(banks[bk][:, (c % 4) * 128:(c % 4 + 1) * 128], lhsT=wqb[:, k, c * 128:(c + 1) * 128],
                                                              rhs=h2T[:, k, :], start=(k == 0), stop=(k == 7)),
                      ["wqb", "h2T_a", "h2T_b"], [f"bank{bk}"])
        p.add("act", lambda e: e.activation(out=qTf[:, 0:4, :], in_=banks[2][:].rearrange("p (k n) -> p k n", k=4), func=AF.Copy), ["bank2"], ["qTf_a"])
        p.add("dve", lambda e: e.tensor_copy(out=qTf[:, 4:8, :], in_=banks[3][:].rearrange("p (k n) -> p k n", k=4)), ["bank3"], ["qTf_b"])
        for h in range(8):
            bk = 4 + h // 2
            p.add("pe", lambda e, h=h, bk=bk: e.matmul(banks[bk][:, (h % 2) * 256:(h % 2 + 1) * 256], lhsT=qTf[:, h, :], rhs=kbd[:, h, :],
                                                      start=True, stop=True), ["qTf_a", "qTf_b", "kbd"], [f"bank{bk}"])
        for bk in range(4, 8):
            g0 = (bk - 4) * 4
            if bk % 2 == 0:
                p.add("act", lambda e, bk=bk, g0=g0: e.activation(out=sc[:, g0:g0 + 4, :], in_=banks[bk][:].rearrange("p (g n) -> p g n", g=4), func=AF.Copy),
                      [f"bank{bk}"], [f"sc{bk}"])
            else:
                p.add("dve", lambda e, bk=bk, g0=g0: e.tensor_copy(out=sc[:, g0:g0 + 4, :], in_=banks[bk][:].rearrange("p (g n) -> p g n", g=4)),
                      [f"bank{bk}"], [f"sc{bk}"])
        for g in range(16):
            ks = f"sc{4 + g // 4}"
            p.add("dve", lambda e, g=g: e.max(out=st[:, g, 0:8], in_=sc[:, g, :]), [ks], [f"st{g}a"])
            p.add("dve", lambda e, g=g: e.max_index(out=it[:, g, 0:8], in_max=st[:, g, 0:8], in_values=sc[:, g, :]), [ks, f"st{g}a"], [f"it{g}a"])
            p.add("dve", lambda e, g=g: e.match_replace(out=wk[:, g, :], in_to_replace=st[:, g, 0:8], in_values=sc[:, g, :], imm_value=-1e30),
                  [ks, f"st{g}a"], [f"wk{g}"])
            p.add("dve", lambda e, g=g: e.max(out=st[:, g, 8:16], in_=wk[:, g, :]), [f"wk{g}"], [f"st{g}b"])
            p.add("dve", lambda e, g=g: e.max_index(out=it[:, g, 8:16], in_max=st[:, g, 8:16], in_values=wk[:, g, :]), [f"wk{g}", f"st{g}b"], [f"it{g}b"])
        allst = [f"st{g}{x}" for g in range(16) for x in "ab"]
        allit = [f"it{g}{x}" for g in range(16) for x in "ab"]
        p.add("dve", lambda e: e.tensor_copy(out=itf[:], in_=it[:]), allit, ["itf"])
        st4 = st[:].rearrange("p (h two) k -> p h two k", two=2)
        itf4 = itf[:].rearrange("p (h two) k -> p h two k", two=2)
        cand4 = cand[:].rearrange("p h (i j) -> p h i j", i=16)
        cidx4 = cidx[:].rearrange("p h (i j) -> p h i j", i=16)
        for h in range(8):
            p.add("dve", lambda e, h=h: e.tensor_tensor(out=cand4[:, h], in0=st4[:, h, 0, :, None].to_broadcast([128, 16, 16]),
                                                        in1=st4[:, h, 1, None, :].to_broadcast([128, 16, 16]), op=ALU.add), allst, [f"cand{h}"])
            p.add("dve", lambda e, h=h: e.scalar_tensor_tensor(out=cidx4[:, h], in0=itf4[:, h, 0, :, None].to_broadcast([128, 16, 16]), scalar=128.0,
                                                               in1=itf4[:, h, 1, None, :].to_broadcast([128, 16, 16]), op0=ALU.mult, op1=ALU.add),
                  ["itf"], [f"cidx{h}"])
            p.add("dve", lambda e, h=h: e.max(out=best[:, h, 0:8], in_=cand[:, h, :]), [f"cand{h}"], [f"best{h}a"])
            p.add("dve", lambda e, h=h: e.match_replace(out=wk2[:, h, :], in_to_replace=best[:, h, 0:8], in_values=cand[:, h, :], imm_value=-1e30),
                  [f"cand{h}", f"best{h}a"], [f"wk2{h}"])
            p.add("dve", lambda e, h=h: e.max(out=best[:, h, 8:16], in_=wk2[:, h, :]), [f"wk2{h}"], [f"best{h}b"])
            for k in range(16):
                hk = h * 16 + k
                p.add("dve", lambda e, h=h, k=k, hk=hk: e.scalar_tensor_tensor(
                    out=junk[:, 0:256], in0=cand[:, h, :], scalar=best[:, h, k:k + 1], in1=cidx[:, h, :], op0=ALU.is_equal, op1=ALU.mult,
                    accum_out=eidf[:, hk:hk + 1]), [f"cand{h}", f"cidx{h}", f"best{h}a", f"best{h}b"], ["junk", f"eidf{h}"])
        alle = [f"eidf{h}" for h in range(8)]
        allb = [f"best{h}{x}" for h in range(8) for x in "ab"]
        p.add("dve", lambda e: e.tensor_scalar(out=eidf[:], in0=eidf[:], scalar1=float(NE - 1), scalar2=0.0, op0=ALU.min, op1=ALU.max), alle, ["eidf"])
        p.add("dve", lambda e: e.tensor_copy(out=eidi[:], in_=eidf[:]), ["eidf"], ["eidi"])
        p.add("dve", lambda e: e.tensor_tensor(out=gw[:], in0=best[:], in1=best[:, :, 0:1].to_broadcast([128, 8, 16]), op=ALU.subtract), allb, ["gw"])
        p.add("act", lambda e: e.activation(out=gw[:], in_=gw[:], func=AF.Exp), ["gw"], ["gw"])
        p.add("dve", lambda e: e.tensor_reduce(out=sm[:, 0:8], in_=gw[:], axis=AX.X, op=ALU.add), ["gw"], ["sm0"])
        p.add("dve", lambda e: e.reciprocal(out=sm[:, 8:16], in_=sm[:, 0:8]), ["sm0"], ["sm1"])
        p.add("dve", lambda e: e.tensor_tensor(out=gw[:], in0=gw[:], in1=sm[:, 8:16, None].to_broadcast([128, 8, 16]), op=ALU.mult), ["gw", "sm1"], ["gw"])
        for hk in range(128):
            b = ngat % NGB; ngat += 1
            p.add("pool", lambda e, b=b, hk=hk: e.indirect_dma_start(
                out=rows[b][:], out_offset=None, in_=pu, in_offset=bass.IndirectOffsetOnAxis(ap=eidi[:, hk:hk + 1], axis=0)),
                ["eidi"], [f"rows{b}"])
            p.add("dve", lambda e, b=b, hk=hk: e.scalar_tensor_tensor(out=junk[:], in0=rows[b][:], scalar=1.0, in1=h2t[:], op0=ALU.mult, op1=ALU.mult,
                                                                     accum_out=act[:, hk:hk + 1]), [f"rows{b}", "h2t"], ["junk", "act"])
        p.add("act", lambda e: e.activation(out=coef[:], in_=act[:], func=AF.Gelu), ["act"], ["coef"])
        p.add("dve", lambda e: e.tensor_tensor(out=coef[:], in0=coef[:], in1=gw[:].rearrange("p h k -> p (h k)"), op=ALU.mult), ["coef", "gw"], ["coef"])
        p.add("dve", lambda e: e.memset(acc[:], 0.0), [], ["acc"])
        for hk in range(128):
            b = ngat % NGB; ngat += 1
            p.add("pool", lambda e, b=b, hk=hk: e.indirect_dma_start(
                out=rows[b][:], out_offset=None, in_=pv, in_offset=bass.IndirectOffsetOnAxis(ap=eidi[:, hk:hk + 1], axis=0)),
                ["eidi"], [f"rows{b}"])
            p.add("dve", lambda e, b=b, hk=hk: e.scalar_tensor_tensor(out=acc[:], in0=rows[b][:], scalar=coef[:, hk:hk + 1], in1=acc[:],
                                                                     op0=ALU.mult, op1=ALU.add), [f"rows{b}", "coef", "acc"], ["acc"])
        p.add("dve", lambda e, gb=gb: e.tensor_tensor(out=x2t[:], in0=acc[:], in1=gb[:], op=ALU.mult), ["acc", kgb], ["x2t"])
        p.add("dve", lambda e: e.tensor_tensor(out=x2t[:], in0=x2t[:], in1=x1t[:], op=ALU.add), ["x2t", "x1t"], ["x2t"])
        p.dma(x2_o[r0:r0 + 128, :], x2t[:], reads=["x2t"])
    p.emit()
    return nc


def build_F(NT):
    nc = bass.Bass("TRN2", target_bir_lowering=False)
    L = NT * 128
    xs = _din(nc, "xs", [L, D])
    fg_d = _din(nc, "fg", [128, D])
    yo = _dout(nc, "y", [L, D])
    p = Prog(nc)
    fg = p.sb([128, D], name="fg_sb"); p.dma(fg[:], fg_d, writes=["fg"])
    junk = p.sb([128, D], name="junk")
    NB = 2
    xt = [p.sb([128, D], name=f"xt{i}") for i in range(NB)]
    yt = [p.sb([128, D], name=f"yt{i}") for i in range(NB)]
    ss = [p.sb([128, 4], name=f"ss{i}") for i in range(NB)]
    for t in range(NT):
        i = t % NB
        r0 = t * 128
        p.dma(xt[i][:], xs[r0:r0 + 128, :], writes=[f"xt{i}"])
        p.add("act", lambda e, i=i: e.activation(out=junk[:], in_=xt[i][:], func=AF.Square, accum_out=ss[i][:, 0:1]), [f"xt{i}"], ["junk", f"ss0{i}"])
        p.add("dve", lambda e, i=i: e.tensor_scalar(out=ss[i][:, 1:2], in0=ss[i][:, 0:1], scalar1=1.0 / D, scalar2=EPS, op0=ALU.mult, op1=ALU.add),
              [f"ss0{i}"], [f"ss1{i}"])
        p.add("act", lambda e, i=i: e.activation(out=ss[i][:, 2:3], in_=ss[i][:, 1:2], func=AF.Sqrt), [f"ss1{i}"], [f"ss2{i}"])
        p.add("dve", lambda e, i=i: e.reciprocal(out=ss[i][:, 3:4], in_=ss[i][:, 2:3]), [f"ss2{i}"], [f"ss3{i}"])
        p.add("dve", lambda e, i=i: e.scalar_tensor_tensor(out=yt[i][:], in0=xt[i][:], scalar=ss[i][:, 3:4], in1=fg[:], op0=ALU.mult, op1=ALU.mult),
              [f"xt{i}", f"ss3{i}", "fg"], [f"yt{i}"])
        p.dma(yo[r0:r0 + 128, :], yt[i][:], reads=[f"yt{i}"])
    p.emit()
    return nc


_PROGS = {}


def _prog(name, fn):
    if name not in _PROGS:
        _PROGS[name] = fn()
    return _PROGS[name]


def _run(nc, maps):
    res = run_bass_kernel_spmd(nc, maps, core_ids=list(range(NCORES)))
    return res.results


def _rep(v, n=128):
    return np.ascontiguousarray(np.broadcast_to(np.asarray(v, np.float32)[None], (n, v.shape[0])))


def _rope_tables(n_tokens):
    rows = n_tokens // 64
    row = np.repeat(np.arange(rows), 64).astype(np.float32)
    col = np.tile(np.arange(64), rows).astype(np.float32)
    inv = (np.float32(10000.0) ** (-np.arange(0, 32, 2, dtype=np.float32) / np.float32(32))).astype(np.float32)
    ang = np.concatenate([row[:, None] * inv, col[:, None] * inv], axis=-1).astype(np.float32)
    return np.cos(ang).astype(np.float32), np.sin(ang).astype(np.float32)


def kernel_unfused(x, c, ctx, c_ctx, ada_w, ada_b, norm1_g, norm2_g, w_in, conv_w, conv_b, conv_ln_g, conv_ln_b,
           mlstm_gate_b, mlstm_norm_g, diff_lambda, diff_norm_g, w_out, peer_wq, peer_keys, peer_u, peer_v, final_g):
    f32 = np.float32
    x = np.asarray(x, f32); ctx = np.asarray(ctx, f32)
    B, S, _ = x.shape
    CT = ctx.shape[1]
    DEPTH = ada_w.shape[0]
    HS, HC = S // 2, CT // 2
    NT = (HS + HC) // 128
    NKT = (S + CT) // 128
    NCT = CT // 128
    ident = np.eye(128, dtype=f32)
    ones = np.ones((128, 128), f32)
    tri = np.triu(np.ones((128, 128), f32))
    cos, sin = _rope_tables(S)
    cores = [(cc // 2, cc % 2) for cc in range(NCORES)]

    cT = np.concatenate([np.asarray(c, f32), np.asarray(c_ctx, f32)[None]], 0).T
    cTl = np.ascontiguousarray(cT.reshape(8, 128, 5).transpose(1, 0, 2))
    maps = []
    for cc in range(NCORES):
        ll, half = cc // 2, cc % 2
        maps.append({"w": np.ascontiguousarray(ada_w[ll][:, half * 3072:(half + 1) * 3072]),
                     "b": np.ascontiguousarray(np.asarray(ada_b[ll], f32)[half * 3072:(half + 1) * 3072].reshape(24, 128).T),
                     "cT": cTl})
    res = _run(_prog("P0", build_P0), maps)
    mod = np.zeros((DEPTH, 6144, 5), f32)
    for cc in range(NCORES):
        ll, half = cc // 2, cc % 2
        mod[ll, half * 3072:(half + 1) * 3072] = res[cc]["modT"].transpose(1, 0, 2).reshape(3072, 5)

    xs = [np.concatenate([ctx[b, z * HC:(z + 1) * HC], x[b, z * HS:(z + 1) * HS]], 0) for (b, z) in cores]
    cs_core = []
    for (b, z) in cores:
        cs = np.zeros((HC + HS, 64), f32)
        cs[:HC, :32] = 1.0
        cs[HC:, :32] = cos[z * HS:(z + 1) * HS]
        cs[HC:, 32:] = sin[z * HS:(z + 1) * HS]
        cs_core.append(cs)

    def fv(v):
        return np.asarray(v, f32).reshape(8, 128).T

    for l in range(DEPTH):
        m6 = mod[l]
        sh1, sc1, g1, sh2, sc2, g2 = [m6[i * 1024:(i + 1) * 1024] for i in range(6)]
        lam_init = 0.8 - 0.6 * math.exp(-0.3 * l)
        maps = []
        for cc, (b, z) in enumerate(cores):
            pv = np.ascontiguousarray(np.stack([fv(norm1_g[l]), fv(sc1[:, b]), fv(sh1[:, b]), fv(sc1[:, 4]), fv(sh1[:, 4])], -1))
            maps.append({"xs": xs[cc], "w_in": np.asarray(w_in[l], f32), "pv": pv, "cs": cs_core[cc], "ident": ident})
        res = _run(_prog("A", lambda: build_A(NT, ctx_tiles=tuple(range(HC // 128)))), maps)
        proj = []
        for b in range(B):
            p0, p1 = res[2 * b]["proj"], res[2 * b + 1]["proj"]
            proj.append(np.concatenate([p0[:HC], p1[:HC], p0[HC:], p1[HC:]], 0))
        del res

        def flipseq(a):
            return np.concatenate([a[:CT][::-1], a[CT:][::-1]], 0)

        maps = []
        for cc, (b, z) in enumerate(cores):
            P = proj[b]
            mqT = np.zeros((4, 64, CT + S), f32); mkT = np.zeros((4, 64, CT + S), f32)
            mvk = np.zeros((4, CT + S, 128), f32); mg = np.zeros((4, CT + S, 2), f32); mgb = np.zeros((4, 128, 2), f32)
            for j in range(2):
                h = 2 * z + j
                q = P[:, 512 + h * 64:512 + (h + 1) * 64]; k = P[:, 768 + h * 64:768 + (h + 1) * 64]
                v = P[:, 1024 + h * 64:1024 + (h + 1) * 64]
                for d in range(2):
                    s = j * 2 + d
                    gi = P[:, 1536 + (2 * d) * 4 + h]; gf = P[:, 1536 + (2 * d + 1) * 4 + h]
                    qq, kk, vv, ii, ff = (q, k, v, gi, gf) if d == 0 else tuple(flipseq(a) for a in (q, k, v, gi, gf))
                    mqT[s] = qq.T; mkT[s] = kk.T
                    mvk[s, :, :64] = vv; mvk[s, :, 64:] = kk
                    mg[s, :, 0] = ii; mg[s, :, 1] = ff
                    mgb[s, :, 0] = mlstm_gate_b[l][2 * d, h]; mgb[s, :, 1] = mlstm_gate_b[l][2 * d + 1, h]
            maps.append({"mqT": mqT, "mkT": mkT, "mvk": mvk, "mg": mg, "mgb": mgb, "tri": tri, "ones": ones})
        res = _run(_prog("M1", lambda: build_M1(NKT)), maps)
        hf = [np.zeros((CT + S, 256), f32) for _ in range(B)]
        hb = [np.zeros((CT + S, 256), f32) for _ in range(B)]
        for cc, (b, z) in enumerate(cores):
            mh = res[cc]["mh"]
            for j in range(2):
                h = 2 * z + j
                hf[b][:, h * 64:(h + 1) * 64] = mh[j * 2]
                hb[b][:, h * 64:(h + 1) * 64] = flipseq(mh[j * 2 + 1])
        del res, maps

        maps = []
        dl = _rep(np.asarray(diff_lambda[l], f32).reshape(256))
        dng = _rep(np.asarray(diff_norm_g[l], f32))
        lami = _rep(np.array([lam_init, 1.0 - lam_init], f32))
        for cc, (b, z) in enumerate(cores):
            P = proj[b]
            aqT = np.zeros((2, 2, 64, CT + S), f32); akT = np.zeros((2, 2, 64, CT + S), f32); av = np.zeros((2, CT + S, 128), f32)
            for j in range(2):
                h = 2 * z + j
                for comp in range(2):
                    o = h * 128 + comp * 64
                    aqT[j, comp] = P[:, 1552 + o:1552 + o + 64].T
                    akT[j, comp] = P[:, 2064 + o:2064 + o + 64].T
                av[j] = P[:, 2576 + h * 128:2576 + (h + 1) * 128]
            maps.append({"aqT": aqT, "akT": akT, "av": av, "dl": dl, "dng": dng, "lami": lami, "ones": ones})
        res = _run(_prog("M2", lambda: build_M2(NKT, NCT)), maps)
        ao = [np.zeros((CT + S, 512), f32) for _ in range(B)]
        for cc, (b, z) in enumerate(cores):
            for j in range(2):
                h = 2 * z + j
                ao[b][:, h * 128:(h + 1) * 128] = res[cc]["ao"][j]
        del res, maps

        cw = np.ascontiguousarray(np.asarray(conv_w[l], f32)[:, 0, :].T.reshape(2, 128, 31).transpose(1, 0, 2))
        cp = np.ascontiguousarray(np.stack([conv_b[l], conv_ln_g[l], conv_ln_b[l]], -1).astype(f32).reshape(2, 128, 3).transpose(1, 0, 2))
        maps = []
        for cc, (b, z) in enumerate(cores):
            P = proj[b]
            aT = np.zeros((512, HC + 30 + HS + 30), f32)
            actx = np.zeros((CT + 30, 512), f32); actx[15:15 + CT] = P[:CT, 0:512]
            alat = np.zeros((S + 30, 512), f32); alat[15:15 + S] = P[CT:, 0:512]
            aT[:, 0:HC + 30] = actx[z * HC:z * HC + HC + 30].T
            aT[:, HC + 30:] = alat[z * HS:z * HS + HS + 30].T
            maps.append({"aT": aT, "cw": cw, "cp": cp, "ones": ones})
        res = _run(_prog("C1", lambda: build_C1([HC, HS])), maps)
        convT = [res[cc]["convT"] for cc in range(NCORES)]
        del res, maps

        maps = []
        mng = _rep(np.asarray(mlstm_norm_g[l], f32))
        for cc, (b, z) in enumerate(cores):
            sel = lambda a: np.ascontiguousarray(np.concatenate([a[z * HC:(z + 1) * HC], a[CT + z * HS:CT + (z + 1) * HS]], 0))
            bc = np.stack([_rep(norm2_g[l]), _rep(g1[:, b]), _rep(g1[:, 4]), _rep(sc2[:, b]), _rep(sc2[:, 4]), _rep(sh2[:, b]), _rep(sh2[:, 4])])
            maps.append({"xs": xs[cc], "convT": convT[cc], "hf": sel(hf[b]), "hb": sel(hb[b]), "mo": sel(proj[b][:, 1280:1536]),
                         "ao": sel(ao[b]), "mng": mng, "w_out": np.asarray(w_out[l], f32), "bc": bc, "ident": ident})
        res = _run(_prog("C2", lambda: build_C2(NT, ctx_tiles=tuple(range(HC // 128)))), maps)
        x1 = [res[cc]["x1"] for cc in range(NCORES)]
        h2 = [res[cc]["h2"] for cc in range(NCORES)]
        del res, maps, proj, hf, hb, ao, convT

        keys = np.asarray(peer_keys[l], f32)
        kbd = np.zeros((128, 8, 256), f32)
        for pp in range(2):
            kbd[pp * 64:(pp + 1) * 64, :, pp * 128:(pp + 1) * 128] = keys[:, pp].transpose(2, 0, 1)
        pu = np.asarray(peer_u[l], f32); pvv = np.asarray(peer_v[l], f32); wq = np.asarray(peer_wq[l], f32)
        maps = []
        for cc, (b, z) in enumerate(cores):
            maps.append({"h2": h2[cc], "x1": x1[cc], "wq": wq, "kbd": kbd, "peer_u": pu, "peer_v": pvv,
                         "g2b": np.stack([_rep(g2[:, b]), _rep(g2[:, 4])]), "ident": ident})
        res = _run(_prog("C3", lambda: build_C3(NT, ctx_tiles=tuple(range(HC // 128)))), maps)
        xs = [res[cc]["x2"] for cc in range(NCORES)]
        del res, maps, x1, h2

    fg = _rep(np.asarray(final_g, f32))
    maps = [{"xs": np.ascontiguousarray(xs[cc][HC:]), "fg": fg} for cc in range(NCORES)]
    res = _run(_prog("F", lambda: build_F(HS // 128)), maps)
    out = np.zeros((B, S, D), f32)
    for cc, (b, z) in enumerate(cores):
        out[b, z * HS:(z + 1) * HS] = res[cc]["y"]
    return out


class Cfg:
    def __init__(self, S, CT, DEPTH):
        self.S, self.CT, self.DEPTH = S, CT, DEPTH
        self.NTOK = S + CT
        self.NKT = self.NTOK // 128
        self.NCT = CT // 128
        self.NE = 16384


def _ph_P0(p, g, T):
    D_ = g.DEPTH
    ct = p.sb([128, 8, 2], name="ct"); st = p.sb([128, 8, 2], name="st")
    p.dma(ct[:], T["cT"], writes=["ct"])
    p.add("act", lambda e: e.activation(out=st[:], in_=ct[:], func=AF.Silu), ["ct"], ["st"])
    sbc = p.sb([128, 8, 2, 128], name="sbc")
    p.add("dve", lambda e: e.tensor_copy(out=sbc[:], in_=st[:, :, :, None].to_broadcast([128, 8, 2, 128])), ["st"], ["sbc"])
    wts = [p.sb([128, 8, 512], name=f"wt{i}") for i in range(2)]
    bbt = [p.sb([128, 512], name=f"bbt{i}") for i in range(2)]
    ob = [p.sb([128, 2, 512], name=f"ob{i}") for i in range(2)]
    btT = p.sb([128, 48], name="btT")
    oT = p.sb([128, 48, 2], name="oT")
    pTb = [p.ps([128, 512], name=f"ppT{i}") for i in range(2)]
    pT = [t[:, 0:32].rearrange("p (a b) -> p a b", a=4) for t in pTb]
    pB = [p.ps([128, 512], name=f"ppB{i}") for i in range(4)]
    nblk = 0
    for l in range(D_):
        p.dma(btT[:], T["ada_bT"][l], writes=["btT"])
        wv = T["ada_w"][l].rearrange("(k p) n -> p k n", p=128)
        for blk in range(12):
            i = nblk % 2; nblk += 1
            wt = wts[i]
            p.dma(wt[:], wv[:, :, blk * 512:(blk + 1) * 512], writes=[f"wt{i}"])
            p.dma(bbt[i][:], T["ada_bB"][l, :, blk * 512:(blk + 1) * 512], writes=[f"bbt{i}"])
            for jj in range(4):
                for k in range(8):
                    p.add("pe", lambda e, wt=wt, i=i, jj=jj, k=k: e.matmul(pT[i][:, jj, 0:2], lhsT=wt[:, k, jj * 128:(jj + 1) * 128], rhs=st[:, k, :],
                                                                          start=(k == 0), stop=(k == 7)), [f"wt{i}", "st"], [f"ppT{i}"])
            for jj in range(4):
                j = blk * 4 + jj
                p.add("dve", lambda e, i=i, jj=jj, j=j: e.tensor_scalar(out=oT[:, j, :], in0=pT[i][:, jj, 0:2], scalar1=btT[:, j:j + 1], scalar2=None,
                                                                       op0=ALU.add), [f"ppT{i}", "btT"], ["oT"])
            for n in range(2):
                pb = (2 * blk + n) % 4
                for k in range(8):
                    p.add("pe", lambda e, wt=wt, n=n, k=k, pb=pb: e.matmul(pB[pb][:], lhsT=sbc[:, k, n, :], rhs=wt[:, k, :], start=(k == 0), stop=(k == 7)),
                          [f"wt{i}", "sbc"], [f"ppB{pb}"])
                p.add("dve", lambda e, i=i, n=n, pb=pb: e.tensor_tensor(out=ob[i][:, n, :], in0=pB[pb][:], in1=bbt[i][:], op=ALU.add),
                      [f"ppB{pb}", f"bbt{i}"], [f"ob{i}_{n}"])
                p.dma(T["modB"][l, n, :, blk * 512:(blk + 1) * 512], ob[i][:, n, :], reads=[f"ob{i}_{n}"])
        p.dma(T["modT"][l], oT[:], reads=["oT"])


def _ph_A(p, g, T, l):
    NT = g.NKT
    xs, proj, projT = T["xs"], T["proj"], T["projT"]
    wb = p.sb([128, 8, IN_W], BF16, name="wb")
    load_cast_weight(p, wb, T["w_in"][l], IN_W, "wb", bw=256)
    idt = p.sb([128, 128], name="idt"); p.dma(idt[:], T["ident"], writes=["idt"])
    mT = p.sb([128, 48, 2], name="mT"); p.dma(mT[:], T["modT"][l], writes=["mT"])
    n1 = p.sb([128, 8], name="n1"); p.dma(n1[:], T["n1g"][l], writes=["n1"])
    gm = p.sb([128, 8, 2], name="gm")
    for n in range(2):
        p.add("dve", lambda e, n=n: e.scalar_tensor_tensor(out=gm[:, :, n], in0=mT[:, 8:16, n], scalar=1.0, in1=n1[:], op0=ALU.add, op1=ALU.mult),
              ["mT", "n1"], ["gm"])
    NB = 2
    mk = lambda shape, nm, dt=F32: [p.sb(shape, dt, name=f"{nm}{i}") for i in range(NB)]
    xt = mk([128, D], "xt"); xn = mk([128, D], "xn"); junk = p.sb([128, D], name="junk")
    ss = mk([128, 1], "ss"); rstd = mk([128, 1], "rstd"); hT = mk([128, 8, 128], "hT", BF16)
    ot = mk([128, IN_W], "ot"); ro = mk([128, 1024], "ro"); tmp = mk([128, 4, 512], "tmp"); cst = mk([128, 64], "cst")
    tT = mk([128, 16, 128], "tT")
    tp = [p.ps([128, 512], name=f"tp{i}") for i in range(4)]
    acc = [p.ps([128, 512], name=f"acc{i}") for i in range(4)]
    nacc = 0
    ntp = 0
    for t in range(NT):
        i = t % NB
        isc = t < g.NCT
        n_ = 1 if isc else 0
        r0 = t * 128
        p.dma(xt[i][:], xs[r0:r0 + 128, :], writes=[f"xt{i}"])
        p.dma(cst[i][:], T["cs"][r0:r0 + 128, :], writes=[f"cst{i}"])
        p.add("act", lambda e, i=i: e.activation(out=junk[:], in_=xt[i][:], func=AF.Square, accum_out=ss[i][:, 0:1]), [f"xt{i}"], ["junk", f"ss{i}"])
        p.add("dve", lambda e, i=i: e.tensor_scalar(out=rstd[i][:], in0=ss[i][:], scalar1=1.0 / D, scalar2=EPS, op0=ALU.mult, op1=ALU.add),
              [f"ss{i}"], [f"rstd{i}"])
        p.add("act", lambda e, i=i: e.activation(out=ss[i][:], in_=rstd[i][:], func=AF.Sqrt), [f"rstd{i}"], [f"ss{i}"])
        p.add("dve", lambda e, i=i: e.reciprocal(out=rstd[i][:], in_=ss[i][:]), [f"ss{i}"], [f"rstd{i}"])
        p.add("dve", lambda e, i=i: e.tensor_scalar(out=xn[i][:], in0=xt[i][:], scalar1=rstd[i][:, 0:1], scalar2=None, op0=ALU.mult),
              [f"xt{i}", f"rstd{i}"], [f"xn{i}"])
        for half in range(2):
            tpi = ntp % 4; ntp += 1
            for k in range(half * 4, half * 4 + 4):
                p.add("pe", lambda e, i=i, k=k, tpi=tpi: e.transpose(tp[tpi][:, (k % 4) * 128:(k % 4 + 1) * 128], xn[i][:, k * 128:(k + 1) * 128], idt[:]),
                      [f"xn{i}", "idt"], [f"tp{tpi}"])
            for k in range(half * 4, half * 4 + 4):
                if half == 0:
                    p.add("act", lambda e, i=i, k=k, tpi=tpi, n_=n_: e.activation(
                        out=hT[i][:, k, :], in_=tp[tpi][:, (k % 4) * 128:(k % 4 + 1) * 128], func=AF.Identity,
                        scale=gm[:, k, n_:n_ + 1], bias=mT[:, k, n_:n_ + 1]), [f"tp{tpi}", "gm", "mT"], [f"hT{i}_{k}"])
                else:
                    p.add("dve", lambda e, i=i, k=k, tpi=tpi, n_=n_: e.tensor_scalar(
                        out=hT[i][:, k, :], in0=tp[tpi][:, (k % 4) * 128:(k % 4 + 1) * 128],
                        scalar1=gm[:, k, n_:n_ + 1], scalar2=mT[:, k, n_:n_ + 1], op0=ALU.mult, op1=ALU.add), [f"tp{tpi}", "gm", "mT"], [f"hT{i}_{k}"])
        for blk in range(7):
            c0 = blk * 512
            cw = min(512, IN_W - c0)
            a = nacc % 4; nacc += 1
            for k in range(8):
                p.add("pe", lambda e, i=i, k=k, a=a, c0=c0, cw=cw: e.matmul(acc[a][:, 0:cw], lhsT=hT[i][:, k, :], rhs=wb[:, k, c0:c0 + cw],
                                                                            start=(k == 0), stop=(k == 7)), [f"hT{i}_{k}", "wb"], [f"acc{a}"])
            if blk % 2 == 0:
                p.add("act", lambda e, i=i, a=a, c0=c0, cw=cw: e.activation(out=ot[i][:, c0:c0 + cw], in_=acc[a][:, 0:cw], func=AF.Copy),
                      [f"acc{a}"], [f"ot{i}_{blk}"])
            else:
                p.add("dve", lambda e, i=i, a=a, c0=c0, cw=cw: e.tensor_copy(out=ot[i][:, c0:c0 + cw], in_=acc[a][:, 0:cw]), [f"acc{a}"], [f"ot{i}_{blk}"])
        src = ot[i][:, 1552:2576].rearrange("p (g r two) -> p g r two", g=16, two=2)
        dst = ro[i][:].rearrange("p (g r two) -> p g r two", g=16, two=2)
        t1 = src[:, :, :, 0]; t2 = src[:, :, :, 1]
        cosb = cst[i][:, None, 0:32].to_broadcast([128, 16, 32])
        sinb = cst[i][:, None, 32:64].to_broadcast([128, 16, 32])
        tm = [tmp[i][:, j, :].rearrange("p (g r) -> p g r", g=16) for j in range(4)]
        rk = [f"ot{i}_3", f"ot{i}_4", f"ot{i}_5", f"cst{i}"]
        p.add("dve", lambda e, tm=tm, t1=t1, cosb=cosb: e.tensor_tensor(out=tm[0], in0=t1, in1=cosb, op=ALU.mult), rk, [f"tmp{i}_0"])
        p.add("dve", lambda e, tm=tm, t2=t2, sinb=sinb: e.tensor_tensor(out=tm[1], in0=t2, in1=sinb, op=ALU.mult), rk, [f"tmp{i}_1"])
        p.add("dve", lambda e, tm=tm, t1=t1, sinb=sinb: e.tensor_tensor(out=tm[2], in0=t1, in1=sinb, op=ALU.mult), rk, [f"tmp{i}_2"])
        p.add("dve", lambda e, tm=tm, t2=t2, cosb=cosb: e.tensor_tensor(out=tm[3], in0=t2, in1=cosb, op=ALU.mult), rk, [f"tmp{i}_3"])
        p.add("dve", lambda e, tm=tm, dst=dst: e.tensor_tensor(out=dst[:, :, :, 0], in0=tm[0], in1=tm[1], op=ALU.subtract),
              [f"tmp{i}_0", f"tmp{i}_1"], [f"ro{i}a"])
        p.add("dve", lambda e, tm=tm, dst=dst: e.tensor_tensor(out=dst[:, :, :, 1], in0=tm[2], in1=tm[3], op=ALU.add),
              [f"tmp{i}_2", f"tmp{i}_3"], [f"ro{i}b"])
        p.dma(proj[r0:r0 + 128, 0:1552], ot[i][:, 0:1552], reads=[f"ot{i}_{b}" for b in range(4)])
        p.dma(proj[r0:r0 + 128, 1552:2576], ro[i][:], reads=[f"ro{i}a", f"ro{i}b"])
        p.dma(proj[r0:r0 + 128, 2576:IN_W], ot[i][:, 2576:IN_W], reads=[f"ot{i}_5", f"ot{i}_6"])
        srcs = [(ot[i], c * 128, [f"ot{i}_0", f"ot{i}_1"]) for c in range(8)] + [(ro[i], c * 128, [f"ro{i}a", f"ro{i}b"]) for c in range(8)]
        for q4 in range(4):
            tpi = ntp % 4; ntp += 1
            for c in range(4):
                sap, c0, keys = srcs[q4 * 4 + c]
                p.add("pe", lambda e, sap=sap, c0=c0, tpi=tpi, c=c: e.transpose(tp[tpi][:, c * 128:(c + 1) * 128], sap[:, c0:c0 + 128], idt[:]),
                      keys + ["idt"], [f"tp{tpi}"])
            if q4 % 2 == 0:
                p.add("act", lambda e, i=i, q4=q4, tpi=tpi: e.activation(out=tT[i][:, q4 * 4:q4 * 4 + 4, :], in_=tp[tpi][:].rearrange("p (k n) -> p k n", k=4),
                                                                         func=AF.Copy), [f"tp{tpi}"], [f"tT{i}_{q4}"])
            else:
                p.add("dve", lambda e, i=i, q4=q4, tpi=tpi: e.tensor_copy(out=tT[i][:, q4 * 4:q4 * 4 + 4, :], in_=tp[tpi][:].rearrange("p (k n) -> p k n", k=4)),
                      [f"tp{tpi}"], [f"tT{i}_{q4}"])
        p.dma(projT[:, r0:r0 + 128].rearrange("(k p) n -> p k n", p=128), tT[i][:], reads=[f"tT{i}_{q}" for q in range(4)])


def _ph_M1(p, g, T, l, GC=4):
    NKT, NCT = g.NKT, g.NCT
    proj, projT, mixo = T["proj"], T["projT"], T["mixo"]
    tri = p.sb([128, 128], name="tri_sb"); p.dma(tri[:], T["tri"], writes=["tri"])
    triL = p.sb([128, 128], name="triL_sb"); p.dma(triL[:], T["triL"], writes=["triL"])
    ones = p.sb([128, 128], name="ones_sb"); p.dma(ones[:], T["ones"], writes=["ones"])
    banks = [p.ps([128, 512], name=f"bank{i}") for i in range(7)]
    NS = 4
    NBUF = 2
    B = {}
    for s in range(NS):
        B[s, "gb"] = p.sb([128, 2], name=f"gb{s}")
        B[s, "Cf"] = p.sb([64, 65], name=f"Cf{s}")
        B[s, "Cb"] = p.sb([64, 65], BF16, name=f"Cb{s}")
        B[s, "tmpC"] = p.sb([64, 65], name=f"tmpC{s}")
        for i in range(NBUF):
            k = (s, i)
            B[k, "qTf"] = p.sb([64, GC * 128], name=f"qTf{s}_{i}")
            B[k, "kTf"] = p.sb([64, GC * 128], name=f"kTf{s}_{i}")
            B[k, "vf"] = p.sb([128, GC, 64], name=f"vf{s}_{i}")
            B[k, "kf"] = p.sb([128, GC, 64], name=f"kf{s}_{i}")
            B[k, "gf"] = p.sb([128, GC, 16], name=f"gf{s}_{i}")
            B[k, "qTb"] = p.sb([64, GC * 128], BF16, name=f"qTb{s}_{i}")
            B[k, "kTb"] = p.sb([64, GC * 128], BF16, name=f"kTb{s}_{i}")
            B[k, "vaug"] = p.sb([128, GC, 65], BF16, name=f"vaug{s}_{i}")
            B[k, "kb"] = p.sb([128, GC, 64], BF16, name=f"kb{s}_{i}")
            B[k, "h"] = p.sb([128, GC, 64], name=f"h{s}_{i}")
            for nm in ("gi", "sp", "a", "b", "eG"):
                B[k, nm] = p.sb([128, GC], name=f"{nm}{s}_{i}")
            B[k, "WT"] = p.sb([128, 128], BF16, name=f"WT{s}_{i}")
            B[k, "d"] = p.sb([128, 4], name=f"d{s}_{i}")
            p.add("dve", lambda e, k=k: e.memset(B[k, "vaug"][:, :, 64:65], 1.0), [], [f"vaug1_{s}_{i}"])
    def groups(rev):
        out = []
        for lo, hi in ((0, NCT), (NCT, NKT)):
            rng = list(range(lo, hi))
            for g0 in range(0, len(rng), GC):
                out.append(rng[g0:g0 + GC])
        if rev:
            out = []
            for lo, hi in ((0, NCT), (NCT, NKT)):
                rng = list(range(lo, hi))[::-1]
                for g0 in range(0, len(rng), GC):
                    out.append(rng[g0:g0 + GC])
        return out
    gcount = {s: 0 for s in range(NS)}
    for hp in range(2):
        for s in range(NS):
            h, dr = 2 * hp + s // 2, s % 2
            p.dma(B[s, "gb"][:], T["mgb"][l, h * 2 + dr], writes=[f"gb{s}"])
            p.add("dve", lambda e, s=s: e.memset(B[s, "Cf"][:], 0.0), [], [f"Cf{s}"])
            p.add("dve", lambda e, s=s: e.memset(B[s, "Cb"][:], 0.0), [], [f"Cb{s}"])
        glists = [groups(s % 2 == 1) for s in range(NS)]
        for gi_ in range(len(glists[0])):
            for s in range(NS):
                h, dr = 2 * hp + s // 2, s % 2
                chunks = glists[s][gi_]
                lo, hi = min(chunks), max(chunks) + 1
                gc = hi - lo
                i = gcount[s] % NBUF; gcount[s] += 1
                k = (s, i)
                sfx = f"{s}_{i}"
                Tt = {n: B[k, n] for n in ("qTf", "kTf", "vf", "kf", "gf", "qTb", "kTb", "vaug", "kb", "h", "gi", "sp", "a", "b", "eG", "WT", "d")}
                Cf, Cb, tmpC, gb = B[s, "Cf"], B[s, "Cb"], B[s, "tmpC"], B[s, "gb"]
                bS, bX, bU = banks[(s % 2) * 3], banks[(s % 2) * 3 + 1], banks[(s % 2) * 3 + 2]
                kS, kX, kU = f"bank{(s%2)*3}", f"bank{(s%2)*3+1}", f"bank{(s%2)*3+2}"
                bP = banks[6]
                trm = triL if dr else tri
                ktr = "triL" if dr else "tri"
                W = gc * 128
                t0 = lo * 128
                p.dma(Tt["qTf"][:, 0:W], projT[512 + h * 64:512 + (h + 1) * 64, t0:t0 + W], writes=[f"qTf{sfx}"])
                p.dma(Tt["kTf"][:, 0:W], projT[768 + h * 64:768 + (h + 1) * 64, t0:t0 + W], writes=[f"kTf{sfx}"])
                pr = proj[t0:t0 + W, :].rearrange("(c p) f -> p c f", p=128)
                p.dma(Tt["vf"][:, 0:gc, :], pr[:, :, 1024 + h * 64:1024 + (h + 1) * 64], writes=[f"vf{sfx}"])
                p.dma(Tt["kf"][:, 0:gc, :], pr[:, :, 768 + h * 64:768 + (h + 1) * 64], writes=[f"kf{sfx}"])
                ci = (2 * dr) * 4 + h
                cf = (2 * dr + 1) * 4 + h
                p.dma(Tt["gf"][:, 0:gc, :], pr[:, :, 1536:1552], writes=[f"gf{sfx}a"])
                gk = [f"gf{sfx}a"]
                p.add("dve", lambda e, Tt=Tt, gb=gb, gc=gc, ci=ci: e.tensor_scalar(out=Tt["gi"][:, 0:gc], in0=Tt["gf"][:, 0:gc, ci], scalar1=gb[:, 0:1],
                                                                       scalar2=None, op0=ALU.add), gk + [f"gb{s}"], [f"gi{sfx}"])
                p.add("dve", lambda e, Tt=Tt, gb=gb, gc=gc, cf=cf: e.tensor_scalar(out=Tt["sp"][:, 0:gc], in0=Tt["gf"][:, 0:gc, cf], scalar1=gb[:, 1:2],
                                                                       scalar2=None, op0=ALU.add), gk + [f"gb{s}"], [f"sp{sfx}"])
                p.add("act", lambda e, Tt=Tt, gc=gc: e.activation(out=Tt["sp"][:, 0:gc], in_=Tt["sp"][:, 0:gc], func=AF.Exp, scale=-1.0),
                      [f"sp{sfx}"], [f"sp{sfx}"])
                p.add("dve", lambda e, Tt=Tt, gc=gc: e.tensor_scalar(out=Tt["sp"][:, 0:gc], in0=Tt["sp"][:, 0:gc], scalar1=1.0, scalar2=None,
                                                                op0=ALU.add), [f"sp{sfx}"], [f"sp{sfx}"])
                p.add("act", lambda e, Tt=Tt, gc=gc: e.activation(out=Tt["sp"][:, 0:gc], in_=Tt["sp"][:, 0:gc], func=AF.Ln), [f"sp{sfx}"], [f"sp{sfx}"])
                p.add("pe", lambda e, Tt=Tt, gc=gc, bP=bP, trm=trm: e.matmul(bP[:, 0:gc], lhsT=trm[:], rhs=Tt["sp"][:, 0:gc], start=True, stop=True),
                      [ktr, f"sp{sfx}"], ["bank6"])
                p.add("pe", lambda e, Tt=Tt, gc=gc, bP=bP: e.matmul(bP[:, 64:64 + gc], lhsT=ones[:], rhs=Tt["sp"][:, 0:gc], start=True, stop=True),
                      ["ones", f"sp{sfx}"], ["bank6"])
                p.add("act", lambda e, Tt=Tt, gc=gc, bP=bP: e.activation(out=Tt["a"][:, 0:gc], in_=bP[:, 0:gc], func=AF.Exp, scale=-1.0), ["bank6"], [f"a{sfx}"])
                p.add("act", lambda e, Tt=Tt, gc=gc, bP=bP: e.activation(out=Tt["eG"][:, 0:gc], in_=bP[:, 64:64 + gc], func=AF.Exp, scale=-1.0), ["bank6"], [f"eG{sfx}"])
                p.add("act", lambda e, Tt=Tt, gc=gc, bP=bP: e.activation(out=Tt["b"][:, 0:gc], in_=bP[:, 0:gc], func=AF.Identity), ["bank6"], [f"b{sfx}"])
                p.add("dve", lambda e, Tt=Tt, gc=gc: e.tensor_tensor(out=Tt["b"][:, 0:gc], in0=Tt["b"][:, 0:gc], in1=Tt["gi"][:, 0:gc], op=ALU.add),
                      [f"b{sfx}", f"gi{sfx}"], [f"b{sfx}"])
                p.add("act", lambda e, Tt=Tt, gc=gc: e.activation(out=Tt["b"][:, 0:gc], in_=Tt["b"][:, 0:gc], func=AF.Exp), [f"b{sfx}"], [f"b{sfx}"])
                p.add("act", lambda e, Tt=Tt, W=W: e.activation(out=Tt["qTb"][:, 0:W], in_=Tt["qTf"][:, 0:W], func=AF.Copy), [f"qTf{sfx}"], [f"qTb{sfx}"])
                p.add("dve", lambda e, Tt=Tt, W=W: e.tensor_scalar(out=Tt["kTb"][:, 0:W], in0=Tt["kTf"][:, 0:W], scalar1=0.125, scalar2=None, op0=ALU.mult),
                      [f"kTf{sfx}"], [f"kTb{sfx}"])
                p.add("act", lambda e, Tt=Tt, gc=gc: e.activation(out=Tt["vaug"][:, 0:gc, 0:64], in_=Tt["vf"][:, 0:gc, :], func=AF.Copy),
                      [f"vf{sfx}"], [f"vaug{sfx}"])
                p.add("dve", lambda e, Tt=Tt, gc=gc: e.scalar_tensor_tensor(
                    out=Tt["kb"][:, 0:gc, :], in0=Tt["kf"][:, 0:gc, :], scalar=0.125,
                    in1=Tt["b"][:, 0:gc, None].to_broadcast([128, gc, 64]), op0=ALU.mult, op1=ALU.mult), [f"kf{sfx}", f"b{sfx}"], [f"kb{sfx}"])
                for c in chunks:
                    cc = c - lo
                    cs = slice(cc * 128, (cc + 1) * 128)
                    p.add("pe", lambda e, Tt=Tt, cs=cs, bS=bS: e.matmul(bS[:, 0:128], lhsT=Tt["kTb"][:, cs], rhs=Tt["qTb"][:, cs], start=True, stop=True),
                          [f"kTb{sfx}", f"qTb{sfx}"], [kS])
                    p.add("dve", lambda e, Tt=Tt, cc=cc, bS=bS, trm=trm: e.scalar_tensor_tensor(
                        out=Tt["WT"][:], in0=bS[:, 0:128], scalar=Tt["b"][:, cc:cc + 1], in1=trm[:], op0=ALU.mult, op1=ALU.mult),
                        [kS, f"b{sfx}", ktr], [f"WT{sfx}"])
                    p.add("pe", lambda e, Tt=Tt, cc=cc, bX=bX: e.matmul(bX[:, 0:65], lhsT=Tt["WT"][:], rhs=Tt["vaug"][:, cc, :], start=True, stop=False),
                          [f"WT{sfx}", f"vaug{sfx}", f"vaug1_{sfx}"], [kX])
                    p.add("pe", lambda e, Tt=Tt, cs=cs, bX=bX, Cb=Cb: e.matmul(bX[:, 0:65], lhsT=Tt["qTb"][:, cs], rhs=Cb[:], start=False, stop=True),
                          [f"qTb{sfx}", f"Cb{s}"], [kX])
                    p.add("pe", lambda e, Tt=Tt, cc=cc, bU=bU: e.matmul(bU[0:64, 0:65], lhsT=Tt["kb"][:, cc, :], rhs=Tt["vaug"][:, cc, :], start=True, stop=True),
                          [f"kb{sfx}", f"vaug{sfx}", f"vaug1_{sfx}"], [kU])
                    d = Tt["d"]
                    p.add("act", lambda e, Tt=Tt, cc=cc, bX=bX, d=d: e.activation(out=d[:, 0:1], in_=bX[:, 64:65], func=AF.Abs, scale=Tt["a"][:, cc:cc + 1]),
                          [kX, f"a{sfx}"], [f"d0{sfx}"])
                    p.add("dve", lambda e, d=d: e.tensor_scalar(out=d[:, 3:4], in0=d[:, 0:1], scalar1=1.0, scalar2=None, op0=ALU.max), [f"d0{sfx}"], [f"d3{sfx}"])
                    p.add("dve", lambda e, d=d: e.reciprocal(out=d[:, 1:2], in_=d[:, 3:4]), [f"d3{sfx}"], [f"d1{sfx}"])
                    p.add("dve", lambda e, Tt=Tt, cc=cc, d=d: e.tensor_tensor(out=d[:, 2:3], in0=d[:, 1:2], in1=Tt["a"][:, cc:cc + 1], op=ALU.mult),
                          [f"d1{sfx}", f"a{sfx}"], [f"d2{sfx}"])
                    p.add("dve", lambda e, Tt=Tt, cc=cc, bX=bX, d=d: e.tensor_scalar(out=Tt["h"][:, cc, :], in0=bX[:, 0:64], scalar1=d[:, 2:3], scalar2=None,
                                                                                op0=ALU.mult), [kX, f"d2{sfx}"], [f"h{sfx}"])
                    p.add("dve", lambda e, bU=bU, Cf=Cf, tmpC=tmpC: e.tensor_tensor(out=tmpC[:], in0=bU[0:64, 0:65], in1=Cf[:], op=ALU.add),
                          [kU, f"Cf{s}"], [f"tmpC{s}"])
                    p.add("dve", lambda e, Tt=Tt, cc=cc, Cf=Cf, tmpC=tmpC: e.tensor_scalar(out=Cf[:], in0=tmpC[:], scalar1=Tt["eG"][0:64, cc:cc + 1], scalar2=None,
                                                                                      op0=ALU.mult), [f"tmpC{s}", f"eG{sfx}"], [f"Cf{s}"])
                    p.add("act", lambda e, Tt=Tt, cc=cc, Cb=Cb, tmpC=tmpC: e.activation(out=Cb[:], in_=tmpC[:], func=AF.Copy, scale=Tt["eG"][0:64, cc:cc + 1]),
                          [f"tmpC{s}", f"eG{sfx}"], [f"Cb{s}"])
                oc = dr * 256 + h * 64
                p.dma(mixo[t0:t0 + W, oc:oc + 64].rearrange("(c p) f -> p c f", p=128), Tt["h"][:, 0:gc, :], reads=[f"h{sfx}"])


def _ph_M2(p, g, T, l):
    NKT, NCT = g.NKT, g.NCT
    L = NKT * 128
    proj, projT, mixo = T["proj"], T["projT"], T["mixo"]
    ones = p.sb([128, 128], name="ones_sb"); p.dma(ones[:], T["ones"], writes=["ones"])
    dl = p.sb([128, 256], name="dl_sb"); p.dma(dl[:], T["dl"][l], writes=["dl"])
    gsc = p.sb([128, 128], name="gsc"); p.dma(gsc[:], T["dng"][l], writes=["gsc"])
    lami = p.sb([128, 2], name="lami_sb"); p.dma(lami[:], T["lami"][l], writes=["lami"])
    junk = p.sb([128, 128], name="junk")
    lm = p.sb([128, 4], name="lm")
    p.add("dve", lambda e: e.scalar_tensor_tensor(out=junk[:, 0:64], in0=dl[:, 0:64], scalar=1.0, in1=dl[:, 64:128],
                                                  op0=ALU.mult, op1=ALU.mult, accum_out=lm[:, 0:1]), ["dl"], ["junk", "lm0"])
    p.add("dve", lambda e: e.scalar_tensor_tensor(out=junk[:, 64:128], in0=dl[:, 128:192], scalar=1.0, in1=dl[:, 192:256],
                                                  op0=ALU.mult, op1=ALU.mult, accum_out=lm[:, 1:2]), ["dl"], ["junk", "lm1"])
    p.add("act", lambda e: e.activation(out=lm[:, 0:2], in_=lm[:, 0:2], func=AF.Exp), ["lm0", "lm1"], ["lm01"])
    p.add("dve", lambda e: e.tensor_tensor(out=lm[:, 2:3], in0=lm[:, 1:2], in1=lm[:, 0:1], op=ALU.subtract), ["lm01"], ["lm2"])
    p.add("dve", lambda e: e.tensor_scalar(out=lm[:, 3:4], in0=lm[:, 2:3], scalar1=lami[:, 0:1], scalar2=None, op0=ALU.subtract), ["lm2", "lami"], ["nlam"])
    p.add("dve", lambda e: e.tensor_scalar(out=gsc[:], in0=gsc[:], scalar1=lami[:, 1:2], scalar2=None, op0=ALU.mult), ["gsc", "lami"], ["gsc"])
    banks = [p.ps([128, 512], name=f"bank{i}") for i in range(8)]
    qTb = p.sb([64, 2, L], BF16, name="qTb")
    kTb = p.sb([64, 2, L], BF16, name="kTb")
    vaug = p.sb([128, NKT, 129], BF16, name="vaug")
    p.add("dve", lambda e: e.memset(vaug[:, :, 128:129], 1.0), [], ["vaug1"])
    PW = 2048
    stg = [p.sb([64, PW], name=f"stg{i}") for i in range(2)]
    sq = p.sb([64, PW], name="sq")
    VG = 16
    vst = [p.sb([128, VG, 128], name=f"vst{i}") for i in range(2)]
    mx = p.sb([128, 8], name="mx")
    nb = p.sb([128, 4], name="nb")
    pT = [p.sb([128, 512], BF16, name=f"pT{i}") for i in range(6)]
    ev = [p.sb([128, 8], name=f"ev{i}") for i in range(2)]
    o1 = [p.sb([128, 128], name=f"o1_{i}") for i in range(2)]
    oo = [p.sb([128, 128], name=f"oo_{i}") for i in range(2)]
    yy = [p.sb([128, 128], name=f"yy_{i}") for i in range(2)]
    nstg = nvst = nit = nblk = nev = 0
    for s in range(4):
        h = s
        p.add("dve", lambda e: e.memset(mx[:], 0.0), [], ["mx", "mx4"])
        for which, (rbase, dst) in enumerate(((1024, qTb), (1536, kTb))):
            for comp in range(2):
                rr = rbase + h * 128 + comp * 64
                for c0 in range(0, L, PW):
                    cw = min(PW, L - c0)
                    si = nstg % 2; nstg += 1
                    st = stg[si]
                    p.dma(st[:, 0:cw], projT[rr:rr + 64, c0:c0 + cw], writes=[f"stg{si}"])
                    p.add("dve", lambda e, st=st, dst=dst, comp=comp, c0=c0, cw=cw: e.tensor_copy(out=dst[:, comp, c0:c0 + cw], in_=st[:, 0:cw]),
                          [f"stg{si}"], ["qTb" if which == 0 else "kTb"])
                    p.add("act", lambda e, st=st, cw=cw: e.activation(out=sq[:, 0:cw], in_=st[:, 0:cw], func=AF.Square), [f"stg{si}"], ["sq"])
                    for b0 in range(0, cw, 512):
                        bw = min(512, cw - b0)
                        p.add("pe", lambda e, b0=b0, bw=bw: e.matmul(banks[6][:, 0:bw], lhsT=ones[0:64, :], rhs=sq[:, b0:b0 + bw], start=True, stop=True),
                              ["ones", "sq"], ["bank6"])
                        p.add("dve", lambda e, bw=bw: e.reduce_max(out=mx[:, 4:5], in_=banks[6][:, 0:bw], axis=AX.X), ["bank6"], ["mx4"])
                        col = which * 2 + comp
                        p.add("dve", lambda e, col=col: e.tensor_tensor(out=mx[:, col:col + 1], in0=mx[:, col:col + 1], in1=mx[:, 4:5], op=ALU.max),
                              ["mx4", "mx"], ["mx"])
        for g0 in range(0, NKT, VG):
            gn = min(VG, NKT - g0)
            vi = nvst % 2; nvst += 1
            p.dma(vst[vi][:, 0:gn, :], proj[g0 * 128:(g0 + gn) * 128, 2576 + h * 128:2576 + (h + 1) * 128].rearrange("(c p) f -> p c f", p=128),
                  writes=[f"vst{vi}"])
            p.add("act", lambda e, vi=vi, g0=g0, gn=gn: e.activation(out=vaug[:, g0:g0 + gn, 0:128], in_=vst[vi][:, 0:gn, :], func=AF.Copy),
                  [f"vst{vi}"], ["vaug"])
        p.add("dve", lambda e: e.tensor_tensor(out=nb[:, 2:4], in0=mx[:, 0:2], in1=mx[:, 2:4], op=ALU.mult), ["mx"], ["nb2"])
        p.add("act", lambda e: e.activation(out=nb[:, 2:4], in_=nb[:, 2:4], func=AF.Sqrt, scale=1.0 / 64.0), ["nb2"], ["nb2"])
        p.add("dve", lambda e: e.tensor_scalar(out=nb[:, 0:2], in0=nb[:, 2:4], scalar1=60.0, scalar2=-1.0, op0=ALU.min, op1=ALU.mult), ["nb2"], ["nb"])
        blocks = [(0, NCT, 0, NCT)]
        for q0 in range(NCT, NKT, 4):
            blocks.append((q0, min(4, NKT - q0), 0, NKT))
        for (q0, nq, k0, nk) in blocks:
            oset = 0; nblk += 1
            obanks = [banks[oset * 3 + i] for i in range(3)]
            okeys = [f"bank{oset*3+i}" for i in range(3)]
            for i in range(3):
                p.add("dve", lambda e, i=i, obanks=obanks: e.memset(obanks[i][:], 0.0), [], [okeys[i]])

            def oacc(comp, j, obanks=obanks, okeys=okeys):
                a = comp * 4 + j
                return obanks[a // 3][:, (a % 3) * 129:(a % 3) * 129 + 129], okeys[a // 3]
            QW = nq * 128
            for kt in range(k0, k0 + nk):
                for comp in range(2):
                    sb_i = 3 + nit % 5
                    pi = nit % 6
                    nit += 1
                    p.add("pe", lambda e, comp=comp, kt=kt, sb_i=sb_i, q0=q0, QW=QW: e.matmul(
                        banks[sb_i][:, 0:QW], lhsT=kTb[:, comp, kt * 128:(kt + 1) * 128], rhs=qTb[:, comp, q0 * 128:q0 * 128 + QW],
                        start=True, stop=True), ["kTb", "qTb"], [f"bank{sb_i}"])
                    p.add("act", lambda e, comp=comp, sb_i=sb_i, pi=pi, QW=QW: e.activation(
                        out=pT[pi][:, 0:QW], in_=banks[sb_i][:, 0:QW], func=AF.Exp, scale=0.125, bias=nb[:, comp:comp + 1]),
                        [f"bank{sb_i}", "nb"], [f"pT{pi}"])
                    for j in range(nq):
                        oap, okey = oacc(comp, j)
                        p.add("pe", lambda e, oap=oap, pi=pi, j=j, kt=kt: e.matmul(
                            oap, lhsT=pT[pi][:, j * 128:(j + 1) * 128], rhs=vaug[:, kt, :], start=False, stop=False,
                            skip_group_check=True), [f"pT{pi}", "vaug", "vaug1"], [okey])
            for j in range(nq):
                ei = nev % 2; nev += 1
                E = ev[ei]
                (o1ap, k1), (o2ap, k2) = oacc(0, j), oacc(1, j)
                sfx = f"_{ei}"
                p.add("dve", lambda e, E=E, o1ap=o1ap: e.reciprocal(out=E[:, 0:1], in_=o1ap[:, 128:129]), [k1], ["ev0" + sfx])
                p.add("dve", lambda e, E=E, o2ap=o2ap: e.reciprocal(out=E[:, 1:2], in_=o2ap[:, 128:129]), [k2], ["ev1" + sfx])
                p.add("dve", lambda e, E=E: e.tensor_tensor(out=E[:, 2:3], in0=E[:, 1:2], in1=lm[:, 3:4], op=ALU.mult), ["ev1" + sfx, "nlam"], ["ev2" + sfx])
                p.add("dve", lambda e, E=E, o1ap=o1ap, ei=ei: e.tensor_scalar(out=o1[ei][:], in0=o1ap[:, 0:128], scalar1=E[:, 0:1], scalar2=None, op0=ALU.mult),
                      [k1, "ev0" + sfx], ["o1" + sfx])
                p.add("dve", lambda e, E=E, o2ap=o2ap, ei=ei: e.scalar_tensor_tensor(out=oo[ei][:], in0=o2ap[:, 0:128], scalar=E[:, 2:3], in1=o1[ei][:],
                                                                                     op0=ALU.mult, op1=ALU.add), [k2, "ev2" + sfx, "o1" + sfx], ["oo" + sfx])
                p.add("dve", lambda e, E=E, ei=ei: e.scalar_tensor_tensor(out=junk[:], in0=oo[ei][:], scalar=1.0, in1=oo[ei][:], op0=ALU.mult, op1=ALU.mult,
                                                                          accum_out=E[:, 3:4]), ["oo" + sfx], ["junk", "ev3" + sfx])
                p.add("dve", lambda e, E=E: e.tensor_scalar(out=E[:, 4:5], in0=E[:, 3:4], scalar1=1.0 / 128.0, scalar2=EPS, op0=ALU.mult, op1=ALU.add),
                      ["ev3" + sfx], ["ev4" + sfx])
                p.add("act", lambda e, E=E: e.activation(out=E[:, 5:6], in_=E[:, 4:5], func=AF.Sqrt), ["ev4" + sfx], ["ev5" + sfx])
                p.add("dve", lambda e, E=E: e.reciprocal(out=E[:, 6:7], in_=E[:, 5:6]), ["ev5" + sfx], ["ev6" + sfx])
                p.add("dve", lambda e, E=E, ei=ei: e.scalar_tensor_tensor(out=yy[ei][:], in0=oo[ei][:], scalar=E[:, 6:7], in1=gsc[:], op0=ALU.mult, op1=ALU.mult),
                      ["oo" + sfx, "ev6" + sfx, "gsc"], ["yy" + sfx])
                r0 = (q0 + j) * 128
                p.dma(mixo[r0:r0 + 128, 512 + h * 128:512 + (h + 1) * 128], yy[ei][:], reads=["yy" + sfx])


def _ph_C1(p, g, T, l):
    projT, co = T["projT"], T["convT"]
    ones = p.sb([128, 128], name="ones_sb"); p.dma(ones[:], T["ones"], writes=["ones"])
    cw = p.sb([128, 2, 31], name="cw_sb"); p.dma(cw[:], T["cw"][l], writes=["cw"])
    cp = p.sb([128, 2, 3], name="cp_sb"); p.dma(cp[:], T["cp"][l], writes=["cp"])
    N = 512
    a1 = [p.sb([128, N + 30], name=f"a1_{i}") for i in range(2)]
    a2 = [p.sb([128, N + 30], name=f"a2_{i}") for i in range(2)]
    u = [p.sb([128, N + 30], name=f"u_{i}") for i in range(2)]
    acc = [[p.sb([128, N], name=f"acc_{i}_{r}") for r in range(2)] for i in range(2)]
    y = [p.sb([128, N], name=f"y_{i}") for i in range(2)]
    ysq = [p.sb([128, N], name=f"ysq_{i}") for i in range(2)]
    mean = p.sb([128, N], name="mean"); msq = p.sb([128, N], name="msq"); rstd = p.sb([128, N], name="rstd")
    zz = [p.sb([128, N], name=f"zz_{i}") for i in range(2)]
    bA = p.ps([128, 512], name="bankA"); bB = p.ps([128, 512], name="bankB")
    for (s0, ns) in ((0, g.CT), (g.CT, g.S)):
        for t0 in range(0, ns, N):
            n = min(N, ns - t0)
            lo = max(t0 - 15, 0); hi = min(t0 + n + 15, ns)
            d0 = lo - (t0 - 15)
            clip = (lo != t0 - 15) or (hi != t0 + n + 15)
            for j in range(2):
                for (buf, nm, rb) in ((a1[j], f"a1_{j}", j * 128), (a2[j], f"a2_{j}", 256 + j * 128)):
                    if clip:
                        p.add("dve", lambda e, buf=buf, n=n: e.memset(buf[:, 0:n + 30], 0.0), [], [nm])
                    p.dma(buf[:, d0:d0 + hi - lo], projT[rb:rb + 128, s0 + lo:s0 + hi], writes=[nm])
                p.add("act", lambda e, j=j, n=n: e.activation(out=a2[j][:, 0:n + 30], in_=a2[j][:, 0:n + 30], func=AF.Sigmoid), [f"a2_{j}"], [f"a2_{j}"])
                p.add("dve", lambda e, j=j, n=n: e.tensor_tensor(out=u[j][:, 0:n + 30], in0=a1[j][:, 0:n + 30], in1=a2[j][:, 0:n + 30], op=ALU.mult),
                      [f"a1_{j}", f"a2_{j}"], [f"u_{j}"])
                p.add("dve", lambda e, j=j, n=n: e.tensor_scalar(out=acc[j][0][:, 0:n], in0=u[j][:, 0:n], scalar1=cw[:, j, 0:1], scalar2=None, op0=ALU.mult),
                      [f"u_{j}", "cw"], [f"acc_{j}_0"])
                for k in range(1, 31):
                    src, dst = acc[j][(k - 1) % 2], acc[j][k % 2]
                    p.add("dve", lambda e, j=j, n=n, k=k, src=src, dst=dst: e.scalar_tensor_tensor(
                        out=dst[:, 0:n], in0=u[j][:, k:k + n], scalar=cw[:, j, k:k + 1], in1=src[:, 0:n], op0=ALU.mult, op1=ALU.add),
                        [f"u_{j}", "cw", f"acc_{j}_{(k-1)%2}"], [f"acc_{j}_{k%2}"])
                p.add("dve", lambda e, j=j, n=n: e.tensor_scalar(out=y[j][:, 0:n], in0=acc[j][0][:, 0:n], scalar1=cp[:, j, 0:1], scalar2=None, op0=ALU.add),
                      [f"acc_{j}_0", "cp"], [f"y_{j}"])
                p.add("act", lambda e, j=j, n=n: e.activation(out=ysq[j][:, 0:n], in_=y[j][:, 0:n], func=AF.Square), [f"y_{j}"], [f"ysq_{j}"])
            for j in range(2):
                p.add("pe", lambda e, j=j, n=n: e.matmul(bA[:, 0:n], lhsT=ones[:], rhs=y[j][:, 0:n], start=(j == 0), stop=(j == 1)), ["ones", f"y_{j}"], ["bankA"])
            for j in range(2):
                p.add("pe", lambda e, j=j, n=n: e.matmul(bB[:, 0:n], lhsT=ones[:], rhs=ysq[j][:, 0:n], start=(j == 0), stop=(j == 1)), ["ones", f"ysq_{j}"], ["bankB"])
            p.add("act", lambda e, n=n: e.activation(out=mean[:, 0:n], in_=bA[:, 0:n], func=AF.Copy, scale=1.0 / 256), ["bankA"], ["mean"])
            p.add("act", lambda e, n=n: e.activation(out=msq[:, 0:n], in_=bA[:, 0:n], func=AF.Square, scale=1.0 / 256), ["bankA"], ["msq"])
            p.add("dve", lambda e, n=n: e.scalar_tensor_tensor(out=rstd[:, 0:n], in0=bB[:, 0:n], scalar=1.0 / 256, in1=msq[:, 0:n], op0=ALU.mult, op1=ALU.subtract),
                  ["bankB", "msq"], ["rstd"])
            p.add("dve", lambda e, n=n: e.tensor_scalar(out=rstd[:, 0:n], in0=rstd[:, 0:n], scalar1=EPS, scalar2=None, op0=ALU.add), ["rstd"], ["rstd"])
            p.add("act", lambda e, n=n: e.activation(out=rstd[:, 0:n], in_=rstd[:, 0:n], func=AF.Sqrt), ["rstd"], ["rstd"])
            p.add("dve", lambda e, n=n: e.reciprocal(out=rstd[:, 0:n], in_=rstd[:, 0:n]), ["rstd"], ["rstd"])
            for j in range(2):
                p.add("dve", lambda e, j=j, n=n: e.tensor_tensor(out=zz[j][:, 0:n], in0=y[j][:, 0:n], in1=mean[:, 0:n], op=ALU.subtract), [f"y_{j}", "mean"], [f"zz_{j}"])
                p.add("dve", lambda e, j=j, n=n: e.tensor_tensor(out=zz[j][:, 0:n], in0=zz[j][:, 0:n], in1=rstd[:, 0:n], op=ALU.mult), [f"zz_{j}", "rstd"], [f"zz_{j}"])
                p.add("act", lambda e, j=j, n=n: e.activation(out=zz[j][:, 0:n], in_=zz[j][:, 0:n], func=AF.Silu, scale=cp[:, j, 1:2], bias=cp[:, j, 2:3]),
                      [f"zz_{j}", "cp"], [f"zz_{j}"])
                p.dma(co[j, :, s0 + t0:s0 + t0 + n], zz[j][:, 0:n], reads=[f"zz_{j}"])


def _ph_C2(p, g, T, l):
    NT = g.NKT
    xs, convT, mixo, proj = T["xs"], T["convT"], T["mixo"], T["proj"]
    wob = p.sb([128, 8, D], BF16, name="wob")
    load_cast_weight(p, wob, T["w_out"][l], D, "wob")
    idt = p.sb([128, 128], name="idt"); p.dma(idt[:], T["ident"], writes=["idt"])
    mng = p.sb([128, 64], name="mng_sb"); p.dma(mng[:], T["mng"][l], writes=["mng"])
    bc = [p.sb([128, D], name=f"bc{i}") for i in range(7)]
    mB = T["modB"]
    srcs = [T["n2gB"][l], mB[l, 0, :, 2048:3072], mB[l, 1, :, 2048:3072], mB[l, 0, :, 4096:5120], mB[l, 1, :, 4096:5120],
            mB[l, 0, :, 3072:4096], mB[l, 1, :, 3072:4096]]
    for i in range(7):
        p.dma(bc[i][:], srcs[i], writes=[f"bc{i}"])
    for i in (3, 4):
        p.add("dve", lambda e, i=i: e.scalar_tensor_tensor(out=bc[i][:], in0=bc[i][:], scalar=1.0, in1=bc[0][:], op0=ALU.add, op1=ALU.mult),
              [f"bc{i}", "bc0"], [f"bc{i}"])
    NB = 2
    mk = lambda shape, nm, dt=F32: [p.sb(shape, dt, name=f"{nm}{i}") for i in range(NB)]
    xt = mk([128, D], "xt"); hf = mk([128, 256], "hf"); hb = mk([128, 256], "hb"); mo = mk([128, 256], "mo")
    aot = mk([128, 512], "aot"); cvt = mk([128, 2, 128], "cvt"); ym = mk([128, 256], "ym"); sqm = mk([128, 256], "sqm")
    st4 = mk([128, 8], "st4"); mixT = mk([128, 8, 128], "mixT", BF16); tmp = mk([128, D], "tmp"); x1 = mk([128, D], "x1")
    h2 = mk([128, D], "h2"); ss = mk([128, 4], "ss")
    junk = p.sb([128, D], name="junk")
    bT = [p.ps([128, 512], name=f"bT{i}") for i in range(4)]
    bAcc = [p.ps([128, 512], name=f"bAcc{i}") for i in range(4)]
    for t in range(NT):
        i = t % NB
        isc = t < g.NCT
        r0 = t * 128
        g1b = bc[2] if isc else bc[1]
        gm2 = bc[4] if isc else bc[3]
        sh2 = bc[6] if isc else bc[5]
        kg1, kgm2, ksh2 = (f"bc{2 if isc else 1}", f"bc{4 if isc else 3}", f"bc{6 if isc else 5}")
        p.dma(xt[i][:], xs[r0:r0 + 128, :], writes=[f"xt{i}"])
        p.dma(hf[i][:], mixo[r0:r0 + 128, 0:256], writes=[f"hf{i}"])
        p.dma(hb[i][:], mixo[r0:r0 + 128, 256:512], writes=[f"hb{i}"])
        p.dma(mo[i][:], proj[r0:r0 + 128, 1280:1536], writes=[f"mo{i}"])
        p.dma(aot[i][:], mixo[r0:r0 + 128, 512:1024], writes=[f"aot{i}"])
        p.dma(cvt[i][:], convT[:, :, r0:r0 + 128].rearrange("j p n -> p j n"), writes=[f"cvt{i}"])
        p.add("dve", lambda e, i=i: e.tensor_tensor(out=ym[i][:], in0=hf[i][:], in1=hb[i][:], op=ALU.add), [f"hf{i}", f"hb{i}"], [f"ym{i}"])
        p.add("act", lambda e, i=i: e.activation(out=sqm[i][:], in_=ym[i][:], func=AF.Square), [f"ym{i}"], [f"sqm{i}"])
        p.add("dve", lambda e, i=i: e.tensor_reduce(out=st4[i][:, 0:4], in_=sqm[i][:].rearrange("p (h d) -> p h d", h=4), axis=AX.X, op=ALU.add),
              [f"sqm{i}"], [f"st4a{i}"])
        p.add("dve", lambda e, i=i: e.tensor_scalar(out=st4[i][:, 4:8], in0=st4[i][:, 0:4], scalar1=1.0 / 64, scalar2=EPS, op0=ALU.mult, op1=ALU.add),
              [f"st4a{i}"], [f"st4b{i}"])
        p.add("act", lambda e, i=i: e.activation(out=st4[i][:, 0:4], in_=st4[i][:, 4:8], func=AF.Sqrt), [f"st4b{i}"], [f"st4a{i}"])
        p.add("dve", lambda e, i=i: e.reciprocal(out=st4[i][:, 4:8], in_=st4[i][:, 0:4]), [f"st4a{i}"], [f"st4b{i}"])
        p.add("act", lambda e, i=i: e.activation(out=mo[i][:], in_=mo[i][:], func=AF.Sigmoid), [f"mo{i}"], [f"mo{i}"])
        ym3 = ym[i][:].rearrange("p (h d) -> p h d", h=4)
        p.add("dve", lambda e, i=i, ym3=ym3: e.tensor_tensor(out=ym3, in0=ym3, in1=st4[i][:, 4:8, None].to_broadcast([128, 4, 64]), op=ALU.mult),
              [f"ym{i}", f"st4b{i}"], [f"ym{i}"])
        p.add("dve", lambda e, i=i, ym3=ym3: e.tensor_tensor(out=ym3, in0=ym3, in1=mng[:, None, :].to_broadcast([128, 4, 64]), op=ALU.mult),
              [f"ym{i}", "mng"], [f"ym{i}"])
        p.add("dve", lambda e, i=i: e.tensor_tensor(out=ym[i][:], in0=ym[i][:], in1=mo[i][:], op=ALU.mult), [f"ym{i}", f"mo{i}"], [f"ym{i}"])
        p.add("act", lambda e, i=i: e.activation(out=mixT[i][:, 0:2, :], in_=cvt[i][:], func=AF.Copy), [f"cvt{i}"], [f"mixT{i}_c"])
        ta, tb = (2 * t) % 4, (2 * t + 1) % 4
        tsrc = [(ym[i], 0, f"ym{i}"), (ym[i], 128, f"ym{i}"), (aot[i], 0, f"aot{i}"), (aot[i], 128, f"aot{i}"),
                (aot[i], 256, f"aot{i}"), (aot[i], 384, f"aot{i}")]
        for n_, (src, c0, key) in enumerate(tsrc):
            bank = ta if n_ < 4 else tb
            col = (n_ % 4) * 128
            p.add("pe", lambda e, src=src, c0=c0, bank=bank, col=col: e.transpose(bT[bank][:, col:col + 128], src[:, c0:c0 + 128], idt[:]),
                  [key, "idt"], [f"bT{bank}"])
        p.add("act", lambda e, i=i, ta=ta: e.activation(out=mixT[i][:, 2:6, :], in_=bT[ta][:].rearrange("p (k n) -> p k n", k=4), func=AF.Copy),
              [f"bT{ta}"], [f"mixT{i}_a"])
        p.add("dve", lambda e, i=i, tb=tb: e.tensor_copy(out=mixT[i][:, 6:8, :], in_=bT[tb][:, 0:256].rearrange("p (k n) -> p k n", k=2)),
              [f"bT{tb}"], [f"mixT{i}_b"])
        for blk in range(2):
            a = (2 * t + blk) % 4
            for k in range(8):
                p.add("pe", lambda e, i=i, k=k, a=a, blk=blk: e.matmul(bAcc[a][:], lhsT=mixT[i][:, k, :], rhs=wob[:, k, blk * 512:(blk + 1) * 512],
                                                                      start=(k == 0), stop=(k == 7)),
                      [f"mixT{i}_c", f"mixT{i}_a", f"mixT{i}_b", "wob"], [f"bAcc{a}"])
            cs = slice(blk * 512, (blk + 1) * 512)
            p.add("dve", lambda e, i=i, a=a, cs=cs, g1b=g1b: e.tensor_tensor(out=tmp[i][:, cs], in0=bAcc[a][:], in1=g1b[:, cs], op=ALU.mult),
                  [f"bAcc{a}", kg1], [f"tmp{i}_{blk}"])
            p.add("dve", lambda e, i=i, cs=cs: e.tensor_tensor(out=x1[i][:, cs], in0=tmp[i][:, cs], in1=xt[i][:, cs], op=ALU.add),
                  [f"tmp{i}_{blk}", f"xt{i}"], [f"x1{i}_{blk}"])
        p.dma(T["x1"][r0:r0 + 128, :], x1[i][:], reads=[f"x1{i}_0", f"x1{i}_1"])
        p.add("act", lambda e, i=i: e.activation(out=junk[:], in_=x1[i][:], func=AF.Square, accum_out=ss[i][:, 0:1]),
              [f"x1{i}_0", f"x1{i}_1"], ["junk", f"ss0{i}"])
        p.add("dve", lambda e, i=i: e.tensor_scalar(out=ss[i][:, 1:2], in0=ss[i][:, 0:1], scalar1=1.0 / D, scalar2=EPS, op0=ALU.mult, op1=ALU.add),
              [f"ss0{i}"], [f"ss1{i}"])
        p.add("act", lambda e, i=i: e.activation(out=ss[i][:, 2:3], in_=ss[i][:, 1:2], func=AF.Sqrt), [f"ss1{i}"], [f"ss2{i}"])
        p.add("dve", lambda e, i=i: e.reciprocal(out=ss[i][:, 3:4], in_=ss[i][:, 2:3]), [f"ss2{i}"], [f"ss3{i}"])
        p.add("dve", lambda e, i=i, gm2=gm2: e.scalar_tensor_tensor(out=h2[i][:], in0=x1[i][:], scalar=ss[i][:, 3:4], in1=gm2[:], op0=ALU.mult, op1=ALU.mult),
              [f"x1{i}_0", f"x1{i}_1", f"ss3{i}", kgm2], [f"h2{i}"])
        p.add("dve", lambda e, i=i, sh2=sh2: e.tensor_tensor(out=h2[i][:], in0=h2[i][:], in1=sh2[:], op=ALU.add), [f"h2{i}", ksh2], [f"h2{i}"])
        p.dma(T["h2"][r0:r0 + 128, :], h2[i][:], reads=[f"h2{i}"])


def _ph_CV(p, g, T, l, RC=4):
    NE = g.NE
    RPP = NE // 128
    nst = 0
    stf = [p.sb([128, RC, D], name=f"cvf{i}") for i in range(2)]
    stb = [p.sb([128, RC, D], BF16, name=f"cvb{i}") for i in range(2)]
    for src, dst in ((T["peer_u"][l], T["pub"]), (T["peer_v"][l], T["pvb"])):
        sv = src.rearrange("(p r) d -> p r d", r=RPP)
        dv = dst.rearrange("(p r) d -> p r d", r=RPP)
        for r0 in range(0, RPP, RC):
            i = nst % 2; nst += 1
            p.dma(stf[i][:], sv[:, r0:r0 + RC, :], writes=[f"cvf{i}"])
            if i == 0:
                p.add("act", lambda e, i=i: e.activation(out=stb[i][:], in_=stf[i][:], func=AF.Copy), [f"cvf{i}"], [f"cvb{i}"])
            else:
                p.add("dve", lambda e, i=i: e.tensor_copy(out=stb[i][:], in_=stf[i][:]), [f"cvf{i}"], [f"cvb{i}"])
            p.dma(dv[:, r0:r0 + RC, :], stb[i][:], reads=[f"cvb{i}"])


def _ph_C3(p, g, T, l, NGB=8):
    NT = g.NKT
    NE = g.NE
    pu, pv = T["pub"], T["pvb"]
    wqb = p.sb([128, 8, D], F32, name="wqb")
    p.dma(wqb[:], T["wq"][l].rearrange("(k p) n -> p k n", p=128), writes=["wqb"])
    idt = p.sb([128, 128], name="idt"); p.dma(idt[:], T["ident"], writes=["idt"])
    kbd = p.sb([128, 8, 256], name="kbd_sb"); p.dma(kbd[:], T["kbd"][l], writes=["kbd"])
    g2b = [p.sb([128, D], name=f"g2b{i}") for i in range(2)]
    for i in range(2):
        p.dma(g2b[i][:], T["modB"][l, i, :, 5120:6144], writes=[f"g2b{i}"])
    h2t = p.sb([128, D], name="h2t"); x1t = p.sb([128, D], name="x1t")
    h2T = p.sb([128, 8, 128], F32, name="h2T")
    qTf = p.sb([128, 8, 128], name="qTf")
    sc = p.sb([128, 16, 128], name="sc"); wk = p.sb([128, 16, 128], name="wk")
    st = p.sb([128, 16, 16], name="st"); it = p.sb([128, 16, 16], U32, name="it"); itf = p.sb([128, 16, 16], name="itf")
    cand = p.sb([128, 8, 256], name="cand"); cidx = p.sb([128, 8, 256], name="cidx"); wk2 = p.sb([128, 8, 256], name="wk2")
    best = p.sb([128, 8, 16], name="best"); gw = p.sb([128, 8, 16], name="gw")
    eidf = p.sb([128, 128], name="eidf"); eidi = p.sb([128, 128], I32, name="eidi")
    sm = p.sb([128, 16], name="sm")
    act = p.sb([128, 128], name="act_sb"); coef = p.sb([128, 128], name="coef")
    junk = p.sb([128, D], name="junk")
    acc = p.sb([128, D], name="acc"); x2t = p.sb([128, D], name="x2t")
    rows = [p.sb([128, D], BF16, name=f"rows{i}") for i in range(NGB)]
    banks = [p.ps([128, 512], name=f"bank{i}") for i in range(8)]
    ngat = 0
    for t in range(NT):
        isc = t < g.NCT
        r0 = t * 128
        gb = g2b[1] if isc else g2b[0]
        kgb = "g2b1" if isc else "g2b0"
        p.dma(h2t[:], T["h2"][r0:r0 + 128, :], writes=["h2t"])
        p.dma(x1t[:], T["x1"][r0:r0 + 128, :], writes=["x1t"])
        for half in range(2):
            for k in range(half * 4, half * 4 + 4):
                p.add("pe", lambda e, k=k, half=half: e.transpose(banks[half][:, (k % 4) * 128:(k % 4 + 1) * 128], h2t[:, k * 128:(k + 1) * 128], idt[:]),
                      ["h2t", "idt"], [f"bank{half}"])
        p.add("act", lambda e: e.activation(out=h2T[:, 0:4, :], in_=banks[0][:].rearrange("p (k n) -> p k n", k=4), func=AF.Copy), ["bank0"], ["h2T_a"])
        p.add("dve", lambda e: e.tensor_copy(out=h2T[:, 4:8, :], in_=banks[1][:].rearrange("p (k n) -> p k n", k=4)), ["bank1"], ["h2T_b"])
        for c in range(8):
            bk = 2 + c // 4
            for k in range(8):
                p.add("pe", lambda e, c=c, k=k, bk=bk: e.matmul(banks[bk][:, (c % 4) * 128:(c % 4 + 1) * 128], lhsT=wqb[:, k, c * 128:(c + 1) * 128],
                                                              rhs=h2T[:, k, :], start=(k == 0), stop=(k == 7)), ["wqb", "h2T_a", "h2T_b"], [f"bank{bk}"])
        p.add("act", lambda e: e.activation(out=qTf[:, 0:4, :], in_=banks[2][:].rearrange("p (k n) -> p k n", k=4), func=AF.Copy), ["bank2"], ["qTf_a"])
        p.add("dve", lambda e: e.tensor_copy(out=qTf[:, 4:8, :], in_=banks[3][:].rearrange("p (k n) -> p k n", k=4)), ["bank3"], ["qTf_b"])
        for h in range(8):
            bk = 4 + h // 2
            p.add("pe", lambda e, h=h, bk=bk: e.matmul(banks[bk][:, (h % 2) * 256:(h % 2 + 1) * 256], lhsT=qTf[:, h, :], rhs=kbd[:, h, :],
                                                      start=True, stop=True), ["qTf_a", "qTf_b", "kbd"], [f"bank{bk}"])
        for bk in range(4, 8):
            g0 = (bk - 4) * 4
            if bk % 2 == 0:
                p.add("act", lambda e, bk=bk, g0=g0: e.activation(out=sc[:, g0:g0 + 4, :], in_=banks[bk][:].rearrange("p (g n) -> p g n", g=4), func=AF.Copy),
                      [f"bank{bk}"], [f"sc{bk}"])
            else:
                p.add("dve", lambda e, bk=bk, g0=g0: e.tensor_copy(out=sc[:, g0:g0 + 4, :], in_=banks[bk][:].rearrange("p (g n) -> p g n", g=4)),
                      [f"bank{bk}"], [f"sc{bk}"])
        for gq in range(16):
            ks = f"sc{4 + gq // 4}"
            p.add("dve", lambda e, gq=gq: e.max(out=st[:, gq, 0:8], in_=sc[:, gq, :]), [ks], [f"st{gq}a"])
            p.add("dve", lambda e, gq=gq: e.max_index(out=it[:, gq, 0:8], in_max=st[:, gq, 0:8], in_values=sc[:, gq, :]), [ks, f"st{gq}a"], [f"it{gq}a"])
            p.add("dve", lambda e, gq=gq: e.match_replace(out=wk[:, gq, :], in_to_replace=st[:, gq, 0:8], in_values=sc[:, gq, :], imm_value=-1e30),
                  [ks, f"st{gq}a"], [f"wk{gq}"])
            p.add("dve", lambda e, gq=gq: e.max(out=st[:, gq, 8:16], in_=wk[:, gq, :]), [f"wk{gq}"], [f"st{gq}b"])
            p.add("dve", lambda e, gq=gq: e.max_index(out=it[:, gq, 8:16], in_max=st[:, gq, 8:16], in_values=wk[:, gq, :]), [f"wk{gq}", f"st{gq}b"], [f"it{gq}b"])
        allst = [f"st{gq}{x}" for gq in range(16) for x in "ab"]
        allit = [f"it{gq}{x}" for gq in range(16) for x in "ab"]
        p.add("dve", lambda e: e.tensor_copy(out=itf[:], in_=it[:]), allit, ["itf"])
        st4 = st[:].rearrange("p (h two) k -> p h two k", two=2)
        itf4 = itf[:].rearrange("p (h two) k -> p h two k", two=2)
        cand4 = cand[:].rearrange("p h (i j) -> p h i j", i=16)
        cidx4 = cidx[:].rearrange("p h (i j) -> p h i j", i=16)
        for h in range(8):
            p.add("dve", lambda e, h=h: e.tensor_tensor(out=cand4[:, h], in0=st4[:, h, 0, :, None].to_broadcast([128, 16, 16]),
                                                        in1=st4[:, h, 1, None, :].to_broadcast([128, 16, 16]), op=ALU.add), allst, [f"cand{h}"])
            p.add("dve", lambda e, h=h: e.scalar_tensor_tensor(out=cidx4[:, h], in0=itf4[:, h, 0, :, None].to_broadcast([128, 16, 16]), scalar=128.0,
                                                               in1=itf4[:, h, 1, None, :].to_broadcast([128, 16, 16]), op0=ALU.mult, op1=ALU.add),
                  ["itf"], [f"cidx{h}"])
            p.add("dve", lambda e, h=h: e.max(out=best[:, h, 0:8], in_=cand[:, h, :]), [f"cand{h}"], [f"best{h}a"])
            p.add("dve", lambda e, h=h: e.match_replace(out=wk2[:, h, :], in_to_replace=best[:, h, 0:8], in_values=cand[:, h, :], imm_value=-1e30),
                  [f"cand{h}", f"best{h}a"], [f"wk2{h}"])
            p.add("dve", lambda e, h=h: e.max(out=best[:, h, 8:16], in_=wk2[:, h, :]), [f"wk2{h}"], [f"best{h}b"])
            for k in range(16):
                hk = h * 16 + k
                p.add("dve", lambda e, h=h, k=k, hk=hk: e.scalar_tensor_tensor(
                    out=junk[:, 0:256], in0=cand[:, h, :], scalar=best[:, h, k:k + 1], in1=cidx[:, h, :], op0=ALU.is_equal, op1=ALU.mult,
                    accum_out=eidf[:, hk:hk + 1]), [f"cand{h}", f"cidx{h}", f"best{h}a", f"best{h}b"], ["junk", f"eidf{h}"])
        alle = [f"eidf{h}" for h in range(8)]
        allb = [f"best{h}{x}" for h in range(8) for x in "ab"]
        p.add("dve", lambda e: e.tensor_scalar(out=eidf[:], in0=eidf[:], scalar1=float(NE - 1), scalar2=0.0, op0=ALU.min, op1=ALU.max), alle, ["eidf"])
        p.add("dve", lambda e: e.tensor_copy(out=eidi[:], in_=eidf[:]), ["eidf"], ["eidi"])
        p.add("dve", lambda e: e.tensor_tensor(out=gw[:], in0=best[:], in1=best[:, :, 0:1].to_broadcast([128, 8, 16]), op=ALU.subtract), allb, ["gw"])
        p.add("act", lambda e: e.activation(out=gw[:], in_=gw[:], func=AF.Exp), ["gw"], ["gw"])
        p.add("dve", lambda e: e.tensor_reduce(out=sm[:, 0:8], in_=gw[:], axis=AX.X, op=ALU.add), ["gw"], ["sm0"])
        p.add("dve", lambda e: e.reciprocal(out=sm[:, 8:16], in_=sm[:, 0:8]), ["sm0"], ["sm1"])
        p.add("dve", lambda e: e.tensor_tensor(out=gw[:], in0=gw[:], in1=sm[:, 8:16, None].to_broadcast([128, 8, 16]), op=ALU.mult), ["gw", "sm1"], ["gw"])
        for hk in range(128):
            b = ngat % NGB; ngat += 1
            p.add("pool", lambda e, b=b, hk=hk: e.indirect_dma_start(
                out=rows[b][:], out_offset=None, in_=pu, in_offset=bass.IndirectOffsetOnAxis(ap=eidi[:, hk:hk + 1], axis=0)), ["eidi"], [f"rows{b}"])
            p.add("dve", lambda e, b=b, hk=hk: e.scalar_tensor_tensor(out=junk[:], in0=rows[b][:], scalar=1.0, in1=h2t[:], op0=ALU.mult, op1=ALU.mult,
                                                                     accum_out=act[:, hk:hk + 1]), [f"rows{b}", "h2t"], ["junk", "act"])
        p.add("act", lambda e: e.activation(out=coef[:], in_=act[:], func=AF.Gelu), ["act"], ["coef"])
        p.add("dve", lambda e: e.tensor_tensor(out=coef[:], in0=coef[:], in1=gw[:].rearrange("p h k -> p (h k)"), op=ALU.mult), ["coef", "gw"], ["coef"])
        p.add("dve", lambda e: e.memset(acc[:], 0.0), [], ["acc"])
        for hk in range(128):
            b = ngat % NGB; ngat += 1
            p.add("pool", lambda e, b=b, hk=hk: e.indirect_dma_start(
                out=rows[b][:], out_offset=None, in_=pv, in_offset=bass.IndirectOffsetOnAxis(ap=eidi[:, hk:hk + 1], axis=0)), ["eidi"], [f"rows{b}"])
            p.add("dve", lambda e, b=b, hk=hk: e.scalar_tensor_tensor(out=acc[:], in0=rows[b][:], scalar=coef[:, hk:hk + 1], in1=acc[:],
                                                                     op0=ALU.mult, op1=ALU.add), [f"rows{b}", "coef", "acc"], ["acc"])
        p.add("dve", lambda e, gb=gb: e.tensor_tensor(out=x2t[:], in0=acc[:], in1=gb[:], op=ALU.mult), ["acc", kgb], ["x2t"])
        p.add("dve", lambda e: e.tensor_tensor(out=x2t[:], in0=x2t[:], in1=x1t[:], op=ALU.add), ["x2t", "x1t"], ["x2t"])
        p.dma(T["xs"][r0:r0 + 128, :], x2t[:], reads=["x2t"])


def _ph_F(p, g, T):
    fg = p.sb([128, D], name="fg_sb"); p.dma(fg[:], T["fg"], writes=["fg"])
    junk = p.sb([128, D], name="junk")
    NB = 2
    xt = [p.sb([128, D], name=f"xt{i}") for i in range(NB)]
    yt = [p.sb([128, D], name=f"yt{i}") for i in range(NB)]
    ss = [p.sb([128, 4], name=f"ss{i}") for i in range(NB)]
    for t in range(g.S // 128):
        i = t % NB
        r0 = t * 128
        p.dma(xt[i][:], T["xs"][g.CT + r0:g.CT + r0 + 128, :], writes=[f"xt{i}"])
        p.add("act", lambda e, i=i: e.activation(out=junk[:], in_=xt[i][:], func=AF.Square, accum_out=ss[i][:, 0:1]), [f"xt{i}"], ["junk", f"ss0{i}"])
        p.add("dve", lambda e, i=i: e.tensor_scalar(out=ss[i][:, 1:2], in0=ss[i][:, 0:1], scalar1=1.0 / D, scalar2=EPS, op0=ALU.mult, op1=ALU.add),
              [f"ss0{i}"], [f"ss1{i}"])
        p.add("act", lambda e, i=i: e.activation(out=ss[i][:, 2:3], in_=ss[i][:, 1:2], func=AF.Sqrt), [f"ss1{i}"], [f"ss2{i}"])
        p.add("dve", lambda e, i=i: e.reciprocal(out=ss[i][:, 3:4], in_=ss[i][:, 2:3]), [f"ss2{i}"], [f"ss3{i}"])
        p.add("dve", lambda e, i=i: e.scalar_tensor_tensor(out=yt[i][:], in0=xt[i][:], scalar=ss[i][:, 3:4], in1=fg[:], op0=ALU.mult, op1=ALU.mult),
              [f"xt{i}", f"ss3{i}", "fg"], [f"yt{i}"])
        p.dma(T["y"][r0:r0 + 128, :], yt[i][:], reads=[f"yt{i}"])


def build_fused(S, CT, DEPTH):
    g = Cfg(S, CT, DEPTH)
    nc = bass.Bass("TRN2", target_bir_lowering=False)
    NTOK = g.NTOK
    T = {}
    ins = {"xs0": [NTOK, D], "cT": [128, 8, 2], "ada_w": [DEPTH, D, 6144], "ada_bB": [DEPTH, 128, 6144], "ada_bT": [DEPTH, 128, 48],
           "n1g": [DEPTH, 128, 8], "n2gB": [DEPTH, 128, D], "w_in": [DEPTH, D, IN_W], "cs": [NTOK, 64],
           "cw": [DEPTH, 128, 2, 31], "cp": [DEPTH, 128, 2, 3], "mgb": [DEPTH, 8, 128, 2], "mng": [DEPTH, 128, 64],
           "dl": [DEPTH, 128, 256], "dng": [DEPTH, 128, 128], "lami": [DEPTH, 128, 2],
           "w_out": [DEPTH, D, D], "wq": [DEPTH, D, D], "kbd": [DEPTH, 128, 8, 256],
           "fg": [128, D],
           "ident": [128, 128], "ones": [128, 128], "tri": [128, 128], "triL": [128, 128]}
    for k, shp in ins.items():
        T[k] = _din(nc, k, shp)
    T["peer_u"] = [_din(nc, f"peer_u{l}", [g.NE, D]) for l in range(DEPTH)]
    T["peer_v"] = [_din(nc, f"peer_v{l}", [g.NE, D]) for l in range(DEPTH)]
    T["y"] = _dout(nc, "y", [S, D])
    scr = {"xs": [NTOK, D], "proj": [NTOK, IN_W], "projT": [2048, NTOK], "mixo": [NTOK, D], "convT": [2, 128, NTOK],
           "x1": [NTOK, D], "h2": [NTOK, D], "modT": [DEPTH, 128, 48, 2], "modB": [DEPTH, 2, 128, 6144]}
    for k, shp in scr.items():
        T[k] = nc.dram_tensor("scr_" + k, shp, F32, kind="Internal").ap()
    T["pub"] = nc.dram_tensor("scr_pub", [g.NE, D], BF16, kind="Internal").ap()
    T["pvb"] = nc.dram_tensor("scr_pvb", [g.NE, D], BF16, kind="Internal").ap()
    p = Prog(nc)
    CH = 2048
    for r0 in range(0, NTOK, CH):
        r1 = min(NTOK, r0 + CH)
        p.dma(T["xs"][r0:r1, :], T["xs0"][r0:r1, :])
    _ph_P0(p, g, T)
    p.end_phase()
    for l in range(DEPTH):
        for ph in (_ph_A, _ph_M1, _ph_M2, _ph_C1, _ph_C2, _ph_CV, _ph_C3):
            p.begin_phase()
            ph(p, g, T, l)
            p.end_phase()
    p.begin_phase()
    _ph_F(p, g, T)
    p.emit()
    return nc


def fused_inputs(b, x, c, ctx, c_ctx, ada_w, ada_b, norm1_g, norm2_g, w_in, conv_w, conv_b, conv_ln_g, conv_ln_b,
                 mlstm_gate_b, mlstm_norm_g, diff_lambda, diff_norm_g, w_out, peer_wq, peer_keys, peer_u, peer_v, final_g, shared=None):
    f32 = np.float32
    DEPTH = ada_w.shape[0]
    S, CT = x.shape[1], ctx.shape[1]
    if shared is None:
        shared = {}
    if not shared:
        cos, sin = _rope_tables(S)
        cs = np.zeros((CT + S, 64), f32); cs[:CT, :32] = 1.0; cs[CT:, :32] = cos; cs[CT:, 32:] = sin
        shared["cs"] = cs
        shared["ada_w"] = np.asarray(ada_w, f32)
        ab = np.asarray(ada_b, f32)
        shared["ada_bB"] = np.ascontiguousarray(np.broadcast_to(ab[:, None, :], (DEPTH, 128, 6144)))
        shared["ada_bT"] = np.ascontiguousarray(ab.reshape(DEPTH, 48, 128).transpose(0, 2, 1))
        shared["n1g"] = np.ascontiguousarray(np.asarray(norm1_g, f32).reshape(DEPTH, 8, 128).transpose(0, 2, 1))
        shared["n2gB"] = np.ascontiguousarray(np.broadcast_to(np.asarray(norm2_g, f32)[:, None, :], (DEPTH, 128, D)))
        shared["w_in"] = np.asarray(w_in, f32)
        shared["cw"] = np.ascontiguousarray(np.asarray(conv_w, f32)[:, :, 0, :].transpose(0, 2, 1).reshape(DEPTH, 2, 128, 31).transpose(0, 2, 1, 3))
        cp = np.stack([np.asarray(conv_b, f32), np.asarray(conv_ln_g, f32), np.asarray(conv_ln_b, f32)], -1)
        shared["cp"] = np.ascontiguousarray(cp.reshape(DEPTH, 2, 128, 3).transpose(0, 2, 1, 3))
        gbv = np.asarray(mlstm_gate_b, f32)
        mgb = np.zeros((DEPTH, 8, 128, 2), f32)
        for h in range(4):
            for d in range(2):
                mgb[:, h * 2 + d, :, 0] = gbv[:, 2 * d, h][:, None]
                mgb[:, h * 2 + d, :, 1] = gbv[:, 2 * d + 1, h][:, None]
        shared["mgb"] = mgb
        shared["mng"] = np.ascontiguousarray(np.broadcast_to(np.asarray(mlstm_norm_g, f32)[:, None, :], (DEPTH, 128, 64)))
        shared["dl"] = np.ascontiguousarray(np.broadcast_to(np.asarray(diff_lambda, f32).reshape(DEPTH, 1, 256), (DEPTH, 128, 256)))
        shared["dng"] = np.ascontiguousarray(np.broadcast_to(np.asarray(diff_norm_g, f32)[:, None, :], (DEPTH, 128, 128)))
        lami = np.zeros((DEPTH, 128, 2), f32)
        for l in range(DEPTH):
            li = 0.8 - 0.6 * math.exp(-0.3 * l)
            lami[l, :, 0] = li; lami[l, :, 1] = 1.0 - li
        shared["lami"] = lami
        shared["w_out"] = np.asarray(w_out, f32)
        shared["wq"] = np.asarray(peer_wq, f32)
        keys = np.asarray(peer_keys, f32)
        kbd = np.zeros((DEPTH, 128, 8, 256), f32)
        for pp in range(2):
            kbd[:, pp * 64:(pp + 1) * 64, :, pp * 128:(pp + 1) * 128] = keys[:, :, pp].transpose(0, 3, 1, 2)
        shared["kbd"] = kbd
        for l in range(DEPTH):
            shared[f"peer_u{l}"] = np.ascontiguousarray(np.asarray(peer_u[l], f32))
            shared[f"peer_v{l}"] = np.ascontiguousarray(np.asarray(peer_v[l], f32))
        shared["fg"] = _rep(np.asarray(final_g, f32))
        shared["ident"] = np.eye(128, dtype=f32)
        shared["ones"] = np.ones((128, 128), f32)
        shared["tri"] = np.triu(np.ones((128, 128), f32))
        shared["triL"] = np.tril(np.ones((128, 128), f32))
    m = dict(shared)
    m["xs0"] = np.ascontiguousarray(np.concatenate([np.asarray(ctx[b], f32), np.asarray(x[b], f32)], 0))
    cT = np.stack([np.asarray(c[b], f32), np.asarray(c_ctx, f32)], -1)
    m["cT"] = np.ascontiguousarray(cT.reshape(8, 128, 2).transpose(1, 0, 2))
    return m


_FUSED = {}


def kernel(**inp):
    x = np.asarray(inp["x"], np.float32)
    B, S, _ = x.shape
    CT = inp["ctx"].shape[1]
    DEPTH = inp["ada_w"].shape[0]
    key = (S, CT, DEPTH)
    if key not in _FUSED:
        _FUSED[key] = build_fused(S, CT, DEPTH)
    nc = _FUSED[key]
    shared = {}
    per_b = [fused_inputs(b, shared=shared, **inp) for b in range(B)]
    maps = [per_b[(cc // 2) % B] for cc in range(NCORES)]
    res = run_bass_kernel_spmd(nc, maps, core_ids=list(range(NCORES))).results
    out = np.zeros((B, S, D), np.float32)
    for b in range(B):
        out[b] = res[2 * b]["y"]
    return out
```

```python
import math
from contextlib import ExitStack
import numpy as np
import ml_dtypes
import concourse.bass as bass
import concourse.mybir as mybir
from concourse.bass_utils import run_bass_kernel_spmd

F32 = mybir.dt.float32
BF16 = mybir.dt.bfloat16
I32 = mybir.dt.int32
U32 = mybir.dt.uint32
AF = mybir.ActivationFunctionType
ALU = mybir.AluOpType
AX = mybir.AxisListType

D = 1024
IN_W = 3088
EPS = 1e-6
R_DMA = 8
R_SLOTS = {"sp": 8, "pool": 8}
NCORES = 8


class Prog:
    ENGS = ("pe", "act", "dve", "pool", "sp")

    def __init__(self, nc):
        self.nc = nc
        self.gs = ExitStack()
        self.sems = {}
        for e in ("pe", "act", "dve"):
            self.sems[e] = self.gs.enter_context(nc.semaphore(f"s_{e}"))
        for e in ("sp", "pool"):
            self.sems[e] = [self.gs.enter_context(nc.semaphore(f"s_{e}{i}")) for i in range(R_SLOTS[e])]
        self.cnt = {e: 0 for e in self.ENGS}
        self.waited = {e: {} for e in self.ENGS}
        self.nalloc = 0
        self.phase_open = False
        self.begin_phase()

    def begin_phase(self):
        assert not self.phase_open
        self.phase_open = True
        self.es = ExitStack()
        self.ops = {e: [] for e in self.ENGS}
        self.last_w = {}
        self.readers = {}
        self.bar = dict(self.cnt)

    def sb(self, shape, dtype=F32, name=None):
        self.nalloc += 1
        name = f"{name or 'sb'}_{self.nalloc}"
        return self.es.enter_context(self.nc.sbuf_tensor(name, list(shape), dtype))

    def ps(self, shape, dtype=F32, name=None):
        self.nalloc += 1
        name = f"{name or 'ps'}_{self.nalloc}"
        return self.es.enter_context(self.nc.psum_tensor(name, list(shape), dtype))

    def add(self, eng, fn, reads=(), writes=()):
        deps = set()
        for b in reads:
            if b in self.last_w:
                deps.add(self.last_w[b])
        for b in writes:
            if b in self.last_w:
                deps.add(self.last_w[b])
            for r in self.readers.get(b, ()):
                deps.add(r)
        self.cnt[eng] += 1
        idx = self.cnt[eng]
        me = (eng, idx)
        for b in reads:
            self.readers.setdefault(b, []).append(me)
        for b in writes:
            self.last_w[b] = me
            self.readers[b] = []
        deps.discard(me)
        self.ops[eng].append((fn, deps, idx))
        return me

    def dma(self, out, in_, reads=(), writes=(), eng="sp", **kw):
        return self.add(eng, lambda e: e.dma_start(out=out, in_=in_, **kw), reads, writes)

    def _dep_wait(self, engname, engobj, dep):
        waited = self.waited[engname]
        f, k = dep
        if f in ("sp", "pool"):
            n = k - 1
            R = R_SLOTS[f]
            sem = self.sems[f][n % R]
            val = 16 * (n // R + 1)
            key = (f, n % R)
        else:
            sem = self.sems[f]
            val = k
            key = f
        if waited.get(key, 0) < val:
            engobj.wait_ge(sem, val)
            waited[key] = val

    def _wait_all(self, engname, engobj, counts):
        for f in self.ENGS:
            tot = counts[f]
            if tot == 0:
                continue
            if f in ("sp", "pool"):
                for k in range(max(1, tot - R_SLOTS[f] + 1), tot + 1):
                    self._dep_wait(engname, engobj, (f, k))
            else:
                self._dep_wait(engname, engobj, (f, tot))

    def end_phase(self, final=False):
        assert self.phase_open
        self.phase_open = False
        nc = self.nc

        def run(engname, engobj):
            first = True
            for fn, deps, idx in self.ops[engname]:
                if first:
                    self._wait_all(engname, engobj, self.bar)
                    first = False
                for d in sorted(deps):
                    if d[0] == engname and engname == "pe":
                        continue
                    self._dep_wait(engname, engobj, d)
                if engname in ("sp", "pool"):
                    n = idx - 1
                    R = R_SLOTS[engname]
                    if n >= R:
                        self._dep_wait(engname, engobj, (engname, idx - R))
                    inst = fn(engobj)
                    inst.then_inc(self.sems[engname][n % R], 16)
                else:
                    inst = fn(engobj)
                    inst.then_inc(self.sems[engname], 1)
            if final and engname in ("sp", "pool"):
                self._wait_all(engname, engobj, {e: (self.cnt[e] if e == engname else 0) for e in self.ENGS})

        with nc.Block() as block:
            @block.tensor
            def _(e):
                run("pe", e)

            @block.scalar
            def _(e):
                run("act", e)

            @block.vector
            def _(e):
                run("dve", e)

            @block.gpsimd
            def _(e):
                run("pool", e)

            @block.sync
            def _(e):
                run("sp", e)
        self.es.close()

    def emit(self):
        self.end_phase(final=True)
        self.gs.close()


def _din(nc, name, shape, dt=F32):
    return nc.dram_tensor(name, list(shape), dt, kind="ExternalInput").ap()


def _dout(nc, name, shape, dt=F32):
    return nc.dram_tensor(name, list(shape), dt, kind="ExternalOutput").ap()


def build_P0():
    nc = bass.Bass("TRN2", target_bir_lowering=False)
    w = _din(nc, "w", [1024, 3072])
    b = _din(nc, "b", [128, 24])
    cT = _din(nc, "cT", [128, 8, 5])
    o = _dout(nc, "modT", [128, 24, 5])
    p = Prog(nc)
    ct = p.sb([128, 8, 5]); st = p.sb([128, 8, 5]); bt = p.sb([128, 24]); ot = p.sb([128, 24, 5])
    p.dma(ct[:], cT, writes=["ct"])
    p.dma(bt[:], b, writes=["bt"])
    p.add("act", lambda e: e.activation(out=st[:], in_=ct[:], func=AF.Silu), ["ct"], ["st"])
    wv = w.rearrange("(k p) n -> p k n", p=128)
    NB = 6
    wts = [p.sb([128, 8, 512], name=f"wt{i}") for i in range(2)]
    pss = [p.ps([128, 4, 8], name=f"pp{i}") for i in range(2)]
    for blk in range(NB):
        wt = wts[blk % 2]; ps = pss[blk % 2]
        p.dma(wt[:], wv[:, :, blk * 512:(blk + 1) * 512], writes=[f"wt{blk%2}"])
        for jj in range(4):
            for k in range(8):
                p.add("pe", lambda e, wt=wt, ps=ps, jj=jj, k=k: e.matmul(
                    ps[:, jj, 0:5], lhsT=wt[:, k, jj * 128:(jj + 1) * 128], rhs=st[:, k, :],
                    start=(k == 0), stop=(k == 7)), [f"wt{blk%2}", "st"], [f"pp{blk%2}"])
        for jj in range(4):
            j = blk * 4 + jj
            p.add("dve", lambda e, ps=ps, jj=jj, j=j: e.tensor_scalar(
                out=ot[:, j, :], in0=ps[:, jj, 0:5], scalar1=bt[:, j:j + 1], scalar2=None,
                op0=ALU.add), [f"pp{blk%2}", "bt"], ["ot"])
    p.dma(o, ot[:], reads=["ot"])
    p.emit()
    return nc


def load_cast_weight(p, wb, w, ncols, key, nk=8, bw=512):
    wv = w.rearrange("(k p) n -> p k n", p=128)
    stg = [p.sb([128, nk, bw], name=f"stg_{key}{i}") for i in range(2)]
    for bi, c0 in enumerate(range(0, ncols, bw)):
        cw = min(bw, ncols - c0)
        st = stg[bi % 2]
        p.dma(st[:, :, 0:cw], wv[:, :, c0:c0 + cw], writes=[f"stg_{key}{bi%2}"])
        h = nk // 2
        p.add("act", lambda e, st=st, c0=c0, cw=cw: e.activation(out=wb[:, 0:h, c0:c0 + cw], in_=st[:, 0:h, 0:cw], func=AF.Copy),
              [f"stg_{key}{bi%2}"], [f"{key}_a{bi}"])
        p.add("dve", lambda e, st=st, c0=c0, cw=cw: e.tensor_copy(out=wb[:, h:nk, c0:c0 + cw], in_=st[:, h:nk, 0:cw]),
              [f"stg_{key}{bi%2}"], [f"{key}_b{bi}"])
    p.add("act", lambda e: e.activation(out=wb[0:1, 0, 0:1], in_=wb[0:1, 0, 0:1], func=AF.Copy),
          [f"{key}_a{bi}" for bi in range((ncols + bw - 1) // bw)] + [f"{key}_b{bi}" for bi in range((ncols + bw - 1) // bw)], [key])


def build_A(NT, ctx_tiles=(0,), stage=9):
    nc = bass.Bass("TRN2", target_bir_lowering=False)
    xs = _din(nc, "xs", [NT * 128, D])
    w_in = _din(nc, "w_in", [D, IN_W])
    pv = _din(nc, "pv", [128, 8, 5])
    cs = _din(nc, "cs", [NT * 128, 64])
    ident = _din(nc, "ident", [128, 128])
    proj = _dout(nc, "proj", [NT * 128, IN_W])
    p = Prog(nc)
    wb = p.sb([128, 8, IN_W], BF16, name="wb")
    load_cast_weight(p, wb, w_in, IN_W, "wb")
    idt = p.sb([128, 128]); p.dma(idt[:], ident, writes=["idt"])
    pvt = p.sb([128, 8, 5]); p.dma(pvt[:], pv, writes=["pvt"])
    gm = p.sb([128, 8, 2])
    for i, col in enumerate((1, 3)):
        p.add("dve", lambda e, i=i, col=col: e.scalar_tensor_tensor(
            out=gm[:, :, i], in0=pvt[:, :, col], scalar=1.0, in1=pvt[:, :, 0],
            op0=ALU.add, op1=ALU.mult), ["pvt"], ["gm"])
    NB = 2
    xt = [p.sb([128, D], name=f"xt{i}") for i in range(NB)]
    xn = [p.sb([128, D], name=f"xn{i}") for i in range(NB)]
    junk = p.sb([128, D], name="junk")
    ss = [p.sb([128, 1], name=f"ss{i}") for i in range(NB)]
    rstd = [p.sb([128, 1], name=f"rstd{i}") for i in range(NB)]
    hT = [p.sb([128, 8, 128], BF16, name=f"hT{i}") for i in range(NB)]
    ot = [p.sb([128, IN_W], name=f"ot{i}") for i in range(NB)]
    ro = [p.sb([128, 1024], name=f"ro{i}") for i in range(NB)]
    tmp = [p.sb([128, 4, 512], name=f"tmp{i}") for i in range(NB)]
    cst = [p.sb([128, 64], name=f"cst{i}") for i in range(NB)]
    tp = [p.ps([128, 512], name=f"tp{i}") for i in range(4)]
    acc = [p.ps([128, 512], name=f"acc{i}") for i in range(4)]
    nacc = 0
    for t in range(NT):
        i = t % NB
        isc = t in ctx_tiles
        r0 = t * 128
        p.dma(xt[i][:], xs[r0:r0 + 128, :], writes=[f"xt{i}"])
        p.dma(cst[i][:], cs[r0:r0 + 128, :], writes=[f"cst{i}"])
        p.add("act", lambda e, i=i: e.activation(out=junk[:], in_=xt[i][:], func=AF.Square,
                                                  accum_out=ss[i][:, 0:1]), [f"xt{i}"], ["junk", f"ss{i}"])
        p.add("dve", lambda e, i=i: e.tensor_scalar(out=rstd[i][:], in0=ss[i][:], scalar1=1.0 / D, scalar2=EPS,
                                                     op0=ALU.mult, op1=ALU.add), [f"ss{i}"], [f"rstd{i}"])
        p.add("act", lambda e, i=i: e.activation(out=ss[i][:], in_=rstd[i][:], func=AF.Sqrt), [f"rstd{i}"], [f"ss{i}"])
        p.add("dve", lambda e, i=i: e.reciprocal(out=rstd[i][:], in_=ss[i][:]), [f"ss{i}"], [f"rstd{i}"])
        p.add("dve", lambda e, i=i: e.tensor_scalar(out=xn[i][:], in0=xt[i][:], scalar1=rstd[i][:, 0:1], scalar2=None,
                                                     op0=ALU.mult), [f"xt{i}", f"rstd{i}"], [f"xn{i}"])
        if stage == 1:
            p.dma(proj[r0:r0 + 128, 0:1024], xn[i][:], reads=[f"xn{i}"])
            continue
        for half in range(2):
            tpi = (2 * t + half) % 4
            for k in range(half * 4, half * 4 + 4):
                p.add("pe", lambda e, i=i, k=k, tpi=tpi: e.transpose(
                    tp[tpi][:, (k % 4) * 128:(k % 4 + 1) * 128], xn[i][:, k * 128:(k + 1) * 128], idt[:]),
                    [f"xn{i}", "idt"], [f"tp{tpi}"])
            sc_col = 1 if isc else 0
            sh_col = 4 if isc else 2
            for k in range(half * 4, half * 4 + 4):
                if half == 0:
                    p.add("act", lambda e, i=i, k=k, tpi=tpi, sc_col=sc_col, sh_col=sh_col: e.activation(
                        out=hT[i][:, k, :], in_=tp[tpi][:, (k % 4) * 128:(k % 4 + 1) * 128], func=AF.Identity,
                        scale=gm[:, k, sc_col:sc_col + 1], bias=pvt[:, k, sh_col:sh_col + 1]),
                        [f"tp{tpi}", "gm", "pvt"], [f"hT{i}_{k}"])
                else:
                    p.add("dve", lambda e, i=i, k=k, tpi=tpi, sc_col=sc_col, sh_col=sh_col: e.tensor_scalar(
                        out=hT[i][:, k, :], in0=tp[tpi][:, (k % 4) * 128:(k % 4 + 1) * 128],
                        scalar1=gm[:, k, sc_col:sc_col + 1], scalar2=pvt[:, k, sh_col:sh_col + 1],
                        op0=ALU.mult, op1=ALU.add), [f"tp{tpi}", "gm", "pvt"], [f"hT{i}_{k}"])
        if stage == 2:
            p.add("dve", lambda e, i=i: e.tensor_copy(out=ot[i][:, 0:1024], in_=hT[i][:].rearrange("p k n -> p (k n)")),
                  [f"hT{i}_{k}" for k in range(8)], [f"ot{i}_0"])
            p.dma(proj[r0:r0 + 128, 0:1024], ot[i][:, 0:1024], reads=[f"ot{i}_0"])
            continue
        for blk in range(7):
            c0 = blk * 512
            cw = min(512, IN_W - c0)
            a = nacc % 4; nacc += 1
            for k in range(8):
                p.add("pe", lambda e, i=i, k=k, a=a, c0=c0, cw=cw: e.matmul(
                    acc[a][:, 0:cw], lhsT=hT[i][:, k, :], rhs=wb[:, k, c0:c0 + cw],
                    start=(k == 0), stop=(k == 7)), [f"hT{i}_{k}", "wb"], [f"acc{a}"])
            if blk % 2 == 0:
                p.add("act", lambda e, i=i, a=a, c0=c0, cw=cw: e.activation(
                    out=ot[i][:, c0:c0 + cw], in_=acc[a][:, 0:cw], func=AF.Copy), [f"acc{a}"], [f"ot{i}_{blk}"])
            else:
                p.add("dve", lambda e, i=i, a=a, c0=c0, cw=cw: e.tensor_copy(
                    out=ot[i][:, c0:c0 + cw], in_=acc[a][:, 0:cw]), [f"acc{a}"], [f"ot{i}_{blk}"])
        if stage == 3:
            p.dma(proj[r0:r0 + 128, :], ot[i][:], reads=[f"ot{i}_{b}" for b in range(7)])
            continue
        src = ot[i][:, 1552:2576].rearrange("p (g r two) -> p g r two", g=16, two=2)
        dst = ro[i][:].rearrange("p (g r two) -> p g r two", g=16, two=2)
        t1 = src[:, :, :, 0]; t2 = src[:, :, :, 1]
        cosb = cst[i][:, None, 0:32].to_broadcast([128, 16, 32])
        sinb = cst[i][:, None, 32:64].to_broadcast([128, 16, 32])
        tm = [tmp[i][:, j, :].rearrange("p (g r) -> p g r", g=16) for j in range(4)]
        rk = [f"ot{i}_3", f"ot{i}_4", f"ot{i}_5", f"cst{i}"]
        p.add("dve", lambda e, tm=tm, t1=t1, cosb=cosb: e.tensor_tensor(out=tm[0], in0=t1, in1=cosb, op=ALU.mult), rk, [f"tmp{i}_0"])
        p.add("dve", lambda e, tm=tm, t2=t2, sinb=sinb: e.tensor_tensor(out=tm[1], in0=t2, in1=sinb, op=ALU.mult), rk, [f"tmp{i}_1"])
        p.add("dve", lambda e, tm=tm, t1=t1, sinb=sinb: e.tensor_tensor(out=tm[2], in0=t1, in1=sinb, op=ALU.mult), rk, [f"tmp{i}_2"])
        p.add("dve", lambda e, tm=tm, t2=t2, cosb=cosb: e.tensor_tensor(out=tm[3], in0=t2, in1=cosb, op=ALU.mult), rk, [f"tmp{i}_3"])
        p.add("dve", lambda e, tm=tm, dst=dst: e.tensor_tensor(out=dst[:, :, :, 0], in0=tm[0], in1=tm[1], op=ALU.subtract),
              [f"tmp{i}_0", f"tmp{i}_1"], [f"ro{i}a"])
        p.add("dve", lambda e, tm=tm, dst=dst: e.tensor_tensor(out=dst[:, :, :, 1], in0=tm[2], in1=tm[3], op=ALU.add),
              [f"tmp{i}_2", f"tmp{i}_3"], [f"ro{i}b"])
        p.dma(proj[r0:r0 + 128, 0:1552], ot[i][:, 0:1552], reads=[f"ot{i}_{b}" for b in range(4)])
        p.dma(proj[r0:r0 + 128, 1552:2576], ro[i][:], reads=[f"ro{i}a", f"ro{i}b"])
        p.dma(proj[r0:r0 + 128, 2576:IN_W], ot[i][:, 2576:IN_W], reads=[f"ot{i}_5", f"ot{i}_6"])
    p.emit()
    return nc


def build_M1(NCH, NS=4, GC=4):
    nc = bass.Bass("TRN2", target_bir_lowering=False)
    L = NCH * 128
    mqT = _din(nc, "mqT", [NS, 64, L])
    mkT = _din(nc, "mkT", [NS, 64, L])
    mvk = _din(nc, "mvk", [NS, L, 128])
    mg = _din(nc, "mg", [NS, L, 2])
    mgb = _din(nc, "mgb", [NS, 128, 2])
    tri_d = _din(nc, "tri", [128, 128])
    ones_d = _din(nc, "ones", [128, 128])
    mh = _dout(nc, "mh", [NS, L, 64])
    p = Prog(nc)
    tri = p.sb([128, 128], name="tri_sb"); p.dma(tri[:], tri_d, writes=["tri"])
    ones = p.sb([128, 128], name="ones_sb"); p.dma(ones[:], ones_d, writes=["ones"])
    banks = [p.ps([128, 512], name=f"bank{i}") for i in range(7)]
    NBUF = 2
    B = {}
    for s in range(NS):
        gb = p.sb([128, 2], name=f"gb{s}"); p.dma(gb[:], mgb[s], writes=[f"gb{s}"])
        B[s, "gb"] = gb
        B[s, "Cf"] = p.sb([64, 65], name=f"Cf{s}")
        B[s, "Cb"] = p.sb([64, 65], BF16, name=f"Cb{s}")
        B[s, "tmpC"] = p.sb([64, 65], name=f"tmpC{s}")
        p.add("dve", lambda e, s=s: e.memset(B[s, "Cf"][:], 0.0), [], [f"Cf{s}"])
        p.add("dve", lambda e, s=s: e.memset(B[s, "Cb"][:], 0.0), [], [f"Cb{s}"])
        for i in range(NBUF):
            k = (s, i)
            B[k, "qTf"] = p.sb([64, GC * 128], name=f"qTf{s}_{i}")
            B[k, "kTf"] = p.sb([64, GC * 128], name=f"kTf{s}_{i}")
            B[k, "vkf"] = p.sb([128, GC, 128], name=f"vkf{s}_{i}")
            B[k, "gf"] = p.sb([128, GC, 2], name=f"gf{s}_{i}")
            B[k, "qTb"] = p.sb([64, GC * 128], BF16, name=f"qTb{s}_{i}")
            B[k, "kTb"] = p.sb([64, GC * 128], BF16, name=f"kTb{s}_{i}")
            B[k, "vaug"] = p.sb([128, GC, 65], BF16, name=f"vaug{s}_{i}")
            B[k, "kb"] = p.sb([128, GC, 64], BF16, name=f"kb{s}_{i}")
            B[k, "h"] = p.sb([128, GC, 64], name=f"h{s}_{i}")
            B[k, "gi"] = p.sb([128, GC], name=f"gi{s}_{i}")
            B[k, "sp"] = p.sb([128, GC], name=f"sp{s}_{i}")
            B[k, "a"] = p.sb([128, GC], name=f"a{s}_{i}")
            B[k, "b"] = p.sb([128, GC], name=f"b{s}_{i}")
            B[k, "eG"] = p.sb([128, GC], name=f"eG{s}_{i}")
            B[k, "WT"] = p.sb([128, 128], BF16, name=f"WT{s}_{i}")
            B[k, "d"] = p.sb([128, 4], name=f"d{s}_{i}")
            p.add("dve", lambda e, k=k: e.memset(B[k, "vaug"][:, :, 64:65], 1.0), [], [f"vaug1_{s}_{i}"])
    ngroups = (NCH + GC - 1) // GC
    for g in range(ngroups):
        c0 = g * GC
        gc = min(GC, NCH - c0)
        i = g % NBUF
        for s in range(NS):
            k = (s, i)
            sfx = f"{s}_{i}"
            T = {n: B[k, n] for n in ("qTf", "kTf", "vkf", "gf", "qTb", "kTb", "vaug", "kb", "h", "gi", "sp", "a", "b", "eG", "WT", "d")}
            Cf, Cb, tmpC, gb = B[s, "Cf"], B[s, "Cb"], B[s, "tmpC"], B[s, "gb"]
            bS, bX, bU = banks[(s % 2) * 3], banks[(s % 2) * 3 + 1], banks[(s % 2) * 3 + 2]
            kS, kX, kU = f"bank{(s%2)*3}", f"bank{(s%2)*3+1}", f"bank{(s%2)*3+2}"
            bP = banks[6]
            W = gc * 128
            p.dma(T["qTf"][:, 0:W], mqT[s, :, c0 * 128:c0 * 128 + W], writes=[f"qTf{sfx}"])
            p.dma(T["kTf"][:, 0:W], mkT[s, :, c0 * 128:c0 * 128 + W], writes=[f"kTf{sfx}"])
            p.dma(T["vkf"][:, 0:gc, :], mvk[s].rearrange("(c p) f -> p c f", p=128)[:, c0:c0 + gc, :], writes=[f"vkf{sfx}"])
            p.dma(T["gf"][:, 0:gc, :], mg[s].rearrange("(c p) f -> p c f", p=128)[:, c0:c0 + gc, :], writes=[f"gf{sfx}"])
            p.add("dve", lambda e, T=T, gb=gb, gc=gc: e.tensor_scalar(out=T["gi"][:, 0:gc], in0=T["gf"][:, 0:gc, 0], scalar1=gb[:, 0:1],
                                                                     scalar2=None, op0=ALU.add), [f"gf{sfx}", f"gb{s}"], [f"gi{sfx}"])
            p.add("dve", lambda e, T=T, gb=gb, gc=gc: e.tensor_scalar(out=T["sp"][:, 0:gc], in0=T["gf"][:, 0:gc, 1], scalar1=gb[:, 1:2],
                                                                     scalar2=None, op0=ALU.add), [f"gf{sfx}", f"gb{s}"], [f"sp{sfx}"])
            p.add("act", lambda e, T=T, gc=gc: e.activation(out=T["sp"][:, 0:gc], in_=T["sp"][:, 0:gc], func=AF.Exp, scale=-1.0),
                  [f"sp{sfx}"], [f"sp{sfx}"])
            p.add("dve", lambda e, T=T, gc=gc: e.tensor_scalar(out=T["sp"][:, 0:gc], in0=T["sp"][:, 0:gc], scalar1=1.0, scalar2=None,
                                                              op0=ALU.add), [f"sp{sfx}"], [f"sp{sfx}"])
            p.add("act", lambda e, T=T, gc=gc: e.activation(out=T["sp"][:, 0:gc], in_=T["sp"][:, 0:gc], func=AF.Ln),
                  [f"sp{sfx}"], [f"sp{sfx}"])
            p.add("pe", lambda e, T=T, gc=gc, bP=bP: e.matmul(bP[:, 0:gc], lhsT=tri[:], rhs=T["sp"][:, 0:gc], start=True, stop=True),
                  ["tri", f"sp{sfx}"], ["bank6"])
            p.add("pe", lambda e, T=T, gc=gc, bP=bP: e.matmul(bP[:, 64:64 + gc], lhsT=ones[:], rhs=T["sp"][:, 0:gc], start=True, stop=True),
                  ["ones", f"sp{sfx}"], ["bank6"])
            p.add("act", lambda e, T=T, gc=gc, bP=bP: e.activation(out=T["a"][:, 0:gc], in_=bP[:, 0:gc], func=AF.Exp, scale=-1.0),
                  ["bank6"], [f"a{sfx}"])
            p.add("act", lambda e, T=T, gc=gc, bP=bP: e.activation(out=T["eG"][:, 0:gc], in_=bP[:, 64:64 + gc], func=AF.Exp, scale=-1.0),
                  ["bank6"], [f"eG{sfx}"])
            p.add("act", lambda e, T=T, gc=gc, bP=bP: e.activation(out=T["b"][:, 0:gc], in_=bP[:, 0:gc], func=AF.Identity),
                  ["bank6"], [f"b{sfx}"])
            p.add("dve", lambda e, T=T, gc=gc: e.tensor_tensor(out=T["b"][:, 0:gc], in0=T["b"][:, 0:gc], in1=T["gi"][:, 0:gc], op=ALU.add),
                  [f"b{sfx}", f"gi{sfx}"], [f"b{sfx}"])
            p.add("act", lambda e, T=T, gc=gc: e.activation(out=T["b"][:, 0:gc], in_=T["b"][:, 0:gc], func=AF.Exp),
                  [f"b{sfx}"], [f"b{sfx}"])
            p.add("act", lambda e, T=T, W=W: e.activation(out=T["qTb"][:, 0:W], in_=T["qTf"][:, 0:W], func=AF.Copy),
                  [f"qTf{sfx}"], [f"qTb{sfx}"])
            p.add("dve", lambda e, T=T, W=W: e.tensor_scalar(out=T["kTb"][:, 0:W], in0=T["kTf"][:, 0:W], scalar1=0.125, scalar2=None,
                                                            op0=ALU.mult), [f"kTf{sfx}"], [f"kTb{sfx}"])
            p.add("act", lambda e, T=T, gc=gc: e.activation(out=T["vaug"][:, 0:gc, 0:64], in_=T["vkf"][:, 0:gc, 0:64], func=AF.Copy),
                  [f"vkf{sfx}"], [f"vaug{sfx}"])
            p.add("dve", lambda e, T=T, gc=gc: e.scalar_tensor_tensor(
                out=T["kb"][:, 0:gc, :], in0=T["vkf"][:, 0:gc, 64:128], scalar=0.125,
                in1=T["b"][:, 0:gc, None].to_broadcast([128, gc, 64]), op0=ALU.mult, op1=ALU.mult),
                [f"vkf{sfx}", f"b{sfx}"], [f"kb{sfx}"])
            for cc in range(gc):
                cs = slice(cc * 128, (cc + 1) * 128)
                p.add("pe", lambda e, T=T, cs=cs, bS=bS: e.matmul(bS[:, 0:128], lhsT=T["kTb"][:, cs], rhs=T["qTb"][:, cs],
                                                                  start=True, stop=True), [f"kTb{sfx}", f"qTb{sfx}"], [kS])
                p.add("dve", lambda e, T=T, cc=cc, bS=bS: e.scalar_tensor_tensor(
                    out=T["WT"][:], in0=bS[:, 0:128], scalar=T["b"][:, cc:cc + 1], in1=tri[:], op0=ALU.mult, op1=ALU.mult),
                    [kS, f"b{sfx}", "tri"], [f"WT{sfx}"])
                p.add("pe", lambda e, T=T, cc=cc, bX=bX: e.matmul(bX[:, 0:65], lhsT=T["WT"][:], rhs=T["vaug"][:, cc, :],
                                                                  start=True, stop=False), [f"WT{sfx}", f"vaug{sfx}", f"vaug1_{sfx}"], [kX])
                p.add("pe", lambda e, T=T, cs=cs, bX=bX, Cb=Cb: e.matmul(bX[:, 0:65], lhsT=T["qTb"][:, cs], rhs=Cb[:],
                                                                         start=False, stop=True), [f"qTb{sfx}", f"Cb{s}"], [kX])
                p.add("pe", lambda e, T=T, cc=cc, bU=bU: e.matmul(bU[0:64, 0:65], lhsT=T["kb"][:, cc, :], rhs=T["vaug"][:, cc, :],
                                                                  start=True, stop=True), [f"kb{sfx}", f"vaug{sfx}", f"vaug1_{sfx}"], [kU])
                d = T["d"]
                p.add("act", lambda e, T=T, cc=cc, bX=bX, d=d: e.activation(
                    out=d[:, 0:1], in_=bX[:, 64:65], func=AF.Abs, scale=T["a"][:, cc:cc + 1]),
                    [kX, f"a{sfx}"], [f"d0{sfx}"])
                p.add("dve", lambda e, d=d: e.tensor_scalar(out=d[:, 3:4], in0=d[:, 0:1], scalar1=1.0, scalar2=None, op0=ALU.max),
                      [f"d0{sfx}"], [f"d3{sfx}"])
                p.add("dve", lambda e, d=d: e.reciprocal(out=d[:, 1:2], in_=d[:, 3:4]), [f"d3{sfx}"], [f"d1{sfx}"])
                p.add("dve", lambda e, T=T, cc=cc, d=d: e.tensor_tensor(out=d[:, 2:3], in0=d[:, 1:2], in1=T["a"][:, cc:cc + 1], op=ALU.mult),
                      [f"d1{sfx}", f"a{sfx}"], [f"d2{sfx}"])
                p.add("dve", lambda e, T=T, cc=cc, bX=bX, d=d: e.tensor_scalar(
                    out=T["h"][:, cc, :], in0=bX[:, 0:64], scalar1=d[:, 2:3], scalar2=None, op0=ALU.mult),
                    [kX, f"d2{sfx}"], [f"h{sfx}"])
                p.add("dve", lambda e, bU=bU, Cf=Cf, tmpC=tmpC: e.tensor_tensor(out=tmpC[:], in0=bU[0:64, 0:65], in1=Cf[:], op=ALU.add),
                      [kU, f"Cf{s}"], [f"tmpC{s}"])
                p.add("dve", lambda e, T=T, cc=cc, Cf=Cf, tmpC=tmpC: e.tensor_scalar(
                    out=Cf[:], in0=tmpC[:], scalar1=T["eG"][0:64, cc:cc + 1], scalar2=None, op0=ALU.mult),
                    [f"tmpC{s}", f"eG{sfx}"], [f"Cf{s}"])
                p.add("act", lambda e, T=T, cc=cc, Cb=Cb, tmpC=tmpC: e.activation(
                    out=Cb[:], in_=tmpC[:], func=AF.Copy, scale=T["eG"][0:64, cc:cc + 1]),
                    [f"tmpC{s}", f"eG{sfx}"], [f"Cb{s}"])
            p.dma(mh[s].rearrange("(c p) f -> p c f", p=128)[:, c0:c0 + gc, :], T["h"][:, 0:gc, :], reads=[f"h{sfx}"])
    p.emit()
    return nc


def build_M2(NKT, NCT, NSL=2):
    nc = bass.Bass("TRN2", target_bir_lowering=False)
    L = NKT * 128
    aqT = _din(nc, "aqT", [NSL, 2, 64, L])
    akT = _din(nc, "akT", [NSL, 2, 64, L])
    av = _din(nc, "av", [NSL, L, 128])
    dl_d = _din(nc, "dl", [128, 256])
    dng_d = _din(nc, "dng", [128, 128])
    lami_d = _din(nc, "lami", [128, 2])
    ones_d = _din(nc, "ones", [128, 128])
    ao = _dout(nc, "ao", [NSL, L, 128])
    p = Prog(nc)
    ones = p.sb([128, 128], name="ones_sb"); p.dma(ones[:], ones_d, writes=["ones"])
    dl = p.sb([128, 256], name="dl_sb"); p.dma(dl[:], dl_d, writes=["dl"])
    gsc = p.sb([128, 128], name="gsc"); p.dma(gsc[:], dng_d, writes=["gsc"])
    lami = p.sb([128, 2], name="lami_sb"); p.dma(lami[:], lami_d, writes=["lami"])
    junk = p.sb([128, 128], name="junk")
    lm = p.sb([128, 4], name="lm")
    p.add("dve", lambda e: e.scalar_tensor_tensor(out=junk[:, 0:64], in0=dl[:, 0:64], scalar=1.0, in1=dl[:, 64:128],
                                                  op0=ALU.mult, op1=ALU.mult, accum_out=lm[:, 0:1]), ["dl"], ["junk", "lm0"])
    p.add("dve", lambda e: e.scalar_tensor_tensor(out=junk[:, 64:128], in0=dl[:, 128:192], scalar=1.0, in1=dl[:, 192:256],
                                                  op0=ALU.mult, op1=ALU.mult, accum_out=lm[:, 1:2]), ["dl"], ["junk", "lm1"])
    p.add("act", lambda e: e.activation(out=lm[:, 0:2], in_=lm[:, 0:2], func=AF.Exp), ["lm0", "lm1"], ["lm01"])
    p.add("dve", lambda e: e.tensor_tensor(out=lm[:, 2:3], in0=lm[:, 1:2], in1=lm[:, 0:1], op=ALU.subtract), ["lm01"], ["lm2"])
    p.add("dve", lambda e: e.tensor_scalar(out=lm[:, 3:4], in0=lm[:, 2:3], scalar1=lami[:, 0:1], scalar2=None, op0=ALU.subtract),
          ["lm2", "lami"], ["nlam"])
    p.add("dve", lambda e: e.tensor_scalar(out=gsc[:], in0=gsc[:], scalar1=lami[:, 1:2], scalar2=None, op0=ALU.mult),
          ["gsc", "lami"], ["gsc"])
    banks = [p.ps([128, 512], name=f"bank{i}") for i in range(8)]
    qTb = p.sb([64, 2, L], BF16, name="qTb")
    kTb = p.sb([64, 2, L], BF16, name="kTb")
    vaug = p.sb([128, NKT, 129], BF16, name="vaug")
    p.add("dve", lambda e: e.memset(vaug[:, :, 128:129], 1.0), [], ["vaug1"])
    PW = 2048
    stg = [p.sb([64, PW], name=f"stg{i}") for i in range(2)]
    sq = p.sb([64, PW], name="sq")
    VG = 16
    vst = [p.sb([128, VG, 128], name=f"vst{i}") for i in range(2)]
    mx = p.sb([128, 8], name="mx")
    nb = p.sb([128, 4], name="nb")
    pT = [p.sb([128, 512], BF16, name=f"pT{i}") for i in range(3)]
    ev = [p.sb([128, 8], name=f"ev{i}") for i in range(2)]
    o1 = [p.sb([128, 128], name=f"o1_{i}") for i in range(2)]
    oo = [p.sb([128, 128], name=f"oo_{i}") for i in range(2)]
    yy = [p.sb([128, 128], name=f"yy_{i}") for i in range(2)]
    nstg = 0
    nvst = 0
    nit = 0
    nblk = 0
    nev = 0
    for s in range(NSL):
        p.add("dve", lambda e: e.memset(mx[:], 0.0), [], ["mx", "mx4"])
        for which, (src, dst) in enumerate(((aqT, qTb), (akT, kTb))):
            for comp in range(2):
                for c0 in range(0, L, PW):
                    cw = min(PW, L - c0)
                    si = nstg % 2; nstg += 1
                    st = stg[si]
                    p.dma(st[:, 0:cw], src[s, comp, :, c0:c0 + cw], writes=[f"stg{si}"])
                    p.add("dve", lambda e, st=st, dst=dst, comp=comp, c0=c0, cw=cw: e.tensor_copy(out=dst[:, comp, c0:c0 + cw], in_=st[:, 0:cw]),
                          [f"stg{si}"], ["qTb" if which == 0 else "kTb"])
                    p.add("act", lambda e, st=st, cw=cw: e.activation(out=sq[:, 0:cw], in_=st[:, 0:cw], func=AF.Square), [f"stg{si}"], ["sq"])
                    for b0 in range(0, cw, 512):
                        bw = min(512, cw - b0)
                        p.add("pe", lambda e, b0=b0, bw=bw: e.matmul(banks[6][:, 0:bw], lhsT=ones[0:64, :], rhs=sq[:, b0:b0 + bw], start=True, stop=True),
                              ["ones", "sq"], ["bank6"])
                        p.add("dve", lambda e, bw=bw: e.reduce_max(out=mx[:, 4:5], in_=banks[6][:, 0:bw], axis=AX.X), ["bank6"], ["mx4"])
                        col = which * 2 + comp
                        p.add("dve", lambda e, col=col: e.tensor_tensor(out=mx[:, col:col + 1], in0=mx[:, col:col + 1], in1=mx[:, 4:5], op=ALU.max),
                              ["mx4", "mx"], ["mx"])
        for g0 in range(0, NKT, VG):
            gn = min(VG, NKT - g0)
            vi = nvst % 2; nvst += 1
            p.dma(vst[vi][:, 0:gn, :], av[s].rearrange("(c p) f -> p c f", p=128)[:, g0:g0 + gn, :], writes=[f"vst{vi}"])
            p.add("act", lambda e, vi=vi, g0=g0, gn=gn: e.activation(out=vaug[:, g0:g0 + gn, 0:128], in_=vst[vi][:, 0:gn, :], func=AF.Copy),
                  [f"vst{vi}"], ["vaug"])
        p.add("dve", lambda e: e.tensor_tensor(out=nb[:, 2:4], in0=mx[:, 0:2], in1=mx[:, 2:4], op=ALU.mult), ["mx"], ["nb2"])
        p.add("act", lambda e: e.activation(out=nb[:, 2:4], in_=nb[:, 2:4], func=AF.Sqrt, scale=1.0 / 64.0), ["nb2"], ["nb2"])
        p.add("dve", lambda e: e.tensor_scalar(out=nb[:, 0:2], in0=nb[:, 2:4], scalar1=60.0, scalar2=-1.0, op0=ALU.min, op1=ALU.mult),
              ["nb2"], ["nb"])
        blocks = [(0, NCT, 0, NCT)]
        for q0 in range(NCT, NKT, 4):
            blocks.append((q0, min(4, NKT - q0), 0, NKT))
        for (q0, nq, k0, nk) in blocks:
            oset = nblk % 2; nblk += 1
            obanks = [banks[oset * 3 + i] for i in range(3)]
            okeys = [f"bank{oset*3+i}" for i in range(3)]
            for i in range(3):
                p.add("dve", lambda e, i=i, obanks=obanks: e.memset(obanks[i][:], 0.0), [], [okeys[i]])

            def oacc(comp, j):
                a = comp * 4 + j
                return obanks[a // 3][:, (a % 3) * 129:(a % 3) * 129 + 129], okeys[a // 3]
            QW = nq * 128
            for kt in range(k0, k0 + nk):
                for comp in range(2):
                    sb_i = 6 + nit % 2
                    pi = nit % 3
                    nit += 1
                    p.add("pe", lambda e, comp=comp, kt=kt, sb_i=sb_i, q0=q0, QW=QW: e.matmul(
                        banks[sb_i][:, 0:QW], lhsT=kTb[:, comp, kt * 128:(kt + 1) * 128], rhs=qTb[:, comp, q0 * 128:q0 * 128 + QW],
                        start=True, stop=True), ["kTb", "qTb"], [f"bank{sb_i}"])
                    p.add("act", lambda e, comp=comp, sb_i=sb_i, pi=pi, QW=QW: e.activation(
                        out=pT[pi][:, 0:QW], in_=banks[sb_i][:, 0:QW], func=AF.Exp, scale=0.125, bias=nb[:, comp:comp + 1]),
                        [f"bank{sb_i}", "nb"], [f"pT{pi}"])
                    for j in range(nq):
                        oap, okey = oacc(comp, j)
                        p.add("pe", lambda e, oap=oap, pi=pi, j=j, kt=kt: e.matmul(
                            oap, lhsT=pT[pi][:, j * 128:(j + 1) * 128], rhs=vaug[:, kt, :], start=False, stop=False,
                            skip_group_check=True), [f"pT{pi}", "vaug", "vaug1"], [okey])
            for j in range(nq):
                ei = nev % 2; nev += 1
                E = ev[ei]
                (o1ap, k1), (o2ap, k2) = oacc(0, j), oacc(1, j)
                sfx = f"_{ei}"
                p.add("dve", lambda e, E=E, o1ap=o1ap: e.reciprocal(out=E[:, 0:1], in_=o1ap[:, 128:129]), [k1], ["ev0" + sfx])
                p.add("dve", lambda e, E=E, o2ap=o2ap: e.reciprocal(out=E[:, 1:2], in_=o2ap[:, 128:129]), [k2], ["ev1" + sfx])
                p.add("dve", lambda e, E=E: e.tensor_tensor(out=E[:, 2:3], in0=E[:, 1:2], in1=lm[:, 3:4], op=ALU.mult),
                      ["ev1" + sfx, "nlam"], ["ev2" + sfx])
                p.add("dve", lambda e, E=E, o1ap=o1ap, ei=ei: e.tensor_scalar(out=o1[ei][:], in0=o1ap[:, 0:128], scalar1=E[:, 0:1], scalar2=None,
                                                                           op0=ALU.mult), [k1, "ev0" + sfx], ["o1" + sfx])
                p.add("dve", lambda e, E=E, o2ap=o2ap, ei=ei: e.scalar_tensor_tensor(
                    out=oo[ei][:], in0=o2ap[:, 0:128], scalar=E[:, 2:3], in1=o1[ei][:], op0=ALU.mult, op1=ALU.add),
                    [k2, "ev2" + sfx, "o1" + sfx], ["oo" + sfx])
                p.add("dve", lambda e, E=E, ei=ei: e.scalar_tensor_tensor(
                    out=junk[:], in0=oo[ei][:], scalar=1.0, in1=oo[ei][:], op0=ALU.mult, op1=ALU.mult, accum_out=E[:, 3:4]),
                    ["oo" + sfx], ["junk", "ev3" + sfx])
                p.add("dve", lambda e, E=E: e.tensor_scalar(out=E[:, 4:5], in0=E[:, 3:4], scalar1=1.0 / 128.0, scalar2=EPS,
                                                            op0=ALU.mult, op1=ALU.add), ["ev3" + sfx], ["ev4" + sfx])
                p.add("act", lambda e, E=E: e.activation(out=E[:, 5:6], in_=E[:, 4:5], func=AF.Sqrt), ["ev4" + sfx], ["ev5" + sfx])
                p.add("dve", lambda e, E=E: e.reciprocal(out=E[:, 6:7], in_=E[:, 5:6]), ["ev5" + sfx], ["ev6" + sfx])
                p.add("dve", lambda e, E=E, ei=ei: e.scalar_tensor_tensor(
                    out=yy[ei][:], in0=oo[ei][:], scalar=E[:, 6:7], in1=gsc[:], op0=ALU.mult, op1=ALU.mult),
                    ["oo" + sfx, "ev6" + sfx, "gsc"], ["yy" + sfx])
                r0 = (q0 + j) * 128
                p.dma(ao[s, r0:r0 + 128, :], yy[ei][:], reads=["yy" + sfx])
    p.emit()
    return nc


def build_C1(segs):
    nc = bass.Bass("TRN2", target_bir_lowering=False)
    W = sum(n + 30 for n in segs)
    NT = sum(segs)
    aT = _din(nc, "aT", [512, W])
    cw_d = _din(nc, "cw", [128, 2, 31])
    cp_d = _din(nc, "cp", [128, 2, 3])
    ones_d = _din(nc, "ones", [128, 128])
    co = _dout(nc, "convT", [2, 128, NT])
    p = Prog(nc)
    ones = p.sb([128, 128], name="ones_sb"); p.dma(ones[:], ones_d, writes=["ones"])
    cw = p.sb([128, 2, 31], name="cw_sb"); p.dma(cw[:], cw_d, writes=["cw"])
    cp = p.sb([128, 2, 3], name="cp_sb"); p.dma(cp[:], cp_d, writes=["cp"])
    N = 512
    a1 = [p.sb([128, N + 30], name=f"a1_{i}") for i in range(2)]
    a2 = [p.sb([128, N + 30], name=f"a2_{i}") for i in range(2)]
    u = [p.sb([128, N + 30], name=f"u_{i}") for i in range(2)]
    acc = [[p.sb([128, N], name=f"acc_{i}_{r}") for r in range(2)] for i in range(2)]
    y = [p.sb([128, N], name=f"y_{i}") for i in range(2)]
    ysq = [p.sb([128, N], name=f"ysq_{i}") for i in range(2)]
    mean = p.sb([128, N], name="mean"); msq = p.sb([128, N], name="msq"); rstd = p.sb([128, N], name="rstd")
    zz = [p.sb([128, N], name=f"zz_{i}") for i in range(2)]
    bA = p.ps([128, 512], name="bankA"); bB = p.ps([128, 512], name="bankB")
    off = 0
    tok0 = 0
    for ns in segs:
        for t0 in range(0, ns, N):
            n = min(N, ns - t0)
            for j in range(2):
                c0 = off + t0
                p.dma(a1[j][:, 0:n + 30], aT[j * 128:(j + 1) * 128, c0:c0 + n + 30], writes=[f"a1_{j}"])
                p.dma(a2[j][:, 0:n + 30], aT[256 + j * 128:256 + (j + 1) * 128, c0:c0 + n + 30], writes=[f"a2_{j}"])
                p.add("act", lambda e, j=j, n=n: e.activation(out=a2[j][:, 0:n + 30], in_=a2[j][:, 0:n + 30], func=AF.Sigmoid),
                      [f"a2_{j}"], [f"a2_{j}"])
                p.add("dve", lambda e, j=j, n=n: e.tensor_tensor(out=u[j][:, 0:n + 30], in0=a1[j][:, 0:n + 30], in1=a2[j][:, 0:n + 30], op=ALU.mult),
                      [f"a1_{j}", f"a2_{j}"], [f"u_{j}"])
                p.add("dve", lambda e, j=j, n=n: e.tensor_scalar(out=acc[j][0][:, 0:n], in0=u[j][:, 0:n], scalar1=cw[:, j, 0:1], scalar2=None,
                                                                op0=ALU.mult), [f"u_{j}", "cw"], [f"acc_{j}_0"])
                for k in range(1, 31):
                    src, dst = acc[j][(k - 1) % 2], acc[j][k % 2]
                    p.add("dve", lambda e, j=j, n=n, k=k, src=src, dst=dst: e.scalar_tensor_tensor(
                        out=dst[:, 0:n], in0=u[j][:, k:k + n], scalar=cw[:, j, k:k + 1], in1=src[:, 0:n], op0=ALU.mult, op1=ALU.add),
                        [f"u_{j}", "cw", f"acc_{j}_{(k-1)%2}"], [f"acc_{j}_{k%2}"])
                p.add("dve", lambda e, j=j, n=n: e.tensor_scalar(out=y[j][:, 0:n], in0=acc[j][0][:, 0:n], scalar1=cp[:, j, 0:1], scalar2=None,
                                                                op0=ALU.add), [f"acc_{j}_0", "cp"], [f"y_{j}"])
                p.add("act", lambda e, j=j, n=n: e.activation(out=ysq[j][:, 0:n], in_=y[j][:, 0:n], func=AF.Square), [f"y_{j}"], [f"ysq_{j}"])
            for j in range(2):
                p.add("pe", lambda e, j=j, n=n: e.matmul(bA[:, 0:n], lhsT=ones[:], rhs=y[j][:, 0:n], start=(j == 0), stop=(j == 1)),
                      ["ones", f"y_{j}"], ["bankA"])
            for j in range(2):
                p.add("pe", lambda e, j=j, n=n: e.matmul(bB[:, 0:n], lhsT=ones[:], rhs=ysq[j][:, 0:n], start=(j == 0), stop=(j == 1)),
                      ["ones", f"ysq_{j}"], ["bankB"])
            p.add("act", lambda e, n=n: e.activation(out=mean[:, 0:n], in_=bA[:, 0:n], func=AF.Copy, scale=1.0 / 256), ["bankA"], ["mean"])
            p.add("act", lambda e, n=n: e.activation(out=msq[:, 0:n], in_=bA[:, 0:n], func=AF.Square, scale=1.0 / 256), ["bankA"], ["msq"])
            p.add("dve", lambda e, n=n: e.scalar_tensor_tensor(out=rstd[:, 0:n], in0=bB[:, 0:n], scalar=1.0 / 256, in1=msq[:, 0:n],
                                                               op0=ALU.mult, op1=ALU.subtract), ["bankB", "msq"], ["rstd"])
            p.add("dve", lambda e, n=n: e.tensor_scalar(out=rstd[:, 0:n], in0=rstd[:, 0:n], scalar1=EPS, scalar2=None, op0=ALU.add),
                  ["rstd"], ["rstd"])
            p.add("act", lambda e, n=n: e.activation(out=rstd[:, 0:n], in_=rstd[:, 0:n], func=AF.Sqrt), ["rstd"], ["rstd"])
            p.add("dve", lambda e, n=n: e.reciprocal(out=rstd[:, 0:n], in_=rstd[:, 0:n]), ["rstd"], ["rstd"])
            for j in range(2):
                p.add("dve", lambda e, j=j, n=n: e.tensor_tensor(out=zz[j][:, 0:n], in0=y[j][:, 0:n], in1=mean[:, 0:n], op=ALU.subtract),
                      [f"y_{j}", "mean"], [f"zz_{j}"])
                p.add("dve", lambda e, j=j, n=n: e.tensor_tensor(out=zz[j][:, 0:n], in0=zz[j][:, 0:n], in1=rstd[:, 0:n], op=ALU.mult),
                      [f"zz_{j}", "rstd"], [f"zz_{j}"])
                p.add("act", lambda e, j=j, n=n: e.activation(out=zz[j][:, 0:n], in_=zz[j][:, 0:n], func=AF.Silu,
                                                              scale=cp[:, j, 1:2], bias=cp[:, j, 2:3]), [f"zz_{j}", "cp"], [f"zz_{j}"])
                p.dma(co[j, :, tok0 + t0:tok0 + t0 + n], zz[j][:, 0:n], reads=[f"zz_{j}"])
        off += ns + 30
        tok0 += ns
    p.emit()
    return nc


def build_C2(NT, ctx_tiles=(0,)):
    nc = bass.Bass("TRN2", target_bir_lowering=False)
    L = NT * 128
    xs = _din(nc, "xs", [L, D])
    convT = _din(nc, "convT", [2, 128, L])
    hf_d = _din(nc, "hf", [L, 256])
    hb_d = _din(nc, "hb", [L, 256])
    mo_d = _din(nc, "mo", [L, 256])
    ao_d = _din(nc, "ao", [L, 512])
    mng_d = _din(nc, "mng", [128, 64])
    w_out = _din(nc, "w_out", [D, D])
    bc_d = _din(nc, "bc", [7, 128, D])
    ident = _din(nc, "ident", [128, 128])
    x1_o = _dout(nc, "x1", [L, D])
    h2_o = _dout(nc, "h2", [L, D])
    p = Prog(nc)
    wob = p.sb([128, 8, D], BF16, name="wob")
    load_cast_weight(p, wob, w_out, D, "wob")
    idt = p.sb([128, 128], name="idt"); p.dma(idt[:], ident, writes=["idt"])
    mng = p.sb([128, 64], name="mng_sb"); p.dma(mng[:], mng_d, writes=["mng"])
    bc = [p.sb([128, D], name=f"bc{i}") for i in range(7)]
    for i in range(7):
        p.dma(bc[i][:], bc_d[i], writes=[f"bc{i}"])
    for i in (3, 4):
        p.add("dve", lambda e, i=i: e.scalar_tensor_tensor(out=bc[i][:], in0=bc[i][:], scalar=1.0, in1=bc[0][:], op0=ALU.add, op1=ALU.mult),
              [f"bc{i}", "bc0"], [f"bc{i}"])
    NB = 2
    def mk(shape, nm, dt=F32):
        return [p.sb(shape, dt, name=f"{nm}{i}") for i in range(NB)]
    xt = mk([128, D], "xt"); hf = mk([128, 256], "hf"); hb = mk([128, 256], "hb"); mo = mk([128, 256], "mo")
    aot = mk([128, 512], "aot"); cvt = mk([128, 2, 128], "cvt"); ym = mk([128, 256], "ym"); sqm = mk([128, 256], "sqm")
    st4 = mk([128, 8], "st4"); mixT = mk([128, 8, 128], "mixT", BF16); tmp = mk([128, D], "tmp"); x1 = mk([128, D], "x1")
    h2 = mk([128, D], "h2"); ss = mk([128, 4], "ss")
    junk = p.sb([128, D], name="junk")
    bT = [p.ps([128, 512], name=f"bT{i}") for i in range(4)]
    bAcc = [p.ps([128, 512], name=f"bAcc{i}") for i in range(4)]
    for t in range(NT):
        i = t % NB
        isc = t in ctx_tiles
        r0 = t * 128
        g1b = bc[2] if isc else bc[1]
        gm2 = bc[4] if isc else bc[3]
        sh2 = bc[6] if isc else bc[5]
        kg1, kgm2, ksh2 = (f"bc{2 if isc else 1}", f"bc{4 if isc else 3}", f"bc{6 if isc else 5}")
        p.dma(xt[i][:], xs[r0:r0 + 128, :], writes=[f"xt{i}"])
        p.dma(hf[i][:], hf_d[r0:r0 + 128, :], writes=[f"hf{i}"])
        p.dma(hb[i][:], hb_d[r0:r0 + 128, :], writes=[f"hb{i}"])
        p.dma(mo[i][:], mo_d[r0:r0 + 128, :], writes=[f"mo{i}"])
        p.dma(aot[i][:], ao_d[r0:r0 + 128, :], writes=[f"aot{i}"])
        p.dma(cvt[i][:], convT[:, :, r0:r0 + 128].rearrange("j p n -> p j n"), writes=[f"cvt{i}"])
        p.add("dve", lambda e, i=i: e.tensor_tensor(out=ym[i][:], in0=hf[i][:], in1=hb[i][:], op=ALU.add), [f"hf{i}", f"hb{i}"], [f"ym{i}"])
        p.add("act", lambda e, i=i: e.activation(out=sqm[i][:], in_=ym[i][:], func=AF.Square), [f"ym{i}"], [f"sqm{i}"])
        p.add("dve", lambda e, i=i: e.tensor_reduce(out=st4[i][:, 0:4], in_=sqm[i][:].rearrange("p (h d) -> p h d", h=4), axis=AX.X, op=ALU.add),
              [f"sqm{i}"], [f"st4a{i}"])
        p.add("dve", lambda e, i=i: e.tensor_scalar(out=st4[i][:, 4:8], in0=st4[i][:, 0:4], scalar1=1.0 / 64, scalar2=EPS, op0=ALU.mult, op1=ALU.add),
              [f"st4a{i}"], [f"st4b{i}"])
        p.add("act", lambda e, i=i: e.activation(out=st4[i][:, 0:4], in_=st4[i][:, 4:8], func=AF.Sqrt), [f"st4b{i}"], [f"st4a{i}"])
        p.add("dve", lambda e, i=i: e.reciprocal(out=st4[i][:, 4:8], in_=st4[i][:, 0:4]), [f"st4a{i}"], [f"st4b{i}"])
        p.add("act", lambda e, i=i: e.activation(out=mo[i][:], in_=mo[i][:], func=AF.Sigmoid), [f"mo{i}"], [f"mo{i}"])
        ym3 = ym[i][:].rearrange("p (h d) -> p h d", h=4)
        p.add("dve", lambda e, i=i, ym3=ym3: e.tensor_tensor(out=ym3, in0=ym3, in1=st4[i][:, 4:8, None].to_broadcast([128, 4, 64]), op=ALU.mult),
              [f"ym{i}", f"st4b{i}"], [f"ym{i}"])
        p.add("dve", lambda e, i=i, ym3=ym3: e.tensor_tensor(out=ym3, in0=ym3, in1=mng[:, None, :].to_broadcast([128, 4, 64]), op=ALU.mult),
              [f"ym{i}", "mng"], [f"ym{i}"])
        p.add("dve", lambda e, i=i: e.tensor_tensor(out=ym[i][:], in0=ym[i][:], in1=mo[i][:], op=ALU.mult), [f"ym{i}", f"mo{i}"], [f"ym{i}"])
        p.add("act", lambda e, i=i: e.activation(out=mixT[i][:, 0:2, :], in_=cvt[i][:], func=AF.Copy), [f"cvt{i}"], [f"mixT{i}_c"])
        ta, tb = (2 * t) % 4, (2 * t + 1) % 4
        srcs = [(ym[i], 0, f"ym{i}"), (ym[i], 128, f"ym{i}"), (aot[i], 0, f"aot{i}"), (aot[i], 128, f"aot{i}"),
                (aot[i], 256, f"aot{i}"), (aot[i], 384, f"aot{i}")]
        for n_, (src, c0, key) in enumerate(srcs):
            bank = ta if n_ < 4 else tb
            col = (n_ % 4) * 128
            p.add("pe", lambda e, src=src, c0=c0, bank=bank, col=col: e.transpose(bT[bank][:, col:col + 128], src[:, c0:c0 + 128], idt[:]),
                  [key, "idt"], [f"bT{bank}"])
        p.add("act", lambda e, i=i, ta=ta: e.activation(out=mixT[i][:, 2:6, :], in_=bT[ta][:].rearrange("p (k n) -> p k n", k=4), func=AF.Copy),
              [f"bT{ta}"], [f"mixT{i}_a"])
        p.add("dve", lambda e, i=i, tb=tb: e.tensor_copy(out=mixT[i][:, 6:8, :], in_=bT[tb][:, 0:256].rearrange("p (k n) -> p k n", k=2)),
              [f"bT{tb}"], [f"mixT{i}_b"])
        for blk in range(2):
            a = (2 * t + blk) % 4
            for k in range(8):
                p.add("pe", lambda e, i=i, k=k, a=a, blk=blk: e.matmul(bAcc[a][:], lhsT=mixT[i][:, k, :], rhs=wob[:, k, blk * 512:(blk + 1) * 512],
                                                                      start=(k == 0), stop=(k == 7)),
                      [f"mixT{i}_c", f"mixT{i}_a", f"mixT{i}_b", "wob"], [f"bAcc{a}"])
            cs = slice(blk * 512, (blk + 1) * 512)
            p.add("dve", lambda e, i=i, a=a, cs=cs, g1b=g1b: e.tensor_tensor(out=tmp[i][:, cs], in0=bAcc[a][:], in1=g1b[:, cs], op=ALU.mult),
                  [f"bAcc{a}", kg1], [f"tmp{i}_{blk}"])
            p.add("dve", lambda e, i=i, cs=cs: e.tensor_tensor(out=x1[i][:, cs], in0=tmp[i][:, cs], in1=xt[i][:, cs], op=ALU.add),
                  [f"tmp{i}_{blk}", f"xt{i}"], [f"x1{i}_{blk}"])
        p.dma(x1_o[r0:r0 + 128, :], x1[i][:], reads=[f"x1{i}_0", f"x1{i}_1"])
        p.add("act", lambda e, i=i: e.activation(out=junk[:], in_=x1[i][:], func=AF.Square, accum_out=ss[i][:, 0:1]),
              [f"x1{i}_0", f"x1{i}_1"], ["junk", f"ss0{i}"])
        p.add("dve", lambda e, i=i: e.tensor_scalar(out=ss[i][:, 1:2], in0=ss[i][:, 0:1], scalar1=1.0 / D, scalar2=EPS, op0=ALU.mult, op1=ALU.add),
              [f"ss0{i}"], [f"ss1{i}"])
        p.add("act", lambda e, i=i: e.activation(out=ss[i][:, 2:3], in_=ss[i][:, 1:2], func=AF.Sqrt), [f"ss1{i}"], [f"ss2{i}"])
        p.add("dve", lambda e, i=i: e.reciprocal(out=ss[i][:, 3:4], in_=ss[i][:, 2:3]), [f"ss2{i}"], [f"ss3{i}"])
        p.add("dve", lambda e, i=i, gm2=gm2: e.scalar_tensor_tensor(out=h2[i][:], in0=x1[i][:], scalar=ss[i][:, 3:4], in1=gm2[:],
                                                                     op0=ALU.mult, op1=ALU.mult),
              [f"x1{i}_0", f"x1{i}_1", f"ss3{i}", kgm2], [f"h2{i}"])
        p.add("dve", lambda e, i=i, sh2=sh2: e.tensor_tensor(out=h2[i][:], in0=h2[i][:], in1=sh2[:], op=ALU.add), [f"h2{i}", ksh2], [f"h2{i}"])
        p.dma(h2_o[r0:r0 + 128, :], h2[i][:], reads=[f"h2{i}"])
    p.emit()
    return nc


def build_C3(NT, ctx_tiles=(0,), NGB=4):
    nc = bass.Bass("TRN2", target_bir_lowering=False)
    L = NT * 128
    NE = 16384
    h2_d = _din(nc, "h2", [L, D])
    x1_d = _din(nc, "x1", [L, D])
    wq = _din(nc, "wq", [D, D])
    kbd_d = _din(nc, "kbd", [128, 8, 256])
    pu = _din(nc, "peer_u", [NE, D])
    pv = _din(nc, "peer_v", [NE, D])
    g2_d = _din(nc, "g2b", [2, 128, D])
    ident = _din(nc, "ident", [128, 128])
    x2_o = _dout(nc, "x2", [L, D])
    p = Prog(nc)
    wqb = p.sb([128, 8, D], F32, name="wqb")
    p.dma(wqb[:], wq.rearrange("(k p) n -> p k n", p=128), writes=["wqb"])
    idt = p.sb([128, 128], name="idt"); p.dma(idt[:], ident, writes=["idt"])
    kbd = p.sb([128, 8, 256], name="kbd_sb"); p.dma(kbd[:], kbd_d, writes=["kbd"])
    g2b = [p.sb([128, D], name=f"g2b{i}") for i in range(2)]
    for i in range(2):
        p.dma(g2b[i][:], g2_d[i], writes=[f"g2b{i}"])
    h2t = p.sb([128, D], name="h2t"); x1t = p.sb([128, D], name="x1t")
    h2T = p.sb([128, 8, 128], F32, name="h2T")
    qTf = p.sb([128, 8, 128], name="qTf")
    sc = p.sb([128, 16, 128], name="sc"); wk = p.sb([128, 16, 128], name="wk")
    st = p.sb([128, 16, 16], name="st"); it = p.sb([128, 16, 16], U32, name="it"); itf = p.sb([128, 16, 16], name="itf")
    cand = p.sb([128, 8, 256], name="cand"); cidx = p.sb([128, 8, 256], name="cidx"); wk2 = p.sb([128, 8, 256], name="wk2")
    best = p.sb([128, 8, 16], name="best"); gw = p.sb([128, 8, 16], name="gw")
    eidf = p.sb([128, 128], name="eidf"); eidi = p.sb([128, 128], I32, name="eidi")
    sm = p.sb([128, 16], name="sm")
    act = p.sb([128, 128], name="act_sb"); coef = p.sb([128, 128], name="coef")
    junk = p.sb([128, D], name="junk")
    acc = p.sb([128, D], name="acc"); x2t = p.sb([128, D], name="x2t")
    rows = [p.sb([128, D], name=f"rows{i}") for i in range(NGB)]
    banks = [p.ps([128, 512], name=f"bank{i}") for i in range(8)]
    ngat = 0
    for t in range(NT):
        isc = t in ctx_tiles
        r0 = t * 128
        gb = g2b[1] if isc else g2b[0]
        kgb = "g2b1" if isc else "g2b0"
        p.dma(h2t[:], h2_d[r0:r0 + 128, :], writes=["h2t"])
        p.dma(x1t[:], x1_d[r0:r0 + 128, :], writes=["x1t"])
        for half in range(2):
            for k in range(half * 4, half * 4 + 4):
                p.add("pe", lambda e, k=k, half=half: e.transpose(banks[half][:, (k % 4) * 128:(k % 4 + 1) * 128], h2t[:, k * 128:(k + 1) * 128], idt[:]),
                      ["h2t", "idt"], [f"bank{half}"])
        p.add("act", lambda e: e.activation(out=h2T[:, 0:4, :], in_=banks[0][:].rearrange("p (k n) -> p k n", k=4), func=AF.Copy), ["bank0"], ["h2T_a"])
        p.add("dve", lambda e: e.tensor_copy(out=h2T[:, 4:8, :], in_=banks[1][:].rearrange("p (k n) -> p k n", k=4)), ["bank1"], ["h2T_b"])
        for c in range(8):
            bk = 2 + c // 4
            for k in range(8):
                p.add("pe", lambda e, c=c, k=k, bk=bk: e.matmul(banks[bk][:, (c % 4) * 128:(c % 4 + 1) * 128], lhsT=wqb[:, k, c * 128:(c + 1) * 128],
                                                              rhs=h2T[:, k, :], start=(k == 0), stop=(k == 7)),
                      ["wqb", "h2T_a", "h2T_b"], [f"bank{bk}"])
        p.add("act", lambda e: e.activation(out=qTf[:, 0:4, :], in_=banks[2][:].rearrange("p (k n) -> p k n", k=4), func=AF.Copy), ["bank2"], ["qTf_a"])
        p.add("dve", lambda e: e.tensor_copy(out=qTf[:, 4:8, :], in_=banks[3][:].rearrange("p (k n) -> p k n", k=4)), ["bank3"], ["qTf_b"])
        for h in range(8):
            bk = 4 + h // 2
            p.add("pe", lambda e, h=h, bk=bk: e.matmul(banks[bk][:, (h % 2) * 256:(h % 2 + 1) * 256], lhsT=qTf[:, h, :], rhs=kbd[:, h, :],
                                                      start=True, stop=True), ["qTf_a", "qTf_b", "kbd"], [f"bank{bk}"])
        for bk in range(4, 8):
            g0 = (bk - 4) * 4
            if bk % 2 == 0:
                p.add("act", lambda e, bk=bk, g0=g0: e.activation(out=sc[:, g0:g0 + 4, :], in_=banks[bk][:].rearrange("p (g n) -> p g n", g=4), func=AF.Copy),
                      [f"bank{bk}"], [f"sc{bk}"])
            else:
                p.add("dve", lambda e, bk=bk, g0=g0: e.tensor_copy(out=sc[:, g0:g0 + 4, :], in_=banks[bk][:].rearrange("p (g n) -> p g n", g=4)),
                      [f"bank{bk}"], [f"sc{bk}"])
        for g in range(16):
            ks = f"sc{4 + g // 4}"
            p.add("dve", lambda e, g=g: e.max(out=st[:, g, 0:8], in_=sc[:, g, :]), [ks], [f"st{g}a"])
            p.add("dve", lambda e, g=g: e.max_index(out=it[:, g, 0:8], in_max=st[:, g, 0:8], in_values=sc[:, g, :]), [ks, f"st{g}a"], [f"it{g}a"])
            p.add("dve", lambda e, g=g: e.match_replace(out=wk[:, g, :], in_to_replace=st[:, g, 0:8], in_values=sc[:, g, :], imm_value=-1e30),
                  [ks, f"st{g}a"], [f"wk{g}"])
            p.add("dve", lambda e, g=g: e.max(out=st[:, g, 8:16], in_=wk[:, g, :]), [f"wk{g}"], [f"st{g}b"])
            p.add("dve", lambda e, g=g: e.max_index(out=it[:, g, 8:16], in_max=st[:, g, 8:16], in_values=wk[:, g, :]), [f"wk{g}", f"st{g}b"], [f"it{g}b"])
        allst = [f"st{g}{x}" for g in range(16) for x in "ab"]
        allit = [f"it{g}{x}" for g in range(16) for x in "ab"]
        p.add("dve", lambda e: e.tensor_copy(out=itf[:], in_=it[:]), allit, ["itf"])
        st4 = st[:].rearrange("p (h two) k -> p h two k", two=2)
        itf4 = itf[:].rearrange("p (h two) k -> p h two k", two=2)
        cand4 = cand[:].rearrange("p h (i j) -> p h i j", i=16)
        cidx4 = cidx[:].rearrange("p h (i j) -> p h i j", i=16)
        for h in range(8):
            p.add("dve", lambda e, h=h: e.tensor_tensor(out=cand4[:, h], in0=st4[:, h, 0, :, None].to_broadcast([128, 16, 16]),
                                                        in1=st4[:, h, 1, None, :].to_broadcast([128, 16, 16]), op=ALU.add), allst, [f"cand{h}"])
            p.add("dve", lambda e, h=h: e.scalar_tensor_tensor(out=cidx4[:, h], in0=itf4[:, h, 0, :, None].to_broadcast([128, 16, 16]), scalar=128.0,
                                                               in1=itf4[:, h, 1, None, :].to_broadcast([128, 16, 16]), op0=ALU.mult, op1=ALU.add),
                  ["itf"], [f"cidx{h}"])
            p.add("dve", lambda e, h=h: e.max(out=best[:, h, 0:8], in_=cand[:, h, :]), [f"cand{h}"], [f"best{h}a"])
            p.add("dve", lambda e, h=h: e.match_replace(out=wk2[:, h, :], in_to_replace=best[:, h, 0:8], in_values=cand[:, h, :], imm_value=-1e30),
                  [f"cand{h}", f"best{h}a"], [f"wk2{h}"])
            p.add("dve", lambda e, h=h: e.max(out=best[:, h, 8:16], in_=wk2[:, h, :]), [f"wk2{h}"], [f"best{h}b"])
            for k in range(16):
                hk = h * 16 + k
                p.add("dve", lambda e, h=h, k=k, hk=hk: e.scalar_tensor_tensor(
                    out=junk[:, 0:256], in0=cand[:, h, :], scalar=best[:, h, k:k + 1], in1=cidx[:, h, :], op0=ALU.is_equal, op1=ALU.mult,
                    accum_out=eidf[:, hk:hk + 1]), [f"cand{h}", f"cidx{h}", f"best{h}a", f"best{h}b"], ["junk", f"eidf{h}"])
        alle = [f"eidf{h}" for h in range(8)]
        allb = [f"best{h}{x}" for h in range(8) for x in "ab"]
        p.add("dve", lambda e: e.tensor_scalar(out=eidf[:], in0=eidf[:], scalar1=float(NE - 1), scalar2=0.0, op0=ALU.min, op1=ALU.max), alle, ["eidf"])
        p.add("dve", lambda e: e.tensor_copy(out=eidi[:], in_=eidf[:]), ["eidf"], ["eidi"])
        p.add("dve", lambda e: e.tensor_tensor(out=gw[:], in0=best[:], in1=best[:, :, 0:1].to_broadcast([128, 8, 16]), op=ALU.subtract), allb, ["gw"])
        p.add("act", lambda e: e.activation(out=gw[:], in_=gw[:], func=AF.Exp), ["gw"], ["gw"])
        p.add("dve", lambda e: e.tensor_reduce(out=sm[:, 0:8], in_=gw[:], axis=AX.X, op=ALU.add), ["gw"], ["sm0"])
        p.add("dve", lambda e: e.reciprocal(out=sm[:, 8:16], in_=sm[:, 0:8]), ["sm0"], ["sm1"])
        p.add("dve", lambda e: e.tensor_tensor(out=gw[:], in0=gw[:], in1=sm[:, 8:16, None].to_broadcast([128, 8, 16]), op=ALU.mult), ["gw", "sm1"], ["gw"])
        for hk in range(128):
            b = ngat % NGB; ngat += 1
            p.add("pool", lambda e, b=b, hk=hk: e.indirect_dma_start(
                out=rows[b][:], out_offset=None, in_=pu, in_offset=bass.IndirectOffsetOnAxis(ap=eidi[:, hk:hk + 1], axis=0)),
                ["eidi"], [f"rows{b}"])
            p.add("dve", lambda e, b=b, hk=hk: e.scalar_tensor_tensor(out=junk[:], in0=rows[b][:], scalar=1.0, in1=h2t[:], op0=ALU.mult, op1=ALU.mult,
                                                                     accum_out=act[:, hk:hk + 1]), [f"rows{b}", "h2t"], ["junk", "act"])
        p.add("act", lambda e: e.activation(out=coef[:], in_=act[:], func=AF.Gelu), ["act"], ["coef"])
        p.add("dve", lambda e: e.tensor_tensor(out=coef[:], in0=coef[:], in1=gw[:].rearrange("p h k -> p (h k)"), op=ALU.mult), ["coef", "gw"], ["coef"])
        p.add("dve", lambda e: e.memset(acc[:], 0.0), [], ["acc"])
        for hk in range(128):
            b = ngat % NGB; ngat += 1
            p.add("pool", lambda e, b=b, hk=hk: e.indirect_dma_start(
                out=rows[b][:], out_offset=None, in_=pv, in_offset=bass.IndirectOffsetOnAxis(ap=eidi[:, hk:hk + 1], axis=0)),
                ["eidi"], [f"rows{b}"])
            p.add("dve", lambda e, b=b, hk=hk: e.scalar_tensor_tensor(out=acc[:], in0=rows[b][:], scalar=coef[:, hk:hk + 1], in1=acc[:],
                                                                     op0=ALU.mult, op1=ALU.add), [f"rows{b}", "coef", "acc"], ["acc"])
        p.add("dve", lambda e, gb=gb: e.tensor_tensor(out=x2t[:], in0=acc[:], in1=gb[:], op=ALU.mult), ["acc", kgb], ["x2t"])
        p.add("dve", lambda e: e.tensor_tensor(out=x2t[:], in0=x2t[:], in1=x1t[:], op=ALU.add), ["x2t", "x1t"], ["x2t"])
        p.dma(x2_o[r0:r0 + 128, :], x2t[:], reads=["x2t"])
    p.emit()
    return nc


def build_F(NT):
    nc = bass.Bass("TRN2", target_bir_lowering=False)
    L = NT * 128
    xs = _din(nc, "xs", [L, D])
    fg_d = _din(nc, "fg", [128, D])
    yo = _dout(nc, "y", [L, D])
    p = Prog(nc)
    fg = p.sb([128, D], name="fg_sb"); p.dma(fg[:], fg_d, writes=["fg"])
    junk = p.sb([128, D], name="junk")
    NB = 2
    xt = [p.sb([128, D], name=f"xt{i}") for i in range(NB)]
    yt = [p.sb([128, D], name=f"yt{i}") for i in range(NB)]
    ss = [p.sb([128, 4], name=f"ss{i}") for i in range(NB)]
    for t in range(NT):
        i = t % NB
        r0 = t * 128
        p.dma(xt[i][:], xs[r0:r0 + 128, :], writes=[f"xt{i}"])
        p.add("act", lambda e, i=i: e.activation(out=junk[:], in_=xt[i][:], func=AF.Square, accum_out=ss[i][:, 0:1]), [f"xt{i}"], ["junk", f"ss0{i}"])
        p.add("dve", lambda e, i=i: e.tensor_scalar(out=ss[i][:, 1:2], in0=ss[i][:, 0:1], scalar1=1.0 / D, scalar2=EPS, op0=ALU.mult, op1=ALU.add),
              [f"ss0{i}"], [f"ss1{i}"])
        p.add("act", lambda e, i=i: e.activation(out=ss[i][:, 2:3], in_=ss[i][:, 1:2], func=AF.Sqrt), [f"ss1{i}"], [f"ss2{i}"])
        p.add("dve", lambda e, i=i: e.reciprocal(out=ss[i][:, 3:4], in_=ss[i][:, 2:3]), [f"ss2{i}"], [f"ss3{i}"])
        p.add("dve", lambda e, i=i: e.scalar_tensor_tensor(out=yt[i][:], in0=xt[i][:], scalar=ss[i][:, 3:4], in1=fg[:], op0=ALU.mult, op1=ALU.mult),
              [f"xt{i}", f"ss3{i}", "fg"], [f"yt{i}"])
        p.dma(yo[r0:r0 + 128, :], yt[i][:], reads=[f"yt{i}"])
    p.emit()
    return nc


_PROGS = {}


def _prog(name, fn):
    if name not in _PROGS:
        _PROGS[name] = fn()
    return _PROGS[name]


def _run(nc, maps):
    res = run_bass_kernel_spmd(nc, maps, core_ids=list(range(NCORES)))
    return res.results


def _rep(v, n=128):
    return np.ascontiguousarray(np.broadcast_to(np.asarray(v, np.float32)[None], (n, v.shape[0])))


def _rope_tables(n_tokens):
    rows = n_tokens // 64
    row = np.repeat(np.arange(rows), 64).astype(np.float32)
    col = np.tile(np.arange(64), rows).astype(np.float32)
    inv = (np.float32(10000.0) ** (-np.arange(0, 32, 2, dtype=np.float32) / np.float32(32))).astype(np.float32)
    ang = np.concatenate([row[:, None] * inv, col[:, None] * inv], axis=-1).astype(np.float32)
    return np.cos(ang).astype(np.float32), np.sin(ang).astype(np.float32)


def kernel_unfused(x, c, ctx, c_ctx, ada_w, ada_b, norm1_g, norm2_g, w_in, conv_w, conv_b, conv_ln_g, conv_ln_b,
           mlstm_gate_b, mlstm_norm_g, diff_lambda, diff_norm_g, w_out, peer_wq, peer_keys, peer_u, peer_v, final_g):
    f32 = np.float32
    x = np.asarray(x, f32); ctx = np.asarray(ctx, f32)
    B, S, _ = x.shape
    CT = ctx.shape[1]
    DEPTH = ada_w.shape[0]
    HS, HC = S // 2, CT // 2
    NT = (HS + HC) // 128
    NKT = (S + CT) // 128
    NCT = CT // 128
    ident = np.eye(128, dtype=f32)
    ones = np.ones((128, 128), f32)
    tri = np.triu(np.ones((128, 128), f32))
    cos, sin = _rope_tables(S)
    cores = [(cc // 2, cc % 2) for cc in range(NCORES)]

    cT = np.concatenate([np.asarray(c, f32), np.asarray(c_ctx, f32)[None]], 0).T
    cTl = np.ascontiguousarray(cT.reshape(8, 128, 5).transpose(1, 0, 2))
    maps = []
    for cc in range(NCORES):
        ll, half = cc // 2, cc % 2
        maps.append({"w": np.ascontiguousarray(ada_w[ll][:, half * 3072:(half + 1) * 3072]),
                     "b": np.ascontiguousarray(np.asarray(ada_b[ll], f32)[half * 3072:(half + 1) * 3072].reshape(24, 128).T),
                     "cT": cTl})
    res = _run(_prog("P0", build_P0), maps)
    mod = np.zeros((DEPTH, 6144, 5), f32)
    for cc in range(NCORES):
        ll, half = cc // 2, cc % 2
        mod[ll, half * 3072:(half + 1) * 3072] = res[cc]["modT"].transpose(1, 0, 2).reshape(3072, 5)

    xs = [np.concatenate([ctx[b, z * HC:(z + 1) * HC], x[b, z * HS:(z + 1) * HS]], 0) for (b, z) in cores]
    cs_core = []
    for (b, z) in cores:
        cs = np.zeros((HC + HS, 64), f32)
        cs[:HC, :32] = 1.0
        cs[HC:, :32] = cos[z * HS:(z + 1) * HS]
        cs[HC:, 32:] = sin[z * HS:(z + 1) * HS]
        cs_core.append(cs)

    def fv(v):
        return np.asarray(v, f32).reshape(8, 128).T

    for l in range(DEPTH):
        m6 = mod[l]
        sh1, sc1, g1, sh2, sc2, g2 = [m6[i * 1024:(i + 1) * 1024] for i in range(6)]
        lam_init = 0.8 - 0.6 * math.exp(-0.3 * l)
        maps = []
        for cc, (b, z) in enumerate(cores):
            pv = np.ascontiguousarray(np.stack([fv(norm1_g[l]), fv(sc1[:, b]), fv(sh1[:, b]), fv(sc1[:, 4]), fv(sh1[:, 4])], -1))
            maps.append({"xs": xs[cc], "w_in": np.asarray(w_in[l], f32), "pv": pv, "cs": cs_core[cc], "ident": ident})
        res = _run(_prog("A", lambda: build_A(NT, ctx_tiles=tuple(range(HC // 128)))), maps)
        proj = []
        for b in range(B):
            p0, p1 = res[2 * b]["proj"], res[2 * b + 1]["proj"]
            proj.append(np.concatenate([p0[:HC], p1[:HC], p0[HC:], p1[HC:]], 0))
        del res

        def flipseq(a):
            return np.concatenate([a[:CT][::-1], a[CT:][::-1]], 0)

        maps = []
        for cc, (b, z) in enumerate(cores):
            P = proj[b]
            mqT = np.zeros((4, 64, CT + S), f32); mkT = np.zeros((4, 64, CT + S), f32)
            mvk = np.zeros((4, CT + S, 128), f32); mg = np.zeros((4, CT + S, 2), f32); mgb = np.zeros((4, 128, 2), f32)
            for j in range(2):
                h = 2 * z + j
                q = P[:, 512 + h * 64:512 + (h + 1) * 64]; k = P[:, 768 + h * 64:768 + (h + 1) * 64]
                v = P[:, 1024 + h * 64:1024 + (h + 1) * 64]
                for d in range(2):
                    s = j * 2 + d
                    gi = P[:, 1536 + (2 * d) * 4 + h]; gf = P[:, 1536 + (2 * d + 1) * 4 + h]
                    qq, kk, vv, ii, ff = (q, k, v, gi, gf) if d == 0 else tuple(flipseq(a) for a in (q, k, v, gi, gf))
                    mqT[s] = qq.T; mkT[s] = kk.T
                    mvk[s, :, :64] = vv; mvk[s, :, 64:] = kk
                    mg[s, :, 0] = ii; mg[s, :, 1] = ff
                    mgb[s, :, 0] = mlstm_gate_b[l][2 * d, h]; mgb[s, :, 1] = mlstm_gate_b[l][2 * d + 1, h]
            maps.append({"mqT": mqT, "mkT": mkT, "mvk": mvk, "mg": mg, "mgb": mgb, "tri": tri, "ones": ones})
        res = _run(_prog("M1", lambda: build_M1(NKT)), maps)
        hf = [np.zeros((CT + S, 256), f32) for _ in range(B)]
        hb = [np.zeros((CT + S, 256), f32) for _ in range(B)]
        for cc, (b, z) in enumerate(cores):
            mh = res[cc]["mh"]
            for j in range(2):
                h = 2 * z + j
                hf[b][:, h * 64:(h + 1) * 64] = mh[j * 2]
                hb[b][:, h * 64:(h + 1) * 64] = flipseq(mh[j * 2 + 1])
        del res, maps

        maps = []
        dl = _rep(np.asarray(diff_lambda[l], f32).reshape(256))
        dng = _rep(np.asarray(diff_norm_g[l], f32))
        lami = _rep(np.array([lam_init, 1.0 - lam_init], f32))
        for cc, (b, z) in enumerate(cores):
            P = proj[b]
            aqT = np.zeros((2, 2, 64, CT + S), f32); akT = np.zeros((2, 2, 64, CT + S), f32); av = np.zeros((2, CT + S, 128), f32)
            for j in range(2):
                h = 2 * z + j
                for comp in range(2):
                    o = h * 128 + comp * 64
                    aqT[j, comp] = P[:, 1552 + o:1552 + o + 64].T
                    akT[j, comp] = P[:, 2064 + o:2064 + o + 64].T
                av[j] = P[:, 2576 + h * 128:2576 + (h + 1) * 128]
            maps.append({"aqT": aqT, "akT": akT, "av": av, "dl": dl, "dng": dng, "lami": lami, "ones": ones})
        res = _run(_prog("M2", lambda: build_M2(NKT, NCT)), maps)
        ao = [np.zeros((CT + S, 512), f32) for _ in range(B)]
        for cc, (b, z) in enumerate(cores):
            for j in range(2):
                h = 2 * z + j
                ao[b][:, h * 128:(h + 1) * 128] = res[cc]["ao"][j]
        del res, maps

        cw = np.ascontiguousarray(np.asarray(conv_w[l], f32)[:, 0, :].T.reshape(2, 128, 31).transpose(1, 0, 2))
        cp = np.ascontiguousarray(np.stack([conv_b[l], conv_ln_g[l], conv_ln_b[l]], -1).astype(f32).reshape(2, 128, 3).transpose(1, 0, 2))
        maps = []
        for cc, (b, z) in enumerate(cores):
            P = proj[b]
            aT = np.zeros((512, HC + 30 + HS + 30), f32)
            actx = np.zeros((CT + 30, 512), f32); actx[15:15 + CT] = P[:CT, 0:512]
            alat = np.zeros((S + 30, 512), f32); alat[15:15 + S] = P[CT:, 0:512]
            aT[:, 0:HC + 30] = actx[z * HC:z * HC + HC + 30].T
            aT[:, HC + 30:] = alat[z * HS:z * HS + HS + 30].T
            maps.append({"aT": aT, "cw": cw, "cp": cp, "ones": ones})
        res = _run(_prog("C1", lambda: build_C1([HC, HS])), maps)
        convT = [res[cc]["convT"] for cc in range(NCORES)]
        del res, maps

        maps = []
        mng = _rep(np.asarray(mlstm_norm_g[l], f32))
        for cc, (b, z) in enumerate(cores):
            sel = lambda a: np.ascontiguousarray(np.concatenate([a[z * HC:(z + 1) * HC], a[CT + z * HS:CT + (z + 1) * HS]], 0))
            bc = np.stack([_rep(norm2_g[l]), _rep(g1[:, b]), _rep(g1[:, 4]), _rep(sc2[:, b]), _rep(sc2[:, 4]), _rep(sh2[:, b]), _rep(sh2[:, 4])])
            maps.append({"xs": xs[cc], "convT": convT[cc], "hf": sel(hf[b]), "hb": sel(hb[b]), "mo": sel(proj[b][:, 1280:1536]),
                         "ao": sel(ao[b]), "mng": mng, "w_out": np.asarray(w_out[l], f32), "bc": bc, "ident": ident})
        res = _run(_prog("C2", lambda: build_C2(NT, ctx_tiles=tuple(range(HC // 128)))), maps)
        x1 = [res[cc]["x1"] for cc in range(NCORES)]
        h2 = [res[cc]["h2"] for cc in range(NCORES)]
        del res, maps, proj, hf, hb, ao, convT

        keys = np.asarray(peer_keys[l], f32)
        kbd = np.zeros((128, 8, 256), f32)
        for pp in range(2):
            kbd[pp * 64:(pp + 1) * 64, :, pp * 128:(pp + 1) * 128] = keys[:, pp].transpose(2, 0, 1)
        pu = np.asarray(peer_u[l], f32); pvv = np.asarray(peer_v[l], f32); wq = np.asarray(peer_wq[l], f32)
        maps = []
        for cc, (b, z) in enumerate(cores):
            maps.append({"h2": h2[cc], "x1": x1[cc], "wq": wq, "kbd": kbd, "peer_u": pu, "peer_v": pvv,
                         "g2b": np.stack([_rep(g2[:, b]), _rep(g2[:, 4])]), "ident": ident})
        res = _run(_prog("C3", lambda: build_C3(NT, ctx_tiles=tuple(range(HC // 128)))), maps)
        xs = [res[cc]["x2"] for cc in range(NCORES)]
        del res, maps, x1, h2

    fg = _rep(np.asarray(final_g, f32))
    maps = [{"xs": np.ascontiguousarray(xs[cc][HC:]), "fg": fg} for cc in range(NCORES)]
    res = _run(_prog("F", lambda: build_F(HS // 128)), maps)
    out = np.zeros((B, S, D), f32)
    for cc, (b, z) in enumerate(cores):
        out[b, z * HS:(z + 1) * HS] = res[cc]["y"]
    return out


class Cfg:
    def __init__(self, S, CT, DEPTH):
        self.S, self.CT, self.DEPTH = S, CT, DEPTH
        self.NTOK = S + CT
        self.NKT = self.NTOK // 128
        self.NCT = CT // 128
        self.NE = 16384


def _ph_P0(p, g, T):
    D_ = g.DEPTH
    ct = p.sb([128, 8, 2], name="ct"); st = p.sb([128, 8, 2], name="st")
    p.dma(ct[:], T["cT"], writes=["ct"])
    p.add("act", lambda e: e.activation(out=st[:], in_=ct[:], func=AF.Silu), ["ct"], ["st"])
    sbc = p.sb([128, 8, 2, 128], name="sbc")
    p.add("dve", lambda e: e.tensor_copy(out=sbc[:], in_=st[:, :, :, None].to_broadcast([128, 8, 2, 128])), ["st"], ["sbc"])
    wts = [p.sb([128, 8, 512], name=f"wt{i}") for i in range(2)]
    bbt = [p.sb([128, 512], name=f"bbt{i}") for i in range(2)]
    ob = [p.sb([128, 2, 512], name=f"ob{i}") for i in range(2)]
    btT = p.sb([128, 48], name="btT")
    oT = p.sb([128, 48, 2], name="oT")
    pTb = [p.ps([128, 512], name=f"ppT{i}") for i in range(2)]
    pT = [t[:, 0:32].rearrange("p (a b) -> p a b", a=4) for t in pTb]
    pB = [p.ps([128, 512], name=f"ppB{i}") for i in range(4)]
    nblk = 0
    for l in range(D_):
        p.dma(btT[:], T["ada_bT"][l], writes=["btT"])
        wv = T["ada_w"][l].rearrange("(k p) n -> p k n", p=128)
        for blk in range(12):
            i = nblk % 2; nblk += 1
            wt = wts[i]
            p.dma(wt[:], wv[:, :, blk * 512:(blk + 1) * 512], writes=[f"wt{i}"])
            p.dma(bbt[i][:], T["ada_bB"][l, :, blk * 512:(blk + 1) * 512], writes=[f"bbt{i}"])
            for jj in range(4):
                for k in range(8):
                    p.add("pe", lambda e, wt=wt, i=i, jj=jj, k=k: e.matmul(pT[i][:, jj, 0:2], lhsT=wt[:, k, jj * 128:(jj + 1) * 128], rhs=st[:, k, :],
                                                                          start=(k == 0), stop=(k == 7)), [f"wt{i}", "st"], [f"ppT{i}"])
            for jj in range(4):
                j = blk * 4 + jj
                p.add("dve", lambda e, i=i, jj=jj, j=j: e.tensor_scalar(out=oT[:, j, :], in0=pT[i][:, jj, 0:2], scalar1=btT[:, j:j + 1], scalar2=None,
                                                                       op0=ALU.add), [f"ppT{i}", "btT"], ["oT"])
            for n in range(2):
                pb = (2 * blk + n) % 4
                for k in range(8):
                    p.add("pe", lambda e, wt=wt, n=n, k=k, pb=pb: e.matmul(pB[pb][:], lhsT=sbc[:, k, n, :], rhs=wt[:, k, :], start=(k == 0), stop=(k == 7)),
                          [f"wt{i}", "sbc"], [f"ppB{pb}"])
                p.add("dve", lambda e, i=i, n=n, pb=pb: e.tensor_tensor(out=ob[i][:, n, :], in0=pB[pb][:], in1=bbt[i][:], op=ALU.add),
                      [f"ppB{pb}", f"bbt{i}"], [f"ob{i}_{n}"])
                p.dma(T["modB"][l, n, :, blk * 512:(blk + 1) * 512], ob[i][:, n, :], reads=[f"ob{i}_{n}"])
        p.dma(T["modT"][l], oT[:], reads=["oT"])


def _ph_A(p, g, T, l):
    NT = g.NKT
    xs, proj, projT = T["xs"], T["proj"], T["projT"]
    wb = p.sb([128, 8, IN_W], BF16, name="wb")
    load_cast_weight(p, wb, T["w_in"][l], IN_W, "wb", bw=256)
    idt = p.sb([128, 128], name="idt"); p.dma(idt[:], T["ident"], writes=["idt"])
    mT = p.sb([128, 48, 2], name="mT"); p.dma(mT[:], T["modT"][l], writes=["mT"])
    n1 = p.sb([128, 8], name="n1"); p.dma(n1[:], T["n1g"][l], writes=["n1"])
    gm = p.sb([128, 8, 2], name="gm")
    for n in range(2):
        p.add("dve", lambda e, n=n: e.scalar_tensor_tensor(out=gm[:, :, n], in0=mT[:, 8:16, n], scalar=1.0, in1=n1[:], op0=ALU.add, op1=ALU.mult),
              ["mT", "n1"], ["gm"])
    NB = 2
    mk = lambda shape, nm, dt=F32: [p.sb(shape, dt, name=f"{nm}{i}") for i in range(NB)]
    xt = mk([128, D], "xt"); xn = mk([128, D], "xn"); junk = p.sb([128, D], name="junk")
    ss = mk([128, 1], "ss"); rstd = mk([128, 1], "rstd"); hT = mk([128, 8, 128], "hT", BF16)
    ot = mk([128, IN_W], "ot"); ro = mk([128, 1024], "ro"); tmp = mk([128, 4, 512], "tmp"); cst = mk([128, 64], "cst")
    tT = mk([128, 16, 128], "tT")
    tp = [p.ps([128, 512], name=f"tp{i}") for i in range(4)]
    acc = [p.ps([128, 512], name=f"acc{i}") for i in range(4)]
    nacc = 0
    ntp = 0
    for t in range(NT):
        i = t % NB
        isc = t < g.NCT
        n_ = 1 if isc else 0
        r0 = t * 128
        p.dma(xt[i][:], xs[r0:r0 + 128, :], writes=[f"xt{i}"])
        p.dma(cst[i][:], T["cs"][r0:r0 + 128, :], writes=[f"cst{i}"])
        p.add("act", lambda e, i=i: e.activation(out=junk[:], in_=xt[i][:], func=AF.Square, accum_out=ss[i][:, 0:1]), [f"xt{i}"], ["junk", f"ss{i}"])
        p.add("dve", lambda e, i=i: e.tensor_scalar(out=rstd[i][:], in0=ss[i][:], scalar1=1.0 / D, scalar2=EPS, op0=ALU.mult, op1=ALU.add),
              [f"ss{i}"], [f"rstd{i}"])
        p.add("act", lambda e, i=i: e.activation(out=ss[i][:], in_=rstd[i][:], func=AF.Sqrt), [f"rstd{i}"], [f"ss{i}"])
        p.add("dve", lambda e, i=i: e.reciprocal(out=rstd[i][:], in_=ss[i][:]), [f"ss{i}"], [f"rstd{i}"])
        p.add("dve", lambda e, i=i: e.tensor_scalar(out=xn[i][:], in0=xt[i][:], scalar1=rstd[i][:, 0:1], scalar2=None, op0=ALU.mult),
              [f"xt{i}", f"rstd{i}"], [f"xn{i}"])
        for half in range(2):
            tpi = ntp % 4; ntp += 1
            for k in range(half * 4, half * 4 + 4):
                p.add("pe", lambda e, i=i, k=k, tpi=tpi: e.transpose(tp[tpi][:, (k % 4) * 128:(k % 4 + 1) * 128], xn[i][:, k * 128:(k + 1) * 128], idt[:]),
                      [f"xn{i}", "idt"], [f"tp{tpi}"])
            for k in range(half * 4, half * 4 + 4):
                if half == 0:
                    p.add("act", lambda e, i=i, k=k, tpi=tpi, n_=n_: e.activation(
                        out=hT[i][:, k, :], in_=tp[tpi][:, (k % 4) * 128:(k % 4 + 1) * 128], func=AF.Identity,
                        scale=gm[:, k, n_:n_ + 1], bias=mT[:, k, n_:n_ + 1]), [f"tp{tpi}", "gm", "mT"], [f"hT{i}_{k}"])
                else:
                    p.add("dve", lambda e, i=i, k=k, tpi=tpi, n_=n_: e.tensor_scalar(
                        out=hT[i][:, k, :], in0=tp[tpi][:, (k % 4) * 128:(k % 4 + 1) * 128],
                        scalar1=gm[:, k, n_:n_ + 1], scalar2=mT[:, k, n_:n_ + 1], op0=ALU.mult, op1=ALU.add), [f"tp{tpi}", "gm", "mT"], [f"hT{i}_{k}"])
        for blk in range(7):
            c0 = blk * 512
            cw = min(512, IN_W - c0)
            a = nacc % 4; nacc += 1
            for k in range(8):
                p.add("pe", lambda e, i=i, k=k, a=a, c0=c0, cw=cw: e.matmul(acc[a][:, 0:cw], lhsT=hT[i][:, k, :], rhs=wb[:, k, c0:c0 + cw],
                                                                            start=(k == 0), stop=(k == 7)), [f"hT{i}_{k}", "wb"], [f"acc{a}"])
            if blk % 2 == 0:
                p.add("act", lambda e, i=i, a=a, c0=c0, cw=cw: e.activation(out=ot[i][:, c0:c0 + cw], in_=acc[a][:, 0:cw], func=AF.Copy),
                      [f"acc{a}"], [f"ot{i}_{blk}"])
            else:
                p.add("dve", lambda e, i=i, a=a, c0=c0, cw=cw: e.tensor_copy(out=ot[i][:, c0:c0 + cw], in_=acc[a][:, 0:cw]), [f"acc{a}"], [f"ot{i}_{blk}"])
        src = ot[i][:, 1552:2576].rearrange("p (g r two) -> p g r two", g=16, two=2)
        dst = ro[i][:].rearrange("p (g r two) -> p g r two", g=16, two=2)
        t1 = src[:, :, :, 0]; t2 = src[:, :, :, 1]
        cosb = cst[i][:, None, 0:32].to_broadcast([128, 16, 32])
        sinb = cst[i][:, None, 32:64].to_broadcast([128, 16, 32])
        tm = [tmp[i][:, j, :].rearrange("p (g r) -> p g r", g=16) for j in range(4)]
        rk = [f"ot{i}_3", f"ot{i}_4", f"ot{i}_5", f"cst{i}"]
        p.add("dve", lambda e, tm=tm, t1=t1, cosb=cosb: e.tensor_tensor(out=tm[0], in0=t1, in1=cosb, op=ALU.mult), rk, [f"tmp{i}_0"])
        p.add("dve", lambda e, tm=tm, t2=t2, sinb=sinb: e.tensor_tensor(out=tm[1], in0=t2, in1=sinb, op=ALU.mult), rk, [f"tmp{i}_1"])
        p.add("dve", lambda e, tm=tm, t1=t1, sinb=sinb: e.tensor_tensor(out=tm[2], in0=t1, in1=sinb, op=ALU.mult), rk, [f"tmp{i}_2"])
        p.add("dve", lambda e, tm=tm, t2=t2, cosb=cosb: e.tensor_tensor(out=tm[3], in0=t2, in1=cosb, op=ALU.mult), rk, [f"tmp{i}_3"])
        p.add("dve", lambda e, tm=tm, dst=dst: e.tensor_tensor(out=dst[:, :, :, 0], in0=tm[0], in1=tm[1], op=ALU.subtract),
              [f"tmp{i}_0", f"tmp{i}_1"], [f"ro{i}a"])
        p.add("dve", lambda e, tm=tm, dst=dst: e.tensor_tensor(out=dst[:, :, :, 1], in0=tm[2], in1=tm[3], op=ALU.add),
              [f"tmp{i}_2", f"tmp{i}_3"], [f"ro{i}b"])
        p.dma(proj[r0:r0 + 128, 0:1552], ot[i][:, 0:1552], reads=[f"ot{i}_{b}" for b in range(4)])
        p.dma(proj[r0:r0 + 128, 1552:2576], ro[i][:], reads=[f"ro{i}a", f"ro{i}b"])
        p.dma(proj[r0:r0 + 128, 2576:IN_W], ot[i][:, 2576:IN_W], reads=[f"ot{i}_5", f"ot{i}_6"])
        srcs = [(ot[i], c * 128, [f"ot{i}_0", f"ot{i}_1"]) for c in range(8)] + [(ro[i], c * 128, [f"ro{i}a", f"ro{i}b"]) for c in range(8)]
        for q4 in range(4):
            tpi = ntp % 4; ntp += 1
            for c in range(4):
                sap, c0, keys = srcs[q4 * 4 + c]
                p.add("pe", lambda e, sap=sap, c0=c0, tpi=tpi, c=c: e.transpose(tp[tpi][:, c * 128:(c + 1) * 128], sap[:, c0:c0 + 128], idt[:]),
                      keys + ["idt"], [f"tp{tpi}"])
            if q4 % 2 == 0:
                p.add("act", lambda e, i=i, q4=q4, tpi=tpi: e.activation(out=tT[i][:, q4 * 4:q4 * 4 + 4, :], in_=tp[tpi][:].rearrange("p (k n) -> p k n", k=4),
                                                                         func=AF.Copy), [f"tp{tpi}"], [f"tT{i}_{q4}"])
            else:
                p.add("dve", lambda e, i=i, q4=q4, tpi=tpi: e.tensor_copy(out=tT[i][:, q4 * 4:q4 * 4 + 4, :], in_=tp[tpi][:].rearrange("p (k n) -> p k n", k=4)),
                      [f"tp{tpi}"], [f"tT{i}_{q4}"])
        p.dma(projT[:, r0:r0 + 128].rearrange("(k p) n -> p k n", p=128), tT[i][:], reads=[f"tT{i}_{q}" for q in range(4)])


def _ph_M1(p, g, T, l, GC=4):
    NKT, NCT = g.NKT, g.NCT
    proj, projT, mixo = T["proj"], T["projT"], T["mixo"]
    tri = p.sb([128, 128], name="tri_sb"); p.dma(tri[:], T["tri"], writes=["tri"])
    triL = p.sb([128, 128], name="triL_sb"); p.dma(triL[:], T["triL"], writes=["triL"])
    ones = p.sb([128, 128], name="ones_sb"); p.dma(ones[:], T["ones"], writes=["ones"])
    banks = [p.ps([128, 512], name=f"bank{i}") for i in range(7)]
    NS = 4
    NBUF = 2
    B = {}
    for s in range(NS):
        B[s, "gb"] = p.sb([128, 2], name=f"gb{s}")
        B[s, "Cf"] = p.sb([64, 65], name=f"Cf{s}")
        B[s, "Cb"] = p.sb([64, 65], BF16, name=f"Cb{s}")
        B[s, "tmpC"] = p.sb([64, 65], name=f"tmpC{s}")
        for i in range(NBUF):
            k = (s, i)
            B[k, "qTf"] = p.sb([64, GC * 128], name=f"qTf{s}_{i}")
            B[k, "kTf"] = p.sb([64, GC * 128], name=f"kTf{s}_{i}")
            B[k, "vf"] = p.sb([128, GC, 64], name=f"vf{s}_{i}")
            B[k, "kf"] = p.sb([128, GC, 64], name=f"kf{s}_{i}")
            B[k, "gf"] = p.sb([128, GC, 16], name=f"gf{s}_{i}")
            B[k, "qTb"] = p.sb([64, GC * 128], BF16, name=f"qTb{s}_{i}")
            B[k, "kTb"] = p.sb([64, GC * 128], BF16, name=f"kTb{s}_{i}")
            B[k, "vaug"] = p.sb([128, GC, 65], BF16, name=f"vaug{s}_{i}")
            B[k, "kb"] = p.sb([128, GC, 64], BF16, name=f"kb{s}_{i}")
            B[k, "h"] = p.sb([128, GC, 64], name=f"h{s}_{i}")
            for nm in ("gi", "sp", "a", "b", "eG"):
                B[k, nm] = p.sb([128, GC], name=f"{nm}{s}_{i}")
            B[k, "WT"] = p.sb([128, 128], BF16, name=f"WT{s}_{i}")
            B[k, "d"] = p.sb([128, 4], name=f"d{s}_{i}")
            p.add("dve", lambda e, k=k: e.memset(B[k, "vaug"][:, :, 64:65], 1.0), [], [f"vaug1_{s}_{i}"])
    def groups(rev):
        out = []
        for lo, hi in ((0, NCT), (NCT, NKT)):
            rng = list(range(lo, hi))
            for g0 in range(0, len(rng), GC):
                out.append(rng[g0:g0 + GC])
        if rev:
            out = []
            for lo, hi in ((0, NCT), (NCT, NKT)):
                rng = list(range(lo, hi))[::-1]
                for g0 in range(0, len(rng), GC):
                    out.append(rng[g0:g0 + GC])
        return out
    gcount = {s: 0 for s in range(NS)}
    for hp in range(2):
        for s in range(NS):
            h, dr = 2 * hp + s // 2, s % 2
            p.dma(B[s, "gb"][:], T["mgb"][l, h * 2 + dr], writes=[f"gb{s}"])
            p.add("dve", lambda e, s=s: e.memset(B[s, "Cf"][:], 0.0), [], [f"Cf{s}"])
            p.add("dve", lambda e, s=s: e.memset(B[s, "Cb"][:], 0.0), [], [f"Cb{s}"])
        glists = [groups(s % 2 == 1) for s in range(NS)]
        for gi_ in range(len(glists[0])):
            for s in range(NS):
                h, dr = 2 * hp + s // 2, s % 2
                chunks = glists[s][gi_]
                lo, hi = min(chunks), max(chunks) + 1
                gc = hi - lo
                i = gcount[s] % NBUF; gcount[s] += 1
                k = (s, i)
                sfx = f"{s}_{i}"
                Tt = {n: B[k, n] for n in ("qTf", "kTf", "vf", "kf", "gf", "qTb", "kTb", "vaug", "kb", "h", "gi", "sp", "a", "b", "eG", "WT", "d")}
                Cf, Cb, tmpC, gb = B[s, "Cf"], B[s, "Cb"], B[s, "tmpC"], B[s, "gb"]
                bS, bX, bU = banks[(s % 2) * 3], banks[(s % 2) * 3 + 1], banks[(s % 2) * 3 + 2]
                kS, kX, kU = f"bank{(s%2)*3}", f"bank{(s%2)*3+1}", f"bank{(s%2)*3+2}"
                bP = banks[6]
                trm = triL if dr else tri
                ktr = "triL" if dr else "tri"
                W = gc * 128
                t0 = lo * 128
                p.dma(Tt["qTf"][:, 0:W], projT[512 + h * 64:512 + (h + 1) * 64, t0:t0 + W], writes=[f"qTf{sfx}"])
                p.dma(Tt["kTf"][:, 0:W], projT[768 + h * 64:768 + (h + 1) * 64, t0:t0 + W], writes=[f"kTf{sfx}"])
                pr = proj[t0:t0 + W, :].rearrange("(c p) f -> p c f", p=128)
                p.dma(Tt["vf"][:, 0:gc, :], pr[:, :, 1024 + h * 64:1024 + (h + 1) * 64], writes=[f"vf{sfx}"])
                p.dma(Tt["kf"][:, 0:gc, :], pr[:, :, 768 + h * 64:768 + (h + 1) * 64], writes=[f"kf{sfx}"])
                ci = (2 * dr) * 4 + h
                cf = (2 * dr + 1) * 4 + h
                p.dma(Tt["gf"][:, 0:gc, :], pr[:, :, 1536:1552], writes=[f"gf{sfx}a"])
                gk = [f"gf{sfx}a"]
                p.add("dve", lambda e, Tt=Tt, gb=gb, gc=gc, ci=ci: e.tensor_scalar(out=Tt["gi"][:, 0:gc], in0=Tt["gf"][:, 0:gc, ci], scalar1=gb[:, 0:1],
                                                                       scalar2=None, op0=ALU.add), gk + [f"gb{s}"], [f"gi{sfx}"])
                p.add("dve", lambda e, Tt=Tt, gb=gb, gc=gc, cf=cf: e.tensor_scalar(out=Tt["sp"][:, 0:gc], in0=Tt["gf"][:, 0:gc, cf], scalar1=gb[:, 1:2],
                                                                       scalar2=None, op0=ALU.add), gk + [f"gb{s}"], [f"sp{sfx}"])
                p.add("act", lambda e, Tt=Tt, gc=gc: e.activation(out=Tt["sp"][:, 0:gc], in_=Tt["sp"][:, 0:gc], func=AF.Exp, scale=-1.0),
                      [f"sp{sfx}"], [f"sp{sfx}"])
                p.add("dve", lambda e, Tt=Tt, gc=gc: e.tensor_scalar(out=Tt["sp"][:, 0:gc], in0=Tt["sp"][:, 0:gc], scalar1=1.0, scalar2=None,
                                                                op0=ALU.add), [f"sp{sfx}"], [f"sp{sfx}"])
                p.add("act", lambda e, Tt=Tt, gc=gc: e.activation(out=Tt["sp"][:, 0:gc], in_=Tt["sp"][:, 0:gc], func=AF.Ln), [f"sp{sfx}"], [f"sp{sfx}"])
                p.add("pe", lambda e, Tt=Tt, gc=gc, bP=bP, trm=trm: e.matmul(bP[:, 0:gc], lhsT=trm[:], rhs=Tt["sp"][:, 0:gc], start=True, stop=True),
                      [ktr, f"sp{sfx}"], ["bank6"])
                p.add("pe", lambda e, Tt=Tt, gc=gc, bP=bP: e.matmul(bP[:, 64:64 + gc], lhsT=ones[:], rhs=Tt["sp"][:, 0:gc], start=True, stop=True),
                      ["ones", f"sp{sfx}"], ["bank6"])
                p.add("act", lambda e, Tt=Tt, gc=gc, bP=bP: e.activation(out=Tt["a"][:, 0:gc], in_=bP[:, 0:gc], func=AF.Exp, scale=-1.0), ["bank6"], [f"a{sfx}"])
                p.add("act", lambda e, Tt=Tt, gc=gc, bP=bP: e.activation(out=Tt["eG"][:, 0:gc], in_=bP[:, 64:64 + gc], func=AF.Exp, scale=-1.0), ["bank6"], [f"eG{sfx}"])
                p.add("act", lambda e, Tt=Tt, gc=gc, bP=bP: e.activation(out=Tt["b"][:, 0:gc], in_=bP[:, 0:gc], func=AF.Identity), ["bank6"], [f"b{sfx}"])
                p.add("dve", lambda e, Tt=Tt, gc=gc: e.tensor_tensor(out=Tt["b"][:, 0:gc], in0=Tt["b"][:, 0:gc], in1=Tt["gi"][:, 0:gc], op=ALU.add),
                      [f"b{sfx}", f"gi{sfx}"], [f"b{sfx}"])
                p.add("act", lambda e, Tt=Tt, gc=gc: e.activation(out=Tt["b"][:, 0:gc], in_=Tt["b"][:, 0:gc], func=AF.Exp), [f"b{sfx}"], [f"b{sfx}"])
                p.add("act", lambda e, Tt=Tt, W=W: e.activation(out=Tt["qTb"][:, 0:W], in_=Tt["qTf"][:, 0:W], func=AF.Copy), [f"qTf{sfx}"], [f"qTb{sfx}"])
                p.add("dve", lambda e, Tt=Tt, W=W: e.tensor_scalar(out=Tt["kTb"][:, 0:W], in0=Tt["kTf"][:, 0:W], scalar1=0.125, scalar2=None, op0=ALU.mult),
                      [f"kTf{sfx}"], [f"kTb{sfx}"])
                p.add("act", lambda e, Tt=Tt, gc=gc: e.activation(out=Tt["vaug"][:, 0:gc, 0:64], in_=Tt["vf"][:, 0:gc, :], func=AF.Copy),
                      [f"vf{sfx}"], [f"vaug{sfx}"])
                p.add("dve", lambda e, Tt=Tt, gc=gc: e.scalar_tensor_tensor(
                    out=Tt["kb"][:, 0:gc, :], in0=Tt["kf"][:, 0:gc, :], scalar=0.125,
                    in1=Tt["b"][:, 0:gc, None].to_broadcast([128, gc, 64]), op0=ALU.mult, op1=ALU.mult), [f"kf{sfx}", f"b{sfx}"], [f"kb{sfx}"])
                for c in chunks:
                    cc = c - lo
                    cs = slice(cc * 128, (cc + 1) * 128)
                    p.add("pe", lambda e, Tt=Tt, cs=cs, bS=bS: e.matmul(bS[:, 0:128], lhsT=Tt["kTb"][:, cs], rhs=Tt["qTb"][:, cs], start=True, stop=True),
                          [f"kTb{sfx}", f"qTb{sfx}"], [kS])
                    p.add("dve", lambda e, Tt=Tt, cc=cc, bS=bS, trm=trm: e.scalar_tensor_tensor(
                        out=Tt["WT"][:], in0=bS[:, 0:128], scalar=Tt["b"][:, cc:cc + 1], in1=trm[:], op0=ALU.mult, op1=ALU.mult),
                        [kS, f"b{sfx}", ktr], [f"WT{sfx}"])
                    p.add("pe", lambda e, Tt=Tt, cc=cc, bX=bX: e.matmul(bX[:, 0:65], lhsT=Tt["WT"][:], rhs=Tt["vaug"][:, cc, :], start=True, stop=False),
                          [f"WT{sfx}", f"vaug{sfx}", f"vaug1_{sfx}"], [kX])
                    p.add("pe", lambda e, Tt=Tt, cs=cs, bX=bX, Cb=Cb: e.matmul(bX[:, 0:65], lhsT=Tt["qTb"][:, cs], rhs=Cb[:], start=False, stop=True),
                          [f"qTb{sfx}", f"Cb{s}"], [kX])
                    p.add("pe", lambda e, Tt=Tt, cc=cc, bU=bU: e.matmul(bU[0:64, 0:65], lhsT=Tt["kb"][:, cc, :], rhs=Tt["vaug"][:, cc, :], start=True, stop=True),
                          [f"kb{sfx}", f"vaug{sfx}", f"vaug1_{sfx}"], [kU])
                    d = Tt["d"]
                    p.add("act", lambda e, Tt=Tt, cc=cc, bX=bX, d=d: e.activation(out=d[:, 0:1], in_=bX[:, 64:65], func=AF.Abs, scale=Tt["a"][:, cc:cc + 1]),
                          [kX, f"a{sfx}"], [f"d0{sfx}"])
                    p.add("dve", lambda e, d=d: e.tensor_scalar(out=d[:, 3:4], in0=d[:, 0:1], scalar1=1.0, scalar2=None, op0=ALU.max), [f"d0{sfx}"], [f"d3{sfx}"])
                    p.add("dve", lambda e, d=d: e.reciprocal(out=d[:, 1:2], in_=d[:, 3:4]), [f"d3{sfx}"], [f"d1{sfx}"])
                    p.add("dve", lambda e, Tt=Tt, cc=cc, d=d: e.tensor_tensor(out=d[:, 2:3], in0=d[:, 1:2], in1=Tt["a"][:, cc:cc + 1], op=ALU.mult),
                          [f"d1{sfx}", f"a{sfx}"], [f"d2{sfx}"])
                    p.add("dve", lambda e, Tt=Tt, cc=cc, bX=bX, d=d: e.tensor_scalar(out=Tt["h"][:, cc, :], in0=bX[:, 0:64], scalar1=d[:, 2:3], scalar2=None,
                                                                                op0=ALU.mult), [kX, f"d2{sfx}"], [f"h{sfx}"])
                    p.add("dve", lambda e, bU=bU, Cf=Cf, tmpC=tmpC: e.tensor_tensor(out=tmpC[:], in0=bU[0:64, 0:65], in1=Cf[:], op=ALU.add),
                          [kU, f"Cf{s}"], [f"tmpC{s}"])
                    p.add("dve", lambda e, Tt=Tt, cc=cc, Cf=Cf, tmpC=tmpC: e.tensor_scalar(out=Cf[:], in0=tmpC[:], scalar1=Tt["eG"][0:64, cc:cc + 1], scalar2=None,
                                                                                      op0=ALU.mult), [f"tmpC{s}", f"eG{sfx}"], [f"Cf{s}"])
                    p.add("act", lambda e, Tt=Tt, cc=cc, Cb=Cb, tmpC=tmpC: e.activation(out=Cb[:], in_=tmpC[:], func=AF.Copy, scale=Tt["eG"][0:64, cc:cc + 1]),
                          [f"tmpC{s}", f"eG{sfx}"], [f"Cb{s}"])
                oc = dr * 256 + h * 64
                p.dma(mixo[t0:t0 + W, oc:oc + 64].rearrange("(c p) f -> p c f", p=128), Tt["h"][:, 0:gc, :], reads=[f"h{sfx}"])


def _ph_M2(p, g, T, l):
    NKT, NCT = g.NKT, g.NCT
    L = NKT * 128
    proj, projT, mixo = T["proj"], T["projT"], T["mixo"]
    ones = p.sb([128, 128], name="ones_sb"); p.dma(ones[:], T["ones"], writes=["ones"])
    dl = p.sb([128, 256], name="dl_sb"); p.dma(dl[:], T["dl"][l], writes=["dl"])
    gsc = p.sb([128, 128], name="gsc"); p.dma(gsc[:], T["dng"][l], writes=["gsc"])
    lami = p.sb([128, 2], name="lami_sb"); p.dma(lami[:], T["lami"][l], writes=["lami"])
    junk = p.sb([128, 128], name="junk")
    lm = p.sb([128, 4], name="lm")
    p.add("dve", lambda e: e.scalar_tensor_tensor(out=junk[:, 0:64], in0=dl[:, 0:64], scalar=1.0, in1=dl[:, 64:128],
                                                  op0=ALU.mult, op1=ALU.mult, accum_out=lm[:, 0:1]), ["dl"], ["junk", "lm0"])
    p.add("dve", lambda e: e.scalar_tensor_tensor(out=junk[:, 64:128], in0=dl[:, 128:192], scalar=1.0, in1=dl[:, 192:256],
                                                  op0=ALU.mult, op1=ALU.mult, accum_out=lm[:, 1:2]), ["dl"], ["junk", "lm1"])
    p.add("act", lambda e: e.activation(out=lm[:, 0:2], in_=lm[:, 0:2], func=AF.Exp), ["lm0", "lm1"], ["lm01"])
    p.add("dve", lambda e: e.tensor_tensor(out=lm[:, 2:3], in0=lm[:, 1:2], in1=lm[:, 0:1], op=ALU.subtract), ["lm01"], ["lm2"])
    p.add("dve", lambda e: e.tensor_scalar(out=lm[:, 3:4], in0=lm[:, 2:3], scalar1=lami[:, 0:1], scalar2=None, op0=ALU.subtract), ["lm2", "lami"], ["nlam"])
    p.add("dve", lambda e: e.tensor_scalar(out=gsc[:], in0=gsc[:], scalar1=lami[:, 1:2], scalar2=None, op0=ALU.mult), ["gsc", "lami"], ["gsc"])
    banks = [p.ps([128, 512], name=f"bank{i}") for i in range(8)]
    qTb = p.sb([64, 2, L], BF16, name="qTb")
    kTb = p.sb([64, 2, L], BF16, name="kTb")
    vaug = p.sb([128, NKT, 129], BF16, name="vaug")
    p.add("dve", lambda e: e.memset(vaug[:, :, 128:129], 1.0), [], ["vaug1"])
    PW = 2048
    stg = [p.sb([64, PW], name=f"stg{i}") for i in range(2)]
    sq = p.sb([64, PW], name="sq")
    VG = 16
    vst = [p.sb([128, VG, 128], name=f"vst{i}") for i in range(2)]
    mx = p.sb([128, 8], name="mx")
    nb = p.sb([128, 4], name="nb")
    pT = [p.sb([128, 512], BF16, name=f"pT{i}") for i in range(6)]
    ev = [p.sb([128, 8], name=f"ev{i}") for i in range(2)]
    o1 = [p.sb([128, 128], name=f"o1_{i}") for i in range(2)]
    oo = [p.sb([128, 128], name=f"oo_{i}") for i in range(2)]
    yy = [p.sb([128, 128], name=f"yy_{i}") for i in range(2)]
    nstg = nvst = nit = nblk = nev = 0
    for s in range(4):
        h = s
        p.add("dve", lambda e: e.memset(mx[:], 0.0), [], ["mx", "mx4"])
        for which, (rbase, dst) in enumerate(((1024, qTb), (1536, kTb))):
            for comp in range(2):
                rr = rbase + h * 128 + comp * 64
                for c0 in range(0, L, PW):
                    cw = min(PW, L - c0)
                    si = nstg % 2; nstg += 1
                    st = stg[si]
                    p.dma(st[:, 0:cw], projT[rr:rr + 64, c0:c0 + cw], writes=[f"stg{si}"])
                    p.add("dve", lambda e, st=st, dst=dst, comp=comp, c0=c0, cw=cw: e.tensor_copy(out=dst[:, comp, c0:c0 + cw], in_=st[:, 0:cw]),
                          [f"stg{si}"], ["qTb" if which == 0 else "kTb"])
                    p.add("act", lambda e, st=st, cw=cw: e.activation(out=sq[:, 0:cw], in_=st[:, 0:cw], func=AF.Square), [f"stg{si}"], ["sq"])
                    for b0 in range(0, cw, 512):
                        bw = min(512, cw - b0)
                        p.add("pe", lambda e, b0=b0, bw=bw: e.matmul(banks[6][:, 0:bw], lhsT=ones[0:64, :], rhs=sq[:, b0:b0 + bw], start=True, stop=True),
                              ["ones", "sq"], ["bank6"])
                        p.add("dve", lambda e, bw=bw: e.reduce_max(out=mx[:, 4:5], in_=banks[6][:, 0:bw], axis=AX.X), ["bank6"], ["mx4"])
                        col = which * 2 + comp
                        p.add("dve", lambda e, col=col: e.tensor_tensor(out=mx[:, col:col + 1], in0=mx[:, col:col + 1], in1=mx[:, 4:5], op=ALU.max),
                              ["mx4", "mx"], ["mx"])
        for g0 in range(0, NKT, VG):
            gn = min(VG, NKT - g0)
            vi = nvst % 2; nvst += 1
            p.dma(vst[vi][:, 0:gn, :], proj[g0 * 128:(g0 + gn) * 128, 2576 + h * 128:2576 + (h + 1) * 128].rearrange("(c p) f -> p c f", p=128),
                  writes=[f"vst{vi}"])
            p.add("act", lambda e, vi=vi, g0=g0, gn=gn: e.activation(out=vaug[:, g0:g0 + gn, 0:128], in_=vst[vi][:, 0:gn, :], func=AF.Copy),
                  [f"vst{vi}"], ["vaug"])
        p.add("dve", lambda e: e.tensor_tensor(out=nb[:, 2:4], in0=mx[:, 0:2], in1=mx[:, 2:4], op=ALU.mult), ["mx"], ["nb2"])
        p.add("act", lambda e: e.activation(out=nb[:, 2:4], in_=nb[:, 2:4], func=AF.Sqrt, scale=1.0 / 64.0), ["nb2"], ["nb2"])
        p.add("dve", lambda e: e.tensor_scalar(out=nb[:, 0:2], in0=nb[:, 2:4], scalar1=60.0, scalar2=-1.0, op0=ALU.min, op1=ALU.mult), ["nb2"], ["nb"])
        blocks = [(0, NCT, 0, NCT)]
        for q0 in range(NCT, NKT, 4):
            blocks.append((q0, min(4, NKT - q0), 0, NKT))
        for (q0, nq, k0, nk) in blocks:
            oset = 0; nblk += 1
            obanks = [banks[oset * 3 + i] for i in range(3)]
            okeys = [f"bank{oset*3+i}" for i in range(3)]
            for i in range(3):
                p.add("dve", lambda e, i=i, obanks=obanks: e.memset(obanks[i][:], 0.0), [], [okeys[i]])

            def oacc(comp, j, obanks=obanks, okeys=okeys):
                a = comp * 4 + j
                return obanks[a // 3][:, (a % 3) * 129:(a % 3) * 129 + 129], okeys[a // 3]
            QW = nq * 128
            for kt in range(k0, k0 + nk):
                for comp in range(2):
                    sb_i = 3 + nit % 5
                    pi = nit % 6
                    nit += 1
                    p.add("pe", lambda e, comp=comp, kt=kt, sb_i=sb_i, q0=q0, QW=QW: e.matmul(
                        banks[sb_i][:, 0:QW], lhsT=kTb[:, comp, kt * 128:(kt + 1) * 128], rhs=qTb[:, comp, q0 * 128:q0 * 128 + QW],
                        start=True, stop=True), ["kTb", "qTb"], [f"bank{sb_i}"])
                    p.add("act", lambda e, comp=comp, sb_i=sb_i, pi=pi, QW=QW: e.activation(
                        out=pT[pi][:, 0:QW], in_=banks[sb_i][:, 0:QW], func=AF.Exp, scale=0.125, bias=nb[:, comp:comp + 1]),
                        [f"bank{sb_i}", "nb"], [f"pT{pi}"])
                    for j in range(nq):
                        oap, okey = oacc(comp, j)
                        p.add("pe", lambda e, oap=oap, pi=pi, j=j, kt=kt: e.matmul(
                            oap, lhsT=pT[pi][:, j * 128:(j + 1) * 128], rhs=vaug[:, kt, :], start=False, stop=False,
                            skip_group_check=True), [f"pT{pi}", "vaug", "vaug1"], [okey])
            for j in range(nq):
                ei = nev % 2; nev += 1
                E = ev[ei]
                (o1ap, k1), (o2ap, k2) = oacc(0, j), oacc(1, j)
                sfx = f"_{ei}"
                p.add("dve", lambda e, E=E, o1ap=o1ap: e.reciprocal(out=E[:, 0:1], in_=o1ap[:, 128:129]), [k1], ["ev0" + sfx])
                p.add("dve", lambda e, E=E, o2ap=o2ap: e.reciprocal(out=E[:, 1:2], in_=o2ap[:, 128:129]), [k2], ["ev1" + sfx])
                p.add("dve", lambda e, E=E: e.tensor_tensor(out=E[:, 2:3], in0=E[:, 1:2], in1=lm[:, 3:4], op=ALU.mult), ["ev1" + sfx, "nlam"], ["ev2" + sfx])
                p.add("dve", lambda e, E=E, o1ap=o1ap, ei=ei: e.tensor_scalar(out=o1[ei][:], in0=o1ap[:, 0:128], scalar1=E[:, 0:1], scalar2=None, op0=ALU.mult),
                      [k1, "ev0" + sfx], ["o1" + sfx])
                p.add("dve", lambda e, E=E, o2ap=o2ap, ei=ei: e.scalar_tensor_tensor(out=oo[ei][:], in0=o2ap[:, 0:128], scalar=E[:, 2:3], in1=o1[ei][:],
                                                                                     op0=ALU.mult, op1=ALU.add), [k2, "ev2" + sfx, "o1" + sfx], ["oo" + sfx])
                p.add("dve", lambda e, E=E, ei=ei: e.scalar_tensor_tensor(out=junk[:], in0=oo[ei][:], scalar=1.0, in1=oo[ei][:], op0=ALU.mult, op1=ALU.mult,
                                                                          accum_out=E[:, 3:4]), ["oo" + sfx], ["junk", "ev3" + sfx])
                p.add("dve", lambda e, E=E: e.tensor_scalar(out=E[:, 4:5], in0=E[:, 3:4], scalar1=1.0 / 128.0, scalar2=EPS, op0=ALU.mult, op1=ALU.add),
                      ["ev3" + sfx], ["ev4" + sfx])
                p.add("act", lambda e, E=E: e.activation(out=E[:, 5:6], in_=E[:, 4:5], func=AF.Sqrt), ["ev4" + sfx], ["ev5" + sfx])
                p.add("dve", lambda e, E=E: e.reciprocal(out=E[:, 6:7], in_=E[:, 5:6]), ["ev5" + sfx], ["ev6" + sfx])
                p.add("dve", lambda e, E=E, ei=ei: e.scalar_tensor_tensor(out=yy[ei][:], in0=oo[ei][:], scalar=E[:, 6:7], in1=gsc[:], op0=ALU.mult, op1=ALU.mult),
                      ["oo" + sfx, "ev6" + sfx, "gsc"], ["yy" + sfx])
                r0 = (q0 + j) * 128
                p.dma(mixo[r0:r0 + 128, 512 + h * 128:512 + (h + 1) * 128], yy[ei][:], reads=["yy" + sfx])


def _ph_C1(p, g, T, l):
    projT, co = T["projT"], T["convT"]
    ones = p.sb([128, 128], name="ones_sb"); p.dma(ones[:], T["ones"], writes=["ones"])
    cw = p.sb([128, 2, 31], name="cw_sb"); p.dma(cw[:], T["cw"][l], writes=["cw"])
    cp = p.sb([128, 2, 3], name="cp_sb"); p.dma(cp[:], T["cp"][l], writes=["cp"])
    N = 512
    a1 = [p.sb([128, N + 30], name=f"a1_{i}") for i in range(2)]
    a2 = [p.sb([128, N + 30], name=f"a2_{i}") for i in range(2)]
    u = [p.sb([128, N + 30], name=f"u_{i}") for i in range(2)]
    acc = [[p.sb([128, N], name=f"acc_{i}_{r}") for r in range(2)] for i in range(2)]
    y = [p.sb([128, N], name=f"y_{i}") for i in range(2)]
    ysq = [p.sb([128, N], name=f"ysq_{i}") for i in range(2)]
    mean = p.sb([128, N], name="mean"); msq = p.sb([128, N], name="msq"); rstd = p.sb([128, N], name="rstd")
    zz = [p.sb([128, N], name=f"zz_{i}") for i in range(2)]
    bA = p.ps([128, 512], name="bankA"); bB = p.ps([128, 512], name="bankB")
    for (s0, ns) in ((0, g.CT), (g.CT, g.S)):
        for t0 in range(0, ns, N):
            n = min(N, ns - t0)
            lo = max(t0 - 15, 0); hi = min(t0 + n + 15, ns)
            d0 = lo - (t0 - 15)
            clip = (lo != t0 - 15) or (hi != t0 + n + 15)
            for j in range(2):
                for (buf, nm, rb) in ((a1[j], f"a1_{j}", j * 128), (a2[j], f"a2_{j}", 256 + j * 128)):
                    if clip:
                        p.add("dve", lambda e, buf=buf, n=n: e.memset(buf[:, 0:n + 30], 0.0), [], [nm])
                    p.dma(buf[:, d0:d0 + hi - lo], projT[rb:rb + 128, s0 + lo:s0 + hi], writes=[nm])
                p.add("act", lambda e, j=j, n=n: e.activation(out=a2[j][:, 0:n + 30], in_=a2[j][:, 0:n + 30], func=AF.Sigmoid), [f"a2_{j}"], [f"a2_{j}"])
                p.add("dve", lambda e, j=j, n=n: e.tensor_tensor(out=u[j][:, 0:n + 30], in0=a1[j][:, 0:n + 30], in1=a2[j][:, 0:n + 30], op=ALU.mult),
                      [f"a1_{j}", f"a2_{j}"], [f"u_{j}"])
                p.add("dve", lambda e, j=j, n=n: e.tensor_scalar(out=acc[j][0][:, 0:n], in0=u[j][:, 0:n], scalar1=cw[:, j, 0:1], scalar2=None, op0=ALU.mult),
                      [f"u_{j}", "cw"], [f"acc_{j}_0"])
                for k in range(1, 31):
                    src, dst = acc[j][(k - 1) % 2], acc[j][k % 2]
                    p.add("dve", lambda e, j=j, n=n, k=k, src=src, dst=dst: e.scalar_tensor_tensor(
                        out=dst[:, 0:n], in0=u[j][:, k:k + n], scalar=cw[:, j, k:k + 1], in1=src[:, 0:n], op0=ALU.mult, op1=ALU.add),
                        [f"u_{j}", "cw", f"acc_{j}_{(k-1)%2}"], [f"acc_{j}_{k%2}"])
                p.add("dve", lambda e, j=j, n=n: e.tensor_scalar(out=y[j][:, 0:n], in0=acc[j][0][:, 0:n], scalar1=cp[:, j, 0:1], scalar2=None, op0=ALU.add),
                      [f"acc_{j}_0", "cp"], [f"y_{j}"])
                p.add("act", lambda e, j=j, n=n: e.activation(out=ysq[j][:, 0:n], in_=y[j][:, 0:n], func=AF.Square), [f"y_{j}"], [f"ysq_{j}"])
            for j in range(2):
                p.add("pe", lambda e, j=j, n=n: e.matmul(bA[:, 0:n], lhsT=ones[:], rhs=y[j][:, 0:n], start=(j == 0), stop=(j == 1)), ["ones", f"y_{j}"], ["bankA"])
            for j in range(2):
                p.add("pe", lambda e, j=j, n=n: e.matmul(bB[:, 0:n], lhsT=ones[:], rhs=ysq[j][:, 0:n], start=(j == 0), stop=(j == 1)), ["ones", f"ysq_{j}"], ["bankB"])
            p.add("act", lambda e, n=n: e.activation(out=mean[:, 0:n], in_=bA[:, 0:n], func=AF.Copy, scale=1.0 / 256), ["bankA"], ["mean"])
            p.add("act", lambda e, n=n: e.activation(out=msq[:, 0:n], in_=bA[:, 0:n], func=AF.Square, scale=1.0 / 256), ["bankA"], ["msq"])
            p.add("dve", lambda e, n=n: e.scalar_tensor_tensor(out=rstd[:, 0:n], in0=bB[:, 0:n], scalar=1.0 / 256, in1=msq[:, 0:n], op0=ALU.mult, op1=ALU.subtract),
                  ["bankB", "msq"], ["rstd"])
            p.add("dve", lambda e, n=n: e.tensor_scalar(out=rstd[:, 0:n], in0=rstd[:, 0:n], scalar1=EPS, scalar2=None, op0=ALU.add), ["rstd"], ["rstd"])
            p.add("act", lambda e, n=n: e.activation(out=rstd[:, 0:n], in_=rstd[:, 0:n], func=AF.Sqrt), ["rstd"], ["rstd"])
            p.add("dve", lambda e, n=n: e.reciprocal(out=rstd[:, 0:n], in_=rstd[:, 0:n]), ["rstd"], ["rstd"])
            for j in range(2):
                p.add("dve", lambda e, j=j, n=n: e.tensor_tensor(out=zz[j][:, 0:n], in0=y[j][:, 0:n], in1=mean[:, 0:n], op=ALU.subtract), [f"y_{j}", "mean"], [f"zz_{j}"])
                p.add("dve", lambda e, j=j, n=n: e.tensor_tensor(out=zz[j][:, 0:n], in0=zz[j][:, 0:n], in1=rstd[:, 0:n], op=ALU.mult), [f"zz_{j}", "rstd"], [f"zz_{j}"])
                p.add("act", lambda e, j=j, n=n: e.activation(out=zz[j][:, 0:n], in_=zz[j][:, 0:n], func=AF.Silu, scale=cp[:, j, 1:2], bias=cp[:, j, 2:3]),
                      [f"zz_{j}", "cp"], [f"zz_{j}"])
                p.dma(co[j, :, s0 + t0:s0 + t0 + n], zz[j][:, 0:n], reads=[f"zz_{j}"])


def _ph_C2(p, g, T, l):
    NT = g.NKT
    xs, convT, mixo, proj = T["xs"], T["convT"], T["mixo"], T["proj"]
    wob = p.sb([128, 8, D], BF16, name="wob")
    load_cast_weight(p, wob, T["w_out"][l], D, "wob")
    idt = p.sb([128, 128], name="idt"); p.dma(idt[:], T["ident"], writes=["idt"])
    mng = p.sb([128, 64], name="mng_sb"); p.dma(mng[:], T["mng"][l], writes=["mng"])
    bc = [p.sb([128, D], name=f"bc{i}") for i in range(7)]
    mB = T["modB"]
    srcs = [T["n2gB"][l], mB[l, 0, :, 2048:3072], mB[l, 1, :, 2048:3072], mB[l, 0, :, 4096:5120], mB[l, 1, :, 4096:5120],
            mB[l, 0, :, 3072:4096], mB[l, 1, :, 3072:4096]]
    for i in range(7):
        p.dma(bc[i][:], srcs[i], writes=[f"bc{i}"])
    for i in (3, 4):
        p.add("dve", lambda e, i=i: e.scalar_tensor_tensor(out=bc[i][:], in0=bc[i][:], scalar=1.0, in1=bc[0][:], op0=ALU.add, op1=ALU.mult),
              [f"bc{i}", "bc0"], [f"bc{i}"])
    NB = 2
    mk = lambda shape, nm, dt=F32: [p.sb(shape, dt, name=f"{nm}{i}") for i in range(NB)]
    xt = mk([128, D], "xt"); hf = mk([128, 256], "hf"); hb = mk([128, 256], "hb"); mo = mk([128, 256], "mo")
    aot = mk([128, 512], "aot"); cvt = mk([128, 2, 128], "cvt"); ym = mk([128, 256], "ym"); sqm = mk([128, 256], "sqm")
    st4 = mk([128, 8], "st4"); mixT = mk([128, 8, 128], "mixT", BF16); tmp = mk([128, D], "tmp"); x1 = mk([128, D], "x1")
    h2 = mk([128, D], "h2"); ss = mk([128, 4], "ss")
    junk = p.sb([128, D], name="junk")
    bT = [p.ps([128, 512], name=f"bT{i}") for i in range(4)]
    bAcc = [p.ps([128, 512], name=f"bAcc{i}") for i in range(4)]
    for t in range(NT):
        i = t % NB
        isc = t < g.NCT
        r0 = t * 128
        g1b = bc[2] if isc else bc[1]
        gm2 = bc[4] if isc else bc[3]
        sh2 = bc[6] if isc else bc[5]
        kg1, kgm2, ksh2 = (f"bc{2 if isc else 1}", f"bc{4 if isc else 3}", f"bc{6 if isc else 5}")
        p.dma(xt[i][:], xs[r0:r0 + 128, :], writes=[f"xt{i}"])
        p.dma(hf[i][:], mixo[r0:r0 + 128, 0:256], writes=[f"hf{i}"])
        p.dma(hb[i][:], mixo[r0:r0 + 128, 256:512], writes=[f"hb{i}"])
        p.dma(mo[i][:], proj[r0:r0 + 128, 1280:1536], writes=[f"mo{i}"])
        p.dma(aot[i][:], mixo[r0:r0 + 128, 512:1024], writes=[f"aot{i}"])
        p.dma(cvt[i][:], convT[:, :, r0:r0 + 128].rearrange("j p n -> p j n"), writes=[f"cvt{i}"])
        p.add("dve", lambda e, i=i: e.tensor_tensor(out=ym[i][:], in0=hf[i][:], in1=hb[i][:], op=ALU.add), [f"hf{i}", f"hb{i}"], [f"ym{i}"])
        p.add("act", lambda e, i=i: e.activation(out=sqm[i][:], in_=ym[i][:], func=AF.Square), [f"ym{i}"], [f"sqm{i}"])
        p.add("dve", lambda e, i=i: e.tensor_reduce(out=st4[i][:, 0:4], in_=sqm[i][:].rearrange("p (h d) -> p h d", h=4), axis=AX.X, op=ALU.add),
              [f"sqm{i}"], [f"st4a{i}"])
        p.add("dve", lambda e, i=i: e.tensor_scalar(out=st4[i][:, 4:8], in0=st4[i][:, 0:4], scalar1=1.0 / 64, scalar2=EPS, op0=ALU.mult, op1=ALU.add),
              [f"st4a{i}"], [f"st4b{i}"])
        p.add("act", lambda e, i=i: e.activation(out=st4[i][:, 0:4], in_=st4[i][:, 4:8], func=AF.Sqrt), [f"st4b{i}"], [f"st4a{i}"])
        p.add("dve", lambda e, i=i: e.reciprocal(out=st4[i][:, 4:8], in_=st4[i][:, 0:4]), [f"st4a{i}"], [f"st4b{i}"])
        p.add("act", lambda e, i=i: e.activation(out=mo[i][:], in_=mo[i][:], func=AF.Sigmoid), [f"mo{i}"], [f"mo{i}"])
        ym3 = ym[i][:].rearrange("p (h d) -> p h d", h=4)
        p.add("dve", lambda e, i=i, ym3=ym3: e.tensor_tensor(out=ym3, in0=ym3, in1=st4[i][:, 4:8, None].to_broadcast([128, 4, 64]), op=ALU.mult),
              [f"ym{i}", f"st4b{i}"], [f"ym{i}"])
        p.add("dve", lambda e, i=i, ym3=ym3: e.tensor_tensor(out=ym3, in0=ym3, in1=mng[:, None, :].to_broadcast([128, 4, 64]), op=ALU.mult),
              [f"ym{i}", "mng"], [f"ym{i}"])
        p.add("dve", lambda e, i=i: e.tensor_tensor(out=ym[i][:], in0=ym[i][:], in1=mo[i][:], op=ALU.mult), [f"ym{i}", f"mo{i}"], [f"ym{i}"])
        p.add("act", lambda e, i=i: e.activation(out=mixT[i][:, 0:2, :], in_=cvt[i][:], func=AF.Copy), [f"cvt{i}"], [f"mixT{i}_c"])
        ta, tb = (2 * t) % 4, (2 * t + 1) % 4
        tsrc = [(ym[i], 0, f"ym{i}"), (ym[i], 128, f"ym{i}"), (aot[i], 0, f"aot{i}"), (aot[i], 128, f"aot{i}"),
                (aot[i], 256, f"aot{i}"), (aot[i], 384, f"aot{i}")]
        for n_, (src, c0, key) in enumerate(tsrc):
            bank = ta if n_ < 4 else tb
            col = (n_ % 4) * 128
            p.add("pe", lambda e, src=src, c0=c0, bank=bank, col=col: e.transpose(bT[bank][:, col:col + 128], src[:, c0:c0 + 128], idt[:]),
                  [key, "idt"], [f"bT{bank}"])
        p.add("act", lambda e, i=i, ta=ta: e.activation(out=mixT[i][:, 2:6, :], in_=bT[ta][:].rearrange("p (k n) -> p k n", k=4), func=AF.Copy),
              [f"bT{ta}"], [f"mixT{i}_a"])
        p.add("dve", lambda e, i=i, tb=tb: e.tensor_copy(out=mixT[i][:, 6:8, :], in_=bT[tb][:, 0:256].rearrange("p (k n) -> p k n", k=2)),
              [f"bT{tb}"], [f"mixT{i}_b"])
        for blk in range(2):
            a = (2 * t + blk) % 4
            for k in range(8):
                p.add("pe", lambda e, i=i, k=k, a=a, blk=blk: e.matmul(bAcc[a][:], lhsT=mixT[i][:, k, :], rhs=wob[:, k, blk * 512:(blk + 1) * 512],
                                                                      start=(k == 0), stop=(k == 7)),
                      [f"mixT{i}_c", f"mixT{i}_a", f"mixT{i}_b", "wob"], [f"bAcc{a}"])
            cs = slice(blk * 512, (blk + 1) * 512)
            p.add("dve", lambda e, i=i, a=a, cs=cs, g1b=g1b: e.tensor_tensor(out=tmp[i][:, cs], in0=bAcc[a][:], in1=g1b[:, cs], op=ALU.mult),
                  [f"bAcc{a}", kg1], [f"tmp{i}_{blk}"])
            p.add("dve", lambda e, i=i, cs=cs: e.tensor_tensor(out=x1[i][:, cs], in0=tmp[i][:, cs], in1=xt[i][:, cs], op=ALU.add),
                  [f"tmp{i}_{blk}", f"xt{i}"], [f"x1{i}_{blk}"])
        p.dma(T["x1"][r0:r0 + 128, :], x1[i][:], reads=[f"x1{i}_0", f"x1{i}_1"])
        p.add("act", lambda e, i=i: e.activation(out=junk[:], in_=x1[i][:], func=AF.Square, accum_out=ss[i][:, 0:1]),
              [f"x1{i}_0", f"x1{i}_1"], ["junk", f"ss0{i}"])
        p.add("dve", lambda e, i=i: e.tensor_scalar(out=ss[i][:, 1:2], in0=ss[i][:, 0:1], scalar1=1.0 / D, scalar2=EPS, op0=ALU.mult, op1=ALU.add),
              [f"ss0{i}"], [f"ss1{i}"])
        p.add("act", lambda e, i=i: e.activation(out=ss[i][:, 2:3], in_=ss[i][:, 1:2], func=AF.Sqrt), [f"ss1{i}"], [f"ss2{i}"])
        p.add("dve", lambda e, i=i: e.reciprocal(out=ss[i][:, 3:4], in_=ss[i][:, 2:3]), [f"ss2{i}"], [f"ss3{i}"])
        p.add("dve", lambda e, i=i, gm2=gm2: e.scalar_tensor_tensor(out=h2[i][:], in0=x1[i][:], scalar=ss[i][:, 3:4], in1=gm2[:], op0=ALU.mult, op1=ALU.mult),
              [f"x1{i}_0", f"x1{i}_1", f"ss3{i}", kgm2], [f"h2{i}"])
        p.add("dve", lambda e, i=i, sh2=sh2: e.tensor_tensor(out=h2[i][:], in0=h2[i][:], in1=sh2[:], op=ALU.add), [f"h2{i}", ksh2], [f"h2{i}"])
        p.dma(T["h2"][r0:r0 + 128, :], h2[i][:], reads=[f"h2{i}"])


def _ph_CV(p, g, T, l, RC=4):
    NE = g.NE
    RPP = NE // 128
    nst = 0
    stf = [p.sb([128, RC, D], name=f"cvf{i}") for i in range(2)]
    stb = [p.sb([128, RC, D], BF16, name=f"cvb{i}") for i in range(2)]
    for which, src in enumerate((T["peer_u"][l], T["peer_v"][l])):
        sv = src.rearrange("(p r) d -> p r d", r=RPP)
        dv = T["puv"].rearrange("(p r) (two d) -> p r two d", r=RPP, two=2)[:, :, which, :]
        for r0 in range(0, RPP, RC):
            i = nst % 2; nst += 1
            p.dma(stf[i][:], sv[:, r0:r0 + RC, :], writes=[f"cvf{i}"])
            if i == 0:
                p.add("act", lambda e, i=i: e.activation(out=stb[i][:], in_=stf[i][:], func=AF.Copy), [f"cvf{i}"], [f"cvb{i}"])
            else:
                p.add("dve", lambda e, i=i: e.tensor_copy(out=stb[i][:], in_=stf[i][:]), [f"cvf{i}"], [f"cvb{i}"])
            p.dma(dv[:, r0:r0 + RC, :], stb[i][:], reads=[f"cvb{i}"])


DBG = {}


def _ph_C3(p, g, T, l, NGB=8):
    NT = g.NKT
    NE = g.NE
    puv = T["puv"]
    wqb = p.sb([128, 8, D], F32, name="wqb")
    p.dma(wqb[:], T["wq"][l].rearrange("(k p) n -> p k n", p=128), writes=["wqb"])
    idt = p.sb([128, 128], name="idt"); p.dma(idt[:], T["ident"], writes=["idt"])
    kbd = p.sb([128, 8, 256], name="kbd_sb"); p.dma(kbd[:], T["kbd"][l], writes=["kbd"])
    g2b = [p.sb([128, D], name=f"g2b{i}") for i in range(2)]
    for i in range(2):
        p.dma(g2b[i][:], T["modB"][l, i, :, 5120:6144], writes=[f"g2b{i}"])
    h2ts = [p.sb([128, D], name=f"h2t{i}") for i in range(2)]; x1ts = [p.sb([128, D], name=f"x1t{i}") for i in range(2)]
    h2T = p.sb([128, 8, 128], F32, name="h2T")
    qTf = p.sb([128, 8, 128], name="qTf")
    sc = p.sb([128, 16, 128], name="sc"); wk = p.sb([128, 16, 128], name="wk")
    st = p.sb([128, 16, 16], name="st"); it = p.sb([128, 16, 16], U32, name="it"); itf = p.sb([128, 16, 16], name="itf")
    cand = p.sb([128, 8, 256], name="cand"); cidx = p.sb([128, 8, 256], name="cidx"); wk2 = p.sb([128, 8, 256], name="wk2")
    best = p.sb([128, 8, 16], name="best"); gws = [p.sb([128, 8, 16], name=f"gw{i}") for i in range(2)]
    eidf = p.sb([128, 128], name="eidf"); eidis = [p.sb([128, 128], I32, name=f"eidi{i}") for i in range(2)]
    sm = p.sb([128, 16], name="sm")
    act = p.sb([128, 128], name="act_sb"); coef = p.sb([128, 128], name="coef")
    junkq = [p.sb([128, 256], name=f"junkq{i}") for i in range(8)]
    junkd = [p.sb([128, D], BF16, name=f"junkd{i}") for i in range(4)]
    acc = p.sb([128, D], name="acc"); x2t = p.sb([128, D], name="x2t")
    rows = [p.sb([128, 2, D], BF16, name=f"rows{i}") for i in range(NGB)]
    banks = [p.ps([128, 512], name=f"bank{i}") for i in range(8)]
    diag = [p.sb([128, 128], BF16, name=f"diag{i}") for i in range(4)]
    ngat = 0
    def route(t):
        pb = t % 2
        h2t, x1t, eidi, gw = h2ts[pb], x1ts[pb], eidis[pb], gws[pb]
        isc = t < g.NCT
        r0 = t * 128
        gb = g2b[1] if isc else g2b[0]
        kgb = "g2b1" if isc else "g2b0"
        p.dma(h2t[:], T["h2"][r0:r0 + 128, :], writes=[f"h2t{pb}"])
        p.dma(x1t[:], T["x1"][r0:r0 + 128, :], writes=[f"x1t{pb}"])
        for half in range(2):
            for k in range(half * 4, half * 4 + 4):
                p.add("pe", lambda e, k=k, half=half: e.transpose(banks[half][:, (k % 4) * 128:(k % 4 + 1) * 128], h2t[:, k * 128:(k + 1) * 128], idt[:]),
                      [f"h2t{pb}", "idt"], [f"bank{half}"])
        p.add("act", lambda e: e.activation(out=h2T[:, 0:4, :], in_=banks[0][:].rearrange("p (k n) -> p k n", k=4), func=AF.Copy), ["bank0"], ["h2T_a"])
        p.add("dve", lambda e: e.tensor_copy(out=h2T[:, 4:8, :], in_=banks[1][:].rearrange("p (k n) -> p k n", k=4)), ["bank1"], ["h2T_b"])
        for c in range(8):
            bk = 2 + c // 4
            for k in range(8):
                p.add("pe", lambda e, c=c, k=k, bk=bk: e.matmul(banks[bk][:, (c % 4) * 128:(c % 4 + 1) * 128], lhsT=wqb[:, k, c * 128:(c + 1) * 128],
                                                              rhs=h2T[:, k, :], start=(k == 0), stop=(k == 7)), ["wqb", "h2T_a", "h2T_b"], [f"bank{bk}"])
        p.add("act", lambda e: e.activation(out=qTf[:, 0:4, :], in_=banks[2][:].rearrange("p (k n) -> p k n", k=4), func=AF.Copy), ["bank2"], ["qTf_a"])
        p.add("dve", lambda e: e.tensor_copy(out=qTf[:, 4:8, :], in_=banks[3][:].rearrange("p (k n) -> p k n", k=4)), ["bank3"], ["qTf_b"])
        for h in range(8):
            bk = 4 + h // 2
            p.add("pe", lambda e, h=h, bk=bk: e.matmul(banks[bk][:, (h % 2) * 256:(h % 2 + 1) * 256], lhsT=qTf[:, h, :], rhs=kbd[:, h, :],
                                                      start=True, stop=True), ["qTf_a", "qTf_b", "kbd"], [f"bank{bk}"])
        for bk in range(4, 8):
            g0 = (bk - 4) * 4
            if bk % 2 == 0:
                p.add("act", lambda e, bk=bk, g0=g0: e.activation(out=sc[:, g0:g0 + 4, :], in_=banks[bk][:].rearrange("p (g n) -> p g n", g=4), func=AF.Copy),
                      [f"bank{bk}"], [f"sc{bk}"])
            else:
                p.add("dve", lambda e, bk=bk, g0=g0: e.tensor_copy(out=sc[:, g0:g0 + 4, :], in_=banks[bk][:].rearrange("p (g n) -> p g n", g=4)),
                      [f"bank{bk}"], [f"sc{bk}"])
        ksf = lambda gq: f"sc{4 + gq // 4}"
        for gq in range(16):
            p.add("dve", lambda e, gq=gq: e.max(out=st[:, gq, 0:8], in_=sc[:, gq, :]), [ksf(gq)], [f"st{gq}a"])
        for gq in range(16):
            p.add("dve", lambda e, gq=gq: e.max_index(out=it[:, gq, 0:8], in_max=st[:, gq, 0:8], in_values=sc[:, gq, :]), [ksf(gq), f"st{gq}a"], [f"it{gq}a"])
        for gq in range(16):
            p.add("dve", lambda e, gq=gq: e.match_replace(out=wk[:, gq, :], in_to_replace=st[:, gq, 0:8], in_values=sc[:, gq, :], imm_value=-1e30),
                  [ksf(gq), f"st{gq}a"], [f"wk{gq}"])
        for gq in range(16):
            p.add("dve", lambda e, gq=gq: e.max(out=st[:, gq, 8:16], in_=wk[:, gq, :]), [f"wk{gq}"], [f"st{gq}b"])
        for gq in range(16):
            p.add("dve", lambda e, gq=gq: e.max_index(out=it[:, gq, 8:16], in_max=st[:, gq, 8:16], in_values=wk[:, gq, :]), [f"wk{gq}", f"st{gq}b"], [f"it{gq}b"])
        allst = [f"st{gq}{x}" for gq in range(16) for x in "ab"]
        allit = [f"it{gq}{x}" for gq in range(16) for x in "ab"]
        p.add("dve", lambda e: e.tensor_copy(out=itf[:], in_=it[:]), allit, ["itf"])
        st4 = st[:].rearrange("p (h two) k -> p h two k", two=2)
        itf4 = itf[:].rearrange("p (h two) k -> p h two k", two=2)
        cand4 = cand[:].rearrange("p h (i j) -> p h i j", i=16)
        cidx4 = cidx[:].rearrange("p h (i j) -> p h i j", i=16)
        for h in range(8):
            p.add("dve", lambda e, h=h: e.tensor_tensor(out=cand4[:, h], in0=st4[:, h, 0, :, None].to_broadcast([128, 16, 16]),
                                                        in1=st4[:, h, 1, None, :].to_broadcast([128, 16, 16]), op=ALU.add), allst, [f"cand{h}"])
        for h in range(8):
            p.add("dve", lambda e, h=h: e.scalar_tensor_tensor(out=cidx4[:, h], in0=itf4[:, h, 0, :, None].to_broadcast([128, 16, 16]), scalar=128.0,
                                                               in1=itf4[:, h, 1, None, :].to_broadcast([128, 16, 16]), op0=ALU.mult, op1=ALU.add),
                  ["itf"], [f"cidx{h}"])
        for h in range(8):
            p.add("dve", lambda e, h=h: e.max(out=best[:, h, 0:8], in_=cand[:, h, :]), [f"cand{h}"], [f"best{h}a"])
        for h in range(8):
            p.add("dve", lambda e, h=h: e.match_replace(out=wk2[:, h, :], in_to_replace=best[:, h, 0:8], in_values=cand[:, h, :], imm_value=-1e30),
                  [f"cand{h}", f"best{h}a"], [f"wk2{h}"])
        for h in range(8):
            p.add("dve", lambda e, h=h: e.max(out=best[:, h, 8:16], in_=wk2[:, h, :]), [f"wk2{h}"], [f"best{h}b"])
        nj = 0
        for k in range(16):
            for h in range(8):
                hk = h * 16 + k
                jq = junkq[nj % 8]; kj = f"junkq{nj % 8}"; nj += 1
                p.add("dve", lambda e, h=h, k=k, hk=hk, jq=jq: e.scalar_tensor_tensor(
                    out=jq[:], in0=cand[:, h, :], scalar=best[:, h, k:k + 1], in1=cidx[:, h, :], op0=ALU.is_equal, op1=ALU.mult,
                    accum_out=eidf[:, hk:hk + 1]), [f"cand{h}", f"cidx{h}", f"best{h}a", f"best{h}b"], [kj, f"eidf{hk}"])
        alle = [f"eidf{hk}" for hk in range(128)]
        allb = [f"best{h}{x}" for h in range(8) for x in "ab"]
        p.add("dve", lambda e: e.tensor_scalar(out=eidf[:], in0=eidf[:], scalar1=float(NE - 1), scalar2=0.0, op0=ALU.min, op1=ALU.max), alle, ["eidf"])
        p.add("dve", lambda e: e.tensor_copy(out=eidi[:], in_=eidf[:]), ["eidf"], [f"eidi{pb}"])
        p.add("dve", lambda e: e.tensor_tensor(out=gw[:], in0=best[:], in1=best[:, :, 0:1].to_broadcast([128, 8, 16]), op=ALU.subtract), allb, [f"gw{pb}"])
        p.add("act", lambda e: e.activation(out=gw[:], in_=gw[:], func=AF.Exp), [f"gw{pb}"], [f"gw{pb}"])
        p.add("dve", lambda e: e.tensor_reduce(out=sm[:, 0:8], in_=gw[:], axis=AX.X, op=ALU.add), [f"gw{pb}"], ["sm0"])
        p.add("dve", lambda e: e.reciprocal(out=sm[:, 8:16], in_=sm[:, 0:8]), ["sm0"], ["sm1"])
        p.add("dve", lambda e: e.tensor_tensor(out=gw[:], in0=gw[:], in1=sm[:, 8:16, None].to_broadcast([128, 8, 16]), op=ALU.mult), [f"gw{pb}", "sm1"], [f"gw{pb}"])

    def finish(t):
        nonlocal ngat
        pb = t % 2
        h2t, x1t, eidi, gw = h2ts[pb], x1ts[pb], eidis[pb], gws[pb]
        isc = t < g.NCT
        r0 = t * 128
        gb = g2b[1] if isc else g2b[0]
        kgb = "g2b1" if isc else "g2b0"
        gwf = gw[:].rearrange("p h k -> p (h k)")
        for hk in range(128):
            b = ngat % NGB; ngat += 1
            dg = diag[hk % 4]
            jd = junkd[hk % 4]
            p.add("pool", lambda e, b=b, hk=hk: e.indirect_dma_start(
                out=rows[b][:].rearrange("p two d -> p (two d)"), out_offset=None, in_=puv,
                in_offset=bass.IndirectOffsetOnAxis(ap=eidi[:, hk:hk + 1], axis=0)), [f"eidi{pb}"], [f"rows{b}"])
            p.add("dve", lambda e, b=b, hk=hk, jd=jd: e.scalar_tensor_tensor(out=jd[:], in0=rows[b][:, 0, :], scalar=1.0, in1=h2t[:], op0=ALU.mult, op1=ALU.mult,
                                                                            accum_out=act[:, hk:hk + 1]), [f"rows{b}", f"h2t{pb}"], [f"junkd{hk % 4}", f"act{hk}"])
            p.add("act", lambda e, hk=hk: e.activation(out=coef[:, hk:hk + 1], in_=act[:, hk:hk + 1], func=AF.Gelu), [f"act{hk}"], [f"cg{hk}"])
            p.add("dve", lambda e, dg=dg, hk=hk, gwf=gwf: e.tensor_scalar(out=dg[:], in0=idt[:], scalar1=coef[:, hk:hk + 1], scalar2=gwf[:, hk:hk + 1],
                                                                         op0=ALU.mult, op1=ALU.mult), ["idt", f"cg{hk}", f"gw{pb}"], [f"diag{hk%4}"])
            for blk in range(2):
                p.add("pe", lambda e, dg=dg, b=b, blk=blk, hk=hk: e.matmul(banks[blk][:], lhsT=dg[:], rhs=rows[b][:, 1, blk * 512:(blk + 1) * 512],
                                                                          start=(hk == 0), stop=(hk == 127)),
                      [f"diag{hk%4}", f"rows{b}"], [f"bank{blk}"])
        p.add("act", lambda e: e.activation(out=acc[:, 0:512], in_=banks[0][:], func=AF.Copy), ["bank0"], ["acc"])
        p.add("dve", lambda e: e.tensor_copy(out=acc[:, 512:1024], in_=banks[1][:]), ["bank1"], ["acc"])
        p.add("dve", lambda e, gb=gb: e.tensor_tensor(out=x2t[:], in0=acc[:], in1=gb[:], op=ALU.mult), ["acc", kgb], ["x2t"])
        p.add("dve", lambda e: e.tensor_tensor(out=x2t[:], in0=x2t[:], in1=x1t[:], op=ALU.add), ["x2t", f"x1t{pb}"], ["x2t"])
        p.dma(T["xs"][r0:r0 + 128, :], x2t[:], reads=["x2t"])


    for t in range(NT + 1):
        if t < NT:
            route(t)
        if t >= 1:
            finish(t - 1)


def _ph_F(p, g, T):
    fg = p.sb([128, D], name="fg_sb"); p.dma(fg[:], T["fg"], writes=["fg"])
    junk = p.sb([128, D], name="junk")
    NB = 2
    xt = [p.sb([128, D], name=f"xt{i}") for i in range(NB)]
    yt = [p.sb([128, D], name=f"yt{i}") for i in range(NB)]
    ss = [p.sb([128, 4], name=f"ss{i}") for i in range(NB)]
    for t in range(g.S // 128):
        i = t % NB
        r0 = t * 128
        p.dma(xt[i][:], T["xs"][g.CT + r0:g.CT + r0 + 128, :], writes=[f"xt{i}"])
        p.add("act", lambda e, i=i: e.activation(out=junk[:], in_=xt[i][:], func=AF.Square, accum_out=ss[i][:, 0:1]), [f"xt{i}"], ["junk", f"ss0{i}"])
        p.add("dve", lambda e, i=i: e.tensor_scalar(out=ss[i][:, 1:2], in0=ss[i][:, 0:1], scalar1=1.0 / D, scalar2=EPS, op0=ALU.mult, op1=ALU.add),
              [f"ss0{i}"], [f"ss1{i}"])
        p.add("act", lambda e, i=i: e.activation(out=ss[i][:, 2:3], in_=ss[i][:, 1:2], func=AF.Sqrt), [f"ss1{i}"], [f"ss2{i}"])
        p.add("dve", lambda e, i=i: e.reciprocal(out=ss[i][:, 3:4], in_=ss[i][:, 2:3]), [f"ss2{i}"], [f"ss3{i}"])
        p.add("dve", lambda e, i=i: e.scalar_tensor_tensor(out=yt[i][:], in0=xt[i][:], scalar=ss[i][:, 3:4], in1=fg[:], op0=ALU.mult, op1=ALU.mult),
              [f"xt{i}", f"ss3{i}", "fg"], [f"yt{i}"])
        p.dma(T["y"][r0:r0 + 128, :], yt[i][:], reads=[f"yt{i}"])


def build_fused(S, CT, DEPTH):
    g = Cfg(S, CT, DEPTH)
    nc = bass.Bass("TRN2", target_bir_lowering=False)
    NTOK = g.NTOK
    T = {}
    ins = {"xs0": [NTOK, D], "cT": [128, 8, 2], "ada_w": [DEPTH, D, 6144], "ada_bB": [DEPTH, 128, 6144], "ada_bT": [DEPTH, 128, 48],
           "n1g": [DEPTH, 128, 8], "n2gB": [DEPTH, 128, D], "w_in": [DEPTH, D, IN_W], "cs": [NTOK, 64],
           "cw": [DEPTH, 128, 2, 31], "cp": [DEPTH, 128, 2, 3], "mgb": [DEPTH, 8, 128, 2], "mng": [DEPTH, 128, 64],
           "dl": [DEPTH, 128, 256], "dng": [DEPTH, 128, 128], "lami": [DEPTH, 128, 2],
           "w_out": [DEPTH, D, D], "wq": [DEPTH, D, D], "kbd": [DEPTH, 128, 8, 256],
           "fg": [128, D],
           "ident": [128, 128], "ones": [128, 128], "tri": [128, 128], "triL": [128, 128]}
    for k, shp in ins.items():
        T[k] = _din(nc, k, shp)
    T["peer_u"] = [_din(nc, f"peer_u{l}", [g.NE, D]) for l in range(DEPTH)]
    T["peer_v"] = [_din(nc, f"peer_v{l}", [g.NE, D]) for l in range(DEPTH)]
    T["y"] = _dout(nc, "y", [S, D])
    scr = {"xs": [NTOK, D], "proj": [NTOK, IN_W], "projT": [2048, NTOK], "mixo": [NTOK, D], "convT": [2, 128, NTOK],
           "x1": [NTOK, D], "h2": [NTOK, D], "modT": [DEPTH, 128, 48, 2], "modB": [DEPTH, 2, 128, 6144]}
    for k, shp in scr.items():
        T[k] = nc.dram_tensor("scr_" + k, shp, F32, kind="Internal").ap()
    T["puv"] = nc.dram_tensor("scr_puv", [g.NE, 2 * D], BF16, kind="Internal").ap()
    p = Prog(nc)
    CH = 2048
    for r0 in range(0, NTOK, CH):
        r1 = min(NTOK, r0 + CH)
        p.dma(T["xs"][r0:r1, :], T["xs0"][r0:r1, :])
    _ph_P0(p, g, T)
    p.end_phase()
    for l in range(DEPTH):
        for ph in (_ph_A, _ph_M1, _ph_M2, _ph_C1, _ph_C2, _ph_CV, _ph_C3):
            p.begin_phase()
            ph(p, g, T, l)
            p.end_phase()
    p.begin_phase()
    _ph_F(p, g, T)
    p.emit()
    return nc


def fused_inputs(b, x, c, ctx, c_ctx, ada_w, ada_b, norm1_g, norm2_g, w_in, conv_w, conv_b, conv_ln_g, conv_ln_b,
                 mlstm_gate_b, mlstm_norm_g, diff_lambda, diff_norm_g, w_out, peer_wq, peer_keys, peer_u, peer_v, final_g, shared=None):
    f32 = np.float32
    DEPTH = ada_w.shape[0]
    S, CT = x.shape[1], ctx.shape[1]
    if shared is None:
        shared = {}
    if not shared:
        cos, sin = _rope_tables(S)
        cs = np.zeros((CT + S, 64), f32); cs[:CT, :32] = 1.0; cs[CT:, :32] = cos; cs[CT:, 32:] = sin
        shared["cs"] = cs
        shared["ada_w"] = np.asarray(ada_w, f32)
        ab = np.asarray(ada_b, f32)
        shared["ada_bB"] = np.ascontiguousarray(np.broadcast_to(ab[:, None, :], (DEPTH, 128, 6144)))
        shared["ada_bT"] = np.ascontiguousarray(ab.reshape(DEPTH, 48, 128).transpose(0, 2, 1))
        shared["n1g"] = np.ascontiguousarray(np.asarray(norm1_g, f32).reshape(DEPTH, 8, 128).transpose(0, 2, 1))
        shared["n2gB"] = np.ascontiguousarray(np.broadcast_to(np.asarray(norm2_g, f32)[:, None, :], (DEPTH, 128, D)))
        shared["w_in"] = np.asarray(w_in, f32)
        shared["cw"] = np.ascontiguousarray(np.asarray(conv_w, f32)[:, :, 0, :].transpose(0, 2, 1).reshape(DEPTH, 2, 128, 31).transpose(0, 2, 1, 3))
        cp = np.stack([np.asarray(conv_b, f32), np.asarray(conv_ln_g, f32), np.asarray(conv_ln_b, f32)], -1)
        shared["cp"] = np.ascontiguousarray(cp.reshape(DEPTH, 2, 128, 3).transpose(0, 2, 1, 3))
        gbv = np.asarray(mlstm_gate_b, f32)
        mgb = np.zeros((DEPTH, 8, 128, 2), f32)
        for h in range(4):
            for d in range(2):
                mgb[:, h * 2 + d, :, 0] = gbv[:, 2 * d, h][:, None]
                mgb[:, h * 2 + d, :, 1] = gbv[:, 2 * d + 1, h][:, None]
        shared["mgb"] = mgb
        shared["mng"] = np.ascontiguousarray(np.broadcast_to(np.asarray(mlstm_norm_g, f32)[:, None, :], (DEPTH, 128, 64)))
        shared["dl"] = np.ascontiguousarray(np.broadcast_to(np.asarray(diff_lambda, f32).reshape(DEPTH, 1, 256), (DEPTH, 128, 256)))
        shared["dng"] = np.ascontiguousarray(np.broadcast_to(np.asarray(diff_norm_g, f32)[:, None, :], (DEPTH, 128, 128)))
        lami = np.zeros((DEPTH, 128, 2), f32)
        for l in range(DEPTH):
            li = 0.8 - 0.6 * math.exp(-0.3 * l)
            lami[l, :, 0] = li; lami[l, :, 1] = 1.0 - li
        shared["lami"] = lami
        shared["w_out"] = np.asarray(w_out, f32)
        shared["wq"] = np.asarray(peer_wq, f32)
        keys = np.asarray(peer_keys, f32)
        kbd = np.zeros((DEPTH, 128, 8, 256), f32)
        for pp in range(2):
            kbd[:, pp * 64:(pp + 1) * 64, :, pp * 128:(pp + 1) * 128] = keys[:, :, pp].transpose(0, 3, 1, 2)
        shared["kbd"] = kbd
        for l in range(DEPTH):
            shared[f"peer_u{l}"] = np.ascontiguousarray(np.asarray(peer_u[l], f32))
            shared[f"peer_v{l}"] = np.ascontiguousarray(np.asarray(peer_v[l], f32))
        shared["fg"] = _rep(np.asarray(final_g, f32))
        shared["ident"] = np.eye(128, dtype=f32)
        shared["ones"] = np.ones((128, 128), f32)
        shared["tri"] = np.triu(np.ones((128, 128), f32))
        shared["triL"] = np.tril(np.ones((128, 128), f32))
    m = dict(shared)
    m["xs0"] = np.ascontiguousarray(np.concatenate([np.asarray(ctx[b], f32), np.asarray(x[b], f32)], 0))
    cT = np.stack([np.asarray(c[b], f32), np.asarray(c_ctx, f32)], -1)
    m["cT"] = np.ascontiguousarray(cT.reshape(8, 128, 2).transpose(1, 0, 2))
    return m


_FUSED = {}


def kernel(**inp):
    x = np.asarray(inp["x"], np.float32)
    B, S, _ = x.shape
    CT = inp["ctx"].shape[1]
    DEPTH = inp["ada_w"].shape[0]
    key = (S, CT, DEPTH)
    if key not in _FUSED:
        _FUSED[key] = build_fused(S, CT, DEPTH)
    nc = _FUSED[key]
    shared = {}
    per_b = [fused_inputs(b, shared=shared, **inp) for b in range(B)]
    maps = [per_b[(cc // 2) % B] for cc in range(NCORES)]
    res = run_bass_kernel_spmd(nc, maps, core_ids=list(range(NCORES))).results
    out = np.zeros((B, S, D), np.float32)
    for b in range(B):
        out[b] = res[2 * b]["y"]
    return out
```

```python
import math
from contextlib import ExitStack
import numpy as np
import ml_dtypes
import concourse.bass as bass
import concourse.mybir as mybir
from concourse.bass_utils import run_bass_kernel_spmd

F32 = mybir.dt.float32
BF16 = mybir.dt.bfloat16
I32 = mybir.dt.int32
U32 = mybir.dt.uint32
AF = mybir.ActivationFunctionType
ALU = mybir.AluOpType
AX = mybir.AxisListType

D = 1024
IN_W = 3088
EPS = 1e-6
R_DMA = 8
R_SLOTS = {"sp": 8, "pool": 8}
NCORES = 8


class Prog:
    ENGS = ("pe", "act", "dve", "pool", "sp")

    def __init__(self, nc):
        self.nc = nc
        self.gs = ExitStack()
        self.sems = {}
        for e in ("pe", "act", "dve"):
            self.sems[e] = self.gs.enter_context(nc.semaphore(f"s_{e}"))
        for e in ("sp", "pool"):
            self.sems[e] = [self.gs.enter_context(nc.semaphore(f"s_{e}{i}")) for i in range(R_SLOTS[e])]
        self.cnt = {e: 0 for e in self.ENGS}
        self.waited = {e: {} for e in self.ENGS}
        self.nalloc = 0
        self.phase_open = False
        self.begin_phase()

    def begin_phase(self):
        assert not self.phase_open
        self.phase_open = True
        self.es = ExitStack()
        self.ops = {e: [] for e in self.ENGS}
        self.last_w = {}
        self.readers = {}
        self.bar = dict(self.cnt)

    def sb(self, shape, dtype=F32, name=None):
        self.nalloc += 1
        name = f"{name or 'sb'}_{self.nalloc}"
        return self.es.enter_context(self.nc.sbuf_tensor(name, list(shape), dtype))

    def ps(self, shape, dtype=F32, name=None):
        self.nalloc += 1
        name = f"{name or 'ps'}_{self.nalloc}"
        return self.es.enter_context(self.nc.psum_tensor(name, list(shape), dtype))

    def add(self, eng, fn, reads=(), writes=()):
        deps = set()
        for b in reads:
            if b in self.last_w:
                deps.add(self.last_w[b])
        for b in writes:
            if b in self.last_w:
                deps.add(self.last_w[b])
            for r in self.readers.get(b, ()):
                deps.add(r)
        self.cnt[eng] += 1
        idx = self.cnt[eng]
        me = (eng, idx)
        for b in reads:
            self.readers.setdefault(b, []).append(me)
        for b in writes:
            self.last_w[b] = me
            self.readers[b] = []
        deps.discard(me)
        self.ops[eng].append((fn, deps, idx))
        return me

    def dma(self, out, in_, reads=(), writes=(), eng="sp", **kw):
        return self.add(eng, lambda e: e.dma_start(out=out, in_=in_, **kw), reads, writes)

    def _dep_wait(self, engname, engobj, dep):
        waited = self.waited[engname]
        f, k = dep
        if f in ("sp", "pool"):
            n = k - 1
            R = R_SLOTS[f]
            sem = self.sems[f][n % R]
            val = 16 * (n // R + 1)
            key = (f, n % R)
        else:
            sem = self.sems[f]
            val = k
            key = f
        if waited.get(key, 0) < val:
            engobj.wait_ge(sem, val)
            waited[key] = val

    def _wait_all(self, engname, engobj, counts):
        for f in self.ENGS:
            tot = counts[f]
            if tot == 0:
                continue
            if f in ("sp", "pool"):
                for k in range(max(1, tot - R_SLOTS[f] + 1), tot + 1):
                    self._dep_wait(engname, engobj, (f, k))
            else:
                self._dep_wait(engname, engobj, (f, tot))

    def end_phase(self, final=False):
        assert self.phase_open
        self.phase_open = False
        nc = self.nc

        def run(engname, engobj):
            first = True
            for fn, deps, idx in self.ops[engname]:
                if first:
                    self._wait_all(engname, engobj, self.bar)
                    first = False
                for d in sorted(deps):
                    if d[0] == engname and engname == "pe":
                        continue
                    self._dep_wait(engname, engobj, d)
                if engname in ("sp", "pool"):
                    n = idx - 1
                    R = R_SLOTS[engname]
                    if n >= R:
                        self._dep_wait(engname, engobj, (engname, idx - R))
                    inst = fn(engobj)
                    inst.then_inc(self.sems[engname][n % R], 16)
                else:
                    inst = fn(engobj)
                    inst.then_inc(self.sems[engname], 1)
            if final and engname in ("sp", "pool"):
                self._wait_all(engname, engobj, {e: (self.cnt[e] if e == engname else 0) for e in self.ENGS})

        with nc.Block() as block:
            @block.tensor
            def _(e):
                run("pe", e)

            @block.scalar
            def _(e):
                run("act", e)

            @block.vector
            def _(e):
                run("dve", e)

            @block.gpsimd
            def _(e):
                run("pool", e)

            @block.sync
            def _(e):
                run("sp", e)
        self.es.close()

    def emit(self):
        self.end_phase(final=True)
        self.gs.close()


def _din(nc, name, shape, dt=F32):
    return nc.dram_tensor(name, list(shape), dt, kind="ExternalInput").ap()


def _dout(nc, name, shape, dt=F32):
    return nc.dram_tensor(name, list(shape), dt, kind="ExternalOutput").ap()


def build_P0():
    nc = bass.Bass("TRN2", target_bir_lowering=False)
    w = _din(nc, "w", [1024, 3072])
    b = _din(nc, "b", [128, 24])
    cT = _din(nc, "cT", [128, 8, 5])
    o = _dout(nc, "modT", [128, 24, 5])
    p = Prog(nc)
    ct = p.sb([128, 8, 5]); st = p.sb([128, 8, 5]); bt = p.sb([128, 24]); ot = p.sb([128, 24, 5])
    p.dma(ct[:], cT, writes=["ct"])
    p.dma(bt[:], b, writes=["bt"])
    p.add("act", lambda e: e.activation(out=st[:], in_=ct[:], func=AF.Silu), ["ct"], ["st"])
    wv = w.rearrange("(k p) n -> p k n", p=128)
    NB = 6
    wts = [p.sb([128, 8, 512], name=f"wt{i}") for i in range(2)]
    pss = [p.ps([128, 4, 8], name=f"pp{i}") for i in range(2)]
    for blk in range(NB):
        wt = wts[blk % 2]; ps = pss[blk % 2]
        p.dma(wt[:], wv[:, :, blk * 512:(blk + 1) * 512], writes=[f"wt{blk%2}"])
        for jj in range(4):
            for k in range(8):
                p.add("pe", lambda e, wt=wt, ps=ps, jj=jj, k=k: e.matmul(
                    ps[:, jj, 0:5], lhsT=wt[:, k, jj * 128:(jj + 1) * 128], rhs=st[:, k, :],
                    start=(k == 0), stop=(k == 7)), [f"wt{blk%2}", "st"], [f"pp{blk%2}"])
        for jj in range(4):
            j = blk * 4 + jj
            p.add("dve", lambda e, ps=ps, jj=jj, j=j: e.tensor_scalar(
                out=ot[:, j, :], in0=ps[:, jj, 0:5], scalar1=bt[:, j:j + 1], scalar2=None,
                op0=ALU.add), [f"pp{blk%2}", "bt"], ["ot"])
    p.dma(o, ot[:], reads=["ot"])
    p.emit()
    return nc


def load_cast_weight(p, wb, w, ncols, key, nk=8, bw=512):
    wv = w.rearrange("(k p) n -> p k n", p=128)
    stg = [p.sb([128, nk, bw], name=f"stg_{key}{i}") for i in range(2)]
    for bi, c0 in enumerate(range(0, ncols, bw)):
        cw = min(bw, ncols - c0)
        st = stg[bi % 2]
        p.dma(st[:, :, 0:cw], wv[:, :, c0:c0 + cw], writes=[f"stg_{key}{bi%2}"])
        h = nk // 2
        p.add("act", lambda e, st=st, c0=c0, cw=cw: e.activation(out=wb[:, 0:h, c0:c0 + cw], in_=st[:, 0:h, 0:cw], func=AF.Copy),
              [f"stg_{key}{bi%2}"], [f"{key}_a{bi}"])
        p.add("dve", lambda e, st=st, c0=c0, cw=cw: e.tensor_copy(out=wb[:, h:nk, c0:c0 + cw], in_=st[:, h:nk, 0:cw]),
              [f"stg_{key}{bi%2}"], [f"{key}_b{bi}"])
    p.add("act", lambda e: e.activation(out=wb[0:1, 0, 0:1], in_=wb[0:1, 0, 0:1], func=AF.Copy),
          [f"{key}_a{bi}" for bi in range((ncols + bw - 1) // bw)] + [f"{key}_b{bi}" for bi in range((ncols + bw - 1) // bw)], [key])


def build_A(NT, ctx_tiles=(0,), stage=9):
    nc = bass.Bass("TRN2", target_bir_lowering=False)
    xs = _din(nc, "xs", [NT * 128, D])
    w_in = _din(nc, "w_in", [D, IN_W])
    pv = _din(nc, "pv", [128, 8, 5])
    cs = _din(nc, "cs", [NT * 128, 64])
    ident = _din(nc, "ident", [128, 128])
    proj = _dout(nc, "proj", [NT * 128, IN_W])
    p = Prog(nc)
    wb = p.sb([128, 8, IN_W], BF16, name="wb")
    load_cast_weight(p, wb, w_in, IN_W, "wb")
    idt = p.sb([128, 128]); p.dma(idt[:], ident, writes=["idt"])
    pvt = p.sb([128, 8, 5]); p.dma(pvt[:], pv, writes=["pvt"])
    gm = p.sb([128, 8, 2])
    for i, col in enumerate((1, 3)):
        p.add("dve", lambda e, i=i, col=col: e.scalar_tensor_tensor(
            out=gm[:, :, i], in0=pvt[:, :, col], scalar=1.0, in1=pvt[:, :, 0],
            op0=ALU.add, op1=ALU.mult), ["pvt"], ["gm"])
    NB = 2
    xt = [p.sb([128, D], name=f"xt{i}") for i in range(NB)]
    xn = [p.sb([128, D], name=f"xn{i}") for i in range(NB)]
    junk = p.sb([128, D], name="junk")
    ss = [p.sb([128, 1], name=f"ss{i}") for i in range(NB)]
    rstd = [p.sb([128, 1], name=f"rstd{i}") for i in range(NB)]
    hT = [p.sb([128, 8, 128], BF16, name=f"hT{i}") for i in range(NB)]
    ot = [p.sb([128, IN_W], name=f"ot{i}") for i in range(NB)]
    ro = [p.sb([128, 1024], name=f"ro{i}") for i in range(NB)]
    tmp = [p.sb([128, 4, 512], name=f"tmp{i}") for i in range(NB)]
    cst = [p.sb([128, 64], name=f"cst{i}") for i in range(NB)]
    tp = [p.ps([128, 512], name=f"tp{i}") for i in range(4)]
    acc = [p.ps([128, 512], name=f"acc{i}") for i in range(4)]
    nacc = 0
    for t in range(NT):
        i = t % NB
        isc = t in ctx_tiles
        r0 = t * 128
        p.dma(xt[i][:], xs[r0:r0 + 128, :], writes=[f"xt{i}"])
        p.dma(cst[i][:], cs[r0:r0 + 128, :], writes=[f"cst{i}"])
        p.add("act", lambda e, i=i: e.activation(out=junk[:], in_=xt[i][:], func=AF.Square,
                                                  accum_out=ss[i][:, 0:1]), [f"xt{i}"], ["junk", f"ss{i}"])
        p.add("dve", lambda e, i=i: e.tensor_scalar(out=rstd[i][:], in0=ss[i][:], scalar1=1.0 / D, scalar2=EPS,
                                                     op0=ALU.mult, op1=ALU.add), [f"ss{i}"], [f"rstd{i}"])
        p.add("act", lambda e, i=i: e.activation(out=ss[i][:], in_=rstd[i][:], func=AF.Sqrt), [f"rstd{i}"], [f"ss{i}"])
        p.add("dve", lambda e, i=i: e.reciprocal(out=rstd[i][:], in_=ss[i][:]), [f"ss{i}"], [f"rstd{i}"])
        p.add("dve", lambda e, i=i: e.tensor_scalar(out=xn[i][:], in0=xt[i][:], scalar1=rstd[i][:, 0:1], scalar2=None,
                                                     op0=ALU.mult), [f"xt{i}", f"rstd{i}"], [f"xn{i}"])
        if stage == 1:
            p.dma(proj[r0:r0 + 128, 0:1024], xn[i][:], reads=[f"xn{i}"])
            continue
        for half in range(2):
            tpi = (2 * t + half) % 4
            for k in range(half * 4, half * 4 + 4):
                p.add("pe", lambda e, i=i, k=k, tpi=tpi: e.transpose(
                    tp[tpi][:, (k % 4) * 128:(k % 4 + 1) * 128], xn[i][:, k * 128:(k + 1) * 128], idt[:]),
                    [f"xn{i}", "idt"], [f"tp{tpi}"])
            sc_col = 1 if isc else 0
            sh_col = 4 if isc else 2
            for k in range(half * 4, half * 4 + 4):
                if half == 0:
                    p.add("act", lambda e, i=i, k=k, tpi=tpi, sc_col=sc_col, sh_col=sh_col: e.activation(
                        out=hT[i][:, k, :], in_=tp[tpi][:, (k % 4) * 128:(k % 4 + 1) * 128], func=AF.Identity,
                        scale=gm[:, k, sc_col:sc_col + 1], bias=pvt[:, k, sh_col:sh_col + 1]),
                        [f"tp{tpi}", "gm", "pvt"], [f"hT{i}_{k}"])
                else:
                    p.add("dve", lambda e, i=i, k=k, tpi=tpi, sc_col=sc_col, sh_col=sh_col: e.tensor_scalar(
                        out=hT[i][:, k, :], in0=tp[tpi][:, (k % 4) * 128:(k % 4 + 1) * 128],
                        scalar1=gm[:, k, sc_col:sc_col + 1], scalar2=pvt[:, k, sh_col:sh_col + 1],
                        op0=ALU.mult, op1=ALU.add), [f"tp{tpi}", "gm", "pvt"], [f"hT{i}_{k}"])
        if stage == 2:
            p.add("dve", lambda e, i=i: e.tensor_copy(out=ot[i][:, 0:1024], in_=hT[i][:].rearrange("p k n -> p (k n)")),
                  [f"hT{i}_{k}" for k in range(8)], [f"ot{i}_0"])
            p.dma(proj[r0:r0 + 128, 0:1024], ot[i][:, 0:1024], reads=[f"ot{i}_0"])
            continue
        for blk in range(7):
            c0 = blk * 512
            cw = min(512, IN_W - c0)
            a = nacc % 4; nacc += 1
            for k in range(8):
                p.add("pe", lambda e, i=i, k=k, a=a, c0=c0, cw=cw: e.matmul(
                    acc[a][:, 0:cw], lhsT=hT[i][:, k, :], rhs=wb[:, k, c0:c0 + cw],
                    start=(k == 0), stop=(k == 7)), [f"hT{i}_{k}", "wb"], [f"acc{a}"])
            if blk % 2 == 0:
                p.add("act", lambda e, i=i, a=a, c0=c0, cw=cw: e.activation(
                    out=ot[i][:, c0:c0 + cw], in_=acc[a][:, 0:cw], func=AF.Copy), [f"acc{a}"], [f"ot{i}_{blk}"])
            else:
                p.add("dve", lambda e, i=i, a=a, c0=c0, cw=cw: e.tensor_copy(
                    out=ot[i][:, c0:c0 + cw], in_=acc[a][:, 0:cw]), [f"acc{a}"], [f"ot{i}_{blk}"])
        if stage == 3:
            p.dma(proj[r0:r0 + 128, :], ot[i][:], reads=[f"ot{i}_{b}" for b in range(7)])
            continue
        src = ot[i][:, 1552:2576].rearrange("p (g r two) -> p g r two", g=16, two=2)
        dst = ro[i][:].rearrange("p (g r two) -> p g r two", g=16, two=2)
        t1 = src[:, :, :, 0]; t2 = src[:, :, :, 1]
        cosb = cst[i][:, None, 0:32].to_broadcast([128, 16, 32])
        sinb = cst[i][:, None, 32:64].to_broadcast([128, 16, 32])
        tm = [tmp[i][:, j, :].rearrange("p (g r) -> p g r", g=16) for j in range(4)]
        rk = [f"ot{i}_3", f"ot{i}_4", f"ot{i}_5", f"cst{i}"]
        p.add("dve", lambda e, tm=tm, t1=t1, cosb=cosb: e.tensor_tensor(out=tm[0], in0=t1, in1=cosb, op=ALU.mult), rk, [f"tmp{i}_0"])
        p.add("dve", lambda e, tm=tm, t2=t2, sinb=sinb: e.tensor_tensor(out=tm[1], in0=t2, in1=sinb, op=ALU.mult), rk, [f"tmp{i}_1"])
        p.add("dve", lambda e, tm=tm, t1=t1, sinb=sinb: e.tensor_tensor(out=tm[2], in0=t1, in1=sinb, op=ALU.mult), rk, [f"tmp{i}_2"])
        p.add("dve", lambda e, tm=tm, t2=t2, cosb=cosb: e.tensor_tensor(out=tm[3], in0=t2, in1=cosb, op=ALU.mult), rk, [f"tmp{i}_3"])
        p.add("dve", lambda e, tm=tm, dst=dst: e.tensor_tensor(out=dst[:, :, :, 0], in0=tm[0], in1=tm[1], op=ALU.subtract),
              [f"tmp{i}_0", f"tmp{i}_1"], [f"ro{i}a"])
        p.add("dve", lambda e, tm=tm, dst=dst: e.tensor_tensor(out=dst[:, :, :, 1], in0=tm[2], in1=tm[3], op=ALU.add),
              [f"tmp{i}_2", f"tmp{i}_3"], [f"ro{i}b"])
        p.dma(proj[r0:r0 + 128, 0:1552], ot[i][:, 0:1552], reads=[f"ot{i}_{b}" for b in range(4)])
        p.dma(proj[r0:r0 + 128, 1552:2576], ro[i][:], reads=[f"ro{i}a", f"ro{i}b"])
        p.dma(proj[r0:r0 + 128, 2576:IN_W], ot[i][:, 2576:IN_W], reads=[f"ot{i}_5", f"ot{i}_6"])
    p.emit()
    return nc


def build_M1(NCH, NS=4, GC=4):
    nc = bass.Bass("TRN2", target_bir_lowering=False)
    L = NCH * 128
    mqT = _din(nc, "mqT", [NS, 64, L])
    mkT = _din(nc, "mkT", [NS, 64, L])
    mvk = _din(nc, "mvk", [NS, L, 128])
    mg = _din(nc, "mg", [NS, L, 2])
    mgb = _din(nc, "mgb", [NS, 128, 2])
    tri_d = _din(nc, "tri", [128, 128])
    ones_d = _din(nc, "ones", [128, 128])
    mh = _dout(nc, "mh", [NS, L, 64])
    p = Prog(nc)
    tri = p.sb([128, 128], name="tri_sb"); p.dma(tri[:], tri_d, writes=["tri"])
    ones = p.sb([128, 128], name="ones_sb"); p.dma(ones[:], ones_d, writes=["ones"])
    banks = [p.ps([128, 512], name=f"bank{i}") for i in range(7)]
    NBUF = 2
    B = {}
    for s in range(NS):
        gb = p.sb([128, 2], name=f"gb{s}"); p.dma(gb[:], mgb[s], writes=[f"gb{s}"])
        B[s, "gb"] = gb
        B[s, "Cf"] = p.sb([64, 65], name=f"Cf{s}")
        B[s, "Cb"] = p.sb([64, 65], BF16, name=f"Cb{s}")
        B[s, "tmpC"] = p.sb([64, 65], name=f"tmpC{s}")
        p.add("dve", lambda e, s=s: e.memset(B[s, "Cf"][:], 0.0), [], [f"Cf{s}"])
        p.add("dve", lambda e, s=s: e.memset(B[s, "Cb"][:], 0.0), [], [f"Cb{s}"])
        for i in range(NBUF):
            k = (s, i)
            B[k, "qTf"] = p.sb([64, GC * 128], name=f"qTf{s}_{i}")
            B[k, "kTf"] = p.sb([64, GC * 128], name=f"kTf{s}_{i}")
            B[k, "vkf"] = p.sb([128, GC, 128], name=f"vkf{s}_{i}")
            B[k, "gf"] = p.sb([128, GC, 2], name=f"gf{s}_{i}")
            B[k, "qTb"] = p.sb([64, GC * 128], BF16, name=f"qTb{s}_{i}")
            B[k, "kTb"] = p.sb([64, GC * 128], BF16, name=f"kTb{s}_{i}")
            B[k, "vaug"] = p.sb([128, GC, 65], BF16, name=f"vaug{s}_{i}")
            B[k, "kb"] = p.sb([128, GC, 64], BF16, name=f"kb{s}_{i}")
            B[k, "h"] = p.sb([128, GC, 64], name=f"h{s}_{i}")
            B[k, "gi"] = p.sb([128, GC], name=f"gi{s}_{i}")
            B[k, "sp"] = p.sb([128, GC], name=f"sp{s}_{i}")
            B[k, "a"] = p.sb([128, GC], name=f"a{s}_{i}")
            B[k, "b"] = p.sb([128, GC], name=f"b{s}_{i}")
            B[k, "eG"] = p.sb([128, GC], name=f"eG{s}_{i}")
            B[k, "WT"] = p.sb([128, 128], BF16, name=f"WT{s}_{i}")
            B[k, "d"] = p.sb([128, 4], name=f"d{s}_{i}")
            p.add("dve", lambda e, k=k: e.memset(B[k, "vaug"][:, :, 64:65], 1.0), [], [f"vaug1_{s}_{i}"])
    ngroups = (NCH + GC - 1) // GC
    for g in range(ngroups):
        c0 = g * GC
        gc = min(GC, NCH - c0)
        i = g % NBUF
        for s in range(NS):
            k = (s, i)
            sfx = f"{s}_{i}"
            T = {n: B[k, n] for n in ("qTf", "kTf", "vkf", "gf", "qTb", "kTb", "vaug", "kb", "h", "gi", "sp", "a", "b", "eG", "WT", "d")}
            Cf, Cb, tmpC, gb = B[s, "Cf"], B[s, "Cb"], B[s, "tmpC"], B[s, "gb"]
            bS, bX, bU = banks[(s % 2) * 3], banks[(s % 2) * 3 + 1], banks[(s % 2) * 3 + 2]
            kS, kX, kU = f"bank{(s%2)*3}", f"bank{(s%2)*3+1}", f"bank{(s%2)*3+2}"
            bP = banks[6]
            W = gc * 128
            p.dma(T["qTf"][:, 0:W], mqT[s, :, c0 * 128:c0 * 128 + W], writes=[f"qTf{sfx}"])
            p.dma(T["kTf"][:, 0:W], mkT[s, :, c0 * 128:c0 * 128 + W], writes=[f"kTf{sfx}"])
            p.dma(T["vkf"][:, 0:gc, :], mvk[s].rearrange("(c p) f -> p c f", p=128)[:, c0:c0 + gc, :], writes=[f"vkf{sfx}"])
            p.dma(T["gf"][:, 0:gc, :], mg[s].rearrange("(c p) f -> p c f", p=128)[:, c0:c0 + gc, :], writes=[f"gf{sfx}"])
            p.add("dve", lambda e, T=T, gb=gb, gc=gc: e.tensor_scalar(out=T["gi"][:, 0:gc], in0=T["gf"][:, 0:gc, 0], scalar1=gb[:, 0:1],
                                                                     scalar2=None, op0=ALU.add), [f"gf{sfx}", f"gb{s}"], [f"gi{sfx}"])
            p.add("dve", lambda e, T=T, gb=gb, gc=gc: e.tensor_scalar(out=T["sp"][:, 0:gc], in0=T["gf"][:, 0:gc, 1], scalar1=gb[:, 1:2],
                                                                     scalar2=None, op0=ALU.add), [f"gf{sfx}", f"gb{s}"], [f"sp{sfx}"])
            p.add("act", lambda e, T=T, gc=gc: e.activation(out=T["sp"][:, 0:gc], in_=T["sp"][:, 0:gc], func=AF.Exp, scale=-1.0),
                  [f"sp{sfx}"], [f"sp{sfx}"])
            p.add("dve", lambda e, T=T, gc=gc: e.tensor_scalar(out=T["sp"][:, 0:gc], in0=T["sp"][:, 0:gc], scalar1=1.0, scalar2=None,
                                                              op0=ALU.add), [f"sp{sfx}"], [f"sp{sfx}"])
            p.add("act", lambda e, T=T, gc=gc: e.activation(out=T["sp"][:, 0:gc], in_=T["sp"][:, 0:gc], func=AF.Ln),
                  [f"sp{sfx}"], [f"sp{sfx}"])
            p.add("pe", lambda e, T=T, gc=gc, bP=bP: e.matmul(bP[:, 0:gc], lhsT=tri[:], rhs=T["sp"][:, 0:gc], start=True, stop=True),
                  ["tri", f"sp{sfx}"], ["bank6"])
            p.add("pe", lambda e, T=T, gc=gc, bP=bP: e.matmul(bP[:, 64:64 + gc], lhsT=ones[:], rhs=T["sp"][:, 0:gc], start=True, stop=True),
                  ["ones", f"sp{sfx}"], ["bank6"])
            p.add("act", lambda e, T=T, gc=gc, bP=bP: e.activation(out=T["a"][:, 0:gc], in_=bP[:, 0:gc], func=AF.Exp, scale=-1.0),
                  ["bank6"], [f"a{sfx}"])
            p.add("act", lambda e, T=T, gc=gc, bP=bP: e.activation(out=T["eG"][:, 0:gc], in_=bP[:, 64:64 + gc], func=AF.Exp, scale=-1.0),
                  ["bank6"], [f"eG{sfx}"])
            p.add("act", lambda e, T=T, gc=gc, bP=bP: e.activation(out=T["b"][:, 0:gc], in_=bP[:, 0:gc], func=AF.Identity),
                  ["bank6"], [f"b{sfx}"])
            p.add("dve", lambda e, T=T, gc=gc: e.tensor_tensor(out=T["b"][:, 0:gc], in0=T["b"][:, 0:gc], in1=T["gi"][:, 0:gc], op=ALU.add),
                  [f"b{sfx}", f"gi{sfx}"], [f"b{sfx}"])
            p.add("act", lambda e, T=T, gc=gc: e.activation(out=T["b"][:, 0:gc], in_=T["b"][:, 0:gc], func=AF.Exp),
                  [f"b{sfx}"], [f"b{sfx}"])
            p.add("act", lambda e, T=T, W=W: e.activation(out=T["qTb"][:, 0:W], in_=T["qTf"][:, 0:W], func=AF.Copy),
                  [f"qTf{sfx}"], [f"qTb{sfx}"])
            p.add("dve", lambda e, T=T, W=W: e.tensor_scalar(out=T["kTb"][:, 0:W], in0=T["kTf"][:, 0:W], scalar1=0.125, scalar2=None,
                                                            op0=ALU.mult), [f"kTf{sfx}"], [f"kTb{sfx}"])
            p.add("act", lambda e, T=T, gc=gc: e.activation(out=T["vaug"][:, 0:gc, 0:64], in_=T["vkf"][:, 0:gc, 0:64], func=AF.Copy),
                  [f"vkf{sfx}"], [f"vaug{sfx}"])
            p.add("dve", lambda e, T=T, gc=gc: e.scalar_tensor_tensor(
                out=T["kb"][:, 0:gc, :], in0=T["vkf"][:, 0:gc, 64:128], scalar=0.125,
                in1=T["b"][:, 0:gc, None].to_broadcast([128, gc, 64]), op0=ALU.mult, op1=ALU.mult),
                [f"vkf{sfx}", f"b{sfx}"], [f"kb{sfx}"])
            for cc in range(gc):
                cs = slice(cc * 128, (cc + 1) * 128)
                p.add("pe", lambda e, T=T, cs=cs, bS=bS: e.matmul(bS[:, 0:128], lhsT=T["kTb"][:, cs], rhs=T["qTb"][:, cs],
                                                                  start=True, stop=True), [f"kTb{sfx}", f"qTb{sfx}"], [kS])
                p.add("dve", lambda e, T=T, cc=cc, bS=bS: e.scalar_tensor_tensor(
                    out=T["WT"][:], in0=bS[:, 0:128], scalar=T["b"][:, cc:cc + 1], in1=tri[:], op0=ALU.mult, op1=ALU.mult),
                    [kS, f"b{sfx}", "tri"], [f"WT{sfx}"])
                p.add("pe", lambda e, T=T, cc=cc, bX=bX: e.matmul(bX[:, 0:65], lhsT=T["WT"][:], rhs=T["vaug"][:, cc, :],
                                                                  start=True, stop=False), [f"WT{sfx}", f"vaug{sfx}", f"vaug1_{sfx}"], [kX])
                p.add("pe", lambda e, T=T, cs=cs, bX=bX, Cb=Cb: e.matmul(bX[:, 0:65], lhsT=T["qTb"][:, cs], rhs=Cb[:],
                                                                         start=False, stop=True), [f"qTb{sfx}", f"Cb{s}"], [kX])
                p.add("pe", lambda e, T=T, cc=cc, bU=bU: e.matmul(bU[0:64, 0:65], lhsT=T["kb"][:, cc, :], rhs=T["vaug"][:, cc, :],
                                                                  start=True, stop=True), [f"kb{sfx}", f"vaug{sfx}", f"vaug1_{sfx}"], [kU])
                d = T["d"]
                p.add("act", lambda e, T=T, cc=cc, bX=bX, d=d: e.activation(
                    out=d[:, 0:1], in_=bX[:, 64:65], func=AF.Abs, scale=T["a"][:, cc:cc + 1]),
                    [kX, f"a{sfx}"], [f"d0{sfx}"])
                p.add("dve", lambda e, d=d: e.tensor_scalar(out=d[:, 3:4], in0=d[:, 0:1], scalar1=1.0, scalar2=None, op0=ALU.max),
                      [f"d0{sfx}"], [f"d3{sfx}"])
                p.add("dve", lambda e, d=d: e.reciprocal(out=d[:, 1:2], in_=d[:, 3:4]), [f"d3{sfx}"], [f"d1{sfx}"])
                p.add("dve", lambda e, T=T, cc=cc, d=d: e.tensor_tensor(out=d[:, 2:3], in0=d[:, 1:2], in1=T["a"][:, cc:cc + 1], op=ALU.mult),
                      [f"d1{sfx}", f"a{sfx}"], [f"d2{sfx}"])
                p.add("dve", lambda e, T=T, cc=cc, bX=bX, d=d: e.tensor_scalar(
                    out=T["h"][:, cc, :], in0=bX[:, 0:64], scalar1=d[:, 2:3], scalar2=None, op0=ALU.mult),
                    [kX, f"d2{sfx}"], [f"h{sfx}"])
                p.add("dve", lambda e, bU=bU, Cf=Cf, tmpC=tmpC: e.tensor_tensor(out=tmpC[:], in0=bU[0:64, 0:65], in1=Cf[:], op=ALU.add),
                      [kU, f"Cf{s}"], [f"tmpC{s}"])
                p.add("dve", lambda e, T=T, cc=cc, Cf=Cf, tmpC=tmpC: e.tensor_scalar(
                    out=Cf[:], in0=tmpC[:], scalar1=T["eG"][0:64, cc:cc + 1], scalar2=None, op0=ALU.mult),
                    [f"tmpC{s}", f"eG{sfx}"], [f"Cf{s}"])
                p.add("act", lambda e, T=T, cc=cc, Cb=Cb, tmpC=tmpC: e.activation(
                    out=Cb[:], in_=tmpC[:], func=AF.Copy, scale=T["eG"][0:64, cc:cc + 1]),
                    [f"tmpC{s}", f"eG{sfx}"], [f"Cb{s}"])
            p.dma(mh[s].rearrange("(c p) f -> p c f", p=128)[:, c0:c0 + gc, :], T["h"][:, 0:gc, :], reads=[f"h{sfx}"])
    p.emit()
    return nc


def build_M2(NKT, NCT, NSL=2):
    nc = bass.Bass("TRN2", target_bir_lowering=False)
    L = NKT * 128
    aqT = _din(nc, "aqT", [NSL, 2, 64, L])
    akT = _din(nc, "akT", [NSL, 2, 64, L])
    av = _din(nc, "av", [NSL, L, 128])
    dl_d = _din(nc, "dl", [128, 256])
    dng_d = _din(nc, "dng", [128, 128])
    lami_d = _din(nc, "lami", [128, 2])
    ones_d = _din(nc, "ones", [128, 128])
    ao = _dout(nc, "ao", [NSL, L, 128])
    p = Prog(nc)
    ones = p.sb([128, 128], name="ones_sb"); p.dma(ones[:], ones_d, writes=["ones"])
    dl = p.sb([128, 256], name="dl_sb"); p.dma(dl[:], dl_d, writes=["dl"])
    gsc = p.sb([128, 128], name="gsc"); p.dma(gsc[:], dng_d, writes=["gsc"])
    lami = p.sb([128, 2], name="lami_sb"); p.dma(lami[:], lami_d, writes=["lami"])
    junk = p.sb([128, 128], name="junk")
    lm = p.sb([128, 4], name="lm")
    p.add("dve", lambda e: e.scalar_tensor_tensor(out=junk[:, 0:64], in0=dl[:, 0:64], scalar=1.0, in1=dl[:, 64:128],
                                                  op0=ALU.mult, op1=ALU.mult, accum_out=lm[:, 0:1]), ["dl"], ["junk", "lm0"])
    p.add("dve", lambda e: e.scalar_tensor_tensor(out=junk[:, 64:128], in0=dl[:, 128:192], scalar=1.0, in1=dl[:, 192:256],
                                                  op0=ALU.mult, op1=ALU.mult, accum_out=lm[:, 1:2]), ["dl"], ["junk", "lm1"])
    p.add("act", lambda e: e.activation(out=lm[:, 0:2], in_=lm[:, 0:2], func=AF.Exp), ["lm0", "lm1"], ["lm01"])
    p.add("dve", lambda e: e.tensor_tensor(out=lm[:, 2:3], in0=lm[:, 1:2], in1=lm[:, 0:1], op=ALU.subtract), ["lm01"], ["lm2"])
    p.add("dve", lambda e: e.tensor_scalar(out=lm[:, 3:4], in0=lm[:, 2:3], scalar1=lami[:, 0:1], scalar2=None, op0=ALU.subtract),
          ["lm2", "lami"], ["nlam"])
    p.add("dve", lambda e: e.tensor_scalar(out=gsc[:], in0=gsc[:], scalar1=lami[:, 1:2], scalar2=None, op0=ALU.mult),
          ["gsc", "lami"], ["gsc"])
    banks = [p.ps([128, 512], name=f"bank{i}") for i in range(8)]
    qTb = p.sb([64, 2, L], BF16, name="qTb")
    kTb = p.sb([64, 2, L], BF16, name="kTb")
    vaug = p.sb([128, NKT, 129], BF16, name="vaug")
    p.add("dve", lambda e: e.memset(vaug[:, :, 128:129], 1.0), [], ["vaug1"])
    PW = 2048
    stg = [p.sb([64, PW], name=f"stg{i}") for i in range(2)]
    sq = p.sb([64, PW], name="sq")
    VG = 16
    vst = [p.sb([128, VG, 128], name=f"vst{i}") for i in range(2)]
    mx = p.sb([128, 8], name="mx")
    nb = p.sb([128, 4], name="nb")
    pT = [p.sb([128, 512], BF16, name=f"pT{i}") for i in range(3)]
    ev = [p.sb([128, 8], name=f"ev{i}") for i in range(2)]
    o1 = [p.sb([128, 128], name=f"o1_{i}") for i in range(2)]
    oo = [p.sb([128, 128], name=f"oo_{i}") for i in range(2)]
    yy = [p.sb([128, 128], name=f"yy_{i}") for i in range(2)]
    nstg = 0
    nvst = 0
    nit = 0
    nblk = 0
    nev = 0
    for s in range(NSL):
        p.add("dve", lambda e: e.memset(mx[:], 0.0), [], ["mx", "mx4"])
        for which, (src, dst) in enumerate(((aqT, qTb), (akT, kTb))):
            for comp in range(2):
                for c0 in range(0, L, PW):
                    cw = min(PW, L - c0)
                    si = nstg % 2; nstg += 1
                    st = stg[si]
                    p.dma(st[:, 0:cw], src[s, comp, :, c0:c0 + cw], writes=[f"stg{si}"])
                    p.add("dve", lambda e, st=st, dst=dst, comp=comp, c0=c0, cw=cw: e.tensor_copy(out=dst[:, comp, c0:c0 + cw], in_=st[:, 0:cw]),
                          [f"stg{si}"], ["qTb" if which == 0 else "kTb"])
                    p.add("act", lambda e, st=st, cw=cw: e.activation(out=sq[:, 0:cw], in_=st[:, 0:cw], func=AF.Square), [f"stg{si}"], ["sq"])
                    for b0 in range(0, cw, 512):
                        bw = min(512, cw - b0)
                        p.add("pe", lambda e, b0=b0, bw=bw: e.matmul(banks[6][:, 0:bw], lhsT=ones[0:64, :], rhs=sq[:, b0:b0 + bw], start=True, stop=True),
                              ["ones", "sq"], ["bank6"])
                        p.add("dve", lambda e, bw=bw: e.reduce_max(out=mx[:, 4:5], in_=banks[6][:, 0:bw], axis=AX.X), ["bank6"], ["mx4"])
                        col = which * 2 + comp
                        p.add("dve", lambda e, col=col: e.tensor_tensor(out=mx[:, col:col + 1], in0=mx[:, col:col + 1], in1=mx[:, 4:5], op=ALU.max),
                              ["mx4", "mx"], ["mx"])
        for g0 in range(0, NKT, VG):
            gn = min(VG, NKT - g0)
            vi = nvst % 2; nvst += 1
            p.dma(vst[vi][:, 0:gn, :], av[s].rearrange("(c p) f -> p c f", p=128)[:, g0:g0 + gn, :], writes=[f"vst{vi}"])
            p.add("act", lambda e, vi=vi, g0=g0, gn=gn: e.activation(out=vaug[:, g0:g0 + gn, 0:128], in_=vst[vi][:, 0:gn, :], func=AF.Copy),
                  [f"vst{vi}"], ["vaug"])
        p.add("dve", lambda e: e.tensor_tensor(out=nb[:, 2:4], in0=mx[:, 0:2], in1=mx[:, 2:4], op=ALU.mult), ["mx"], ["nb2"])
        p.add("act", lambda e: e.activation(out=nb[:, 2:4], in_=nb[:, 2:4], func=AF.Sqrt, scale=1.0 / 64.0), ["nb2"], ["nb2"])
        p.add("dve", lambda e: e.tensor_scalar(out=nb[:, 0:2], in0=nb[:, 2:4], scalar1=60.0, scalar2=-1.0, op0=ALU.min, op1=ALU.mult),
              ["nb2"], ["nb"])
        blocks = [(0, NCT, 0, NCT)]
        for q0 in range(NCT, NKT, 4):
            blocks.append((q0, min(4, NKT - q0), 0, NKT))
        for (q0, nq, k0, nk) in blocks:
            oset = nblk % 2; nblk += 1
            obanks = [banks[oset * 3 + i] for i in range(3)]
            okeys = [f"bank{oset*3+i}" for i in range(3)]
            for i in range(3):
                p.add("dve", lambda e, i=i, obanks=obanks: e.memset(obanks[i][:], 0.0), [], [okeys[i]])

            def oacc(comp, j):
                a = comp * 4 + j
                return obanks[a // 3][:, (a % 3) * 129:(a % 3) * 129 + 129], okeys[a // 3]
            QW = nq * 128
            for kt in range(k0, k0 + nk):
                for comp in range(2):
                    sb_i = 6 + nit % 2
                    pi = nit % 3
                    nit += 1
                    p.add("pe", lambda e, comp=comp, kt=kt, sb_i=sb_i, q0=q0, QW=QW: e.matmul(
                        banks[sb_i][:, 0:QW], lhsT=kTb[:, comp, kt * 128:(kt + 1) * 128], rhs=qTb[:, comp, q0 * 128:q0 * 128 + QW],
                        start=True, stop=True), ["kTb", "qTb"], [f"bank{sb_i}"])
                    p.add("act", lambda e, comp=comp, sb_i=sb_i, pi=pi, QW=QW: e.activation(
                        out=pT[pi][:, 0:QW], in_=banks[sb_i][:, 0:QW], func=AF.Exp, scale=0.125, bias=nb[:, comp:comp + 1]),
                        [f"bank{sb_i}", "nb"], [f"pT{pi}"])
                    for j in range(nq):
                        oap, okey = oacc(comp, j)
                        p.add("pe", lambda e, oap=oap, pi=pi, j=j, kt=kt: e.matmul(
                            oap, lhsT=pT[pi][:, j * 128:(j + 1) * 128], rhs=vaug[:, kt, :], start=False, stop=False,
                            skip_group_check=True), [f"pT{pi}", "vaug", "vaug1"], [okey])
            for j in range(nq):
                ei = nev % 2; nev += 1
                E = ev[ei]
                (o1ap, k1), (o2ap, k2) = oacc(0, j), oacc(1, j)
                sfx = f"_{ei}"
                p.add("dve", lambda e, E=E, o1ap=o1ap: e.reciprocal(out=E[:, 0:1], in_=o1ap[:, 128:129]), [k1], ["ev0" + sfx])
                p.add("dve", lambda e, E=E, o2ap=o2ap: e.reciprocal(out=E[:, 1:2], in_=o2ap[:, 128:129]), [k2], ["ev1" + sfx])
                p.add("dve", lambda e, E=E: e.tensor_tensor(out=E[:, 2:3], in0=E[:, 1:2], in1=lm[:, 3:4], op=ALU.mult),
                      ["ev1" + sfx, "nlam"], ["ev2" + sfx])
                p.add("dve", lambda e, E=E, o1ap=o1ap, ei=ei: e.tensor_scalar(out=o1[ei][:], in0=o1ap[:, 0:128], scalar1=E[:, 0:1], scalar2=None,
                                                                           op0=ALU.mult), [k1, "ev0" + sfx], ["o1" + sfx])
                p.add("dve", lambda e, E=E, o2ap=o2ap, ei=ei: e.scalar_tensor_tensor(
                    out=oo[ei][:], in0=o2ap[:, 0:128], scalar=E[:, 2:3], in1=o1[ei][:], op0=ALU.mult, op1=ALU.add),
                    [k2, "ev2" + sfx, "o1" + sfx], ["oo" + sfx])
                p.add("dve", lambda e, E=E, ei=ei: e.scalar_tensor_tensor(
                    out=junk[:], in0=oo[ei][:], scalar=1.0, in1=oo[ei][:], op0=ALU.mult, op1=ALU.mult, accum_out=E[:, 3:4]),
                    ["oo" + sfx], ["junk", "ev3" + sfx])
                p.add("dve", lambda e, E=E: e.tensor_scalar(out=E[:, 4:5], in0=E[:, 3:4], scalar1=1.0 / 128.0, scalar2=EPS,
                                                            op0=ALU.mult, op1=ALU.add), ["ev3" + sfx], ["ev4" + sfx])
                p.add("act", lambda e, E=E: e.activation(out=E[:, 5:6], in_=E[:, 4:5], func=AF.Sqrt), ["ev4" + sfx], ["ev5" + sfx])
                p.add("dve", lambda e, E=E: e.reciprocal(out=E[:, 6:7], in_=E[:, 5:6]), ["ev5" + sfx], ["ev6" + sfx])
                p.add("dve", lambda e, E=E, ei=ei: e.scalar_tensor_tensor(
                    out=yy[ei][:], in0=oo[ei][:], scalar=E[:, 6:7], in1=gsc[:], op0=ALU.mult, op1=ALU.mult),
                    ["oo" + sfx, "ev6" + sfx, "gsc"], ["yy" + sfx])
                r0 = (q0 + j) * 128
                p.dma(ao[s, r0:r0 + 128, :], yy[ei][:], reads=["yy" + sfx])
    p.emit()
    return nc


def build_C1(segs):
    nc = bass.Bass("TRN2", target_bir_lowering=False)
    W = sum(n + 30 for n in segs)
    NT = sum(segs)
    aT = _din(nc, "aT", [512, W])
    cw_d = _din(nc, "cw", [128, 2, 31])
    cp_d = _din(nc, "cp", [128, 2, 3])
    ones_d = _din(nc, "ones", [128, 128])
    co = _dout(nc, "convT", [2, 128, NT])
    p = Prog(nc)
    ones = p.sb([128, 128], name="ones_sb"); p.dma(ones[:], ones_d, writes=["ones"])
    cw = p.sb([128, 2, 31], name="cw_sb"); p.dma(cw[:], cw_d, writes=["cw"])
    cp = p.sb([128, 2, 3], name="cp_sb"); p.dma(cp[:], cp_d, writes=["cp"])
    N = 512
    a1 = [p.sb([128, N + 30], name=f"a1_{i}") for i in range(2)]
    a2 = [p.sb([128, N + 30], name=f"a2_{i}") for i in range(2)]
    u = [p.sb([128, N + 30], name=f"u_{i}") for i in range(2)]
    acc = [[p.sb([128, N], name=f"acc_{i}_{r}") for r in range(2)] for i in range(2)]
    y = [p.sb([128, N], name=f"y_{i}") for i in range(2)]
    ysq = [p.sb([128, N], name=f"ysq_{i}") for i in range(2)]
    mean = p.sb([128, N], name="mean"); msq = p.sb([128, N], name="msq"); rstd = p.sb([128, N], name="rstd")
    zz = [p.sb([128, N], name=f"zz_{i}") for i in range(2)]
    bA = p.ps([128, 512], name="bankA"); bB = p.ps([128, 512], name="bankB")
    off = 0
    tok0 = 0
    for ns in segs:
        for t0 in range(0, ns, N):
            n = min(N, ns - t0)
            for j in range(2):
                c0 = off + t0
                p.dma(a1[j][:, 0:n + 30], aT[j * 128:(j + 1) * 128, c0:c0 + n + 30], writes=[f"a1_{j}"])
                p.dma(a2[j][:, 0:n + 30], aT[256 + j * 128:256 + (j + 1) * 128, c0:c0 + n + 30], writes=[f"a2_{j}"])
                p.add("act", lambda e, j=j, n=n: e.activation(out=a2[j][:, 0:n + 30], in_=a2[j][:, 0:n + 30], func=AF.Sigmoid),
                      [f"a2_{j}"], [f"a2_{j}"])
                p.add("dve", lambda e, j=j, n=n: e.tensor_tensor(out=u[j][:, 0:n + 30], in0=a1[j][:, 0:n + 30], in1=a2[j][:, 0:n + 30], op=ALU.mult),
                      [f"a1_{j}", f"a2_{j}"], [f"u_{j}"])
                p.add("dve", lambda e, j=j, n=n: e.tensor_scalar(out=acc[j][0][:, 0:n], in0=u[j][:, 0:n], scalar1=cw[:, j, 0:1], scalar2=None,
                                                                op0=ALU.mult), [f"u_{j}", "cw"], [f"acc_{j}_0"])
                for k in range(1, 31):
                    src, dst = acc[j][(k - 1) % 2], acc[j][k % 2]
                    p.add("dve", lambda e, j=j, n=n, k=k, src=src, dst=dst: e.scalar_tensor_tensor(
                        out=dst[:, 0:n], in0=u[j][:, k:k + n], scalar=cw[:, j, k:k + 1], in1=src[:, 0:n], op0=ALU.mult, op1=ALU.add),
                        [f"u_{j}", "cw", f"acc_{j}_{(k-1)%2}"], [f"acc_{j}_{k%2}"])
                p.add("dve", lambda e, j=j, n=n: e.tensor_scalar(out=y[j][:, 0:n], in0=acc[j][0][:, 0:n], scalar1=cp[:, j, 0:1], scalar2=None,
                                                                op0=ALU.add), [f"acc_{j}_0", "cp"], [f"y_{j}"])
                p.add("act", lambda e, j=j, n=n: e.activation(out=ysq[j][:, 0:n], in_=y[j][:, 0:n], func=AF.Square), [f"y_{j}"], [f"ysq_{j}"])
            for j in range(2):
                p.add("pe", lambda e, j=j, n=n: e.matmul(bA[:, 0:n], lhsT=ones[:], rhs=y[j][:, 0:n], start=(j == 0), stop=(j == 1)),
                      ["ones", f"y_{j}"], ["bankA"])
            for j in range(2):
                p.add("pe", lambda e, j=j, n=n: e.matmul(bB[:, 0:n], lhsT=ones[:], rhs=ysq[j][:, 0:n], start=(j == 0), stop=(j == 1)),
                      ["ones", f"ysq_{j}"], ["bankB"])
            p.add("act", lambda e, n=n: e.activation(out=mean[:, 0:n], in_=bA[:, 0:n], func=AF.Copy, scale=1.0 / 256), ["bankA"], ["mean"])
            p.add("act", lambda e, n=n: e.activation(out=msq[:, 0:n], in_=bA[:, 0:n], func=AF.Square, scale=1.0 / 256), ["bankA"], ["msq"])
            p.add("dve", lambda e, n=n: e.scalar_tensor_tensor(out=rstd[:, 0:n], in0=bB[:, 0:n], scalar=1.0 / 256, in1=msq[:, 0:n],
                                                               op0=ALU.mult, op1=ALU.subtract), ["bankB", "msq"], ["rstd"])
            p.add("dve", lambda e, n=n: e.tensor_scalar(out=rstd[:, 0:n], in0=rstd[:, 0:n], scalar1=EPS, scalar2=None, op0=ALU.add),
                  ["rstd"], ["rstd"])
            p.add("act", lambda e, n=n: e.activation(out=rstd[:, 0:n], in_=rstd[:, 0:n], func=AF.Sqrt), ["rstd"], ["rstd"])
            p.add("dve", lambda e, n=n: e.reciprocal(out=rstd[:, 0:n], in_=rstd[:, 0:n]), ["rstd"], ["rstd"])
            for j in range(2):
                p.add("dve", lambda e, j=j, n=n: e.tensor_tensor(out=zz[j][:, 0:n], in0=y[j][:, 0:n], in1=mean[:, 0:n], op=ALU.subtract),
                      [f"y_{j}", "mean"], [f"zz_{j}"])
                p.add("dve", lambda e, j=j, n=n: e.tensor_tensor(out=zz[j][:, 0:n], in0=zz[j][:, 0:n], in1=rstd[:, 0:n], op=ALU.mult),
                      [f"zz_{j}", "rstd"], [f"zz_{j}"])
                p.add("act", lambda e, j=j, n=n: e.activation(out=zz[j][:, 0:n], in_=zz[j][:, 0:n], func=AF.Silu,
                                                              scale=cp[:, j, 1:2], bias=cp[:, j, 2:3]), [f"zz_{j}", "cp"], [f"zz_{j}"])
                p.dma(co[j, :, tok0 + t0:tok0 + t0 + n], zz[j][:, 0:n], reads=[f"zz_{j}"])
        off += ns + 30
        tok0 += ns
    p.emit()
    return nc


def build_C2(NT, ctx_tiles=(0,)):
    nc = bass.Bass("TRN2", target_bir_lowering=False)
    L = NT * 128
    xs = _din(nc, "xs", [L, D])
    convT = _din(nc, "convT", [2, 128, L])
    hf_d = _din(nc, "hf", [L, 256])
    hb_d = _din(nc, "hb", [L, 256])
    mo_d = _din(nc, "mo", [L, 256])
    ao_d = _din(nc, "ao", [L, 512])
    mng_d = _din(nc, "mng", [128, 64])
    w_out = _din(nc, "w_out", [D, D])
    bc_d = _din(nc, "bc", [7, 128, D])
    ident = _din(nc, "ident", [128, 128])
    x1_o = _dout(nc, "x1", [L, D])
    h2_o = _dout(nc, "h2", [L, D])
    p = Prog(nc)
    wob = p.sb([128, 8, D], BF16, name="wob")
    load_cast_weight(p, wob, w_out, D, "wob")
    idt = p.sb([128, 128], name="idt"); p.dma(idt[:], ident, writes=["idt"])
    mng = p.sb([128, 64], name="mng_sb"); p.dma(mng[:], mng_d, writes=["mng"])
    bc = [p.sb([128, D], name=f"bc{i}") for i in range(7)]
    for i in range(7):
        p.dma(bc[i][:], bc_d[i], writes=[f"bc{i}"])
    for i in (3, 4):
        p.add("dve", lambda e, i=i: e.scalar_tensor_tensor(out=bc[i][:], in0=bc[i][:], scalar=1.0, in1=bc[0][:], op0=ALU.add, op1=ALU.mult),
              [f"bc{i}", "bc0"], [f"bc{i}"])
    NB = 2
    def mk(shape, nm, dt=F32):
        return [p.sb(shape, dt, name=f"{nm}{i}") for i in range(NB)]
    xt = mk([128, D], "xt"); hf = mk([128, 256], "hf"); hb = mk([128, 256], "hb"); mo = mk([128, 256], "mo")
    aot = mk([128, 512], "aot"); cvt = mk([128, 2, 128], "cvt"); ym = mk([128, 256], "ym"); sqm = mk([128, 256], "sqm")
    st4 = mk([128, 8], "st4"); mixT = mk([128, 8, 128], "mixT", BF16); tmp = mk([128, D], "tmp"); x1 = mk([128, D], "x1")
    h2 = mk([128, D], "h2"); ss = mk([128, 4], "ss")
    junk = p.sb([128, D], name="junk")
    bT = [p.ps([128, 512], name=f"bT{i}") for i in range(4)]
    bAcc = [p.ps([128, 512], name=f"bAcc{i}") for i in range(4)]
    for t in range(NT):
        i = t % NB
        isc = t in ctx_tiles
        r0 = t * 128
        g1b = bc[2] if isc else bc[1]
        gm2 = bc[4] if isc else bc[3]
        sh2 = bc[6] if isc else bc[5]
        kg1, kgm2, ksh2 = (f"bc{2 if isc else 1}", f"bc{4 if isc else 3}", f"bc{6 if isc else 5}")
        p.dma(xt[i][:], xs[r0:r0 + 128, :], writes=[f"xt{i}"])
        p.dma(hf[i][:], hf_d[r0:r0 + 128, :], writes=[f"hf{i}"])
        p.dma(hb[i][:], hb_d[r0:r0 + 128, :], writes=[f"hb{i}"])
        p.dma(mo[i][:], mo_d[r0:r0 + 128, :], writes=[f"mo{i}"])
        p.dma(aot[i][:], ao_d[r0:r0 + 128, :], writes=[f"aot{i}"])
        p.dma(cvt[i][:], convT[:, :, r0:r0 + 128].rearrange("j p n -> p j n"), writes=[f"cvt{i}"])
        p.add("dve", lambda e, i=i: e.tensor_tensor(out=ym[i][:], in0=hf[i][:], in1=hb[i][:], op=ALU.add), [f"hf{i}", f"hb{i}"], [f"ym{i}"])
        p.add("act", lambda e, i=i: e.activation(out=sqm[i][:], in_=ym[i][:], func=AF.Square), [f"ym{i}"], [f"sqm{i}"])
        p.add("dve", lambda e, i=i: e.tensor_reduce(out=st4[i][:, 0:4], in_=sqm[i][:].rearrange("p (h d) -> p h d", h=4), axis=AX.X, op=ALU.add),
              [f"sqm{i}"], [f"st4a{i}"])
        p.add("dve", lambda e, i=i: e.tensor_scalar(out=st4[i][:, 4:8], in0=st4[i][:, 0:4], scalar1=1.0 / 64, scalar2=EPS, op0=ALU.mult, op1=ALU.add),
              [f"st4a{i}"], [f"st4b{i}"])
        p.add("act", lambda e, i=i: e.activation(out=st4[i][:, 0:4], in_=st4[i][:, 4:8], func=AF.Sqrt), [f"st4b{i}"], [f"st4a{i}"])
        p.add("dve", lambda e, i=i: e.reciprocal(out=st4[i][:, 4:8], in_=st4[i][:, 0:4]), [f"st4a{i}"], [f"st4b{i}"])
        p.add("act", lambda e, i=i: e.activation(out=mo[i][:], in_=mo[i][:], func=AF.Sigmoid), [f"mo{i}"], [f"mo{i}"])
        ym3 = ym[i][:].rearrange("p (h d) -> p h d", h=4)
        p.add("dve", lambda e, i=i, ym3=ym3: e.tensor_tensor(out=ym3, in0=ym3, in1=st4[i][:, 4:8, None].to_broadcast([128, 4, 64]), op=ALU.mult),
              [f"ym{i}", f"st4b{i}"], [f"ym{i}"])
        p.add("dve", lambda e, i=i, ym3=ym3: e.tensor_tensor(out=ym3, in0=ym3, in1=mng[:, None, :].to_broadcast([128, 4, 64]), op=ALU.mult),
              [f"ym{i}", "mng"], [f"ym{i}"])
        p.add("dve", lambda e, i=i: e.tensor_tensor(out=ym[i][:], in0=ym[i][:], in1=mo[i][:], op=ALU.mult), [f"ym{i}", f"mo{i}"], [f"ym{i}"])
        p.add("act", lambda e, i=i: e.activation(out=mixT[i][:, 0:2, :], in_=cvt[i][:], func=AF.Copy), [f"cvt{i}"], [f"mixT{i}_c"])
        ta, tb = (2 * t) % 4, (2 * t + 1) % 4
        srcs = [(ym[i], 0, f"ym{i}"), (ym[i], 128, f"ym{i}"), (aot[i], 0, f"aot{i}"), (aot[i], 128, f"aot{i}"),
                (aot[i], 256, f"aot{i}"), (aot[i], 384, f"aot{i}")]
        for n_, (src, c0, key) in enumerate(srcs):
            bank = ta if n_ < 4 else tb
            col = (n_ % 4) * 128
            p.add("pe", lambda e, src=src, c0=c0, bank=bank, col=col: e.transpose(bT[bank][:, col:col + 128], src[:, c0:c0 + 128], idt[:]),
                  [key, "idt"], [f"bT{bank}"])
        p.add("act", lambda e, i=i, ta=ta: e.activation(out=mixT[i][:, 2:6, :], in_=bT[ta][:].rearrange("p (k n) -> p k n", k=4), func=AF.Copy),
              [f"bT{ta}"], [f"mixT{i}_a"])
        p.add("dve", lambda e, i=i, tb=tb: e.tensor_copy(out=mixT[i][:, 6:8, :], in_=bT[tb][:, 0:256].rearrange("p (k n) -> p k n", k=2)),
              [f"bT{tb}"], [f"mixT{i}_b"])
        for blk in range(2):
            a = (2 * t + blk) % 4
            for k in range(8):
                p.add("pe", lambda e, i=i, k=k, a=a, blk=blk: e.matmul(bAcc[a][:], lhsT=mixT[i][:, k, :], rhs=wob[:, k, blk * 512:(blk + 1) * 512],
                                                                      start=(k == 0), stop=(k == 7)),
                      [f"mixT{i}_c", f"mixT{i}_a", f"mixT{i}_b", "wob"], [f"bAcc{a}"])
            cs = slice(blk * 512, (blk + 1) * 512)
            p.add("dve", lambda e, i=i, a=a, cs=cs, g1b=g1b: e.tensor_tensor(out=tmp[i][:, cs], in0=bAcc[a][:], in1=g1b[:, cs], op=ALU.mult),
                  [f"bAcc{a}", kg1], [f"tmp{i}_{blk}"])
            p.add("dve", lambda e, i=i, cs=cs: e.tensor_tensor(out=x1[i][:, cs], in0=tmp[i][:, cs], in1=xt[i][:, cs], op=ALU.add),
                  [f"tmp{i}_{blk}", f"xt{i}"], [f"x1{i}_{blk}"])
        p.dma(x1_o[r0:r0 + 128, :], x1[i][:], reads=[f"x1{i}_0", f"x1{i}_1"])
        p.add("act", lambda e, i=i: e.activation(out=junk[:], in_=x1[i][:], func=AF.Square, accum_out=ss[i][:, 0:1]),
              [f"x1{i}_0", f"x1{i}_1"], ["junk", f"ss0{i}"])
        p.add("dve", lambda e, i=i: e.tensor_scalar(out=ss[i][:, 1:2], in0=ss[i][:, 0:1], scalar1=1.0 / D, scalar2=EPS, op0=ALU.mult, op1=ALU.add),
              [f"ss0{i}"], [f"ss1{i}"])
        p.add("act", lambda e, i=i: e.activation(out=ss[i][:, 2:3], in_=ss[i][:, 1:2], func=AF.Sqrt), [f"ss1{i}"], [f"ss2{i}"])
        p.add("dve", lambda e, i=i: e.reciprocal(out=ss[i][:, 3:4], in_=ss[i][:, 2:3]), [f"ss2{i}"], [f"ss3{i}"])
        p.add("dve", lambda e, i=i, gm2=gm2: e.scalar_tensor_tensor(out=h2[i][:], in0=x1[i][:], scalar=ss[i][:, 3:4], in1=gm2[:],
                                                                     op0=ALU.mult, op1=ALU.mult),
              [f"x1{i}_0", f"x1{i}_1", f"ss3{i}", kgm2], [f"h2{i}"])
        p.add("dve", lambda e, i=i, sh2=sh2: e.tensor_tensor(out=h2[i][:], in0=h2[i][:], in1=sh2[:], op=ALU.add), [f"h2{i}", ksh2], [f"h2{i}"])
        p.dma(h2_o[r0:r0 + 128, :], h2[i][:], reads=[f"h2{i}"])
    p.emit()
    return nc


def build_C3(NT, ctx_tiles=(0,), NGB=4):
    nc = bass.Bass("TRN2", target_bir_lowering=False)
    L = NT * 128
    NE = 16384
    h2_d = _din(nc, "h2", [L, D])
    x1_d = _din(nc, "x1", [L, D])
    wq = _din(nc, "wq", [D, D])
    kbd_d = _din(nc, "kbd", [128, 8, 256])
    pu = _din(nc, "peer_u", [NE, D])
    pv = _din(nc, "peer_v", [NE, D])
    g2_d = _din(nc, "g2b", [2, 128, D])
    ident = _din(nc, "ident", [128, 128])
    x2_o = _dout(nc, "x2", [L, D])
    p = Prog(nc)
    wqb = p.sb([128, 8, D], F32, name="wqb")
    p.dma(wqb[:], wq.rearrange("(k p) n -> p k n", p=128), writes=["wqb"])
    idt = p.sb([128, 128], name="idt"); p.dma(idt[:], ident, writes=["idt"])
    kbd = p.sb([128, 8, 256], name="kbd_sb"); p.dma(kbd[:], kbd_d, writes=["kbd"])
    g2b = [p.sb([128, D], name=f"g2b{i}") for i in range(2)]
    for i in range(2):
        p.dma(g2b[i][:], g2_d[i], writes=[f"g2b{i}"])
    h2t = p.sb([128, D], name="h2t"); x1t = p.sb([128, D], name="x1t")
    h2T = p.sb([128, 8, 128], F32, name="h2T")
    qTf = p.sb([128, 8, 128], name="qTf")
    sc = p.sb([128, 16, 128], name="sc"); wk = p.sb([128, 16, 128], name="wk")
    st = p.sb([128, 16, 16], name="st"); it = p.sb([128, 16, 16], U32, name="it"); itf = p.sb([128, 16, 16], name="itf")
    cand = p.sb([128, 8, 256], name="cand"); cidx = p.sb([128, 8, 256], name="cidx"); wk2 = p.sb([128, 8, 256], name="wk2")
    best = p.sb([128, 8, 16], name="best"); gw = p.sb([128, 8, 16], name="gw")
    eidf = p.sb([128, 128], name="eidf"); eidi = p.sb([128, 128], I32, name="eidi")
    sm = p.sb([128, 16], name="sm")
    act = p.sb([128, 128], name="act_sb"); coef = p.sb([128, 128], name="coef")
    junk = p.sb([128, D], name="junk")
    acc = p.sb([128, D], name="acc"); x2t = p.sb([128, D], name="x2t")
    rows = [p.sb([128, D], name=f"rows{i}") for i in range(NGB)]
    banks = [p.ps([128, 512], name=f"bank{i}") for i in range(8)]
    ngat = 0
    for t in range(NT):
        isc = t in ctx_tiles
        r0 = t * 128
        gb = g2b[1] if isc else g2b[0]
        kgb = "g2b1" if isc else "g2b0"
        p.dma(h2t[:], h2_d[r0:r0 + 128, :], writes=["h2t"])
        p.dma(x1t[:], x1_d[r0:r0 + 128, :], writes=["x1t"])
        for half in range(2):
            for k in range(half * 4, half * 4 + 4):
                p.add("pe", lambda e, k=k, half=half: e.transpose(banks[half][:, (k % 4) * 128:(k % 4 + 1) * 128], h2t[:, k * 128:(k + 1) * 128], idt[:]),
                      ["h2t", "idt"], [f"bank{half}"])
        p.add("act", lambda e: e.activation(out=h2T[:, 0:4, :], in_=banks[0][:].rearrange("p (k n) -> p k n", k=4), func=AF.Copy), ["bank0"], ["h2T_a"])
        p.add("dve", lambda e: e.tensor_copy(out=h2T[:, 4:8, :], in_=banks[1][:].rearrange("p (k n) -> p k n", k=4)), ["bank1"], ["h2T_b"])
        for c in range(8):
            bk = 2 + c // 4
            for k in range(8):
                p.add("pe", lambda e, c=c, k=k, bk=bk: e.matmul(banks[bk][:, (c % 4) * 128:(c % 4 + 1) * 128], lhsT=wqb[:, k, c * 128:(c + 1) * 128],
                                                              rhs=h2T[:, k, :], start=(k == 0), stop=(k == 7)),
                      ["wqb", "h2T_a", "h2T_b"], [f"bank{bk}"])
        p.add("act", lambda e: e.activation(out=qTf[:, 0:4, :], in_=banks[2][:].rearrange("p (k n) -> p k n", k=4), func=AF.Copy), ["bank2"], ["qTf_a"])
        p.add("dve", lambda e: e.tensor_copy(out=qTf[:, 4:8, :], in_=banks[3][:].rearrange("p (k n) -> p k n", k=4)), ["bank3"], ["qTf_b"])
        for h in range(8):
            bk = 4 + h // 2
            p.add("pe", lambda e, h=h, bk=bk: e.matmul(banks[bk][:, (h % 2) * 256:(h % 2 + 1) * 256], lhsT=qTf[:, h, :], rhs=kbd[:, h, :],
                                                      start=True, stop=True), ["qTf_a", "qTf_b", "kbd"], [f"bank{bk}"])
        for bk in range(4, 8):
            g0 = (bk - 4) * 4
            if bk % 2 == 0:
                p.add("act", lambda e, bk=bk, g0=g0: e.activation(out=sc[:, g0:g0 + 4, :], in_=banks[bk][:].rearrange("p (g n) -> p g n", g=4), func=AF.Copy),
                      [f"bank{bk}"], [f"sc{bk}"])
            else:
                p.add("dve", lambda e, bk=bk, g0=g0: e.tensor_copy(out=sc[:, g0:g0 + 4, :], in_=banks[bk][:].rearrange("p (g n) -> p g n", g=4)),
                      [f"bank{bk}"], [f"sc{bk}"])
        for g in range(16):
            ks = f"sc{4 + g // 4}"
            p.add("dve", lambda e, g=g: e.max(out=st[:, g, 0:8], in_=sc[:, g, :]), [ks], [f"st{g}a"])
            p.add("dve", lambda e, g=g: e.max_index(out=it[:, g, 0:8], in_max=st[:, g, 0:8], in_values=sc[:, g, :]), [ks, f"st{g}a"], [f"it{g}a"])
            p.add("dve", lambda e, g=g: e.match_replace(out=wk[:, g, :], in_to_replace=st[:, g, 0:8], in_values=sc[:, g, :], imm_value=-1e30),
                  [ks, f"st{g}a"], [f"wk{g}"])
            p.add("dve", lambda e, g=g: e.max(out=st[:, g, 8:16], in_=wk[:, g, :]), [f"wk{g}"], [f"st{g}b"])
            p.add("dve", lambda e, g=g: e.max_index(out=it[:, g, 8:16], in_max=st[:, g, 8:16], in_values=wk[:, g, :]), [f"wk{g}", f"st{g}b"], [f"it{g}b"])
        allst = [f"st{g}{x}" for g in range(16) for x in "ab"]
        allit = [f"it{g}{x}" for g in range(16) for x in "ab"]
        p.add("dve", lambda e: e.tensor_copy(out=itf[:], in_=it[:]), allit, ["itf"])
        st4 = st[:].rearrange("p (h two) k -> p h two k", two=2)
        itf4 = itf[:].rearrange("p (h two) k -> p h two k", two=2)
        cand4 = cand[:].rearrange("p h (i j) -> p h i j", i=16)
        cidx4 = cidx[:].rearrange("p h (i j) -> p h i j", i=16)
        for h in range(8):
            p.add("dve", lambda e, h=h: e.tensor_tensor(out=cand4[:, h], in0=st4[:, h, 0, :, None].to_broadcast([128, 16, 16]),
                                                        in1=st4[:, h, 1, None, :].to_broadcast([128, 16, 16]), op=ALU.add), allst, [f"cand{h}"])
            p.add("dve", lambda e, h=h: e.scalar_tensor_tensor(out=cidx4[:, h], in0=itf4[:, h, 0, :, None].to_broadcast([128, 16, 16]), scalar=128.0,
                                                               in1=itf4[:, h, 1, None, :].to_broadcast([128, 16, 16]), op0=ALU.mult, op1=ALU.add),
                  ["itf"], [f"cidx{h}"])
            p.add("dve", lambda e, h=h: e.max(out=best[:, h, 0:8], in_=cand[:, h, :]), [f"cand{h}"], [f"best{h}a"])
            p.add("dve", lambda e, h=h: e.match_replace(out=wk2[:, h, :], in_to_replace=best[:, h, 0:8], in_values=cand[:, h, :], imm_value=-1e30),
                  [f"cand{h}", f"best{h}a"], [f"wk2{h}"])
            p.add("dve", lambda e, h=h: e.max(out=best[:, h, 8:16], in_=wk2[:, h, :]), [f"wk2{h}"], [f"best{h}b"])
            for k in range(16):
                hk = h * 16 + k
                p.add("dve", lambda e, h=h, k=k, hk=hk: e.scalar_tensor_tensor(
                    out=junk[:, 0:256], in0=cand[:, h, :], scalar=best[:, h, k:k + 1], in1=cidx[:, h, :], op0=ALU.is_equal, op1=ALU.mult,
                    accum_out=eidf[:, hk:hk + 1]), [f"cand{h}", f"cidx{h}", f"best{h}a", f"best{h}b"], ["junk", f"eidf{h}"])
        alle = [f"eidf{h}" for h in range(8)]
        allb = [f"best{h}{x}" for h in range(8) for x in "ab"]
        p.add("dve", lambda e: e.tensor_scalar(out=eidf[:], in0=eidf[:], scalar1=float(NE - 1), scalar2=0.0, op0=ALU.min, op1=ALU.max), alle, ["eidf"])
        p.add("dve", lambda e: e.tensor_copy(out=eidi[:], in_=eidf[:]), ["eidf"], ["eidi"])
        p.add("dve", lambda e: e.tensor_tensor(out=gw[:], in0=best[:], in1=best[:, :, 0:1].to_broadcast([128, 8, 16]), op=ALU.subtract), allb, ["gw"])
        p.add("act", lambda e: e.activation(out=gw[:], in_=gw[:], func=AF.Exp), ["gw"], ["gw"])
        p.add("dve", lambda e: e.tensor_reduce(out=sm[:, 0:8], in_=gw[:], axis=AX.X, op=ALU.add), ["gw"], ["sm0"])
        p.add("dve", lambda e: e.reciprocal(out=sm[:, 8:16], in_=sm[:, 0:8]), ["sm0"], ["sm1"])
        p.add("dve", lambda e: e.tensor_tensor(out=gw[:], in0=gw[:], in1=sm[:, 8:16, None].to_broadcast([128, 8, 16]), op=ALU.mult), ["gw", "sm1"], ["gw"])
        for hk in range(128):
            b = ngat % NGB; ngat += 1
            p.add("pool", lambda e, b=b, hk=hk: e.indirect_dma_start(
                out=rows[b][:], out_offset=None, in_=pu, in_offset=bass.IndirectOffsetOnAxis(ap=eidi[:, hk:hk + 1], axis=0)),
                ["eidi"], [f"rows{b}"])
            p.add("dve", lambda e, b=b, hk=hk: e.scalar_tensor_tensor(out=junk[:], in0=rows[b][:], scalar=1.0, in1=h2t[:], op0=ALU.mult, op1=ALU.mult,
                                                                     accum_out=act[:, hk:hk + 1]), [f"rows{b}", "h2t"], ["junk", "act"])
        p.add("act", lambda e: e.activation(out=coef[:], in_=act[:], func=AF.Gelu), ["act"], ["coef"])
        p.add("dve", lambda e: e.tensor_tensor(out=coef[:], in0=coef[:], in1=gw[:].rearrange("p h k -> p (h k)"), op=ALU.mult), ["coef", "gw"], ["coef"])
        p.add("dve", lambda e: e.memset(acc[:], 0.0), [], ["acc"])
        for hk in range(128):
            b = ngat % NGB; ngat += 1
            p.add("pool", lambda e, b=b, hk=hk: e.indirect_dma_start(
                out=rows[b][:], out_offset=None, in_=pv, in_offset=bass.IndirectOffsetOnAxis(ap=eidi[:, hk:hk + 1], axis=0)),
                ["eidi"], [f"rows{b}"])
            p.add("dve", lambda e, b=b, hk=hk: e.scalar_tensor_tensor(out=acc[:], in0=rows[b][:], scalar=coef[:, hk:hk + 1], in1=acc[:],
                                                                     op0=ALU.mult, op1=ALU.add), [f"rows{b}", "coef", "acc"], ["acc"])
        p.add("dve", lambda e, gb=gb: e.tensor_tensor(out=x2t[:], in0=acc[:], in1=gb[:], op=ALU.mult), ["acc", kgb], ["x2t"])
        p.add("dve", lambda e: e.tensor_tensor(out=x2t[:], in0=x2t[:], in1=x1t[:], op=ALU.add), ["x2t", "x1t"], ["x2t"])
        p.dma(x2_o[r0:r0 + 128, :], x2t[:], reads=["x2t"])
    p.emit()
    return nc


def build_F(NT):
    nc = bass.Bass("TRN2", target_bir_lowering=False)
    L = NT * 128
    xs = _din(nc, "xs", [L, D])
    fg_d = _din(nc, "fg", [128, D])
    yo = _dout(nc, "y", [L, D])
    p = Prog(nc)
    fg = p.sb([128, D], name="fg_sb"); p.dma(fg[:], fg_d, writes=["fg"])
    junk = p.sb([128, D], name="junk")
    NB = 2
    xt = [p.sb([128, D], name=f"xt{i}") for i in range(NB)]
    yt = [p.sb([128, D], name=f"yt{i}") for i in range(NB)]
    ss = [p.sb([128, 4], name=f"ss{i}") for i in range(NB)]
    for t in range(NT):
        i = t % NB
        r0 = t * 128
        p.dma(xt[i][:], xs[r0:r0 + 128, :], writes=[f"xt{i}"])
        p.add("act", lambda e, i=i: e.activation(out=junk[:], in_=xt[i][:], func=AF.Square, accum_out=ss[i][:, 0:1]), [f"xt{i}"], ["junk", f"ss0{i}"])
        p.add("dve", lambda e, i=i: e.tensor_scalar(out=ss[i][:, 1:2], in0=ss[i][:, 0:1], scalar1=1.0 / D, scalar2=EPS, op0=ALU.mult, op1=ALU.add),
              [f"ss0{i}"], [f"ss1{i}"])
        p.add("act", lambda e, i=i: e.activation(out=ss[i][:, 2:3], in_=ss[i][:, 1:2], func=AF.Sqrt), [f"ss1{i}"], [f"ss2{i}"])
        p.add("dve", lambda e, i=i: e.reciprocal(out=ss[i][:, 3:4], in_=ss[i][:, 2:3]), [f"ss2{i}"], [f"ss3{i}"])
        p.add("dve", lambda e, i=i: e.scalar_tensor_tensor(out=yt[i][:], in0=xt[i][:], scalar=ss[i][:, 3:4], in1=fg[:], op0=ALU.mult, op1=ALU.mult),
              [f"xt{i}", f"ss3{i}", "fg"], [f"yt{i}"])
        p.dma(yo[r0:r0 + 128, :], yt[i][:], reads=[f"yt{i}"])
    p.emit()
    return nc


_PROGS = {}


def _prog(name, fn):
    if name not in _PROGS:
        _PROGS[name] = fn()
    return _PROGS[name]


def _run(nc, maps):
    res = run_bass_kernel_spmd(nc, maps, core_ids=list(range(NCORES)))
    return res.results


def _rep(v, n=128):
    return np.ascontiguousarray(np.broadcast_to(np.asarray(v, np.float32)[None], (n, v.shape[0])))


def _rope_tables(n_tokens):
    rows = n_tokens // 64
    row = np.repeat(np.arange(rows), 64).astype(np.float32)
    col = np.tile(np.arange(64), rows).astype(np.float32)
    inv = (np.float32(10000.0) ** (-np.arange(0, 32, 2, dtype=np.float32) / np.float32(32))).astype(np.float32)
    ang = np.concatenate([row[:, None] * inv, col[:, None] * inv], axis=-1).astype(np.float32)
    return np.cos(ang).astype(np.float32), np.sin(ang).astype(np.float32)


def kernel_unfused(x, c, ctx, c_ctx, ada_w, ada_b, norm1_g, norm2_g, w_in, conv_w, conv_b, conv_ln_g, conv_ln_b,
           mlstm_gate_b, mlstm_norm_g, diff_lambda, diff_norm_g, w_out, peer_wq, peer_keys, peer_u, peer_v, final_g):
    f32 = np.float32
    x = np.asarray(x, f32); ctx = np.asarray(ctx, f32)
    B, S, _ = x.shape
    CT = ctx.shape[1]
    DEPTH = ada_w.shape[0]
    HS, HC = S // 2, CT // 2
    NT = (HS + HC) // 128
    NKT = (S + CT) // 128
    NCT = CT // 128
    ident = np.eye(128, dtype=f32)
    ones = np.ones((128, 128), f32)
    tri = np.triu(np.ones((128, 128), f32))
    cos, sin = _rope_tables(S)
    cores = [(cc // 2, cc % 2) for cc in range(NCORES)]

    cT = np.concatenate([np.asarray(c, f32), np.asarray(c_ctx, f32)[None]], 0).T
    cTl = np.ascontiguousarray(cT.reshape(8, 128, 5).transpose(1, 0, 2))
    maps = []
    for cc in range(NCORES):
        ll, half = cc // 2, cc % 2
        maps.append({"w": np.ascontiguousarray(ada_w[ll][:, half * 3072:(half + 1) * 3072]),
                     "b": np.ascontiguousarray(np.asarray(ada_b[ll], f32)[half * 3072:(half + 1) * 3072].reshape(24, 128).T),
                     "cT": cTl})
    res = _run(_prog("P0", build_P0), maps)
    mod = np.zeros((DEPTH, 6144, 5), f32)
    for cc in range(NCORES):
        ll, half = cc // 2, cc % 2
        mod[ll, half * 3072:(half + 1) * 3072] = res[cc]["modT"].transpose(1, 0, 2).reshape(3072, 5)

    xs = [np.concatenate([ctx[b, z * HC:(z + 1) * HC], x[b, z * HS:(z + 1) * HS]], 0) for (b, z) in cores]
    cs_core = []
    for (b, z) in cores:
        cs = np.zeros((HC + HS, 64), f32)
        cs[:HC, :32] = 1.0
        cs[HC:, :32] = cos[z * HS:(z + 1) * HS]
        cs[HC:, 32:] = sin[z * HS:(z + 1) * HS]
        cs_core.append(cs)

    def fv(v):
        return np.asarray(v, f32).reshape(8, 128).T

    for l in range(DEPTH):
        m6 = mod[l]
        sh1, sc1, g1, sh2, sc2, g2 = [m6[i * 1024:(i + 1) * 1024] for i in range(6)]
        lam_init = 0.8 - 0.6 * math.exp(-0.3 * l)
        maps = []
        for cc, (b, z) in enumerate(cores):
            pv = np.ascontiguousarray(np.stack([fv(norm1_g[l]), fv(sc1[:, b]), fv(sh1[:, b]), fv(sc1[:, 4]), fv(sh1[:, 4])], -1))
            maps.append({"xs": xs[cc], "w_in": np.asarray(w_in[l], f32), "pv": pv, "cs": cs_core[cc], "ident": ident})
        res = _run(_prog("A", lambda: build_A(NT, ctx_tiles=tuple(range(HC // 128)))), maps)
        proj = []
        for b in range(B):
            p0, p1 = res[2 * b]["proj"], res[2 * b + 1]["proj"]
            proj.append(np.concatenate([p0[:HC], p1[:HC], p0[HC:], p1[HC:]], 0))
        del res

        def flipseq(a):
            return np.concatenate([a[:CT][::-1], a[CT:][::-1]], 0)

        maps = []
        for cc, (b, z) in enumerate(cores):
            P = proj[b]
            mqT = np.zeros((4, 64, CT + S), f32); mkT = np.zeros((4, 64, CT + S), f32)
            mvk = np.zeros((4, CT + S, 128), f32); mg = np.zeros((4, CT + S, 2), f32); mgb = np.zeros((4, 128, 2), f32)
            for j in range(2):
                h = 2 * z + j
                q = P[:, 512 + h * 64:512 + (h + 1) * 64]; k = P[:, 768 + h * 64:768 + (h + 1) * 64]
                v = P[:, 1024 + h * 64:1024 + (h + 1) * 64]
                for d in range(2):
                    s = j * 2 + d
                    gi = P[:, 1536 + (2 * d) * 4 + h]; gf = P[:, 1536 + (2 * d + 1) * 4 + h]
                    qq, kk, vv, ii, ff = (q, k, v, gi, gf) if d == 0 else tuple(flipseq(a) for a in (q, k, v, gi, gf))
                    mqT[s] = qq.T; mkT[s] = kk.T
                    mvk[s, :, :64] = vv; mvk[s, :, 64:] = kk
                    mg[s, :, 0] = ii; mg[s, :, 1] = ff
                    mgb[s, :, 0] = mlstm_gate_b[l][2 * d, h]; mgb[s, :, 1] = mlstm_gate_b[l][2 * d + 1, h]
            maps.append({"mqT": mqT, "mkT": mkT, "mvk": mvk, "mg": mg, "mgb": mgb, "tri": tri, "ones": ones})
        res = _run(_prog("M1", lambda: build_M1(NKT)), maps)
        hf = [np.zeros((CT + S, 256), f32) for _ in range(B)]
        hb = [np.zeros((CT + S, 256), f32) for _ in range(B)]
        for cc, (b, z) in enumerate(cores):
            mh = res[cc]["mh"]
            for j in range(2):
                h = 2 * z + j
                hf[b][:, h * 64:(h + 1) * 64] = mh[j * 2]
                hb[b][:, h * 64:(h + 1) * 64] = flipseq(mh[j * 2 + 1])
        del res, maps

        maps = []
        dl = _rep(np.asarray(diff_lambda[l], f32).reshape(256))
        dng = _rep(np.asarray(diff_norm_g[l], f32))
        lami = _rep(np.array([lam_init, 1.0 - lam_init], f32))
        for cc, (b, z) in enumerate(cores):
            P = proj[b]
            aqT = np.zeros((2, 2, 64, CT + S), f32); akT = np.zeros((2, 2, 64, CT + S), f32); av = np.zeros((2, CT + S, 128), f32)
            for j in range(2):
                h = 2 * z + j
                for comp in range(2):
                    o = h * 128 + comp * 64
                    aqT[j, comp] = P[:, 1552 + o:1552 + o + 64].T
                    akT[j, comp] = P[:, 2064 + o:2064 + o + 64].T
                av[j] = P[:, 2576 + h * 128:2576 + (h + 1) * 128]
            maps.append({"aqT": aqT, "akT": akT, "av": av, "dl": dl, "dng": dng, "lami": lami, "ones": ones})
        res = _run(_prog("M2", lambda: build_M2(NKT, NCT)), maps)
        ao = [np.zeros((CT + S, 512), f32) for _ in range(B)]
        for cc, (b, z) in enumerate(cores):
            for j in range(2):
                h = 2 * z + j
                ao[b][:, h * 128:(h + 1) * 128] = res[cc]["ao"][j]
        del res, maps

        cw = np.ascontiguousarray(np.asarray(conv_w[l], f32)[:, 0, :].T.reshape(2, 128, 31).transpose(1, 0, 2))
        cp = np.ascontiguousarray(np.stack([conv_b[l], conv_ln_g[l], conv_ln_b[l]], -1).astype(f32).reshape(2, 128, 3).transpose(1, 0, 2))
        maps = []
        for cc, (b, z) in enumerate(cores):
            P = proj[b]
            aT = np.zeros((512, HC + 30 + HS + 30), f32)
            actx = np.zeros((CT + 30, 512), f32); actx[15:15 + CT] = P[:CT, 0:512]
            alat = np.zeros((S + 30, 512), f32); alat[15:15 + S] = P[CT:, 0:512]
            aT[:, 0:HC + 30] = actx[z * HC:z * HC + HC + 30].T
            aT[:, HC + 30:] = alat[z * HS:z * HS + HS + 30].T
            maps.append({"aT": aT, "cw": cw, "cp": cp, "ones": ones})
        res = _run(_prog("C1", lambda: build_C1([HC, HS])), maps)
        convT = [res[cc]["convT"] for cc in range(NCORES)]
        del res, maps

        maps = []
        mng = _rep(np.asarray(mlstm_norm_g[l], f32))
        for cc, (b, z) in enumerate(cores):
            sel = lambda a: np.ascontiguousarray(np.concatenate([a[z * HC:(z + 1) * HC], a[CT + z * HS:CT + (z + 1) * HS]], 0))
            bc = np.stack([_rep(norm2_g[l]), _rep(g1[:, b]), _rep(g1[:, 4]), _rep(sc2[:, b]), _rep(sc2[:, 4]), _rep(sh2[:, b]), _rep(sh2[:, 4])])
            maps.append({"xs": xs[cc], "convT": convT[cc], "hf": sel(hf[b]), "hb": sel(hb[b]), "mo": sel(proj[b][:, 1280:1536]),
                         "ao": sel(ao[b]), "mng": mng, "w_out": np.asarray(w_out[l], f32), "bc": bc, "ident": ident})
        res = _run(_prog("C2", lambda: build_C2(NT, ctx_tiles=tuple(range(HC // 128)))), maps)
        x1 = [res[cc]["x1"] for cc in range(NCORES)]
        h2 = [res[cc]["h2"] for cc in range(NCORES)]
        del res, maps, proj, hf, hb, ao, convT

        keys = np.asarray(peer_keys[l], f32)
        kbd = np.zeros((128, 8, 256), f32)
        for pp in range(2):
            kbd[pp * 64:(pp + 1) * 64, :, pp * 128:(pp + 1) * 128] = keys[:, pp].transpose(2, 0, 1)
        pu = np.asarray(peer_u[l], f32); pvv = np.asarray(peer_v[l], f32); wq = np.asarray(peer_wq[l], f32)
        maps = []
        for cc, (b, z) in enumerate(cores):
            maps.append({"h2": h2[cc], "x1": x1[cc], "wq": wq, "kbd": kbd, "peer_u": pu, "peer_v": pvv,
                         "g2b": np.stack([_rep(g2[:, b]), _rep(g2[:, 4])]), "ident": ident})
        res = _run(_prog("C3", lambda: build_C3(NT, ctx_tiles=tuple(range(HC // 128)))), maps)
        xs = [res[cc]["x2"] for cc in range(NCORES)]
        del res, maps, x1, h2

    fg = _rep(np.asarray(final_g, f32))
    maps = [{"xs": np.ascontiguousarray(xs[cc][HC:]), "fg": fg} for cc in range(NCORES)]
    res = _run(_prog("F", lambda: build_F(HS // 128)), maps)
    out = np.zeros((B, S, D), f32)
    for cc, (b, z) in enumerate(cores):
        out[b, z * HS:(z + 1) * HS] = res[cc]["y"]
    return out


class Cfg:
    def __init__(self, S, CT, DEPTH):
        self.S, self.CT, self.DEPTH = S, CT, DEPTH
        self.NTOK = S + CT
        self.NKT = self.NTOK // 128
        self.NCT = CT // 128
        self.NE = 16384


def _ph_P0(p, g, T):
    D_ = g.DEPTH
    ct = p.sb([128, 8, 2], name="ct"); st = p.sb([128, 8, 2], name="st")
    p.dma(ct[:], T["cT"], writes=["ct"])
    p.add("act", lambda e: e.activation(out=st[:], in_=ct[:], func=AF.Silu), ["ct"], ["st"])
    sbc = p.sb([128, 8, 2, 128], name="sbc")
    p.add("dve", lambda e: e.tensor_copy(out=sbc[:], in_=st[:, :, :, None].to_broadcast([128, 8, 2, 128])), ["st"], ["sbc"])
    wts = [p.sb([128, 8, 512], name=f"wt{i}") for i in range(2)]
    bbt = [p.sb([128, 512], name=f"bbt{i}") for i in range(2)]
    ob = [p.sb([128, 2, 512], name=f"ob{i}") for i in range(2)]
    btT = p.sb([128, 48], name="btT")
    oT = p.sb([128, 48, 2], name="oT")
    pTb = [p.ps([128, 512], name=f"ppT{i}") for i in range(2)]
    pT = [t[:, 0:32].rearrange("p (a b) -> p a b", a=4) for t in pTb]
    pB = [p.ps([128, 512], name=f"ppB{i}") for i in range(4)]
    nblk = 0
    for l in range(D_):
        p.dma(btT[:], T["ada_bT"][l], writes=["btT"])
        wv = T["ada_w"][l].rearrange("(k p) n -> p k n", p=128)
        for blk in range(12):
            i = nblk % 2; nblk += 1
            wt = wts[i]
            p.dma(wt[:], wv[:, :, blk * 512:(blk + 1) * 512], writes=[f"wt{i}"])
            p.dma(bbt[i][:], T["ada_bB"][l, :, blk * 512:(blk + 1) * 512], writes=[f"bbt{i}"])
            for jj in range(4):
                for k in range(8):
                    p.add("pe", lambda e, wt=wt, i=i, jj=jj, k=k: e.matmul(pT[i][:, jj, 0:2], lhsT=wt[:, k, jj * 128:(jj + 1) * 128], rhs=st[:, k, :],
                                                                          start=(k == 0), stop=(k == 7)), [f"wt{i}", "st"], [f"ppT{i}"])
            for jj in range(4):
                j = blk * 4 + jj
                p.add("dve", lambda e, i=i, jj=jj, j=j: e.tensor_scalar(out=oT[:, j, :], in0=pT[i][:, jj, 0:2], scalar1=btT[:, j:j + 1], scalar2=None,
                                                                       op0=ALU.add), [f"ppT{i}", "btT"], ["oT"])
            for n in range(2):
                pb = (2 * blk + n) % 4
                for k in range(8):
                    p.add("pe", lambda e, wt=wt, n=n, k=k, pb=pb: e.matmul(pB[pb][:], lhsT=sbc[:, k, n, :], rhs=wt[:, k, :], start=(k == 0), stop=(k == 7)),
                          [f"wt{i}", "sbc"], [f"ppB{pb}"])
                p.add("dve", lambda e, i=i, n=n, pb=pb: e.tensor_tensor(out=ob[i][:, n, :], in0=pB[pb][:], in1=bbt[i][:], op=ALU.add),
                      [f"ppB{pb}", f"bbt{i}"], [f"ob{i}_{n}"])
                p.dma(T["modB"][l, n, :, blk * 512:(blk + 1) * 512], ob[i][:, n, :], reads=[f"ob{i}_{n}"])
        p.dma(T["modT"][l], oT[:], reads=["oT"])


def _ph_A(p, g, T, l):
    NT = g.NKT
    xs, proj, projT = T["xs"], T["proj"], T["projT"]
    wb = p.sb([128, 8, IN_W], BF16, name="wb")
    load_cast_weight(p, wb, T["w_in"][l], IN_W, "wb", bw=256)
    idt = p.sb([128, 128], name="idt"); p.dma(idt[:], T["ident"], writes=["idt"])
    mT = p.sb([128, 48, 2], name="mT"); p.dma(mT[:], T["modT"][l], writes=["mT"])
    n1 = p.sb([128, 8], name="n1"); p.dma(n1[:], T["n1g"][l], writes=["n1"])
    gm = p.sb([128, 8, 2], name="gm")
    for n in range(2):
        p.add("dve", lambda e, n=n: e.scalar_tensor_tensor(out=gm[:, :, n], in0=mT[:, 8:16, n], scalar=1.0, in1=n1[:], op0=ALU.add, op1=ALU.mult),
              ["mT", "n1"], ["gm"])
    NB = 2
    mk = lambda shape, nm, dt=F32: [p.sb(shape, dt, name=f"{nm}{i}") for i in range(NB)]
    xt = mk([128, D], "xt"); xn = mk([128, D], "xn"); junk = p.sb([128, D], name="junk")
    ss = mk([128, 1], "ss"); rstd = mk([128, 1], "rstd"); hT = mk([128, 8, 128], "hT", BF16)
    ot = mk([128, IN_W], "ot"); ro = mk([128, 1024], "ro"); tmp = mk([128, 4, 512], "tmp"); cst = mk([128, 64], "cst")
    tT = mk([128, 16, 128], "tT")
    tp = [p.ps([128, 512], name=f"tp{i}") for i in range(4)]
    acc = [p.ps([128, 512], name=f"acc{i}") for i in range(4)]
    nacc = 0
    ntp = 0
    for t in range(NT):
        i = t % NB
        isc = t < g.NCT
        n_ = 1 if isc else 0
        r0 = t * 128
        p.dma(xt[i][:], xs[r0:r0 + 128, :], writes=[f"xt{i}"])
        p.dma(cst[i][:], T["cs"][r0:r0 + 128, :], writes=[f"cst{i}"])
        p.add("act", lambda e, i=i: e.activation(out=junk[:], in_=xt[i][:], func=AF.Square, accum_out=ss[i][:, 0:1]), [f"xt{i}"], ["junk", f"ss{i}"])
        p.add("dve", lambda e, i=i: e.tensor_scalar(out=rstd[i][:], in0=ss[i][:], scalar1=1.0 / D, scalar2=EPS, op0=ALU.mult, op1=ALU.add),
              [f"ss{i}"], [f"rstd{i}"])
        p.add("act", lambda e, i=i: e.activation(out=ss[i][:], in_=rstd[i][:], func=AF.Sqrt), [f"rstd{i}"], [f"ss{i}"])
        p.add("dve", lambda e, i=i: e.reciprocal(out=rstd[i][:], in_=ss[i][:]), [f"ss{i}"], [f"rstd{i}"])
        p.add("dve", lambda e, i=i: e.tensor_scalar(out=xn[i][:], in0=xt[i][:], scalar1=rstd[i][:, 0:1], scalar2=None, op0=ALU.mult),
              [f"xt{i}", f"rstd{i}"], [f"xn{i}"])
        for half in range(2):
            tpi = ntp % 4; ntp += 1
            for k in range(half * 4, half * 4 + 4):
                p.add("pe", lambda e, i=i, k=k, tpi=tpi: e.transpose(tp[tpi][:, (k % 4) * 128:(k % 4 + 1) * 128], xn[i][:, k * 128:(k + 1) * 128], idt[:]),
                      [f"xn{i}", "idt"], [f"tp{tpi}"])
            for k in range(half * 4, half * 4 + 4):
                if half == 0:
                    p.add("act", lambda e, i=i, k=k, tpi=tpi, n_=n_: e.activation(
                        out=hT[i][:, k, :], in_=tp[tpi][:, (k % 4) * 128:(k % 4 + 1) * 128], func=AF.Identity,
                        scale=gm[:, k, n_:n_ + 1], bias=mT[:, k, n_:n_ + 1]), [f"tp{tpi}", "gm", "mT"], [f"hT{i}_{k}"])
                else:
                    p.add("dve", lambda e, i=i, k=k, tpi=tpi, n_=n_: e.tensor_scalar(
                        out=hT[i][:, k, :], in0=tp[tpi][:, (k % 4) * 128:(k % 4 + 1) * 128],
                        scalar1=gm[:, k, n_:n_ + 1], scalar2=mT[:, k, n_:n_ + 1], op0=ALU.mult, op1=ALU.add), [f"tp{tpi}", "gm", "mT"], [f"hT{i}_{k}"])
        for blk in range(7):
            c0 = blk * 512
            cw = min(512, IN_W - c0)
            a = nacc % 4; nacc += 1
            for k in range(8):
                p.add("pe", lambda e, i=i, k=k, a=a, c0=c0, cw=cw: e.matmul(acc[a][:, 0:cw], lhsT=hT[i][:, k, :], rhs=wb[:, k, c0:c0 + cw],
                                                                            start=(k == 0), stop=(k == 7)), [f"hT{i}_{k}", "wb"], [f"acc{a}"])
            if blk % 2 == 0:
                p.add("act", lambda e, i=i, a=a, c0=c0, cw=cw: e.activation(out=ot[i][:, c0:c0 + cw], in_=acc[a][:, 0:cw], func=AF.Copy),
                      [f"acc{a}"], [f"ot{i}_{blk}"])
            else:
                p.add("dve", lambda e, i=i, a=a, c0=c0, cw=cw: e.tensor_copy(out=ot[i][:, c0:c0 + cw], in_=acc[a][:, 0:cw]), [f"acc{a}"], [f"ot{i}_{blk}"])
        src = ot[i][:, 1552:2576].rearrange("p (g r two) -> p g r two", g=16, two=2)
        dst = ro[i][:].rearrange("p (g r two) -> p g r two", g=16, two=2)
        t1 = src[:, :, :, 0]; t2 = src[:, :, :, 1]
        cosb = cst[i][:, None, 0:32].to_broadcast([128, 16, 32])
        sinb = cst[i][:, None, 32:64].to_broadcast([128, 16, 32])
        tm = [tmp[i][:, j, :].rearrange("p (g r) -> p g r", g=16) for j in range(4)]
        rk = [f"ot{i}_3", f"ot{i}_4", f"ot{i}_5", f"cst{i}"]
        p.add("dve", lambda e, tm=tm, t1=t1, cosb=cosb: e.tensor_tensor(out=tm[0], in0=t1, in1=cosb, op=ALU.mult), rk, [f"tmp{i}_0"])
        p.add("dve", lambda e, tm=tm, t2=t2, sinb=sinb: e.tensor_tensor(out=tm[1], in0=t2, in1=sinb, op=ALU.mult), rk, [f"tmp{i}_1"])
        p.add("dve", lambda e, tm=tm, t1=t1, sinb=sinb: e.tensor_tensor(out=tm[2], in0=t1, in1=sinb, op=ALU.mult), rk, [f"tmp{i}_2"])
        p.add("dve", lambda e, tm=tm, t2=t2, cosb=cosb: e.tensor_tensor(out=tm[3], in0=t2, in1=cosb, op=ALU.mult), rk, [f"tmp{i}_3"])
        p.add("dve", lambda e, tm=tm, dst=dst: e.tensor_tensor(out=dst[:, :, :, 0], in0=tm[0], in1=tm[1], op=ALU.subtract),
              [f"tmp{i}_0", f"tmp{i}_1"], [f"ro{i}a"])
        p.add("dve", lambda e, tm=tm, dst=dst: e.tensor_tensor(out=dst[:, :, :, 1], in0=tm[2], in1=tm[3], op=ALU.add),
              [f"tmp{i}_2", f"tmp{i}_3"], [f"ro{i}b"])
        p.dma(proj[r0:r0 + 128, 0:1552], ot[i][:, 0:1552], reads=[f"ot{i}_{b}" for b in range(4)])
        p.dma(proj[r0:r0 + 128, 1552:2576], ro[i][:], reads=[f"ro{i}a", f"ro{i}b"])
        p.dma(proj[r0:r0 + 128, 2576:IN_W], ot[i][:, 2576:IN_W], reads=[f"ot{i}_5", f"ot{i}_6"])
        srcs = [(ot[i], c * 128, [f"ot{i}_0", f"ot{i}_1"]) for c in range(8)] + [(ro[i], c * 128, [f"ro{i}a", f"ro{i}b"]) for c in range(8)]
        for q4 in range(4):
            tpi = ntp % 4; ntp += 1
            for c in range(4):
                sap, c0, keys = srcs[q4 * 4 + c]
                p.add("pe", lambda e, sap=sap, c0=c0, tpi=tpi, c=c: e.transpose(tp[tpi][:, c * 128:(c + 1) * 128], sap[:, c0:c0 + 128], idt[:]),
                      keys + ["idt"], [f"tp{tpi}"])
            if q4 % 2 == 0:
                p.add("act", lambda e, i=i, q4=q4, tpi=tpi: e.activation(out=tT[i][:, q4 * 4:q4 * 4 + 4, :], in_=tp[tpi][:].rearrange("p (k n) -> p k n", k=4),
                                                                         func=AF.Copy), [f"tp{tpi}"], [f"tT{i}_{q4}"])
            else:
                p.add("dve", lambda e, i=i, q4=q4, tpi=tpi: e.tensor_copy(out=tT[i][:, q4 * 4:q4 * 4 + 4, :], in_=tp[tpi][:].rearrange("p (k n) -> p k n", k=4)),
                      [f"tp{tpi}"], [f"tT{i}_{q4}"])
        p.dma(projT[:, r0:r0 + 128].rearrange("(k p) n -> p k n", p=128), tT[i][:], reads=[f"tT{i}_{q}" for q in range(4)])


def _ph_M1(p, g, T, l, GC=4):
    NKT, NCT = g.NKT, g.NCT
    proj, projT, mixo = T["proj"], T["projT"], T["mixo"]
    tri = p.sb([128, 128], name="tri_sb"); p.dma(tri[:], T["tri"], writes=["tri"])
    triL = p.sb([128, 128], name="triL_sb"); p.dma(triL[:], T["triL"], writes=["triL"])
    ones = p.sb([128, 128], name="ones_sb"); p.dma(ones[:], T["ones"], writes=["ones"])
    banks = [p.ps([128, 512], name=f"bank{i}") for i in range(7)]
    NS = 4
    NBUF = 2
    B = {}
    for s in range(NS):
        B[s, "gb"] = p.sb([128, 2], name=f"gb{s}")
        B[s, "Cf"] = p.sb([64, 65], name=f"Cf{s}")
        B[s, "Cb"] = p.sb([64, 65], BF16, name=f"Cb{s}")
        B[s, "tmpC"] = p.sb([64, 65], name=f"tmpC{s}")
        for i in range(NBUF):
            k = (s, i)
            B[k, "qTf"] = p.sb([64, GC * 128], name=f"qTf{s}_{i}")
            B[k, "kTf"] = p.sb([64, GC * 128], name=f"kTf{s}_{i}")
            B[k, "vf"] = p.sb([128, GC, 64], name=f"vf{s}_{i}")
            B[k, "kf"] = p.sb([128, GC, 64], name=f"kf{s}_{i}")
            B[k, "gf"] = p.sb([128, GC, 16], name=f"gf{s}_{i}")
            B[k, "qTb"] = p.sb([64, GC * 128], BF16, name=f"qTb{s}_{i}")
            B[k, "kTb"] = p.sb([64, GC * 128], BF16, name=f"kTb{s}_{i}")
            B[k, "vaug"] = p.sb([128, GC, 65], BF16, name=f"vaug{s}_{i}")
            B[k, "kb"] = p.sb([128, GC, 64], BF16, name=f"kb{s}_{i}")
            B[k, "h"] = p.sb([128, GC, 64], name=f"h{s}_{i}")
            for nm in ("gi", "sp", "a", "b", "eG"):
                B[k, nm] = p.sb([128, GC], name=f"{nm}{s}_{i}")
            B[k, "WT"] = p.sb([128, 128], BF16, name=f"WT{s}_{i}")
            B[k, "d"] = p.sb([128, 4], name=f"d{s}_{i}")
            p.add("dve", lambda e, k=k: e.memset(B[k, "vaug"][:, :, 64:65], 1.0), [], [f"vaug1_{s}_{i}"])
    def groups(rev):
        out = []
        for lo, hi in ((0, NCT), (NCT, NKT)):
            rng = list(range(lo, hi))
            for g0 in range(0, len(rng), GC):
                out.append(rng[g0:g0 + GC])
        if rev:
            out = []
            for lo, hi in ((0, NCT), (NCT, NKT)):
                rng = list(range(lo, hi))[::-1]
                for g0 in range(0, len(rng), GC):
                    out.append(rng[g0:g0 + GC])
        return out
    gcount = {s: 0 for s in range(NS)}
    for hp in range(2):
        for s in range(NS):
            h, dr = 2 * hp + s // 2, s % 2
            p.dma(B[s, "gb"][:], T["mgb"][l, h * 2 + dr], writes=[f"gb{s}"])
            p.add("dve", lambda e, s=s: e.memset(B[s, "Cf"][:], 0.0), [], [f"Cf{s}"])
            p.add("dve", lambda e, s=s: e.memset(B[s, "Cb"][:], 0.0), [], [f"Cb{s}"])
        glists = [groups(s % 2 == 1) for s in range(NS)]
        for gi_ in range(len(glists[0])):
            for s in range(NS):
                h, dr = 2 * hp + s // 2, s % 2
                chunks = glists[s][gi_]
                lo, hi = min(chunks), max(chunks) + 1
                gc = hi - lo
                i = gcount[s] % NBUF; gcount[s] += 1
                k = (s, i)
                sfx = f"{s}_{i}"
                Tt = {n: B[k, n] for n in ("qTf", "kTf", "vf", "kf", "gf", "qTb", "kTb", "vaug", "kb", "h", "gi", "sp", "a", "b", "eG", "WT", "d")}
                Cf, Cb, tmpC, gb = B[s, "Cf"], B[s, "Cb"], B[s, "tmpC"], B[s, "gb"]
                bS, bX, bU = banks[(s % 2) * 3], banks[(s % 2) * 3 + 1], banks[(s % 2) * 3 + 2]
                kS, kX, kU = f"bank{(s%2)*3}", f"bank{(s%2)*3+1}", f"bank{(s%2)*3+2}"
                bP = banks[6]
                trm = triL if dr else tri
                ktr = "triL" if dr else "tri"
                W = gc * 128
                t0 = lo * 128
                p.dma(Tt["qTf"][:, 0:W], projT[512 + h * 64:512 + (h + 1) * 64, t0:t0 + W], writes=[f"qTf{sfx}"])
                p.dma(Tt["kTf"][:, 0:W], projT[768 + h * 64:768 + (h + 1) * 64, t0:t0 + W], writes=[f"kTf{sfx}"])
                pr = proj[t0:t0 + W, :].rearrange("(c p) f -> p c f", p=128)
                p.dma(Tt["vf"][:, 0:gc, :], pr[:, :, 1024 + h * 64:1024 + (h + 1) * 64], writes=[f"vf{sfx}"])
                p.dma(Tt["kf"][:, 0:gc, :], pr[:, :, 768 + h * 64:768 + (h + 1) * 64], writes=[f"kf{sfx}"])
                ci = (2 * dr) * 4 + h
                cf = (2 * dr + 1) * 4 + h
                p.dma(Tt["gf"][:, 0:gc, :], pr[:, :, 1536:1552], writes=[f"gf{sfx}a"])
                gk = [f"gf{sfx}a"]
                p.add("dve", lambda e, Tt=Tt, gb=gb, gc=gc, ci=ci: e.tensor_scalar(out=Tt["gi"][:, 0:gc], in0=Tt["gf"][:, 0:gc, ci], scalar1=gb[:, 0:1],
                                                                       scalar2=None, op0=ALU.add), gk + [f"gb{s}"], [f"gi{sfx}"])
                p.add("dve", lambda e, Tt=Tt, gb=gb, gc=gc, cf=cf: e.tensor_scalar(out=Tt["sp"][:, 0:gc], in0=Tt["gf"][:, 0:gc, cf], scalar1=gb[:, 1:2],
                                                                       scalar2=None, op0=ALU.add), gk + [f"gb{s}"], [f"sp{sfx}"])
                p.add("act", lambda e, Tt=Tt, gc=gc: e.activation(out=Tt["sp"][:, 0:gc], in_=Tt["sp"][:, 0:gc], func=AF.Exp, scale=-1.0),
                      [f"sp{sfx}"], [f"sp{sfx}"])
                p.add("dve", lambda e, Tt=Tt, gc=gc: e.tensor_scalar(out=Tt["sp"][:, 0:gc], in0=Tt["sp"][:, 0:gc], scalar1=1.0, scalar2=None,
                                                                op0=ALU.add), [f"sp{sfx}"], [f"sp{sfx}"])
                p.add("act", lambda e, Tt=Tt, gc=gc: e.activation(out=Tt["sp"][:, 0:gc], in_=Tt["sp"][:, 0:gc], func=AF.Ln), [f"sp{sfx}"], [f"sp{sfx}"])
                p.add("pe", lambda e, Tt=Tt, gc=gc, bP=bP, trm=trm: e.matmul(bP[:, 0:gc], lhsT=trm[:], rhs=Tt["sp"][:, 0:gc], start=True, stop=True),
                      [ktr, f"sp{sfx}"], ["bank6"])
                p.add("pe", lambda e, Tt=Tt, gc=gc, bP=bP: e.matmul(bP[:, 64:64 + gc], lhsT=ones[:], rhs=Tt["sp"][:, 0:gc], start=True, stop=True),
                      ["ones", f"sp{sfx}"], ["bank6"])
                p.add("act", lambda e, Tt=Tt, gc=gc, bP=bP: e.activation(out=Tt["a"][:, 0:gc], in_=bP[:, 0:gc], func=AF.Exp, scale=-1.0), ["bank6"], [f"a{sfx}"])
                p.add("act", lambda e, Tt=Tt, gc=gc, bP=bP: e.activation(out=Tt["eG"][:, 0:gc], in_=bP[:, 64:64 + gc], func=AF.Exp, scale=-1.0), ["bank6"], [f"eG{sfx}"])
                p.add("act", lambda e, Tt=Tt, gc=gc, bP=bP: e.activation(out=Tt["b"][:, 0:gc], in_=bP[:, 0:gc], func=AF.Identity), ["bank6"], [f"b{sfx}"])
                p.add("dve", lambda e, Tt=Tt, gc=gc: e.tensor_tensor(out=Tt["b"][:, 0:gc], in0=Tt["b"][:, 0:gc], in1=Tt["gi"][:, 0:gc], op=ALU.add),
                      [f"b{sfx}", f"gi{sfx}"], [f"b{sfx}"])
                p.add("act", lambda e, Tt=Tt, gc=gc: e.activation(out=Tt["b"][:, 0:gc], in_=Tt["b"][:, 0:gc], func=AF.Exp), [f"b{sfx}"], [f"b{sfx}"])
                p.add("act", lambda e, Tt=Tt, W=W: e.activation(out=Tt["qTb"][:, 0:W], in_=Tt["qTf"][:, 0:W], func=AF.Copy), [f"qTf{sfx}"], [f"qTb{sfx}"])
                p.add("dve", lambda e, Tt=Tt, W=W: e.tensor_scalar(out=Tt["kTb"][:, 0:W], in0=Tt["kTf"][:, 0:W], scalar1=0.125, scalar2=None, op0=ALU.mult),
                      [f"kTf{sfx}"], [f"kTb{sfx}"])
                p.add("act", lambda e, Tt=Tt, gc=gc: e.activation(out=Tt["vaug"][:, 0:gc, 0:64], in_=Tt["vf"][:, 0:gc, :], func=AF.Copy),
                      [f"vf{sfx}"], [f"vaug{sfx}"])
                p.add("dve", lambda e, Tt=Tt, gc=gc: e.scalar_tensor_tensor(
                    out=Tt["kb"][:, 0:gc, :], in0=Tt["kf"][:, 0:gc, :], scalar=0.125,
                    in1=Tt["b"][:, 0:gc, None].to_broadcast([128, gc, 64]), op0=ALU.mult, op1=ALU.mult), [f"kf{sfx}", f"b{sfx}"], [f"kb{sfx}"])
                for c in chunks:
                    cc = c - lo
                    cs = slice(cc * 128, (cc + 1) * 128)
                    p.add("pe", lambda e, Tt=Tt, cs=cs, bS=bS: e.matmul(bS[:, 0:128], lhsT=Tt["kTb"][:, cs], rhs=Tt["qTb"][:, cs], start=True, stop=True),
                          [f"kTb{sfx}", f"qTb{sfx}"], [kS])
                    p.add("dve", lambda e, Tt=Tt, cc=cc, bS=bS, trm=trm: e.scalar_tensor_tensor(
                        out=Tt["WT"][:], in0=bS[:, 0:128], scalar=Tt["b"][:, cc:cc + 1], in1=trm[:], op0=ALU.mult, op1=ALU.mult),
                        [kS, f"b{sfx}", ktr], [f"WT{sfx}"])
                    p.add("pe", lambda e, Tt=Tt, cc=cc, bX=bX: e.matmul(bX[:, 0:65], lhsT=Tt["WT"][:], rhs=Tt["vaug"][:, cc, :], start=True, stop=False),
                          [f"WT{sfx}", f"vaug{sfx}", f"vaug1_{sfx}"], [kX])
                    p.add("pe", lambda e, Tt=Tt, cs=cs, bX=bX, Cb=Cb: e.matmul(bX[:, 0:65], lhsT=Tt["qTb"][:, cs], rhs=Cb[:], start=False, stop=True),
                          [f"qTb{sfx}", f"Cb{s}"], [kX])
                    p.add("pe", lambda e, Tt=Tt, cc=cc, bU=bU: e.matmul(bU[0:64, 0:65], lhsT=Tt["kb"][:, cc, :], rhs=Tt["vaug"][:, cc, :], start=True, stop=True),
                          [f"kb{sfx}", f"vaug{sfx}", f"vaug1_{sfx}"], [kU])
                    d = Tt["d"]
                    p.add("act", lambda e, Tt=Tt, cc=cc, bX=bX, d=d: e.activation(out=d[:, 0:1], in_=bX[:, 64:65], func=AF.Abs, scale=Tt["a"][:, cc:cc + 1]),
                          [kX, f"a{sfx}"], [f"d0{sfx}"])
                    p.add("dve", lambda e, d=d: e.tensor_scalar(out=d[:, 3:4], in0=d[:, 0:1], scalar1=1.0, scalar2=None, op0=ALU.max), [f"d0{sfx}"], [f"d3{sfx}"])
                    p.add("dve", lambda e, d=d: e.reciprocal(out=d[:, 1:2], in_=d[:, 3:4]), [f"d3{sfx}"], [f"d1{sfx}"])
                    p.add("dve", lambda e, Tt=Tt, cc=cc, d=d: e.tensor_tensor(out=d[:, 2:3], in0=d[:, 1:2], in1=Tt["a"][:, cc:cc + 1], op=ALU.mult),
                          [f"d1{sfx}", f"a{sfx}"], [f"d2{sfx}"])
                    p.add("dve", lambda e, Tt=Tt, cc=cc, bX=bX, d=d: e.tensor_scalar(out=Tt["h"][:, cc, :], in0=bX[:, 0:64], scalar1=d[:, 2:3], scalar2=None,
                                                                                op0=ALU.mult), [kX, f"d2{sfx}"], [f"h{sfx}"])
                    p.add("dve", lambda e, bU=bU, Cf=Cf, tmpC=tmpC: e.tensor_tensor(out=tmpC[:], in0=bU[0:64, 0:65], in1=Cf[:], op=ALU.add),
                          [kU, f"Cf{s}"], [f"tmpC{s}"])
                    p.add("dve", lambda e, Tt=Tt, cc=cc, Cf=Cf, tmpC=tmpC: e.tensor_scalar(out=Cf[:], in0=tmpC[:], scalar1=Tt["eG"][0:64, cc:cc + 1], scalar2=None,
                                                                                      op0=ALU.mult), [f"tmpC{s}", f"eG{sfx}"], [f"Cf{s}"])
                    p.add("act", lambda e, Tt=Tt, cc=cc, Cb=Cb, tmpC=tmpC: e.activation(out=Cb[:], in_=tmpC[:], func=AF.Copy, scale=Tt["eG"][0:64, cc:cc + 1]),
                          [f"tmpC{s}", f"eG{sfx}"], [f"Cb{s}"])
                oc = dr * 256 + h * 64
                p.dma(mixo[t0:t0 + W, oc:oc + 64].rearrange("(c p) f -> p c f", p=128), Tt["h"][:, 0:gc, :], reads=[f"h{sfx}"])


def _ph_M2(p, g, T, l):
    NKT, NCT = g.NKT, g.NCT
    L = NKT * 128
    proj, projT, mixo = T["proj"], T["projT"], T["mixo"]
    ones = p.sb([128, 128], name="ones_sb"); p.dma(ones[:], T["ones"], writes=["ones"])
    dl = p.sb([128, 256], name="dl_sb"); p.dma(dl[:], T["dl"][l], writes=["dl"])
    gsc = p.sb([128, 128], name="gsc"); p.dma(gsc[:], T["dng"][l], writes=["gsc"])
    lami = p.sb([128, 2], name="lami_sb"); p.dma(lami[:], T["lami"][l], writes=["lami"])
    junk = p.sb([128, 128], name="junk")
    lm = p.sb([128, 4], name="lm")
    p.add("dve", lambda e: e.scalar_tensor_tensor(out=junk[:, 0:64], in0=dl[:, 0:64], scalar=1.0, in1=dl[:, 64:128],
                                                  op0=ALU.mult, op1=ALU.mult, accum_out=lm[:, 0:1]), ["dl"], ["junk", "lm0"])
    p.add("dve", lambda e: e.scalar_tensor_tensor(out=junk[:, 64:128], in0=dl[:, 128:192], scalar=1.0, in1=dl[:, 192:256],
                                                  op0=ALU.mult, op1=ALU.mult, accum_out=lm[:, 1:2]), ["dl"], ["junk", "lm1"])
    p.add("act", lambda e: e.activation(out=lm[:, 0:2], in_=lm[:, 0:2], func=AF.Exp), ["lm0", "lm1"], ["lm01"])
    p.add("dve", lambda e: e.tensor_tensor(out=lm[:, 2:3], in0=lm[:, 1:2], in1=lm[:, 0:1], op=ALU.subtract), ["lm01"], ["lm2"])
    p.add("dve", lambda e: e.tensor_scalar(out=lm[:, 3:4], in0=lm[:, 2:3], scalar1=lami[:, 0:1], scalar2=None, op0=ALU.subtract), ["lm2", "lami"], ["nlam"])
    p.add("dve", lambda e: e.tensor_scalar(out=gsc[:], in0=gsc[:], scalar1=lami[:, 1:2], scalar2=None, op0=ALU.mult), ["gsc", "lami"], ["gsc"])
    banks = [p.ps([128, 512], name=f"bank{i}") for i in range(8)]
    qTb = p.sb([64, 2, L], BF16, name="qTb")
    kTb = p.sb([64, 2, L], BF16, name="kTb")
    vaug = p.sb([128, NKT, 129], BF16, name="vaug")
    p.add("dve", lambda e: e.memset(vaug[:, :, 128:129], 1.0), [], ["vaug1"])
    PW = 2048
    stg = [p.sb([64, PW], name=f"stg{i}") for i in range(2)]
    sq = p.sb([64, PW], name="sq")
    VG = 16
    vst = [p.sb([128, VG, 128], name=f"vst{i}") for i in range(2)]
    mx = p.sb([128, 8], name="mx")
    nb = p.sb([128, 4], name="nb")
    pT = [p.sb([128, 512], BF16, name=f"pT{i}") for i in range(6)]
    ev = [p.sb([128, 8], name=f"ev{i}") for i in range(2)]
    o1 = [p.sb([128, 128], name=f"o1_{i}") for i in range(2)]
    oo = [p.sb([128, 128], name=f"oo_{i}") for i in range(2)]
    yy = [p.sb([128, 128], name=f"yy_{i}") for i in range(2)]
    nstg = nvst = nit = nblk = nev = 0
    for s in range(4):
        h = s
        p.add("dve", lambda e: e.memset(mx[:], 0.0), [], ["mx", "mx4"])
        for which, (rbase, dst) in enumerate(((1024, qTb), (1536, kTb))):
            for comp in range(2):
                rr = rbase + h * 128 + comp * 64
                for c0 in range(0, L, PW):
                    cw = min(PW, L - c0)
                    si = nstg % 2; nstg += 1
                    st = stg[si]
                    p.dma(st[:, 0:cw], projT[rr:rr + 64, c0:c0 + cw], writes=[f"stg{si}"])
                    p.add("dve", lambda e, st=st, dst=dst, comp=comp, c0=c0, cw=cw: e.tensor_copy(out=dst[:, comp, c0:c0 + cw], in_=st[:, 0:cw]),
                          [f"stg{si}"], ["qTb" if which == 0 else "kTb"])
                    p.add("act", lambda e, st=st, cw=cw: e.activation(out=sq[:, 0:cw], in_=st[:, 0:cw], func=AF.Square), [f"stg{si}"], ["sq"])
                    for b0 in range(0, cw, 512):
                        bw = min(512, cw - b0)
                        p.add("pe", lambda e, b0=b0, bw=bw: e.matmul(banks[6][:, 0:bw], lhsT=ones[0:64, :], rhs=sq[:, b0:b0 + bw], start=True, stop=True),
                              ["ones", "sq"], ["bank6"])
                        p.add("dve", lambda e, bw=bw: e.reduce_max(out=mx[:, 4:5], in_=banks[6][:, 0:bw], axis=AX.X), ["bank6"], ["mx4"])
                        col = which * 2 + comp
                        p.add("dve", lambda e, col=col: e.tensor_tensor(out=mx[:, col:col + 1], in0=mx[:, col:col + 1], in1=mx[:, 4:5], op=ALU.max),
                              ["mx4", "mx"], ["mx"])
        for g0 in range(0, NKT, VG):
            gn = min(VG, NKT - g0)
            vi = nvst % 2; nvst += 1
            p.dma(vst[vi][:, 0:gn, :], proj[g0 * 128:(g0 + gn) * 128, 2576 + h * 128:2576 + (h + 1) * 128].rearrange("(c p) f -> p c f", p=128),
                  writes=[f"vst{vi}"])
            p.add("act", lambda e, vi=vi, g0=g0, gn=gn: e.activation(out=vaug[:, g0:g0 + gn, 0:128], in_=vst[vi][:, 0:gn, :], func=AF.Copy),
                  [f"vst{vi}"], ["vaug"])
        p.add("dve", lambda e: e.tensor_tensor(out=nb[:, 2:4], in0=mx[:, 0:2], in1=mx[:, 2:4], op=ALU.mult), ["mx"], ["nb2"])
        p.add("act", lambda e: e.activation(out=nb[:, 2:4], in_=nb[:, 2:4], func=AF.Sqrt, scale=1.0 / 64.0), ["nb2"], ["nb2"])
        p.add("dve", lambda e: e.tensor_scalar(out=nb[:, 0:2], in0=nb[:, 2:4], scalar1=60.0, scalar2=-1.0, op0=ALU.min, op1=ALU.mult), ["nb2"], ["nb"])
        blocks = [(0, NCT, 0, NCT)]
        for q0 in range(NCT, NKT, 4):
            blocks.append((q0, min(4, NKT - q0), 0, NKT))
        for (q0, nq, k0, nk) in blocks:
            oset = 0; nblk += 1
            obanks = [banks[oset * 3 + i] for i in range(3)]
            okeys = [f"bank{oset*3+i}" for i in range(3)]
            for i in range(3):
                p.add("dve", lambda e, i=i, obanks=obanks: e.memset(obanks[i][:], 0.0), [], [okeys[i]])

            def oacc(comp, j, obanks=obanks, okeys=okeys):
                a = comp * 4 + j
                return obanks[a // 3][:, (a % 3) * 129:(a % 3) * 129 + 129], okeys[a // 3]
            QW = nq * 128
            pending = []
            for kt in range(k0, k0 + nk):
                for comp in range(2):
                    sb_i = 3 + nit % 5
                    pi = nit % 6
                    nit += 1
                    p.add("pe", lambda e, comp=comp, kt=kt, sb_i=sb_i, q0=q0, QW=QW: e.matmul(
                        banks[sb_i][:, 0:QW], lhsT=kTb[:, comp, kt * 128:(kt + 1) * 128], rhs=qTb[:, comp, q0 * 128:q0 * 128 + QW],
                        start=True, stop=True), ["kTb", "qTb"], [f"bank{sb_i}"])
                    p.add("act", lambda e, comp=comp, sb_i=sb_i, pi=pi, QW=QW: e.activation(
                        out=pT[pi][:, 0:QW], in_=banks[sb_i][:, 0:QW], func=AF.Exp, scale=0.125, bias=nb[:, comp:comp + 1]),
                        [f"bank{sb_i}", "nb"], [f"pT{pi}"])
                    def emit_pv(comp=comp, pi=pi, kt=kt, nq=nq, oacc=oacc):
                        for j in range(nq):
                            oap, okey = oacc(comp, j)
                            p.add("pe", lambda e, oap=oap, pi=pi, j=j, kt=kt: e.matmul(
                                oap, lhsT=pT[pi][:, j * 128:(j + 1) * 128], rhs=vaug[:, kt, :], start=False, stop=False,
                                skip_group_check=True), [f"pT{pi}", "vaug", "vaug1"], [okey])
                    pending.append(emit_pv)
                    if len(pending) > 2:
                        pending.pop(0)()
            while pending:
                pending.pop(0)()
            for j in range(nq):
                ei = nev % 2; nev += 1
                E = ev[ei]
                (o1ap, k1), (o2ap, k2) = oacc(0, j), oacc(1, j)
                sfx = f"_{ei}"
                p.add("dve", lambda e, E=E, o1ap=o1ap: e.reciprocal(out=E[:, 0:1], in_=o1ap[:, 128:129]), [k1], ["ev0" + sfx])
                p.add("dve", lambda e, E=E, o2ap=o2ap: e.reciprocal(out=E[:, 1:2], in_=o2ap[:, 128:129]), [k2], ["ev1" + sfx])
                p.add("dve", lambda e, E=E: e.tensor_tensor(out=E[:, 2:3], in0=E[:, 1:2], in1=lm[:, 3:4], op=ALU.mult), ["ev1" + sfx, "nlam"], ["ev2" + sfx])
                p.add("dve", lambda e, E=E, o1ap=o1ap, ei=ei: e.tensor_scalar(out=o1[ei][:], in0=o1ap[:, 0:128], scalar1=E[:, 0:1], scalar2=None, op0=ALU.mult),
                      [k1, "ev0" + sfx], ["o1" + sfx])
                p.add("dve", lambda e, E=E, o2ap=o2ap, ei=ei: e.scalar_tensor_tensor(out=oo[ei][:], in0=o2ap[:, 0:128], scalar=E[:, 2:3], in1=o1[ei][:],
                                                                                     op0=ALU.mult, op1=ALU.add), [k2, "ev2" + sfx, "o1" + sfx], ["oo" + sfx])
                p.add("dve", lambda e, E=E, ei=ei: e.scalar_tensor_tensor(out=junk[:], in0=oo[ei][:], scalar=1.0, in1=oo[ei][:], op0=ALU.mult, op1=ALU.mult,
                                                                          accum_out=E[:, 3:4]), ["oo" + sfx], ["junk", "ev3" + sfx])
                p.add("dve", lambda e, E=E: e.tensor_scalar(out=E[:, 4:5], in0=E[:, 3:4], scalar1=1.0 / 128.0, scalar2=EPS, op0=ALU.mult, op1=ALU.add),
                      ["ev3" + sfx], ["ev4" + sfx])
                p.add("act", lambda e, E=E: e.activation(out=E[:, 5:6], in_=E[:, 4:5], func=AF.Sqrt), ["ev4" + sfx], ["ev5" + sfx])
                p.add("dve", lambda e, E=E: e.reciprocal(out=E[:, 6:7], in_=E[:, 5:6]), ["ev5" + sfx], ["ev6" + sfx])
                p.add("dve", lambda e, E=E, ei=ei: e.scalar_tensor_tensor(out=yy[ei][:], in0=oo[ei][:], scalar=E[:, 6:7], in1=gsc[:], op0=ALU.mult, op1=ALU.mult),
                      ["oo" + sfx, "ev6" + sfx, "gsc"], ["yy" + sfx])
                r0 = (q0 + j) * 128
                p.dma(mixo[r0:r0 + 128, 512 + h * 128:512 + (h + 1) * 128], yy[ei][:], reads=["yy" + sfx])


def _ph_C1(p, g, T, l):
    projT, co = T["projT"], T["convT"]
    ones = p.sb([128, 128], name="ones_sb"); p.dma(ones[:], T["ones"], writes=["ones"])
    cw = p.sb([128, 2, 31], name="cw_sb"); p.dma(cw[:], T["cw"][l], writes=["cw"])
    cp = p.sb([128, 2, 3], name="cp_sb"); p.dma(cp[:], T["cp"][l], writes=["cp"])
    N = 512
    a1 = [p.sb([128, N + 30], name=f"a1_{i}") for i in range(2)]
    a2 = [p.sb([128, N + 30], name=f"a2_{i}") for i in range(2)]
    u = [p.sb([128, N + 30], name=f"u_{i}") for i in range(2)]
    acc = [[p.sb([128, N], name=f"acc_{i}_{r}") for r in range(2)] for i in range(2)]
    y = [p.sb([128, N], name=f"y_{i}") for i in range(2)]
    ysq = [p.sb([128, N], name=f"ysq_{i}") for i in range(2)]
    mean = p.sb([128, N], name="mean"); msq = p.sb([128, N], name="msq"); rstd = p.sb([128, N], name="rstd")
    zz = [p.sb([128, N], name=f"zz_{i}") for i in range(2)]
    bA = p.ps([128, 512], name="bankA"); bB = p.ps([128, 512], name="bankB")
    for (s0, ns) in ((0, g.CT), (g.CT, g.S)):
        for t0 in range(0, ns, N):
            n = min(N, ns - t0)
            lo = max(t0 - 15, 0); hi = min(t0 + n + 15, ns)
            d0 = lo - (t0 - 15)
            clip = (lo != t0 - 15) or (hi != t0 + n + 15)
            for j in range(2):
                for (buf, nm, rb) in ((a1[j], f"a1_{j}", j * 128), (a2[j], f"a2_{j}", 256 + j * 128)):
                    if clip:
                        p.add("dve", lambda e, buf=buf, n=n: e.memset(buf[:, 0:n + 30], 0.0), [], [nm])
                    p.dma(buf[:, d0:d0 + hi - lo], projT[rb:rb + 128, s0 + lo:s0 + hi], writes=[nm])
                p.add("act", lambda e, j=j, n=n: e.activation(out=a2[j][:, 0:n + 30], in_=a2[j][:, 0:n + 30], func=AF.Sigmoid), [f"a2_{j}"], [f"a2_{j}"])
                p.add("dve", lambda e, j=j, n=n: e.tensor_tensor(out=u[j][:, 0:n + 30], in0=a1[j][:, 0:n + 30], in1=a2[j][:, 0:n + 30], op=ALU.mult),
                      [f"a1_{j}", f"a2_{j}"], [f"u_{j}"])
                p.add("dve", lambda e, j=j, n=n: e.tensor_scalar(out=acc[j][0][:, 0:n], in0=u[j][:, 0:n], scalar1=cw[:, j, 0:1], scalar2=None, op0=ALU.mult),
                      [f"u_{j}", "cw"], [f"acc_{j}_0"])
                for k in range(1, 31):
                    src, dst = acc[j][(k - 1) % 2], acc[j][k % 2]
                    p.add("dve", lambda e, j=j, n=n, k=k, src=src, dst=dst: e.scalar_tensor_tensor(
                        out=dst[:, 0:n], in0=u[j][:, k:k + n], scalar=cw[:, j, k:k + 1], in1=src[:, 0:n], op0=ALU.mult, op1=ALU.add),
                        [f"u_{j}", "cw", f"acc_{j}_{(k-1)%2}"], [f"acc_{j}_{k%2}"])
                p.add("dve", lambda e, j=j, n=n: e.tensor_scalar(out=y[j][:, 0:n], in0=acc[j][0][:, 0:n], scalar1=cp[:, j, 0:1], scalar2=None, op0=ALU.add),
                      [f"acc_{j}_0", "cp"], [f"y_{j}"])
                p.add("act", lambda e, j=j, n=n: e.activation(out=ysq[j][:, 0:n], in_=y[j][:, 0:n], func=AF.Square), [f"y_{j}"], [f"ysq_{j}"])
            for j in range(2):
                p.add("pe", lambda e, j=j, n=n: e.matmul(bA[:, 0:n], lhsT=ones[:], rhs=y[j][:, 0:n], start=(j == 0), stop=(j == 1)), ["ones", f"y_{j}"], ["bankA"])
            for j in range(2):
                p.add("pe", lambda e, j=j, n=n: e.matmul(bB[:, 0:n], lhsT=ones[:], rhs=ysq[j][:, 0:n], start=(j == 0), stop=(j == 1)), ["ones", f"ysq_{j}"], ["bankB"])
            p.add("act", lambda e, n=n: e.activation(out=mean[:, 0:n], in_=bA[:, 0:n], func=AF.Copy, scale=1.0 / 256), ["bankA"], ["mean"])
            p.add("act", lambda e, n=n: e.activation(out=msq[:, 0:n], in_=bA[:, 0:n], func=AF.Square, scale=1.0 / 256), ["bankA"], ["msq"])
            p.add("dve", lambda e, n=n: e.scalar_tensor_tensor(out=rstd[:, 0:n], in0=bB[:, 0:n], scalar=1.0 / 256, in1=msq[:, 0:n], op0=ALU.mult, op1=ALU.subtract),
                  ["bankB", "msq"], ["rstd"])
            p.add("dve", lambda e, n=n: e.tensor_scalar(out=rstd[:, 0:n], in0=rstd[:, 0:n], scalar1=EPS, scalar2=None, op0=ALU.add), ["rstd"], ["rstd"])
            p.add("act", lambda e, n=n: e.activation(out=rstd[:, 0:n], in_=rstd[:, 0:n], func=AF.Sqrt), ["rstd"], ["rstd"])
            p.add("dve", lambda e, n=n: e.reciprocal(out=rstd[:, 0:n], in_=rstd[:, 0:n]), ["rstd"], ["rstd"])
            for j in range(2):
                p.add("dve", lambda e, j=j, n=n: e.tensor_tensor(out=zz[j][:, 0:n], in0=y[j][:, 0:n], in1=mean[:, 0:n], op=ALU.subtract), [f"y_{j}", "mean"], [f"zz_{j}"])
                p.add("dve", lambda e, j=j, n=n: e.tensor_tensor(out=zz[j][:, 0:n], in0=zz[j][:, 0:n], in1=rstd[:, 0:n], op=ALU.mult), [f"zz_{j}", "rstd"], [f"zz_{j}"])
                p.add("act", lambda e, j=j, n=n: e.activation(out=zz[j][:, 0:n], in_=zz[j][:, 0:n], func=AF.Silu, scale=cp[:, j, 1:2], bias=cp[:, j, 2:3]),
                      [f"zz_{j}", "cp"], [f"zz_{j}"])
                p.dma(co[j, :, s0 + t0:s0 + t0 + n], zz[j][:, 0:n], reads=[f"zz_{j}"])


def _ph_C2(p, g, T, l):
    NT = g.NKT
    xs, convT, mixo, proj = T["xs"], T["convT"], T["mixo"], T["proj"]
    wob = p.sb([128, 8, D], BF16, name="wob")
    load_cast_weight(p, wob, T["w_out"][l], D, "wob")
    idt = p.sb([128, 128], name="idt"); p.dma(idt[:], T["ident"], writes=["idt"])
    mng = p.sb([128, 64], name="mng_sb"); p.dma(mng[:], T["mng"][l], writes=["mng"])
    bc = [p.sb([128, D], name=f"bc{i}") for i in range(7)]
    mB = T["modB"]
    srcs = [T["n2gB"][l], mB[l, 0, :, 2048:3072], mB[l, 1, :, 2048:3072], mB[l, 0, :, 4096:5120], mB[l, 1, :, 4096:5120],
            mB[l, 0, :, 3072:4096], mB[l, 1, :, 3072:4096]]
    for i in range(7):
        p.dma(bc[i][:], srcs[i], writes=[f"bc{i}"])
    for i in (3, 4):
        p.add("dve", lambda e, i=i: e.scalar_tensor_tensor(out=bc[i][:], in0=bc[i][:], scalar=1.0, in1=bc[0][:], op0=ALU.add, op1=ALU.mult),
              [f"bc{i}", "bc0"], [f"bc{i}"])
    NB = 2
    mk = lambda shape, nm, dt=F32: [p.sb(shape, dt, name=f"{nm}{i}") for i in range(NB)]
    xt = mk([128, D], "xt"); hf = mk([128, 256], "hf"); hb = mk([128, 256], "hb"); mo = mk([128, 256], "mo")
    aot = mk([128, 512], "aot"); cvt = mk([128, 2, 128], "cvt"); ym = mk([128, 256], "ym"); sqm = mk([128, 256], "sqm")
    st4 = mk([128, 8], "st4"); mixT = mk([128, 8, 128], "mixT", BF16); tmp = mk([128, D], "tmp"); x1 = mk([128, D], "x1")
    h2 = mk([128, D], "h2"); ss = mk([128, 4], "ss")
    junk = p.sb([128, D], name="junk")
    bT = [p.ps([128, 512], name=f"bT{i}") for i in range(4)]
    bAcc = [p.ps([128, 512], name=f"bAcc{i}") for i in range(4)]
    for t in range(NT):
        i = t % NB
        isc = t < g.NCT
        r0 = t * 128
        g1b = bc[2] if isc else bc[1]
        gm2 = bc[4] if isc else bc[3]
        sh2 = bc[6] if isc else bc[5]
        kg1, kgm2, ksh2 = (f"bc{2 if isc else 1}", f"bc{4 if isc else 3}", f"bc{6 if isc else 5}")
        p.dma(xt[i][:], xs[r0:r0 + 128, :], writes=[f"xt{i}"])
        p.dma(hf[i][:], mixo[r0:r0 + 128, 0:256], writes=[f"hf{i}"])
        p.dma(hb[i][:], mixo[r0:r0 + 128, 256:512], writes=[f"hb{i}"])
        p.dma(mo[i][:], proj[r0:r0 + 128, 1280:1536], writes=[f"mo{i}"])
        p.dma(aot[i][:], mixo[r0:r0 + 128, 512:1024], writes=[f"aot{i}"])
        p.dma(cvt[i][:], convT[:, :, r0:r0 + 128].rearrange("j p n -> p j n"), writes=[f"cvt{i}"])
        p.add("dve", lambda e, i=i: e.tensor_tensor(out=ym[i][:], in0=hf[i][:], in1=hb[i][:], op=ALU.add), [f"hf{i}", f"hb{i}"], [f"ym{i}"])
        p.add("act", lambda e, i=i: e.activation(out=sqm[i][:], in_=ym[i][:], func=AF.Square), [f"ym{i}"], [f"sqm{i}"])
        p.add("dve", lambda e, i=i: e.tensor_reduce(out=st4[i][:, 0:4], in_=sqm[i][:].rearrange("p (h d) -> p h d", h=4), axis=AX.X, op=ALU.add),
              [f"sqm{i}"], [f"st4a{i}"])
        p.add("dve", lambda e, i=i: e.tensor_scalar(out=st4[i][:, 4:8], in0=st4[i][:, 0:4], scalar1=1.0 / 64, scalar2=EPS, op0=ALU.mult, op1=ALU.add),
              [f"st4a{i}"], [f"st4b{i}"])
        p.add("act", lambda e, i=i: e.activation(out=st4[i][:, 0:4], in_=st4[i][:, 4:8], func=AF.Sqrt), [f"st4b{i}"], [f"st4a{i}"])
        p.add("dve", lambda e, i=i: e.reciprocal(out=st4[i][:, 4:8], in_=st4[i][:, 0:4]), [f"st4a{i}"], [f"st4b{i}"])
        p.add("act", lambda e, i=i: e.activation(out=mo[i][:], in_=mo[i][:], func=AF.Sigmoid), [f"mo{i}"], [f"mo{i}"])
        ym3 = ym[i][:].rearrange("p (h d) -> p h d", h=4)
        p.add("dve", lambda e, i=i, ym3=ym3: e.tensor_tensor(out=ym3, in0=ym3, in1=st4[i][:, 4:8, None].to_broadcast([128, 4, 64]), op=ALU.mult),
              [f"ym{i}", f"st4b{i}"], [f"ym{i}"])
        p.add("dve", lambda e, i=i, ym3=ym3: e.tensor_tensor(out=ym3, in0=ym3, in1=mng[:, None, :].to_broadcast([128, 4, 64]), op=ALU.mult),
              [f"ym{i}", "mng"], [f"ym{i}"])
        p.add("dve", lambda e, i=i: e.tensor_tensor(out=ym[i][:], in0=ym[i][:], in1=mo[i][:], op=ALU.mult), [f"ym{i}", f"mo{i}"], [f"ym{i}"])
        p.add("act", lambda e, i=i: e.activation(out=mixT[i][:, 0:2, :], in_=cvt[i][:], func=AF.Copy), [f"cvt{i}"], [f"mixT{i}_c"])
        ta, tb = (2 * t) % 4, (2 * t + 1) % 4
        tsrc = [(ym[i], 0, f"ym{i}"), (ym[i], 128, f"ym{i}"), (aot[i], 0, f"aot{i}"), (aot[i], 128, f"aot{i}"),
                (aot[i], 256, f"aot{i}"), (aot[i], 384, f"aot{i}")]
        for n_, (src, c0, key) in enumerate(tsrc):
            bank = ta if n_ < 4 else tb
            col = (n_ % 4) * 128
            p.add("pe", lambda e, src=src, c0=c0, bank=bank, col=col: e.transpose(bT[bank][:, col:col + 128], src[:, c0:c0 + 128], idt[:]),
                  [key, "idt"], [f"bT{bank}"])
        p.add("act", lambda e, i=i, ta=ta: e.activation(out=mixT[i][:, 2:6, :], in_=bT[ta][:].rearrange("p (k n) -> p k n", k=4), func=AF.Copy),
              [f"bT{ta}"], [f"mixT{i}_a"])
        p.add("dve", lambda e, i=i, tb=tb: e.tensor_copy(out=mixT[i][:, 6:8, :], in_=bT[tb][:, 0:256].rearrange("p (k n) -> p k n", k=2)),
              [f"bT{tb}"], [f"mixT{i}_b"])
        for blk in range(2):
            a = (2 * t + blk) % 4
            for k in range(8):
                p.add("pe", lambda e, i=i, k=k, a=a, blk=blk: e.matmul(bAcc[a][:], lhsT=mixT[i][:, k, :], rhs=wob[:, k, blk * 512:(blk + 1) * 512],
                                                                      start=(k == 0), stop=(k == 7)),
                      [f"mixT{i}_c", f"mixT{i}_a", f"mixT{i}_b", "wob"], [f"bAcc{a}"])
            cs = slice(blk * 512, (blk + 1) * 512)
            p.add("dve", lambda e, i=i, a=a, cs=cs, g1b=g1b: e.tensor_tensor(out=tmp[i][:, cs], in0=bAcc[a][:], in1=g1b[:, cs], op=ALU.mult),
                  [f"bAcc{a}", kg1], [f"tmp{i}_{blk}"])
            p.add("dve", lambda e, i=i, cs=cs: e.tensor_tensor(out=x1[i][:, cs], in0=tmp[i][:, cs], in1=xt[i][:, cs], op=ALU.add),
                  [f"tmp{i}_{blk}", f"xt{i}"], [f"x1{i}_{blk}"])
        p.dma(T["x1"][r0:r0 + 128, :], x1[i][:], reads=[f"x1{i}_0", f"x1{i}_1"])
        p.add("act", lambda e, i=i: e.activation(out=junk[:], in_=x1[i][:], func=AF.Square, accum_out=ss[i][:, 0:1]),
              [f"x1{i}_0", f"x1{i}_1"], ["junk", f"ss0{i}"])
        p.add("dve", lambda e, i=i: e.tensor_scalar(out=ss[i][:, 1:2], in0=ss[i][:, 0:1], scalar1=1.0 / D, scalar2=EPS, op0=ALU.mult, op1=ALU.add),
              [f"ss0{i}"], [f"ss1{i}"])
        p.add("act", lambda e, i=i: e.activation(out=ss[i][:, 2:3], in_=ss[i][:, 1:2], func=AF.Sqrt), [f"ss1{i}"], [f"ss2{i}"])
        p.add("dve", lambda e, i=i: e.reciprocal(out=ss[i][:, 3:4], in_=ss[i][:, 2:3]), [f"ss2{i}"], [f"ss3{i}"])
        p.add("dve", lambda e, i=i, gm2=gm2: e.scalar_tensor_tensor(out=h2[i][:], in0=x1[i][:], scalar=ss[i][:, 3:4], in1=gm2[:], op0=ALU.mult, op1=ALU.mult),
              [f"x1{i}_0", f"x1{i}_1", f"ss3{i}", kgm2], [f"h2{i}"])
        p.add("dve", lambda e, i=i, sh2=sh2: e.tensor_tensor(out=h2[i][:], in0=h2[i][:], in1=sh2[:], op=ALU.add), [f"h2{i}", ksh2], [f"h2{i}"])
        p.dma(T["h2"][r0:r0 + 128, :], h2[i][:], reads=[f"h2{i}"])


def _ph_CV(p, g, T, l, RC=4):
    NE = g.NE
    RPP = NE // 128
    nst = 0
    stf = [p.sb([128, RC, D], name=f"cvf{i}") for i in range(2)]
    stb = [p.sb([128, RC, D], BF16, name=f"cvb{i}") for i in range(2)]
    for which, src in enumerate((T["peer_u"][l], T["peer_v"][l])):
        sv = src.rearrange("(p r) d -> p r d", r=RPP)
        dv = T["puv"].rearrange("(p r) (two d) -> p r two d", r=RPP, two=2)[:, :, which, :]
        for r0 in range(0, RPP, RC):
            i = nst % 2; nst += 1
            p.dma(stf[i][:], sv[:, r0:r0 + RC, :], writes=[f"cvf{i}"])
            if i == 0:
                p.add("act", lambda e, i=i: e.activation(out=stb[i][:], in_=stf[i][:], func=AF.Copy), [f"cvf{i}"], [f"cvb{i}"])
            else:
                p.add("dve", lambda e, i=i: e.tensor_copy(out=stb[i][:], in_=stf[i][:]), [f"cvf{i}"], [f"cvb{i}"])
            p.dma(dv[:, r0:r0 + RC, :], stb[i][:], reads=[f"cvb{i}"])


DBG = {}


def _ph_C3(p, g, T, l, NGB=8):
    NT = g.NKT
    NE = g.NE
    puv = T["puv"]
    wqb = p.sb([128, 8, D], F32, name="wqb")
    p.dma(wqb[:], T["wq"][l].rearrange("(k p) n -> p k n", p=128), writes=["wqb"])
    idt = p.sb([128, 128], name="idt"); p.dma(idt[:], T["ident"], writes=["idt"])
    kbd = p.sb([128, 8, 256], name="kbd_sb"); p.dma(kbd[:], T["kbd"][l], writes=["kbd"])
    g2b = [p.sb([128, D], name=f"g2b{i}") for i in range(2)]
    for i in range(2):
        p.dma(g2b[i][:], T["modB"][l, i, :, 5120:6144], writes=[f"g2b{i}"])
    h2ts = [p.sb([128, D], name=f"h2t{i}") for i in range(2)]; x1ts = [p.sb([128, D], name=f"x1t{i}") for i in range(2)]
    h2T = p.sb([128, 8, 128], F32, name="h2T")
    qTf = p.sb([128, 8, 128], name="qTf")
    sc = p.sb([128, 16, 128], name="sc"); wk = p.sb([128, 16, 128], name="wk")
    st = p.sb([128, 16, 16], name="st"); it = p.sb([128, 16, 16], U32, name="it"); itf = p.sb([128, 16, 16], name="itf")
    cand = p.sb([128, 8, 256], name="cand"); cidx = p.sb([128, 8, 256], name="cidx"); wk2 = p.sb([128, 8, 256], name="wk2")
    best = p.sb([128, 8, 16], name="best"); gws = [p.sb([128, 8, 16], name=f"gw{i}") for i in range(2)]
    eidf = p.sb([128, 128], name="eidf"); eidis = [p.sb([128, 128], I32, name=f"eidi{i}") for i in range(2)]
    sm = p.sb([128, 16], name="sm")
    act = p.sb([128, 128], name="act_sb"); coef = p.sb([128, 128], name="coef")
    junkq = [p.sb([128, 256], name=f"junkq{i}") for i in range(8)]
    junkd = [p.sb([128, D], BF16, name=f"junkd{i}") for i in range(4)]
    acc = p.sb([128, D], name="acc"); x2t = p.sb([128, D], name="x2t")
    rows = [p.sb([128, 2, D], BF16, name=f"rows{i}") for i in range(NGB)]
    banks = [p.ps([128, 512], name=f"bank{i}") for i in range(8)]
    diag = [p.sb([128, 128], BF16, name=f"diag{i}") for i in range(4)]
    ngat = 0
    def route(t):
        pb = t % 2
        h2t, x1t, eidi, gw = h2ts[pb], x1ts[pb], eidis[pb], gws[pb]
        isc = t < g.NCT
        r0 = t * 128
        gb = g2b[1] if isc else g2b[0]
        kgb = "g2b1" if isc else "g2b0"
        p.dma(h2t[:], T["h2"][r0:r0 + 128, :], writes=[f"h2t{pb}"])
        p.dma(x1t[:], T["x1"][r0:r0 + 128, :], writes=[f"x1t{pb}"])
        for half in range(2):
            for k in range(half * 4, half * 4 + 4):
                p.add("pe", lambda e, k=k, half=half: e.transpose(banks[half][:, (k % 4) * 128:(k % 4 + 1) * 128], h2t[:, k * 128:(k + 1) * 128], idt[:]),
                      [f"h2t{pb}", "idt"], [f"bank{half}"])
        p.add("act", lambda e: e.activation(out=h2T[:, 0:4, :], in_=banks[0][:].rearrange("p (k n) -> p k n", k=4), func=AF.Copy), ["bank0"], ["h2T_a"])
        p.add("dve", lambda e: e.tensor_copy(out=h2T[:, 4:8, :], in_=banks[1][:].rearrange("p (k n) -> p k n", k=4)), ["bank1"], ["h2T_b"])
        for c in range(8):
            bk = 2 + c // 4
            for k in range(8):
                p.add("pe", lambda e, c=c, k=k, bk=bk: e.matmul(banks[bk][:, (c % 4) * 128:(c % 4 + 1) * 128], lhsT=wqb[:, k, c * 128:(c + 1) * 128],
                                                              rhs=h2T[:, k, :], start=(k == 0), stop=(k == 7)), ["wqb", "h2T_a", "h2T_b"], [f"bank{bk}"])
        p.add("act", lambda e: e.activation(out=qTf[:, 0:4, :], in_=banks[2][:].rearrange("p (k n) -> p k n", k=4), func=AF.Copy), ["bank2"], ["qTf_a"])
        p.add("dve", lambda e: e.tensor_copy(out=qTf[:, 4:8, :], in_=banks[3][:].rearrange("p (k n) -> p k n", k=4)), ["bank3"], ["qTf_b"])
        for h in range(8):
            bk = 4 + h // 2
            p.add("pe", lambda e, h=h, bk=bk: e.matmul(banks[bk][:, (h % 2) * 256:(h % 2 + 1) * 256], lhsT=qTf[:, h, :], rhs=kbd[:, h, :],
                                                      start=True, stop=True), ["qTf_a", "qTf_b", "kbd"], [f"bank{bk}"])
        for bk in range(4, 8):
            g0 = (bk - 4) * 4
            if bk % 2 == 0:
                p.add("act", lambda e, bk=bk, g0=g0: e.activation(out=sc[:, g0:g0 + 4, :], in_=banks[bk][:].rearrange("p (g n) -> p g n", g=4), func=AF.Copy),
                      [f"bank{bk}"], [f"sc{bk}"])
            else:
                p.add("dve", lambda e, bk=bk, g0=g0: e.tensor_copy(out=sc[:, g0:g0 + 4, :], in_=banks[bk][:].rearrange("p (g n) -> p g n", g=4)),
                      [f"bank{bk}"], [f"sc{bk}"])
        ksf = lambda gq: f"sc{4 + gq // 4}"
        for gq in range(16):
            p.add("dve", lambda e, gq=gq: e.max(out=st[:, gq, 0:8], in_=sc[:, gq, :]), [ksf(gq)], [f"st{gq}a"])
        for gq in range(16):
            p.add("dve", lambda e, gq=gq: e.max_index(out=it[:, gq, 0:8], in_max=st[:, gq, 0:8], in_values=sc[:, gq, :]), [ksf(gq), f"st{gq}a"], [f"it{gq}a"])
        for gq in range(16):
            p.add("dve", lambda e, gq=gq: e.match_replace(out=wk[:, gq, :], in_to_replace=st[:, gq, 0:8], in_values=sc[:, gq, :], imm_value=-1e30),
                  [ksf(gq), f"st{gq}a"], [f"wk{gq}"])
        for gq in range(16):
            p.add("dve", lambda e, gq=gq: e.max(out=st[:, gq, 8:16], in_=wk[:, gq, :]), [f"wk{gq}"], [f"st{gq}b"])
        for gq in range(16):
            p.add("dve", lambda e, gq=gq: e.max_index(out=it[:, gq, 8:16], in_max=st[:, gq, 8:16], in_values=wk[:, gq, :]), [f"wk{gq}", f"st{gq}b"], [f"it{gq}b"])
        allst = [f"st{gq}{x}" for gq in range(16) for x in "ab"]
        allit = [f"it{gq}{x}" for gq in range(16) for x in "ab"]
        p.add("dve", lambda e: e.tensor_copy(out=itf[:], in_=it[:]), allit, ["itf"])
        st4 = st[:].rearrange("p (h two) k -> p h two k", two=2)
        itf4 = itf[:].rearrange("p (h two) k -> p h two k", two=2)
        cand4 = cand[:].rearrange("p h (i j) -> p h i j", i=16)
        cidx4 = cidx[:].rearrange("p h (i j) -> p h i j", i=16)
        for h in range(8):
            p.add("dve", lambda e, h=h: e.tensor_tensor(out=cand4[:, h], in0=st4[:, h, 0, :, None].to_broadcast([128, 16, 16]),
                                                        in1=st4[:, h, 1, None, :].to_broadcast([128, 16, 16]), op=ALU.add), allst, [f"cand{h}"])
        for h in range(8):
            p.add("dve", lambda e, h=h: e.scalar_tensor_tensor(out=cidx4[:, h], in0=itf4[:, h, 0, :, None].to_broadcast([128, 16, 16]), scalar=128.0,
                                                               in1=itf4[:, h, 1, None, :].to_broadcast([128, 16, 16]), op0=ALU.mult, op1=ALU.add),
                  ["itf"], [f"cidx{h}"])
        for h in range(8):
            p.add("dve", lambda e, h=h: e.max(out=best[:, h, 0:8], in_=cand[:, h, :]), [f"cand{h}"], [f"best{h}a"])
        for h in range(8):
            p.add("dve", lambda e, h=h: e.match_replace(out=wk2[:, h, :], in_to_replace=best[:, h, 0:8], in_values=cand[:, h, :], imm_value=-1e30),
                  [f"cand{h}", f"best{h}a"], [f"wk2{h}"])
        for h in range(8):
            p.add("dve", lambda e, h=h: e.max(out=best[:, h, 8:16], in_=wk2[:, h, :]), [f"wk2{h}"], [f"best{h}b"])
        nj = 0
        for k in range(16):
            for h in range(8):
                hk = h * 16 + k
                jq = junkq[nj % 8]; kj = f"junkq{nj % 8}"; nj += 1
                p.add("dve", lambda e, h=h, k=k, hk=hk, jq=jq: e.scalar_tensor_tensor(
                    out=jq[:], in0=cand[:, h, :], scalar=best[:, h, k:k + 1], in1=cidx[:, h, :], op0=ALU.is_equal, op1=ALU.mult,
                    accum_out=eidf[:, hk:hk + 1]), [f"cand{h}", f"cidx{h}", f"best{h}a", f"best{h}b"], [kj, f"eidf{hk}"])
        alle = [f"eidf{hk}" for hk in range(128)]
        allb = [f"best{h}{x}" for h in range(8) for x in "ab"]
        p.add("dve", lambda e: e.tensor_scalar(out=eidf[:], in0=eidf[:], scalar1=float(NE - 1), scalar2=0.0, op0=ALU.min, op1=ALU.max), alle, ["eidf"])
        p.add("dve", lambda e: e.tensor_copy(out=eidi[:], in_=eidf[:]), ["eidf"], [f"eidi{pb}"])
        p.add("dve", lambda e: e.tensor_tensor(out=gw[:], in0=best[:], in1=best[:, :, 0:1].to_broadcast([128, 8, 16]), op=ALU.subtract), allb, [f"gw{pb}"])
        p.add("act", lambda e: e.activation(out=gw[:], in_=gw[:], func=AF.Exp), [f"gw{pb}"], [f"gw{pb}"])
        p.add("dve", lambda e: e.tensor_reduce(out=sm[:, 0:8], in_=gw[:], axis=AX.X, op=ALU.add), [f"gw{pb}"], ["sm0"])
        p.add("dve", lambda e: e.reciprocal(out=sm[:, 8:16], in_=sm[:, 0:8]), ["sm0"], ["sm1"])
        p.add("dve", lambda e: e.tensor_tensor(out=gw[:], in0=gw[:], in1=sm[:, 8:16, None].to_broadcast([128, 8, 16]), op=ALU.mult), [f"gw{pb}", "sm1"], [f"gw{pb}"])

    def finish(t):
        nonlocal ngat
        pb = t % 2
        h2t, x1t, eidi, gw = h2ts[pb], x1ts[pb], eidis[pb], gws[pb]
        isc = t < g.NCT
        r0 = t * 128
        gb = g2b[1] if isc else g2b[0]
        kgb = "g2b1" if isc else "g2b0"
        gwf = gw[:].rearrange("p h k -> p (h k)")
        for hk in range(128):
            b = ngat % NGB; ngat += 1
            dg = diag[hk % 4]
            jd = junkd[hk % 4]
            p.add("pool", lambda e, b=b, hk=hk: e.indirect_dma_start(
                out=rows[b][:].rearrange("p two d -> p (two d)"), out_offset=None, in_=puv,
                in_offset=bass.IndirectOffsetOnAxis(ap=eidi[:, hk:hk + 1], axis=0)), [f"eidi{pb}"], [f"rows{b}"])
            p.add("dve", lambda e, b=b, hk=hk, jd=jd: e.scalar_tensor_tensor(out=jd[:], in0=rows[b][:, 0, :], scalar=1.0, in1=h2t[:], op0=ALU.mult, op1=ALU.mult,
                                                                            accum_out=act[:, hk:hk + 1]), [f"rows{b}", f"h2t{pb}"], [f"junkd{hk % 4}", f"act{hk}"])
            p.add("act", lambda e, hk=hk: e.activation(out=coef[:, hk:hk + 1], in_=act[:, hk:hk + 1], func=AF.Gelu), [f"act{hk}"], [f"cg{hk}"])
            p.add("dve", lambda e, dg=dg, hk=hk, gwf=gwf: e.tensor_scalar(out=dg[:], in0=idt[:], scalar1=coef[:, hk:hk + 1], scalar2=gwf[:, hk:hk + 1],
                                                                         op0=ALU.mult, op1=ALU.mult), ["idt", f"cg{hk}", f"gw{pb}"], [f"diag{hk%4}"])
            for blk in range(2):
                p.add("pe", lambda e, dg=dg, b=b, blk=blk, hk=hk: e.matmul(banks[blk][:], lhsT=dg[:], rhs=rows[b][:, 1, blk * 512:(blk + 1) * 512],
                                                                          start=(hk == 0), stop=(hk == 127)),
                      [f"diag{hk%4}", f"rows{b}"], [f"bank{blk}"])
        p.add("act", lambda e: e.activation(out=acc[:, 0:512], in_=banks[0][:], func=AF.Copy), ["bank0"], ["acc"])
        p.add("dve", lambda e: e.tensor_copy(out=acc[:, 512:1024], in_=banks[1][:]), ["bank1"], ["acc"])
        p.add("dve", lambda e, gb=gb: e.tensor_tensor(out=x2t[:], in0=acc[:], in1=gb[:], op=ALU.mult), ["acc", kgb], ["x2t"])
        p.add("dve", lambda e: e.tensor_tensor(out=x2t[:], in0=x2t[:], in1=x1t[:], op=ALU.add), ["x2t", f"x1t{pb}"], ["x2t"])
        p.dma(T["xs"][r0:r0 + 128, :], x2t[:], reads=["x2t"])


    for t in range(NT + 1):
        if t < NT:
            route(t)
        if t >= 1:
            finish(t - 1)


def _ph_F(p, g, T):
    fg = p.sb([128, D], name="fg_sb"); p.dma(fg[:], T["fg"], writes=["fg"])
    junk = p.sb([128, D], name="junk")
    NB = 2
    xt = [p.sb([128, D], name=f"xt{i}") for i in range(NB)]
    yt = [p.sb([128, D], name=f"yt{i}") for i in range(NB)]
    ss = [p.sb([128, 4], name=f"ss{i}") for i in range(NB)]
    for t in range(g.S // 128):
        i = t % NB
        r0 = t * 128
        p.dma(xt[i][:], T["xs"][g.CT + r0:g.CT + r0 + 128, :], writes=[f"xt{i}"])
        p.add("act", lambda e, i=i: e.activation(out=junk[:], in_=xt[i][:], func=AF.Square, accum_out=ss[i][:, 0:1]), [f"xt{i}"], ["junk", f"ss0{i}"])
        p.add("dve", lambda e, i=i: e.tensor_scalar(out=ss[i][:, 1:2], in0=ss[i][:, 0:1], scalar1=1.0 / D, scalar2=EPS, op0=ALU.mult, op1=ALU.add),
              [f"ss0{i}"], [f"ss1{i}"])
        p.add("act", lambda e, i=i: e.activation(out=ss[i][:, 2:3], in_=ss[i][:, 1:2], func=AF.Sqrt), [f"ss1{i}"], [f"ss2{i}"])
        p.add("dve", lambda e, i=i: e.reciprocal(out=ss[i][:, 3:4], in_=ss[i][:, 2:3]), [f"ss2{i}"], [f"ss3{i}"])
        p.add("dve", lambda e, i=i: e.scalar_tensor_tensor(out=yt[i][:], in0=xt[i][:], scalar=ss[i][:, 3:4], in1=fg[:], op0=ALU.mult, op1=ALU.mult),
              [f"xt{i}", f"ss3{i}", "fg"], [f"yt{i}"])
        p.dma(T["y"][r0:r0 + 128, :], yt[i][:], reads=[f"yt{i}"])


def build_fused(S, CT, DEPTH):
    g = Cfg(S, CT, DEPTH)
    nc = bass.Bass("TRN2", target_bir_lowering=False)
    NTOK = g.NTOK
    T = {}
    ins = {"xs0": [NTOK, D], "cT": [128, 8, 2], "ada_w": [DEPTH, D, 6144], "ada_bB": [DEPTH, 128, 6144], "ada_bT": [DEPTH, 128, 48],
           "n1g": [DEPTH, 128, 8], "n2gB": [DEPTH, 128, D], "w_in": [DEPTH, D, IN_W], "cs": [NTOK, 64],
           "cw": [DEPTH, 128, 2, 31], "cp": [DEPTH, 128, 2, 3], "mgb": [DEPTH, 8, 128, 2], "mng": [DEPTH, 128, 64],
           "dl": [DEPTH, 128, 256], "dng": [DEPTH, 128, 128], "lami": [DEPTH, 128, 2],
           "w_out": [DEPTH, D, D], "wq": [DEPTH, D, D], "kbd": [DEPTH, 128, 8, 256],
           "fg": [128, D],
           "ident": [128, 128], "ones": [128, 128], "tri": [128, 128], "triL": [128, 128]}
    for k, shp in ins.items():
        T[k] = _din(nc, k, shp)
    T["peer_u"] = [_din(nc, f"peer_u{l}", [g.NE, D]) for l in range(DEPTH)]
    T["peer_v"] = [_din(nc, f"peer_v{l}", [g.NE, D]) for l in range(DEPTH)]
    T["y"] = _dout(nc, "y", [S, D])
    scr = {"xs": [NTOK, D], "proj": [NTOK, IN_W], "projT": [2048, NTOK], "mixo": [NTOK, D], "convT": [2, 128, NTOK],
           "x1": [NTOK, D], "h2": [NTOK, D], "modT": [DEPTH, 128, 48, 2], "modB": [DEPTH, 2, 128, 6144]}
    for k, shp in scr.items():
        T[k] = nc.dram_tensor("scr_" + k, shp, F32, kind="Internal").ap()
    T["puv"] = nc.dram_tensor("scr_puv", [g.NE, 2 * D], BF16, kind="Internal").ap()
    p = Prog(nc)
    CH = 2048
    for r0 in range(0, NTOK, CH):
        r1 = min(NTOK, r0 + CH)
        p.dma(T["xs"][r0:r1, :], T["xs0"][r0:r1, :])
    _ph_P0(p, g, T)
    p.end_phase()
    for l in range(DEPTH):
        for ph in (_ph_A, _ph_M1, _ph_M2, _ph_C1, _ph_C2, _ph_CV, _ph_C3):
            p.begin_phase()
            ph(p, g, T, l)
            p.end_phase()
    p.begin_phase()
    _ph_F(p, g, T)
    p.emit()
    return nc


def fused_inputs(b, x, c, ctx, c_ctx, ada_w, ada_b, norm1_g, norm2_g, w_in, conv_w, conv_b, conv_ln_g, conv_ln_b,
                 mlstm_gate_b, mlstm_norm_g, diff_lambda, diff_norm_g, w_out, peer_wq, peer_keys, peer_u, peer_v, final_g, shared=None):
    f32 = np.float32
    DEPTH = ada_w.shape[0]
    S, CT = x.shape[1], ctx.shape[1]
    if shared is None:
        shared = {}
    if not shared:
        cos, sin = _rope_tables(S)
        cs = np.zeros((CT + S, 64), f32); cs[:CT, :32] = 1.0; cs[CT:, :32] = cos; cs[CT:, 32:] = sin
        shared["cs"] = cs
        shared["ada_w"] = np.asarray(ada_w, f32)
        ab = np.asarray(ada_b, f32)
        shared["ada_bB"] = np.ascontiguousarray(np.broadcast_to(ab[:, None, :], (DEPTH, 128, 6144)))
        shared["ada_bT"] = np.ascontiguousarray(ab.reshape(DEPTH, 48, 128).transpose(0, 2, 1))
        shared["n1g"] = np.ascontiguousarray(np.asarray(norm1_g, f32).reshape(DEPTH, 8, 128).transpose(0, 2, 1))
        shared["n2gB"] = np.ascontiguousarray(np.broadcast_to(np.asarray(norm2_g, f32)[:, None, :], (DEPTH, 128, D)))
        shared["w_in"] = np.asarray(w_in, f32)
        shared["cw"] = np.ascontiguousarray(np.asarray(conv_w, f32)[:, :, 0, :].transpose(0, 2, 1).reshape(DEPTH, 2, 128, 31).transpose(0, 2, 1, 3))
        cp = np.stack([np.asarray(conv_b, f32), np.asarray(conv_ln_g, f32), np.asarray(conv_ln_b, f32)], -1)
        shared["cp"] = np.ascontiguousarray(cp.reshape(DEPTH, 2, 128, 3).transpose(0, 2, 1, 3))
        gbv = np.asarray(mlstm_gate_b, f32)
        mgb = np.zeros((DEPTH, 8, 128, 2), f32)
        for h in range(4):
            for d in range(2):
                mgb[:, h * 2 + d, :, 0] = gbv[:, 2 * d, h][:, None]
                mgb[:, h * 2 + d, :, 1] = gbv[:, 2 * d + 1, h][:, None]
        shared["mgb"] = mgb
        shared["mng"] = np.ascontiguousarray(np.broadcast_to(np.asarray(mlstm_norm_g, f32)[:, None, :], (DEPTH, 128, 64)))
        shared["dl"] = np.ascontiguousarray(np.broadcast_to(np.asarray(diff_lambda, f32).reshape(DEPTH, 1, 256), (DEPTH, 128, 256)))
        shared["dng"] = np.ascontiguousarray(np.broadcast_to(np.asarray(diff_norm_g, f32)[:, None, :], (DEPTH, 128, 128)))
        lami = np.zeros((DEPTH, 128, 2), f32)
        for l in range(DEPTH):
            li = 0.8 - 0.6 * math.exp(-0.3 * l)
            lami[l, :, 0] = li; lami[l, :, 1] = 1.0 - li
        shared["lami"] = lami
        shared["w_out"] = np.asarray(w_out, f32)
        shared["wq"] = np.asarray(peer_wq, f32)
        keys = np.asarray(peer_keys, f32)
        kbd = np.zeros((DEPTH, 128, 8, 256), f32)
        for pp in range(2):
            kbd[:, pp * 64:(pp + 1) * 64, :, pp * 128:(pp + 1) * 128] = keys[:, :, pp].transpose(0, 3, 1, 2)
        shared["kbd"] = kbd
        for l in range(DEPTH):
            shared[f"peer_u{l}"] = np.ascontiguousarray(np.asarray(peer_u[l], f32))
            shared[f"peer_v{l}"] = np.ascontiguousarray(np.asarray(peer_v[l], f32))
        shared["fg"] = _rep(np.asarray(final_g, f32))
        shared["ident"] = np.eye(128, dtype=f32)
        shared["ones"] = np.ones((128, 128), f32)
        shared["tri"] = np.triu(np.ones((128, 128), f32))
        shared["triL"] = np.tril(np.ones((128, 128), f32))
    m = dict(shared)
    m["xs0"] = np.ascontiguousarray(np.concatenate([np.asarray(ctx[b], f32), np.asarray(x[b], f32)], 0))
    cT = np.stack([np.asarray(c[b], f32), np.asarray(c_ctx, f32)], -1)
    m["cT"] = np.ascontiguousarray(cT.reshape(8, 128, 2).transpose(1, 0, 2))
    return m


_FUSED = {}


def kernel(**inp):
    x = np.asarray(inp["x"], np.float32)
    B, S, _ = x.shape
    CT = inp["ctx"].shape[1]
    DEPTH = inp["ada_w"].shape[0]
    key = (S, CT, DEPTH)
    if key not in _FUSED:
        _FUSED[key] = build_fused(S, CT, DEPTH)
    nc = _FUSED[key]
    shared = {}
    per_b = [fused_inputs(b, shared=shared, **inp) for b in range(B)]
    maps = [per_b[(cc // 2) % B] for cc in range(NCORES)]
    res = run_bass_kernel_spmd(nc, maps, core_ids=list(range(NCORES))).results
    out = np.zeros((B, S, D), np.float32)
    for b in range(B):
        out[b] = res[2 * b]["y"]
    return out
```
